# Optimizing a Trainium2 kernel written in Bass

```python
import jax
import jax.numpy as jnp
from jax import lax
import numpy as np

D_MODEL = 1024
BATCH = 16
SEQ = 2048
DEPTH = 2

GRID_W = 64
CTX_LEN = 256
RMS_EPS = 1e-6

NA_HEADS = 8
NA_HEAD_DIM = 64
NA_WIDTH = NA_HEADS * NA_HEAD_DIM
NA_KH = 8
NA_KW = 16

POOL_WINDOWS = (2, 4, 8, 16)
POOL_GROUPS = len(POOL_WINDOWS)
POOL_WIDTH = D_MODEL - NA_WIDTH
POOL_GC = POOL_WIDTH // POOL_GROUPS
AB_IN_WIDTH = 3 * NA_WIDTH + POOL_WIDTH
AB_OUT_WIDTH = NA_WIDTH + POOL_WIDTH

RW_HEAD_DIM = 64
RW_HEADS = D_MODEL // RW_HEAD_DIM
RW_DECAY_LORA = 64
RW_AAA_LORA = 64
RW_GATE_LORA = 128
RW_GN_EPS = 64e-5

N_EXPERTS = 32
TOP_K = 4
D_FF = D_MODEL
SWIGLU_LIMIT = 7.0
SWIGLU_ALPHA = 1.702

N_EVEN = (DEPTH + 1) // 2
N_ODD = DEPTH // 2

f32 = jnp.float32

kernel_name = 'hybrid_natten_pool_rwkv7_moe_dit'


def rms_norm(x, g):
    x32 = x.astype(f32)
    y = x32 * lax.rsqrt(jnp.mean(x32 * x32, axis=-1, keepdims=True) + RMS_EPS)
    return (y * g.astype(f32)).astype(x.dtype)


def modulate(x, shift, scale):
    return x * (1.0 + scale) + shift


def ctx_attention(q, k, v):
    s = jnp.einsum('bqhd,bkhd->bhqk', q, k).astype(f32) * (q.shape[-1] ** -0.5)
    p = jax.nn.softmax(s, axis=-1).astype(v.dtype)
    return jnp.einsum('bhqk,bkhd->bqhd', p, v)


def neighbourhood_attention(q, k, v, k_ctx, v_ctx, rpb):
    b, l, h, dh = q.shape
    rows = l // GRID_W
    kh = min(NA_KH, rows)
    scale = dh ** -0.5
    qg = q.reshape(b, rows, GRID_W, h, dh)
    kg = k.reshape(b, rows, GRID_W, h, dh)
    vg = v.reshape(b, rows, GRID_W, h, dh)
    col = jnp.arange(GRID_W)
    c_start = jnp.clip(col - NA_KW // 2, 0, GRID_W - NA_KW)
    c_valid = (col[None, :] >= c_start[:, None]) & (col[None, :] < c_start[:, None] + NA_KW)
    c_rel = jnp.clip(col[None, :] - col[:, None] + NA_KW - 1, 0, 2 * NA_KW - 2)
    n_loc = kh * GRID_W

    def row_block(r):
        r_start = jnp.clip(r - kh // 2, 0, rows - kh)
        q_r = lax.dynamic_index_in_dim(qg, r, axis=1, keepdims=False)
        k_r = lax.dynamic_slice_in_dim(kg, r_start, kh, axis=1)
        v_r = lax.dynamic_slice_in_dim(vg, r_start, kh, axis=1)
        r_rel = r_start + jnp.arange(kh) - r + NA_KH - 1
        bias = rpb[:, r_rel[None, :, None], c_rel[:, None, :]]
        s_loc = jnp.einsum('bqhd,bjkhd->bhqjk', q_r, k_r).astype(f32) * scale + bias.astype(f32)
        s_loc = jnp.where(c_valid[:, None, :], s_loc, -jnp.inf).reshape(b, h, GRID_W, n_loc)
        s_ctx = jnp.einsum('bqhd,bchd->bhqc', q_r, k_ctx).astype(f32) * scale
        p = jax.nn.softmax(jnp.concatenate([s_loc, s_ctx], axis=-1), axis=-1).astype(v.dtype)
        p_loc = p[..., :n_loc].reshape(b, h, GRID_W, kh, GRID_W)
        return (jnp.einsum('bhqjk,bjkhd->bqhd', p_loc, v_r)
                + jnp.einsum('bhqc,bchd->bqhd', p[..., n_loc:], v_ctx))

    o = lax.map(row_block, jnp.arange(rows))
    return jnp.moveaxis(o, 0, 1).reshape(b, l, h * dh)


def multiscale_pool(u, pool_w, pool_scale):
    b, l, _ = u.shape
    ug = u.reshape(b, l, POOL_GROUPS, POOL_GC).astype(f32)
    csum = jnp.concatenate([jnp.zeros_like(ug[:, :1]), jnp.cumsum(ug, axis=1)], axis=1)
    t = jnp.arange(l)
    pooled = []
    for g, w in enumerate(POOL_WINDOWS):
        lo = jnp.clip(t - w // 2, 0, l)
        hi = jnp.clip(t + w - w // 2, 0, l)
        mean = (csum[:, hi, g] - csum[:, lo, g]) / (hi - lo).astype(f32)[:, None]
        pooled.append(mean - ug[:, :, g])
    pooled = jnp.stack(pooled, axis=2)
    y = jnp.einsum('blgc,gcd->blgd', pooled, pool_w.astype(f32)).reshape(b, l, POOL_WIDTH)
    return (y * pool_scale.astype(f32)).astype(u.dtype)


def ab_mixer(xl, xc, need_ctx, w_in, q_g, k_g, rpb, pool_w, pool_scale, w_out):
    def split(p):
        b, l, _ = p.shape
        q, k, v, u = jnp.split(p, [NA_WIDTH, 2 * NA_WIDTH, 3 * NA_WIDTH], axis=-1)
        heads = lambda t: t.reshape(b, l, NA_HEADS, NA_HEAD_DIM)
        return rms_norm(heads(q), q_g), rms_norm(heads(k), k_g), heads(v), u

    q_l, k_l, v_l, u_l = split(xl @ w_in)
    q_c, k_c, v_c, u_c = split(xc @ w_in)
    o_na = neighbourhood_attention(q_l, k_l, v_l, k_c, v_c, rpb)
    out_l = jnp.concatenate([o_na, multiscale_pool(u_l, pool_w, pool_scale)], axis=-1) @ w_out
    out_c = None
    if need_ctx:
        b, lc, _ = xc.shape
        o_na_c = ctx_attention(q_c, k_c, v_c).reshape(b, lc, NA_WIDTH)
        out_c = jnp.concatenate([o_na_c, multiscale_pool(u_c, pool_w, pool_scale)], axis=-1) @ w_out
    return out_l, out_c


def rwkv_features(x, mu, w_r, w_k, w_v, w0, w1, w2, a0, a1, a2, g1, g2, k_k, k_a):
    b, l, _ = x.shape
    x32 = x.astype(f32)
    prev = jnp.pad(x32[:, :-1], ((0, 0), (1, 0), (0, 0)))
    nxt = jnp.pad(x32[:, 1:], ((0, 0), (0, 1), (0, 0)))
    xx = 0.5 * (prev + nxt) - x32
    xr, xw, xk, xv, xa, xg = (x32 + xx * mu[j] for j in range(6))
    heads = lambda t: t.reshape(b, l, RW_HEADS, RW_HEAD_DIM)
    r = xr @ w_r
    k = xk @ w_k
    v = xv @ w_v
    g = jax.nn.sigmoid(xg @ g1) @ g2
    kk = heads(k * k_k)
    kk = kk * lax.rsqrt(jnp.sum(kk * kk, axis=-1, keepdims=True) + 1e-12)
    dirs = []
    for d in range(2):
        w_log = -jax.nn.softplus(-(w0[d] + jnp.tanh(xw @ w1[d]) @ w2[d])) - 0.5
        decay = jnp.exp(-jnp.exp(w_log))
        a = jax.nn.sigmoid(a0[d] + (xa @ a1[d]) @ a2[d])
        k_d = k * (1.0 + (a - 1.0) * k_a)
        dirs.append((heads(decay), heads(k_d), heads(a)))
    return heads(r), heads(v), g, kk, dirs


def rwkv_scan(state0, r, decay, k, v, kk, a, reverse):
    def step(S, inp):
        r_t, w_t, k_t, v_t, kk_t, a_t = inp
        sa = jnp.einsum('bhij,bhj->bhi', S, -kk_t)
        S = (S * w_t[:, :, None, :] + sa[..., None] * (kk_t * a_t)[:, :, None, :]
             + v_t[..., None] * k_t[:, :, None, :])
        return S, jnp.einsum('bhij,bhj->bhi', S, r_t)
    xs = tuple(jnp.moveaxis(t, 1, 0) for t in (r, decay, k, v, kk, a))
    state, ys = lax.scan(step, state0, xs, reverse=reverse)
    return state, jnp.moveaxis(ys, 0, 1)


def rwkv_readout(y, r, v, g, k_dirs, ln_w, ln_b, r_k, w_o, out_dtype):
    b, l, h, dh = y.shape
    mean = jnp.mean(y, axis=-1, keepdims=True)
    var = jnp.mean(jnp.square(y - mean), axis=-1, keepdims=True)
    yn = ((y - mean) * lax.rsqrt(var + RW_GN_EPS)).reshape(b, l, h * dh) * ln_w + ln_b
    bonus = sum(jnp.sum(r * k_d * r_k, axis=-1, keepdims=True) * v for k_d in k_dirs)
    yn = yn + bonus.reshape(b, l, h * dh)
    return ((yn * g) @ w_o).astype(out_dtype)


def rwkv_mixer(xl, xc, need_ctx, mu, w_r, w_k, w_v, w_o, w0, w1, w2, a0, a1, a2, g1, g2,
               k_k, k_a, r_k, ln_w, ln_b):
    feat = (mu, w_r, w_k, w_v, w0, w1, w2, a0, a1, a2, g1, g2, k_k, k_a)
    r_l, v_l, g_l, kk_l, dirs_l = rwkv_features(xl, *feat)
    r_c, v_c, g_c, kk_c, dirs_c = rwkv_features(xc, *feat)
    b = xl.shape[0]
    y_l = 0.0
    y_c = 0.0
    for d, rev in enumerate((False, True)):
        s0 = jnp.zeros((b, RW_HEADS, RW_HEAD_DIM, RW_HEAD_DIM), f32)
        dec_c, kd_c, a_c = dirs_c[d]
        s_ctx, yc = rwkv_scan(s0, r_c, dec_c, kd_c, v_c, kk_c, a_c, rev)
        dec_l, kd_l, a_l = dirs_l[d]
        _, yl = rwkv_scan(s_ctx, r_l, dec_l, kd_l, v_l, kk_l, a_l, rev)
        y_l = y_l + yl
        if need_ctx:
            y_c = y_c + yc
    out_l = rwkv_readout(y_l, r_l, v_l, g_l, [t[1] for t in dirs_l], ln_w, ln_b, r_k, w_o, xl.dtype)
    out_c = None
    if need_ctx:
        out_c = rwkv_readout(y_c, r_c, v_c, g_c, [t[1] for t in dirs_c], ln_w, ln_b, r_k, w_o, xc.dtype)
    return out_l, out_c


def moe_ffn(t, router_w, router_b, w_in, b_in, w_out, b_out):
    logits = (t @ router_w + router_b).astype(f32)
    top_val, top_idx = lax.top_k(logits, TOP_K)
    gates = jax.nn.softmax(top_val, axis=-1)
    combine = jnp.sum(jax.nn.one_hot(top_idx, N_EXPERTS, dtype=f32) * gates[..., None], axis=1)
    y = jnp.zeros(t.shape, f32)
    for e in range(N_EXPERTS):
        hdn = t @ w_in[e] + b_in[e]
        glu = jnp.minimum(hdn[:, 0::2], SWIGLU_LIMIT)
        lin = jnp.clip(hdn[:, 1::2], -SWIGLU_LIMIT, SWIGLU_LIMIT)
        act = glu * jax.nn.sigmoid(SWIGLU_ALPHA * glu) * (lin + 1.0)
        y = y + combine[:, e:e + 1] * (act @ w_out[e] + b_out[e])
    return y.astype(t.dtype)


def setup_inputs(seed: int = 0) -> dict:
    key = jax.random.key(seed)
    ks = iter(jax.random.split(key, 48))
    nrm = lambda shape, s: jax.random.normal(next(ks), shape, f32) * s
    uni = lambda shape, lo, hi: jax.random.uniform(next(ks), shape, f32, lo, hi)
    D = D_MODEL
    return {
        'x': nrm((BATCH, SEQ, D), 1.0),
        'c': nrm((BATCH, D), 1.0),
        'ctx': nrm((BATCH, CTX_LEN, D), 1.0),
        'c_ctx': nrm((D,), 1.0),
        'ada_w': nrm((DEPTH, D, 6 * D), 0.5 * D ** -0.5),
        'ada_b': nrm((DEPTH, 6 * D), 0.02),
        'norm_mix_g': 1.0 + nrm((DEPTH, D), 0.02),
        'norm_ffn_g': 1.0 + nrm((DEPTH, D), 0.02),
        'router_w': nrm((DEPTH, D, N_EXPERTS), D ** -0.5),
        'router_b': nrm((DEPTH, N_EXPERTS), 0.01),
        'exp_w_in': nrm((DEPTH, N_EXPERTS, D, 2 * D_FF), D ** -0.5),
        'exp_b_in': nrm((DEPTH, N_EXPERTS, 2 * D_FF), 0.02),
        'exp_w_out': nrm((DEPTH, N_EXPERTS, D_FF, D), D_FF ** -0.5),
        'exp_b_out': nrm((DEPTH, N_EXPERTS, D), 0.02),
        'ab_w_in': nrm((N_EVEN, D, AB_IN_WIDTH), D ** -0.5),
        'na_q_g': 1.0 + nrm((N_EVEN, NA_HEAD_DIM), 0.02),
        'na_k_g': 1.0 + nrm((N_EVEN, NA_HEAD_DIM), 0.02),
        'na_rpb': nrm((N_EVEN, NA_HEADS, 2 * NA_KH - 1, 2 * NA_KW - 1), 0.5),
        'pool_w': nrm((N_EVEN, POOL_GROUPS, POOL_GC, POOL_GC), POOL_GC ** -0.5),
        'pool_scale': 1.0 + nrm((N_EVEN, POOL_WIDTH), 0.1),
        'ab_w_out': nrm((N_EVEN, AB_OUT_WIDTH, D), AB_OUT_WIDTH ** -0.5),
        'rw_mu': uni((N_ODD, 6, D), 0.0, 1.0),
        'rw_w_r': nrm((N_ODD, D, D), D ** -0.5),
        'rw_w_k': nrm((N_ODD, D, D), D ** -0.5),
        'rw_w_v': nrm((N_ODD, D, D), D ** -0.5),
        'rw_w_o': nrm((N_ODD, D, D), D ** -0.5),
        'rw_w0': uni((N_ODD, 2, D), -4.0, 0.0),
        'rw_w1': nrm((N_ODD, 2, D, RW_DECAY_LORA), D ** -0.5),
        'rw_w2': nrm((N_ODD, 2, RW_DECAY_LORA, D), 0.5 * RW_DECAY_LORA ** -0.5),
        'rw_a0': nrm((N_ODD, 2, D), 0.1),
        'rw_a1': nrm((N_ODD, 2, D, RW_AAA_LORA), D ** -0.5),
        'rw_a2': nrm((N_ODD, 2, RW_AAA_LORA, D), 0.5 * RW_AAA_LORA ** -0.5),
        'rw_g1': nrm((N_ODD, D, RW_GATE_LORA), D ** -0.5),
        'rw_g2': nrm((N_ODD, RW_GATE_LORA, D), RW_GATE_LORA ** -0.5),
        'rw_k_k': 0.85 + nrm((N_ODD, D), 0.05),
        'rw_k_a': 1.0 + nrm((N_ODD, D), 0.05),
        'rw_r_k': nrm((N_ODD, RW_HEADS, RW_HEAD_DIM), 0.1),
        'rw_ln_w': 1.0 + nrm((N_ODD, D), 0.02),
        'rw_ln_b': nrm((N_ODD, D), 0.02),
    }


def reference(x, c, ctx, c_ctx, ada_w, ada_b, norm_mix_g, norm_ffn_g,
              router_w, router_b, exp_w_in, exp_b_in, exp_w_out, exp_b_out,
              ab_w_in, na_q_g, na_k_g, na_rpb, pool_w, pool_scale, ab_w_out,
              rw_mu, rw_w_r, rw_w_k, rw_w_v, rw_w_o, rw_w0, rw_w1, rw_w2, rw_a0, rw_a1, rw_a2,
              rw_g1, rw_g2, rw_k_k, rw_k_a, rw_r_k, rw_ln_w, rw_ln_b):
    h_lat, h_ctx = x, ctx
    for layer in range(DEPTH):
        need_ctx = layer < DEPTH - 1
        mod_l = jax.nn.silu(c) @ ada_w[layer] + ada_b[layer]
        mod_c = jax.nn.silu(c_ctx) @ ada_w[layer] + ada_b[layer]
        sh1_l, sc1_l, g1_l, sh2_l, sc2_l, g2_l = (m[:, None, :] for m in jnp.split(mod_l, 6, axis=-1))
        sh1_c, sc1_c, g1_c, sh2_c, sc2_c, g2_c = jnp.split(mod_c, 6, axis=-1)

        xl = modulate(rms_norm(h_lat, norm_mix_g[layer]), sh1_l, sc1_l)
        xc = modulate(rms_norm(h_ctx, norm_mix_g[layer]), sh1_c, sc1_c)
        i = layer // 2
        if layer % 2 == 0:
            out_l, out_c = ab_mixer(xl, xc, need_ctx, ab_w_in[i], na_q_g[i], na_k_g[i], na_rpb[i],
                                    pool_w[i], pool_scale[i], ab_w_out[i])
        else:
            out_l, out_c = rwkv_mixer(xl, xc, need_ctx, rw_mu[i], rw_w_r[i], rw_w_k[i], rw_w_v[i], rw_w_o[i],
                                      rw_w0[i], rw_w1[i], rw_w2[i], rw_a0[i], rw_a1[i], rw_a2[i],
                                      rw_g1[i], rw_g2[i], rw_k_k[i], rw_k_a[i], rw_r_k[i],
                                      rw_ln_w[i], rw_ln_b[i])
        h_lat = h_lat + g1_l * out_l
        if need_ctx:
            h_ctx = h_ctx + g1_c * out_c

        moe_args = (router_w[layer], router_b[layer], exp_w_in[layer], exp_b_in[layer],
                    exp_w_out[layer], exp_b_out[layer])
        yl = modulate(rms_norm(h_lat, norm_ffn_g[layer]), sh2_l, sc2_l)
        if need_ctx:
            yc = modulate(rms_norm(h_ctx, norm_ffn_g[layer]), sh2_c, sc2_c)
            n_lat = yl.shape[0] * yl.shape[1]
            f = moe_ffn(jnp.concatenate([yl.reshape(-1, D_MODEL), yc.reshape(-1, D_MODEL)], axis=0), *moe_args)
            h_lat = h_lat + g2_l * f[:n_lat].reshape(h_lat.shape)
            h_ctx = h_ctx + g2_c * f[n_lat:].reshape(h_ctx.shape)
        else:
            h_lat = h_lat + g2_l * moe_ffn(yl.reshape(-1, D_MODEL), *moe_args).reshape(h_lat.shape)
    return h_lat
```

```python
import numpy as np
import concourse.bass as bass
import concourse.mybir as mybir

F32 = mybir.dt.float32
BF16 = mybir.dt.bfloat16
ALU = mybir.AluOpType
AF = mybir.ActivationFunctionType
AX = mybir.AxisListType

KDMA = 8
ENGS = ("pe", "dve", "act", "pool", "sp")


class Res:
    __slots__ = ("name", "w", "r")

    def __init__(self, name=""):
        self.name = name
        self.w = None
        self.r = {}


class T:
    def __init__(self, t, name):
        self.t = t
        self.res = Res(name)

    def __getitem__(self, k):
        return self.t[k]

    def parts(self, n):
        if not hasattr(self, "_parts"):
            self._parts = [Res("%s.%d" % (self.res.name, i)) for i in range(n)]
        return self._parts


class V(T):
    def __init__(self, ap, res):
        self.t = ap
        self.res = res


def _res(x):
    return x.res if isinstance(x, T) else x


class Op:
    __slots__ = ("waits", "fn", "marked", "kind", "dma_m")

    def __init__(self, fn, kind):
        self.waits = []
        self.fn = fn
        self.marked = False
        self.kind = kind
        self.dma_m = -1


class FW:
    def __init__(self, nc):
        self.nc = nc
        self.ops = {e: [] for e in ENGS}
        self.seen = {e: {} for e in ENGS}
        self.ndma = {e: 0 for e in ENGS}
        self.ctx = []
        self.pctx = []
        self.emitted = {e: 0 for e in ENGS}
        self.phase_end = {e: [] for e in ENGS}
        self.cnt = {e: [] for e in ENGS}
        self.sems = None
        self.persist = False

    def sbuf(self, name, shape, dt):
        self.uid = getattr(self, "uid", 0) + 1
        name = "s%d_%s" % (self.uid, name)
        g = self.nc.sbuf_tensor(name, list(shape), dt)
        t = g.__enter__()
        (self.ctx if self.persist else self.pctx).append(g)
        return T(t, name)

    def psum(self, name, shape, dt=F32):
        self.uid = getattr(self, "uid", 0) + 1
        name = "p%d_%s" % (self.uid, name)
        g = self.nc.psum_tensor(name, list(shape), dt)
        t = g.__enter__()
        (self.ctx if self.persist else self.pctx).append(g)
        return T(t, name)

    def dram(self, name, shape, dt, kind="Internal"):
        t = self.nc.dram_tensor(name, list(shape), dt, kind=kind)
        return T(t.ap(), name)

    def _need(self, eng, tok, op):
        if tok is None:
            return
        if tok[0] == "c":
            _, e, idx = tok
            if e == "pe" and eng == "pe":
                return
            key = ("c", e)
            if idx < self.emitted[e] and not self.ops[e][idx].marked:
                idx = min(i for i in self.phase_end[e] if i >= idx)
                tok = ("c", e, idx)
            if self.seen[eng].get(key, -1) >= idx:
                return
            self.seen[eng][key] = idx
            self.ops[e][idx].marked = True
            op.waits.append(tok)
        else:
            _, q, m = tok
            key = ("d", q, m % KDMA)
            if self.seen[eng].get(key, -1) >= m:
                return
            self.seen[eng][key] = m
            op.waits.append(tok)

    def _deps(self, eng, op, r, w, ww=()):
        for x in r:
            x = _res(x)
            self._need(eng, x.w, op)
        for x in w:
            x = _res(x)
            self._need(eng, x.w, op)
            for tok in x.r.values():
                self._need(eng, tok, op)
        for x in ww:
            x = _res(x)
            if x.w is not None and not (x.w[0] == "c" and x.w[1] == eng):
                self._need(eng, x.w, op)
            for tok in x.r.values():
                self._need(eng, tok, op)

    def _commit(self, tok, r, w):
        for x in r:
            x = _res(x)
            if tok[0] == "c":
                x.r[("c", tok[1])] = tok
            else:
                x.r[("d", tok[1], tok[2] % KDMA)] = tok
        for x in w:
            x = _res(x)
            x.w = tok
            x.r = {}

    def op(self, eng, fn, r=(), w=(), ww=()):
        o = Op(fn, "c")
        self._deps(eng, o, r, w, ww)
        idx = len(self.ops[eng])
        self.ops[eng].append(o)
        self._commit(("c", eng, idx), r, list(w) + list(ww))
        return o

    def dma(self, eng, fn, r=(), w=()):
        o = Op(fn, "d")
        m = self.ndma[eng]
        self.ndma[eng] += 1
        o.dma_m = m
        if m >= KDMA:
            self._need(eng, ("d", eng, m - KDMA), o)
        self._deps(eng, o, r, w)
        self.ops[eng].append(o)
        self._commit(("d", eng, m), r, w)
        return o

    def pe(self, fn, r=(), w=(), ww=()):
        return self.op("pe", fn, r, w, ww)

    def dve(self, fn, r=(), w=(), ww=()):
        return self.op("dve", fn, r, w, ww)

    def act(self, fn, r=(), w=(), ww=()):
        return self.op("act", fn, r, w, ww)

    def pool(self, fn, r=(), w=(), ww=()):
        return self.op("pool", fn, r, w, ww)

    def barrier(self):
        for eng in ENGS:
            o = Op(None, "n")
            for e in ENGS:
                for i in range(len(self.ops[e]) - 1, -1, -1):
                    if self.ops[e][i].kind == "c":
                        self._need(eng, ("c", e, i), o)
                        break
                n = self.ndma[e]
                for m in range(max(0, n - KDMA), n):
                    self._need(eng, ("d", e, m), o)
            self.ops[eng].append(o)

    def finish_waits(self):
        o = Op(None, "n")
        for q in ENGS:
            n = self.ndma[q]
            for m in range(max(0, n - KDMA), n):
                self._need("sp", ("d", q, m), o)
        self.ops["sp"].append(o)

    def _mksems(self):
        nc = self.nc
        self.sems = {}
        for e in ENGS:
            g = nc.semaphore("c_" + e)
            self.sems[("c", e)] = g.__enter__()
            self.ctx.append(g)
            for k in range(KDMA):
                g = nc.semaphore("d_%s_%d" % (e, k))
                self.sems[("d", e, k)] = g.__enter__()
                self.ctx.append(g)

    def flush(self, final=False):
        nc = self.nc
        if self.sems is None:
            self._mksems()
        if final:
            self.finish_waits()
        sems = self.sems
        start = dict(self.emitted)
        for e in ENGS:
            ops = self.ops[e]
            for i in range(len(ops) - 1, start[e] - 1, -1):
                if ops[i].kind == "c":
                    ops[i].marked = True
                    self.phase_end[e].append(i)
                    break
            c = self.cnt[e][-1] if self.cnt[e] else 0
            for o in ops[start[e]:]:
                if o.kind == "c" and o.marked:
                    c += 1
                self.cnt[e].append(c)
        cnt = self.cnt

        def run(ename, eng):
            for o in self.ops[ename][start[ename]:]:
                for tok in o.waits:
                    if tok[0] == "c":
                        eng.wait_ge(sems[("c", tok[1])], cnt[tok[1]][tok[2]])
                    else:
                        m = tok[2]
                        eng.wait_ge(sems[("d", tok[1], m % KDMA)], 16 * (m // KDMA + 1))
                if o.kind == "n":
                    continue
                ins = o.fn(eng)
                if o.kind == "d":
                    ins.then_inc(sems[("d", ename, o.dma_m % KDMA)], 16)
                elif o.marked:
                    ins.then_inc(sems[("c", ename)], 1)

        blk = nc.Block()
        block = blk.__enter__()

        @block.tensor
        def _(e):
            run("pe", e)

        @block.vector
        def _(e):
            run("dve", e)

        @block.scalar
        def _(e):
            run("act", e)

        @block.gpsimd
        def _(e):
            run("pool", e)

        @block.sync
        def _(e):
            run("sp", e)

        blk.__exit__(None, None, None)
        for e in ENGS:
            self.emitted[e] = len(self.ops[e])
        if not final:
            self.barrier()
        for g in reversed(self.pctx):
            g.__exit__(None, None, None)
        self.pctx = []
        if final:
            for g in reversed(self.ctx):
                g.__exit__(None, None, None)
            self.ctx = []

    def emit(self):
        self.flush(final=True)

    def stats(self):
        return {e: (len(self.ops[e]), sum(1 for o in self.ops[e] if o.marked),
                    sum(len(o.waits) for o in self.ops[e])) for e in ENGS}
D = 1024
NT = 36
NE = 32
EPS = 1e-6


def rowtype(t):
    return 2 if (t % 18) < 2 else t // 18


class K:
    pass


def dma(fw, q, out, in_, r, w, **kw):
    fw.dma(q, lambda e: e.dma_start(out=out, in_=in_, **kw), r=r, w=w)


def phase_ada(fw, k):
    ccT = fw.sbuf("ccT", [128, 8, 3], F32)
    scT = fw.sbuf("scT", [128, 8, 3], F32)
    for r_ in range(3):
        dma(fw, "sp", ccT[:, :, r_], k.cc[r_, :].rearrange("(c p) -> p c", p=128), [k.cc], [ccT],
            allow_slow_non_contiguous=True)
    fw.act(lambda e: e.activation(out=scT[:], in_=ccT[:], func=AF.Silu), r=[ccT], w=[scT])
    aw = [fw.sbuf("aw%d" % i, [128, 8, 768], F32) for i in range(2)]
    abT = fw.sbuf("abT", [128, 48], F32)
    ps = fw.psum("adaps", [128, 48, 3], F32)
    n = 0
    for l in range(2):
        dma(fw, "sp", abT[:], k.ada_b[l, :].rearrange("(j p) -> p j", p=128), [k.ada_b], [abT],
            allow_slow_non_contiguous=True)
        for blk in range(8):
            a = aw[n % 2]
            n += 1
            dma(fw, "sp", a[:], k.ada_w[l, :, blk * 768:(blk + 1) * 768].rearrange("(c p) f -> p c f", p=128),
                [k.ada_w], [a])
            for j in range(6):
                jj = blk * 6 + j
                for c in range(8):
                    fw.pe(lambda e, a=a, j=j, c=c, jj=jj: e.matmul(
                        ps[:, jj, :], a[:, c, j * 128:(j + 1) * 128], scT[:, c, :],
                        start=(c == 0), stop=(c == 7)), r=[a, scT], w=[ps])
        fw.dve(lambda e, l=l: e.tensor_tensor(
            out=k.modT[l][:], in0=ps[:], in1=abT[:].unsqueeze(2).to_broadcast([128, 48, 3]), op=ALU.add),
            r=[ps, abT], w=[k.modT[l]])
    fw.flush()


def load_gain_T(fw, k, name, src_row):
    t = fw.sbuf(name, [128, 8], F32)
    dma(fw, "sp", t[:], src_row.rearrange("(c p) -> p c", p=128), [], [t], allow_slow_non_contiguous=True)
    return t


def make_GS(fw, k, l, which, gain_T):
    G = fw.sbuf("G%d" % which, [128, 8, 3], F32)
    base = 0 if which == 0 else 24
    m = k.modT[l]
    fw.dve(lambda e: e.scalar_tensor_tensor(
        out=G[:], in0=m[:, base + 8:base + 16, :], scalar=1.0,
        in1=gain_T[:].unsqueeze(2).to_broadcast([128, 8, 3]), op0=ALU.add, op1=ALU.mult),
        r=[m, gain_T], w=[G])
    return G, base


class Front:
    def __init__(self, fw, k, nbuf=2):
        self.fw = fw
        self.k = k
        self.xt = [fw.sbuf("fe_xt%d" % i, [128, D], F32) for i in range(nbuf)]
        self.xh = [fw.sbuf("fe_xh%d" % i, [128, D], BF16) for i in range(2)]
        self.ss = [fw.sbuf("fe_ss%d" % i, [128, 1], F32) for i in range(2)]
        self.rstd = [fw.sbuf("fe_rs%d" % i, [128, 1], F32) for i in range(2)]
        self.tp = [fw.psum("fe_tp%d" % i, [128, 8, 128], BF16) for i in range(1)]
        self.n = 0

    def run(self, src_rows, src_res, G, l, base, row, dst_fn, dst_res):
        fw, k = self.fw, self.k
        i = self.n
        self.n += 1
        xt = self.xt[i % len(self.xt)]
        xh = self.xh[i % 2]
        ss = self.ss[i % 2]
        rstd = self.rstd[i % 2]
        tp = self.tp[0]
        junk = xh
        m = k.modT[l]
        dma(fw, "sp", xt[:], src_rows, [src_res], [xt])
        fw.pool(lambda e: e.memset(ss[:], 0.0), w=[ss])
        fw.act(lambda e: e.activation(out=junk[:], in_=xt[:], func=AF.Square, accum_out=ss[:]),
               r=[xt], w=[xh, ss])
        fw.dve(lambda e: e.tensor_scalar(out=rstd[:], in0=ss[:], scalar1=1.0 / D, scalar2=EPS,
                                         op0=ALU.mult, op1=ALU.add), r=[ss], w=[rstd])
        fw.act(lambda e: e.activation(out=rstd[:], in_=rstd[:], func=AF.Sqrt), r=[rstd], w=[rstd])
        fw.dve(lambda e: e.reciprocal(out=rstd[:], in_=rstd[:]), r=[rstd], w=[rstd])
        fw.dve(lambda e: e.tensor_scalar(out=xh[:], in0=xt[:], scalar1=rstd[:, 0:1], scalar2=None,
                                         op0=ALU.mult), r=[xt, rstd], w=[xh])
        for c in range(8):
            fw.pe(lambda e, c=c: e.transpose(tp[:, c, :], xh[:, c * 128:(c + 1) * 128], k.ident_bf[:]),
                  r=[xh, k.ident_bf], w=[tp])
        for c in range(8):
            fw.act(lambda e, c=c: e.activation(out=dst_fn(c), in_=tp[:, c, :], func=AF.Identity,
                                               scale=G[:, c, row:row + 1], bias=m[:, base + c, row:row + 1]),
                   r=[tp, G, m], ww=[dst_res])
        return xt


class Back:
    def __init__(self, fw, k, ht_bufs, ps_big):
        self.fw = fw
        self.k = k
        self.fT = [fw.sbuf("be_fT%d" % i, [128, 8, 128], F32) for i in range(1)]
        self.ht = ht_bufs
        self.ps = [ps_big]
        self.n = 0

    def run(self, srcT_fn, src_res, l, gbase, row, h_rows, h_res, dst_rows, dst_res):
        fw, k = self.fw, self.k
        i = self.n
        self.n += 1
        fT = self.fT[0]
        ht = self.ht[i % len(self.ht)]
        ps = self.ps[0]
        m = k.modT[l]
        dma(fw, "sp", ht[:], h_rows, [h_res], [ht])
        fp = fT.parts(8)
        for c in range(8):
            if c % 2 == 0:
                fw.dve(lambda e, c=c: e.tensor_scalar(out=fT[:, c, :], in0=srcT_fn(c),
                                                      scalar1=m[:, gbase + c, row:row + 1], scalar2=None,
                                                      op0=ALU.mult), r=[src_res, m], w=[fp[c]])
            else:
                fw.act(lambda e, c=c: e.activation(out=fT[:, c, :], in_=srcT_fn(c), func=AF.Copy,
                                                   scale=m[:, gbase + c, row:row + 1]), r=[src_res, m], w=[fp[c]])
        for c in range(8):
            fw.pe(lambda e, c=c: e.transpose(ps[:, c * 128:(c + 1) * 128], fT[:, c, :], k.ident_f[:]),
                  r=[fp[c], k.ident_f], w=[ps])
        fw.dve(lambda e: e.tensor_tensor(out=ht[:], in0=ps[:], in1=ht[:], op=ALU.add), r=[ps, ht], w=[ht])
        dma(fw, "sp", dst_rows, ht[:], [ht], [dst_res])
def subs(T):
    out = []
    o = 0
    while o < T:
        n = min(512, T - o)
        out.append((o, n))
        o += n
    return out


def phase_moe(fw, k, l, tiles, src, dst_fn, first_block):
    nt = len(tiles)
    TB = nt * 128
    SB = subs(TB)
    ynT = fw.sbuf("ynT", [128, 8, TB], BF16)
    acc = fw.sbuf("acc", [128, 8, TB], F32)
    actT = [fw.sbuf("actT%d" % i, [128, 8, TB], BF16) for i in range(2)]
    gbc = [fw.sbuf("gbc%d" % i, [128, TB], F32) for i in range(1)]
    st1 = [fw.sbuf("st1_%d" % i, [128, 8, 256], F32) for i in range(2)]
    w1b = [fw.sbuf("w1b%d" % i, [128, 8, 2, 128], BF16) for i in range(3)]
    st2 = [fw.sbuf("st2_%d" % i, [128, 1024], F32) for i in range(2)]
    w2b = fw.sbuf("w2b", [128, 8, 1024], BF16)
    w2p = w2b.parts(8)
    ub = [[fw.sbuf("ub%d_%d" % (i, j), [128, 512], F32) for j in range(3)] for i in range(2)]
    wr = fw.sbuf("wr", [128, 8, NE], BF16)
    rb = fw.sbuf("rb", [128, NE], F32)
    bout = V(st2[0][0:NE, :], st2[0].res)
    bin_sb = V(st1[0][0:NE, :, :].rearrange("p c f -> p (c f)"), st1[0].res)
    binT = fw.sbuf("binT", [128, 16, NE], F32)
    combT = fw.sbuf("combT", [NE, TB], F32)
    lg = fw.sbuf("lg", [128, NE], F32)
    m8 = fw.sbuf("m8", [128, 8], F32)
    nmx = fw.sbuf("nmx", [128, 1], F32)
    msk = fw.sbuf("msk", [128, NE], F32)
    ex = fw.sbuf("ex", [128, NE], F32)
    sm = fw.sbuf("sm", [128, 1], F32)
    comb = fw.sbuf("comb", [128, NE], F32)
    gn = load_gain_T(fw, k, "gnf", k.norm_ffn_g[l, :])
    G, base = make_GS(fw, k, l, 1, gn)
    fe = Front(fw, k)
    ps_big = fw.psum("ps_big", [128, 1024], F32)
    be = Back(fw, k, fe.xt, ps_big)
    ps_misc = fw.psum("ps_misc", [128, 512], F32)
    ps_g = [V(ps_big[:, 0:512], ps_big.res), fw.psum("ps_g1", [128, 512], F32)]
    ps_l = [V(ps_big[:, 512:1024], Res("ps_l0")), fw.psum("ps_l1", [128, 512], F32)]
    ps_y = [fw.psum("ps_y%d" % i, [128, 512], F32) for i in range(1)]
    combT_d = k.combT_d

    dma(fw, "pool", wr[:], k.router_w[l].rearrange("(c p) e -> p c e", p=128), [], [wr])
    dma(fw, "sp", rb[:], k.router_b[l:l + 1, :].partition_broadcast(128), [], [rb])
    dma(fw, "sp", bout[:], k.exp_b_out[l], [], [bout])
    dma(fw, "sp", bin_sb[:], k.exp_b_in[l], [], [bin_sb])
    for mt in range(16):
        m_, two = mt // 2, mt % 2
        fw.pe(lambda e, mt=mt, m_=m_, two=two: e.transpose(
            ps_misc[:, mt * NE:(mt + 1) * NE],
            bin_sb[:, two + 256 * m_: 256 * m_ + 256: 2], k.ident_f[0:NE, 0:NE]),
            r=[bin_sb, k.ident_f], w=[ps_misc])
    fw.dve(lambda e: e.tensor_copy(binT[:].rearrange("p a b -> p (a b)"), ps_misc[:, 0:16 * NE]),
           r=[ps_misc], w=[binT])
    fw.dve(lambda e: e.tensor_scalar(out=binT[:, 1::2, :], in0=binT[:, 1::2, :], scalar1=1.0, scalar2=None, op0=ALU.add),
           r=[binT], w=[binT])

    for i, t in enumerate(tiles):
        row = rowtype(t)
        fe.run(src[t * 128:(t + 1) * 128, :], src, G, l, base, row,
               lambda c, i=i: ynT[:, c, i * 128:(i + 1) * 128], ynT)
        for c in range(8):
            fw.pe(lambda e, c=c, i=i: e.matmul(ps_misc[:, 0:NE], ynT[:, c, i * 128:(i + 1) * 128], wr[:, c, :],
                                               start=(c == 0), stop=(c == 7)), r=[ynT, wr], w=[ps_misc])
        fw.dve(lambda e: e.tensor_tensor(out=lg[:], in0=ps_misc[:, 0:NE], in1=rb[:], op=ALU.add),
               r=[ps_misc, rb], w=[lg])
        fw.dve(lambda e: e.max(out=m8[:], in_=lg[:]), r=[lg], w=[m8])
        fw.dve(lambda e: e.tensor_scalar(out=msk[:], in0=lg[:], scalar1=m8[:, 3:4], scalar2=None, op0=ALU.is_ge),
               r=[lg, m8], w=[msk])
        fw.dve(lambda e: e.tensor_scalar(out=nmx[:], in0=m8[:, 0:1], scalar1=-1.0, scalar2=None, op0=ALU.mult),
               r=[m8], w=[nmx])
        fw.act(lambda e: e.activation(out=ex[:], in_=lg[:], func=AF.Exp, bias=nmx[:, 0:1]), r=[lg, nmx], w=[ex])
        fw.dve(lambda e: e.tensor_tensor(out=ex[:], in0=ex[:], in1=msk[:], op=ALU.mult), r=[ex, msk], w=[ex])
        fw.dve(lambda e: e.reduce_sum(out=sm[:], in_=ex[:], axis=AX.X), r=[ex], w=[sm])
        fw.dve(lambda e: e.reciprocal(out=sm[:], in_=sm[:]), r=[sm], w=[sm])
        fw.dve(lambda e: e.tensor_scalar(out=comb[:], in0=ex[:], scalar1=sm[:, 0:1], scalar2=None, op0=ALU.mult),
               r=[ex, sm], w=[comb])
        fw.pe(lambda e: e.transpose(ps_misc[0:NE, 128:256], comb[:], k.ident_f[:]), r=[comb, k.ident_f], w=[ps_misc])
        fw.act(lambda e, i=i: e.activation(out=combT[:, i * 128:(i + 1) * 128], in_=ps_misc[0:NE, 128:256], func=AF.Copy),
               r=[ps_misc], ww=[combT])
    dma(fw, "sp", combT_d[:, 0:TB], combT[:], [combT], [combT_d])

    for d in range(8):
        for (o, n) in SB:
            fw.pe(lambda e, d=d, o=o, n=n: e.matmul(ps_misc[:, 0:n], bout[:, d * 128:(d + 1) * 128], combT[:, o:o + n],
                                                    start=True, stop=True), r=[bout, combT], w=[ps_misc])
            fw.act(lambda e, d=d, o=o, n=n: e.activation(out=acc[:, d, o:o + n], in_=ps_misc[:, 0:n], func=AF.Copy),
                   r=[ps_misc], ww=[acc])

    cnt = {"w1": 0, "u": 0, "cast": 0}

    def load_w1(e_, m_):
        i = cnt["w1"]
        cnt["w1"] += 1
        s_ = st1[i % 2]
        wb = w1b[i % 3]
        dma(fw, "sp", s_[:], k.exp_w_in[l, e_].rearrange("(c p) f -> p c f", p=128)[:, :, m_ * 256:(m_ + 1) * 256],
            [], [s_])
        f = fw.act if (i % 2 == 0) else fw.pool
        if i % 2 == 0:
            fw.act(lambda e: e.activation(out=wb[:], in_=s_[:].rearrange("p c (j two) -> p c two j", two=2), func=AF.Copy),
                   r=[s_], w=[wb])
        else:
            fw.pool(lambda e: e.tensor_copy(wb[:], s_[:].rearrange("p c (j two) -> p c two j", two=2)),
                    r=[s_], w=[wb])
        return wb

    def load_w2(e_):
        for m_ in range(8):
            i = cnt["cast"]
            cnt["cast"] += 1
            s_ = st2[i % 2]
            dma(fw, "sp", s_[:], k.exp_w_out[l, e_, m_ * 128:(m_ + 1) * 128, :], [], [s_])
            if i % 2 == 0:
                fw.pool(lambda e, m_=m_, s_=s_: e.tensor_copy(w2b[:, m_, :], s_[:]), r=[s_], w=[w2p[m_]])
            else:
                fw.act(lambda e, m_=m_, s_=s_: e.activation(out=w2b[:, m_, :], in_=s_[:], func=AF.Copy), r=[s_], w=[w2p[m_]])

    def mm1(e_):
        g = gbc[0]
        dma(fw, "sp", g[:], combT_d[e_:e_ + 1, 0:TB].partition_broadcast(128), [combT_d], [g])
        aT = actT[e_ % 2]
        for m_ in range(8):
            wb = load_w1(e_, m_)
            for (o, n) in SB:
                u = cnt["u"]
                cnt["u"] += 1
                pg, pl = ps_g[u % 2], ps_l[u % 2]
                A, B, C = ub[u % 2]
                for c in range(8):
                    fw.pe(lambda e, c=c, o=o, n=n, wb=wb, pg=pg: e.matmul(pg[:, 0:n], wb[:, c, 0, :], ynT[:, c, o:o + n],
                                                                          start=(c == 0), stop=(c == 7)), r=[wb, ynT], w=[pg])
                for c in range(8):
                    fw.pe(lambda e, c=c, o=o, n=n, wb=wb, pl=pl: e.matmul(pl[:, 0:n], wb[:, c, 1, :], ynT[:, c, o:o + n],
                                                                          start=(c == 0), stop=(c == 7)), r=[wb, ynT], w=[pl])
                bg = binT[:, 2 * m_, e_:e_ + 1]
                bl = binT[:, 2 * m_ + 1, e_:e_ + 1]
                fw.dve(lambda e, n=n, pg=pg, A=A, bg=bg: e.tensor_scalar(out=A[:, 0:n], in0=pg[:, 0:n], scalar1=bg, scalar2=7.0,
                                                                        op0=ALU.add, op1=ALU.min), r=[pg, binT], w=[A])
                fw.act(lambda e, n=n, A=A, B=B: e.activation(out=B[:, 0:n], in_=A[:, 0:n], func=AF.Sigmoid, scale=1.702),
                       r=[A], w=[B])
                fw.act(lambda e, n=n, pl=pl, C=C, bl=bl: e.activation(out=C[:, 0:n], in_=pl[:, 0:n], func=AF.Identity, bias=bl),
                       r=[pl, binT], w=[C])
                fw.pool(lambda e, n=n, C=C: e.tensor_scalar(out=C[:, 0:n], in0=C[:, 0:n], scalar1=8.0, scalar2=-6.0,
                                                           op0=ALU.min, op1=ALU.max), r=[C], w=[C])
                fw.pool(lambda e, n=n, A=A, B=B: e.tensor_tensor(out=A[:, 0:n], in0=A[:, 0:n], in1=B[:, 0:n], op=ALU.mult),
                        r=[A, B], w=[A])
                fw.pool(lambda e, n=n, A=A, C=C: e.tensor_tensor(out=C[:, 0:n], in0=C[:, 0:n], in1=A[:, 0:n], op=ALU.mult),
                        r=[A, C], w=[C])
                fw.dve(lambda e, n=n, o=o, C=C, g=g, aT=aT, m_=m_: e.tensor_tensor(out=aT[:, m_, o:o + n], in0=C[:, 0:n],
                                                                                  in1=g[:, o:o + n], op=ALU.mult),
                       r=[C, g], ww=[aT])

    def mm2(e_):
        aT = actT[e_ % 2]
        for d in range(8):
            for (o, n) in SB:
                py = ps_y[0]
                for m_ in range(8):
                    fw.pe(lambda e, m_=m_, d=d, o=o, n=n, py=py: e.matmul(py[:, 0:n], w2b[:, m_, d * 128:(d + 1) * 128],
                                                                          aT[:, m_, o:o + n], start=(m_ == 0), stop=(m_ == 7)),
                          r=[w2p[m_], aT], w=[py])
                fw.dve(lambda e, d=d, o=o, n=n, py=py: e.tensor_tensor(out=acc[:, d, o:o + n], in0=py[:, 0:n],
                                                                      in1=acc[:, d, o:o + n], op=ALU.add),
                       r=[py], ww=[acc])

    ne = k.n_experts_dbg if hasattr(k, "n_experts_dbg") else NE
    mm1(0)
    for e_ in range(ne):
        load_w2(e_)
        if e_ + 1 < ne:
            mm1(e_ + 1)
        mm2(e_)

    for i, t in enumerate(tiles):
        row = rowtype(t)
        drows, dres = dst_fn(t)
        if drows is None:
            continue
        be.run(lambda c, i=i: acc[:, c, i * 128:(i + 1) * 128], acc, l, 40, row,
               src[t * 128:(t + 1) * 128, :], src, drows, dres)
    fw.flush()
def ucol(s):
    return 16 + s if s < 256 else s + 48


def phase_ab(fw, k, b):
    l = 0
    t0 = 18 * b
    TS = 2304
    xnT = fw.sbuf("xnT", [128, 8, TS], BF16)
    OT = xnT
    QT = fw.sbuf("QT", [128, 4, TS], BF16)
    KT = fw.sbuf("KT", [128, 4, TS], BF16)
    Vt = fw.sbuf("Vt", [128, 18, 512], BF16)
    Vo = fw.sbuf("Vo", [128, 17, 512], BF16)
    UT = fw.sbuf("UT", [128, 4, 2368], BF16)
    w_in = fw.sbuf("w_in", [128, 8, 2048], BF16)
    EB = V(w_in[:].rearrange("p h (o jp q) -> p h o jp q", o=8, jp=4), w_in.res)
    w_out = V(UT[:].rearrange("p g x -> p (g x)")[:, 0:8 * D].rearrange("p (c f) -> p c f", c=8), UT.res)
    pw = fw.sbuf("pw", [128, 4, 128], BF16)
    pscale = fw.sbuf("pscale", [128, 4], F32)
    gq = fw.sbuf("gq", [128, 1], F32)
    gk = fw.sbuf("gk", [128, 1], F32)
    bd = fw.sbuf("bd", [128, 128], BF16)
    ones = fw.sbuf("ones", [128, 128], BF16)
    sqb = [fw.sbuf("sqb%d" % i, [128, 512], BF16) for i in range(2)]
    rstd = [fw.sbuf("qrstd%d" % i, [128, 512], F32) for i in range(2)]
    tA = fw.sbuf("ptA", [128, 544], F32)
    tB = fw.sbuf("ptB", [128, 544], F32)
    rcb = fw.sbuf("rcb", [128, 512], F32)
    pooled = fw.sbuf("pooled", [128, 512], BF16)
    mk = fw.sbuf("mk", [128, 4, 64], F32)
    rbt = [fw.sbuf("rbt%d" % i, [128, 4, 64], F32) for i in range(2)]
    Pb = [fw.sbuf("Pb%d" % i, [128, 6, 64], BF16) for i in range(3)]
    rd = [fw.sbuf("rd%d" % i, [128, 64], F32) for i in range(2)]
    resT = fw.sbuf("resT", [128, 8, 128], F32)
    gn = load_gain_T(fw, k, "gnm", k.norm_mix_g[l, :])
    G, base = make_GS(fw, k, l, 0, gn)
    fe = Front(fw, k)
    big = fw.psum("ab_big", [128, 1024], F32)
    pq = fw.psum("ab_pq", [128, 2048], F32)
    be = Back(fw, k, fe.xt, big)
    pAB = [V(big[:, 0:512], Res("pA")), V(big[:, 512:1024], Res("pB"))]
    pS = V(pq[:, 0:512], Res("pS"))
    pO = [V(pq[:, 0:64], pS.res), V(pq[:, 512:576], Res("pO1"))]
    pD = [V(pq[:, 1024:1088], Res("pD0")), V(pq[:, 1536:1600], Res("pD1"))]

    for c in range(8):
        dma(fw, "pool", w_in[:, c, :], k.ab_w_in[0, c * 128:(c + 1) * 128, :], [], [w_in])
    fw.pool(lambda e: e.memset(UT[:], 0.0), w=[UT])
    for (g_, src) in ((gq, k.na_q_g), (gk, k.na_k_g)):
        for hl in range(2):
            dma(fw, "sp", g_[hl * 64:(hl + 1) * 64, :], src[0, :].rearrange("(p o) -> p o", o=1), [], [g_])
    fw.dve(lambda e: e.tensor_scalar(out=gq[:], in0=gq[:], scalar1=0.125, scalar2=None, op0=ALU.mult), r=[gq], w=[gq])
    fw.pool(lambda e: e.memset(bd[:], 0.0), w=[bd])
    fw.pool(lambda e: e.memset(bd[0:64, 0:64], 1.0), w=[bd])
    fw.pool(lambda e: e.memset(bd[64:128, 64:128], 1.0), w=[bd])
    fw.pool(lambda e: e.memset(ones[:], 1.0), w=[ones])
    dma(fw, "pool", pw[:], k.pool_w[0].rearrange("g c d -> c g d"), [], [pw])
    dma(fw, "sp", pscale[:], k.pool_scale[0, :].rearrange("(g p) -> p g", p=128), [], [pscale],
        allow_slow_non_contiguous=True)
    dma(fw, "sp", mk[:], k.c_namask[:, :].rearrange("(jp p) q -> p jp q", p=128), [], [mk])

    for i in range(18):
        t = t0 + i
        fe.run(k.xin[t * 128:(t + 1) * 128, :], k.xin, G, l, base, rowtype(t),
               lambda c, i=i: xnT[:, c, i * 128:(i + 1) * 128], xnT)

    n_ = 0
    for m in range(8):
        dstT, gg = (QT, gq) if m < 4 else (KT, gk)
        mm = m % 4
        for (o, n) in subs(TS):
            ps = pAB[n_ % 2]
            sq = sqb[n_ % 2]
            rs = rstd[n_ % 2]
            n_ += 1
            for c in range(8):
                fw.pe(lambda e, c=c, m=m, o=o, n=n, ps=ps: e.matmul(ps[:, 0:n], w_in[:, c, m * 128:(m + 1) * 128],
                                                                   xnT[:, c, o:o + n], start=(c == 0), stop=(c == 7)),
                      r=[w_in, xnT], w=[ps])
            fw.act(lambda e, n=n, ps=ps, sq=sq: e.activation(out=sq[:, 0:n], in_=ps[:, 0:n], func=AF.Square), r=[ps], w=[sq])
            fw.pe(lambda e, n=n, sq=sq: e.matmul(pS[:, 0:n], bd[:], sq[:, 0:n], start=True, stop=True), r=[bd, sq], w=[pS])
            fw.dve(lambda e, n=n, rs=rs: e.tensor_scalar(out=rs[:, 0:n], in0=pS[:, 0:n], scalar1=1.0 / 64, scalar2=EPS,
                                                        op0=ALU.mult, op1=ALU.add), r=[pS], w=[rs])
            fw.act(lambda e, n=n, rs=rs: e.activation(out=rs[:, 0:n], in_=rs[:, 0:n], func=AF.Sqrt), r=[rs], w=[rs])
            fw.dve(lambda e, n=n, rs=rs: e.reciprocal(out=rs[:, 0:n], in_=rs[:, 0:n]), r=[rs], w=[rs])
            fw.dve(lambda e, n=n, o=o, rs=rs, ps=ps, dstT=dstT, gg=gg, mm=mm: e.scalar_tensor_tensor(
                out=dstT[:, mm, o:o + n], in0=ps[:, 0:n], scalar=gg[:, 0:1], in1=rs[:, 0:n], op0=ALU.mult, op1=ALU.mult),
                r=[ps, rs, gg], ww=[dstT])
    for (dst, ntile, off) in ((Vt, 18, 0), (Vo, 17, 64)):
        for i in range(ntile):
            ps = pAB[n_ % 2]
            n_ += 1
            for c in range(8):
                fw.pe(lambda e, c=c, i=i, off=off, ps=ps: e.matmul(ps[:, :], xnT[:, c, off + i * 128: off + (i + 1) * 128],
                                                                  w_in[:, c, 1024:1536], start=(c == 0), stop=(c == 7)),
                      r=[w_in, xnT], w=[ps])
            fw.act(lambda e, i=i, ps=ps, dst=dst: e.activation(out=dst[:, i, :], in_=ps[:, :], func=AF.Copy), r=[ps], ww=[dst])
    for g in range(4):
        for (o, n) in subs(TS):
            ps = pAB[n_ % 2]
            n_ += 1
            for c in range(8):
                fw.pe(lambda e, c=c, g=g, o=o, n=n, ps=ps: e.matmul(ps[:, 0:n], w_in[:, c, 1536 + g * 128:1536 + (g + 1) * 128],
                                                                   xnT[:, c, o:o + n], start=(c == 0), stop=(c == 7)),
                      r=[w_in, xnT], w=[ps])
            pieces = [(o, n)] if o >= 256 else [(0, 256), (256, n - 256)]
            for (po_, pn) in pieces:
                fw.dve(lambda e, g=g, po_=po_, pn=pn, o=o, ps=ps: e.tensor_copy(UT[:, g, ucol(po_):ucol(po_) + pn],
                                                                              ps[:, po_ - o:po_ - o + pn]), r=[ps], ww=[UT])

    for g, w in enumerate((2, 4, 8, 16)):
        nlev = (2, 4, 8, 16).index(w) + 1
        plist = [(16, 256, 0, 1, 0)] + [(304 + 512 * j, 512, 256 + 512 * j, 0, 512 * j) for j in range(4)]
        for (lo, n, s0, seg, p0) in plist:
            W = n + 32
            xb = lo - 16
            fw.dve(lambda e, g=g, W=W, xb=xb: e.tensor_tensor(out=tA[:, 1:W], in0=UT[:, g, xb + 1:xb + W],
                                                             in1=UT[:, g, xb:xb + W - 1], op=ALU.add), r=[UT], w=[tA])
            cur, oth = tA, tB
            sh = 2
            valid = 1
            for lev in range(1, nlev):
                lo_x = valid + sh
                fw.dve(lambda e, W=W, cur=cur, oth=oth, lo_x=lo_x, sh=sh: e.tensor_tensor(
                    out=oth[:, lo_x:W], in0=cur[:, lo_x:W], in1=cur[:, lo_x - sh:W - sh], op=ALU.add), r=[cur], w=[oth])
                cur, oth = oth, cur
                valid = lo_x
                sh *= 2
            X0 = 16 + w // 2 - 1
            dma(fw, "sp", rcb[:, 0:n], k.c_poolrc[g, seg:seg + 1, 16 + p0:16 + p0 + n].partition_broadcast(128), [], [rcb])
            fw.dve(lambda e, n=n, cur=cur, oth=oth, X0=X0: e.tensor_tensor(out=oth[:, 0:n], in0=cur[:, X0:X0 + n],
                                                                         in1=rcb[:, 0:n], op=ALU.mult), r=[cur, rcb], w=[oth])
            fw.pool(lambda e, n=n, oth=oth, g=g, lo=lo: e.tensor_tensor(out=pooled[:, 0:n], in0=oth[:, 0:n],
                                                                       in1=UT[:, g, lo:lo + n], op=ALU.subtract),
                    r=[oth, UT], w=[pooled])
            ps = pAB[n_ % 2]
            n_ += 1
            fw.pe(lambda e, n=n, g=g, ps=ps: e.matmul(ps[:, 0:n], pw[:, g, :], pooled[:, 0:n], start=True, stop=True),
                  r=[pw, pooled], w=[ps])
            fw.act(lambda e, n=n, g=g, s0=s0, ps=ps: e.activation(out=OT[:, 4 + g, s0:s0 + n], in_=ps[:, 0:n], func=AF.Copy,
                                                                 scale=pscale[:, g:g + 1]), r=[ps, pscale], ww=[OT])

    n2 = 0
    for h in range(8):
        for o in range(8):
            rb_ = rbt[n2 % 2]
            n2 += 1
            dma(fw, "sp", rb_[:], k.rpb_g[h, o].rearrange("(jp p) q -> p jp q", p=128), [], [rb_])
            fw.pool(lambda e, rb_=rb_: e.tensor_tensor(out=rb_[:], in0=rb_[:], in1=mk[:], op=ALU.add), r=[rb_, mk], w=[rb_])
            fw.act(lambda e, rb_=rb_, h=h, o=o: e.activation(out=EB[:, h, o, :, :], in_=rb_[:], func=AF.Exp), r=[rb_], ww=[EB])

    for c in range(8):
        dma(fw, "pool", w_out[:, c, :], k.ab_w_out[0, c * 128:(c + 1) * 128, :], [], [w_out])

    cnt = {"u": 0}
    pst = pAB

    def unit(qs, m, hl, ktiles, eb):
        u = cnt["u"]
        cnt["u"] += 1
        bp = 64 * hl
        ps = pst[u % 2]
        P = Pb[u % 3]
        rdd = rd[u % 2]
        nk = len(ktiles)
        for kt, (ks, vt) in enumerate(ktiles):
            fw.pe(lambda e, kt=kt, ks=ks, ps=ps: e.matmul(ps[:, kt * 64:(kt + 1) * 64], KT[bp:bp + 64, m, ks:ks + 128],
                                                         QT[bp:bp + 64, m, qs:qs + 64], start=True, stop=True),
                  r=[KT, QT], w=[ps])
        fw.act(lambda e, nk=nk, ps=ps, P=P: e.activation(out=P[:, 0:nk, :].rearrange("p a b -> p (a b)"), in_=ps[:, 0:nk * 64],
                                                        func=AF.Exp), r=[ps], w=[P])
        if eb is not None:
            fw.dve(lambda e, P=P, eb=eb: e.tensor_tensor(out=P[:, 0:4, :], in0=P[:, 0:4, :], in1=eb, op=ALU.mult),
                   r=[P, EB], w=[P])
        for kt, (ks, vt) in enumerate(ktiles):
            fw.pe(lambda e, kt=kt, vt=vt, P=P: e.matmul(pO[hl][:, :], vt[:, m * 128:(m + 1) * 128], P[:, kt, :],
                                                       start=(kt == 0), stop=(kt == nk - 1)), r=[Vt, Vo, P], w=[pO[hl]])
        for kt, (ks, vt) in enumerate(ktiles):
            fw.pe(lambda e, kt=kt, P=P: e.matmul(pD[hl][:, :], ones[:], P[:, kt, :],
                                                start=(kt == 0), stop=(kt == nk - 1)), r=[ones, P], w=[pD[hl]])
        fw.dve(lambda e, rdd=rdd: e.reciprocal(out=rdd[bp:bp + 64, :], in_=pD[hl][bp:bp + 64, :]), r=[pD[hl]], w=[rdd])
        fw.dve(lambda e, rdd=rdd: e.tensor_tensor(out=OT[bp:bp + 64, m, qs:qs + 64], in0=pO[hl][bp:bp + 64, :],
                                                 in1=rdd[bp:bp + 64, :], op=ALU.mult), r=[pO[hl], rdd], ww=[OT])

    ctx_tiles = [(0, Vt[:, 0, :]), (128, Vt[:, 1, :])]
    for m in range(4):
        for qb in range(4):
            for hl in range(2):
                unit(qb * 64, m, hl, ctx_tiles, None)
        for r in range(32):
            rs_ = min(max(r - 4, 0), 24)
            o = r - rs_
            kts = []
            for kt in range(4):
                rho = rs_ + 2 * kt
                ks = 256 + rho * 64
                vt = Vt[:, 2 + rho // 2, :] if rho % 2 == 0 else Vo[:, (3 + rho) // 2, :]
                kts.append((ks, vt))
            kts += ctx_tiles
            for hl in range(2):
                unit(256 + r * 64, m, hl, kts, EB[:, 2 * m + hl, o, :, :])

    pOP = V(pq[:, 0:1024].rearrange("p (d t) -> p d t", d=8), pS.res)
    for i in range(18):
        t = t0 + i
        for d in range(8):
            for c in range(8):
                fw.pe(lambda e, c=c, d=d, i=i: e.matmul(pOP[:, d, :], w_out[:, c, d * 128:(d + 1) * 128],
                                                       OT[:, c, i * 128:(i + 1) * 128], start=(c == 0), stop=(c == 7)),
                      r=[w_out, OT], w=[pOP])
        fw.act(lambda e: e.activation(out=resT[:].rearrange("p d t -> p (d t)"), in_=pq[:, 0:1024], func=AF.Copy),
               r=[pOP], w=[resT])
        be.run(lambda c: resT[:, c, :], resT, l, 16, rowtype(t),
               k.xin[t * 128:(t + 1) * 128, :], k.xin, k.H[0][t * 128:(t + 1) * 128, :], k.H[0])
    fw.flush()
C0 = 0.6065306597126334
TSQ = 2304


def rw_declare(fw, k):
    k.rw = {}
    for nm in ("r", "kk", "k0", "k1", "b0", "b1", "s0", "s1", "g", "bonus", "y"):
        k.rw[nm] = fw.dram("rw_" + nm, [TSQ, D], F32)
    k.rw["v"] = fw.dram("rw_v", [TSQ, D], BF16)


def bc_load(fw, name, row_ap):
    t = fw.sbuf(name, [128, D], F32)
    dma(fw, "sp", t[:], row_ap.partition_broadcast(128), [], [t])
    return t


def phase_rw_feat(fw, k, b, src):
    l = 1
    t0 = 18 * b
    A = k.rw
    xp = fw.sbuf("xp", [128, 8, 2310], BF16)
    col = lambda s: 2 + s if s < 256 else 4 + s
    wr = fw.sbuf("rw_wr", [128, 8, D], BF16)
    wk = fw.sbuf("rw_wk", [128, 8, D], BF16)
    wv = fw.sbuf("rw_wv", [128, 8, D], BF16)
    w1 = fw.sbuf("rw_w1", [128, 2, 8, 64], BF16)
    a1 = fw.sbuf("rw_a1", [128, 2, 8, 64], BF16)
    g1 = fw.sbuf("rw_g1", [128, 8, 128], BF16)
    w2 = fw.sbuf("rw_w2", [128, 2, D], BF16)
    a2 = fw.sbuf("rw_a2", [128, 2, D], BF16)
    g2 = fw.sbuf("rw_g2", [128, D], BF16)
    mu = fw.sbuf("rw_mu", [128, 6, 8], F32)
    w0b = [bc_load(fw, "w0b%d" % d, k.rw_w0[0, d:d + 1, :]) for d in range(2)]
    a0b = [bc_load(fw, "a0b%d" % d, k.rw_a0[0, d:d + 1, :]) for d in range(2)]
    kkb = bc_load(fw, "kkb", k.rw_k_k[0:1, :])
    kab = bc_load(fw, "kab", k.rw_k_a[0:1, :])
    rkb = bc_load(fw, "rkb", k.rw_r_k[0:1, :, :].rearrange("o h d -> o (h d)"))
    for (dst, srcw) in ((wr, k.rw_w_r), (wk, k.rw_w_k), (wv, k.rw_w_v)):
        for c in range(8):
            dma(fw, "pool", dst[:, c, :], srcw[0, c * 128:(c + 1) * 128, :], [], [dst])
    for d in range(2):
        dma(fw, "pool", w1[:, d, :, :], k.rw_w1[0, d].rearrange("(c p) n -> p c n", p=128), [], [w1])
        dma(fw, "pool", a1[:, d, :, :], k.rw_a1[0, d].rearrange("(c p) n -> p c n", p=128), [], [a1])
        dma(fw, "pool", w2[0:64, d, :], k.rw_w2[0, d], [], [w2])
        dma(fw, "pool", a2[0:64, d, :], k.rw_a2[0, d], [], [a2])
    dma(fw, "pool", g1[:], k.rw_g1[0].rearrange("(c p) n -> p c n", p=128), [], [g1])
    dma(fw, "pool", g2[:], k.rw_g2[0], [], [g2])
    for j in range(6):
        dma(fw, "sp", mu[:, j, :], k.rw_mu[0, j, :].rearrange("(c p) -> p c", p=128), [], [mu], allow_slow_non_contiguous=True)
    fw.pool(lambda e: e.memset(xp[:], 0.0), w=[xp])
    gn = load_gain_T(fw, k, "gnm1", k.norm_mix_g[l, :])
    G, base = make_GS(fw, k, l, 0, gn)
    fe = Front(fw, k)
    for i in range(18):
        t = t0 + i
        fe.run(src[t * 128:(t + 1) * 128, :], src, G, l, base, rowtype(t),
               lambda c, i=i: xp[:, c, col(i * 128):col(i * 128) + 128], xp)

    xx = fw.sbuf("rw_xx", [128, 8, 128], F32)
    tmpx = fw.sbuf("rw_tmpx", [128, 8, 128], F32)
    xm = [fw.sbuf("rw_xm%d" % j, [128, 8, 128], BF16) for j in range(6)]
    hT = [fw.sbuf("rw_hT%d" % i, [128, 128], BF16) for i in range(5)]
    pp = [fw.psum("rwf_pp%d" % i, [128, 1024], F32) for i in range(3)]
    ph = fw.psum("rwf_ph", [128, 4, 128], F32)
    tr = fw.sbuf("t_r", [128, D], F32)
    tk = fw.sbuf("t_k", [128, D], F32)
    tvb = fe.xh[0]
    tkk = fw.sbuf("t_kk", [128, D], F32)
    tsq = fe.xt[0]
    ta = [fw.sbuf("t_a", [128, D], F32)] * 2
    tsg = [fw.sbuf("t_sg", [128, D], F32)] * 2
    tkd = [fw.sbuf("t_kd", [128, D], F32)] * 2
    tbt = [fw.sbuf("t_bt", [128, D], F32)] * 2
    tg = tsq
    tbo = fe.xt[1]
    s16 = [fw.sbuf("t_s16_%d" % i, [128, 16], F32) for i in range(3)]
    v3 = lambda t_: t_[:].rearrange("p (h d) -> p h d", d=64)
    b16 = lambda s_: s_[:].unsqueeze(2).to_broadcast([128, 16, 64])

    for i in range(getattr(k, "rwf_tiles", 18)):
        c0_ = col(i * 128)
        rows = slice(i * 128, (i + 1) * 128)
        fw.pool(lambda e, c0_=c0_: e.tensor_tensor(out=tmpx[:], in0=xp[:, :, c0_ - 1:c0_ + 127], in1=xp[:, :, c0_ + 1:c0_ + 129],
                                                  op=ALU.add), r=[xp], w=[tmpx])
        fw.dve(lambda e, c0_=c0_: e.scalar_tensor_tensor(out=xx[:], in0=tmpx[:], scalar=0.5, in1=xp[:, :, c0_:c0_ + 128],
                                                        op0=ALU.mult, op1=ALU.subtract), r=[tmpx, xp], w=[xx])
        for j in range(6):
            fw.pool(lambda e, j=j: e.tensor_tensor(out=tmpx[:], in0=xx[:], in1=mu[:, j, :].unsqueeze(2).to_broadcast([128, 8, 128]),
                                                  op=ALU.mult), r=[xx, mu], w=[tmpx])
            fw.pool(lambda e, j=j, c0_=c0_: e.tensor_tensor(out=xm[j][:], in0=tmpx[:], in1=xp[:, :, c0_:c0_ + 128], op=ALU.add),
                    r=[tmpx, xp], w=[xm[j]])
        if getattr(k, 'rwf_stage', 99) <= 1:
            continue
        for (pi, xj, wt) in ((0, 0, wr), (1, 2, wk), (2, 3, wv)):
            for half in range(2):
                for c in range(8):
                    fw.pe(lambda e, pi=pi, xj=xj, wt=wt, half=half, c=c: e.matmul(
                        pp[pi][:, half * 512:(half + 1) * 512], xm[xj][:, c, :], wt[:, c, half * 512:(half + 1) * 512],
                        start=(c == 0), stop=(c == 7)), r=[xm[xj], wt], w=[pp[pi]])
        var = getattr(k, "rwf_var", "")
        if var != "noevac":
            if var != "nor":
                fw.act(lambda e: e.activation(out=tr[:], in_=pp[0][:], func=AF.Copy), r=[pp[0]], w=[tr])
            if var != "nok":
                fw.act(lambda e: e.activation(out=tk[:], in_=pp[1][:], func=AF.Copy), r=[pp[1]], w=[tk])
            if var != "nokk":
                fw.pool(lambda e: e.tensor_tensor(out=tkk[:], in0=tk[:], in1=kkb[:], op=ALU.mult), r=[tk, kkb], w=[tkk])
            if var != "nov":
                fw.act(lambda e: e.activation(out=tvb[:], in_=pp[2][:], func=AF.Copy), r=[pp[2]], w=[tvb])
        if getattr(k, 'rwf_stage', 99) <= 2:
            continue
        for (hi, xj, wt, d, M) in ((0, 1, w1, 0, 64), (1, 1, w1, 1, 64), (2, 4, a1, 0, 64), (3, 4, a1, 1, 64), (4, 5, g1, None, 128)):
            for c in range(8):
                lhs = (lambda wt=wt, d=d, c=c: wt[:, c, :]) if d is None else (lambda wt=wt, d=d, c=c: wt[:, d, c, :])
                if hi < 4:
                    fw.pe(lambda e, hi=hi, xj=xj, c=c, M=M, lhs=lhs: e.matmul(ph[0:M, hi, :], lhs(), xm[xj][:, c, :],
                                                                            start=(c == 0), stop=(c == 7)), r=[xm[xj], wt], w=[ph])
                else:
                    fw.pe(lambda e, xj=xj, c=c, lhs=lhs: e.matmul(pp[2][:, 0:128], lhs(), xm[xj][:, c, :],
                                                                 start=(c == 0), stop=(c == 7)), r=[xm[xj], wt], w=[pp[2]])
        for hi, (fn, M) in enumerate(((AF.Tanh, 64), (AF.Tanh, 64), (AF.Copy, 64), (AF.Copy, 64), (AF.Sigmoid, 128))):
            if hi < 4:
                fw.act(lambda e, hi=hi, fn=fn, M=M: e.activation(out=hT[hi][0:M, :], in_=ph[0:M, hi, :], func=fn), r=[ph], w=[hT[hi]])
            else:
                fw.act(lambda e, hi=hi, fn=fn: e.activation(out=hT[hi][:, :], in_=pp[2][:, 0:128], func=fn), r=[pp[2]], w=[hT[hi]])
        if getattr(k, 'rwf_stage', 99) <= 3:
            continue
        for half in range(2):
            fw.pe(lambda e, half=half: e.matmul(pp[2][:, half * 512:(half + 1) * 512], hT[4][:, :], g2[:, half * 512:(half + 1) * 512],
                                               start=True, stop=True), r=[hT[4], g2], w=[pp[2]])
        fw.pool(lambda e: e.tensor_tensor(out=tsq[:], in0=tkk[:], in1=tkk[:], op=ALU.mult), r=[tkk], w=[tsq])
        fw.dve(lambda e: e.reduce_sum(out=s16[0][:], in_=v3(tsq), axis=AX.X), r=[tsq], w=[s16[0]])
        fw.dve(lambda e: e.tensor_scalar(out=s16[0][:], in0=s16[0][:], scalar1=1e-12, scalar2=None, op0=ALU.add), r=[s16[0]], w=[s16[0]])
        fw.act(lambda e: e.activation(out=s16[0][:], in_=s16[0][:], func=AF.Sqrt), r=[s16[0]], w=[s16[0]])
        fw.dve(lambda e: e.reciprocal(out=s16[0][:], in_=s16[0][:]), r=[s16[0]], w=[s16[0]])
        fw.dve(lambda e: e.tensor_tensor(out=v3(tkk), in0=v3(tkk), in1=b16(s16[0]), op=ALU.mult), r=[tkk, s16[0]], w=[tkk])
        fw.pool(lambda e: e.tensor_tensor(out=tsq[:], in0=tr[:], in1=rkb[:], op=ALU.mult), r=[tr, rkb], w=[tsq])
        for d in range(2):
            for half in range(2):
                fw.pe(lambda e, d=d, half=half: e.matmul(pp[0][:, half * 512:(half + 1) * 512], hT[d][0:64, :],
                                                        w2[0:64, d, half * 512:(half + 1) * 512], start=True, stop=True),
                      r=[hT[d], w2], w=[pp[0]])
                fw.pe(lambda e, d=d, half=half: e.matmul(pp[1][:, half * 512:(half + 1) * 512], hT[2 + d][0:64, :],
                                                        a2[0:64, d, half * 512:(half + 1) * 512], start=True, stop=True),
                      r=[hT[2 + d], a2], w=[pp[1]])
            fw.dve(lambda e, d=d: e.tensor_tensor(out=tsg[d][:], in0=pp[0][:], in1=w0b[d][:], op=ALU.add), r=[pp[0], w0b[d]], w=[tsg[d]])
            fw.act(lambda e, d=d: e.activation(out=tsg[d][:], in_=tsg[d][:], func=AF.Sigmoid), r=[tsg[d]], w=[tsg[d]])
            fw.dve(lambda e, d=d: e.tensor_tensor(out=ta[d][:], in0=pp[1][:], in1=a0b[d][:], op=ALU.add), r=[pp[1], a0b[d]], w=[ta[d]])
            fw.act(lambda e, d=d: e.activation(out=ta[d][:], in_=ta[d][:], func=AF.Sigmoid), r=[ta[d]], w=[ta[d]])
            fw.dve(lambda e, d=d: e.scalar_tensor_tensor(out=tkd[d][:], in0=ta[d][:], scalar=-1.0, in1=kab[:], op0=ALU.add, op1=ALU.mult),
                   r=[ta[d], kab], w=[tkd[d]])
            fw.dve(lambda e, d=d: e.scalar_tensor_tensor(out=tkd[d][:], in0=tkd[d][:], scalar=1.0, in1=tk[:], op0=ALU.add, op1=ALU.mult),
                   r=[tkd[d], tk], w=[tkd[d]])
            fw.pool(lambda e, d=d: e.tensor_tensor(out=tbt[d][:], in0=tkk[:], in1=ta[d][:], op=ALU.mult), r=[tkk, ta[d]], w=[tbt[d]])
            fw.pool(lambda e, d=d: e.tensor_tensor(out=tbo[:], in0=tsq[:], in1=tkd[d][:], op=ALU.mult), r=[tsq, tkd[d]], w=[tbo])
            fw.dve(lambda e, d=d: e.reduce_sum(out=s16[1 + d][:], in_=v3(tbo), axis=AX.X), r=[tbo], w=[s16[1 + d]])
            for (nm, tt) in (("k%d" % d, tkd[d]), ("b%d" % d, tbt[d]), ("s%d" % d, tsg[d])):
                dma(fw, "sp", A[nm][rows, :], tt[:], [tt], [A[nm]])
        fw.dve(lambda e: e.tensor_tensor(out=s16[1][:], in0=s16[1][:], in1=s16[2][:], op=ALU.add), r=[s16[1], s16[2]], w=[s16[1]])
        fw.dve(lambda e: e.tensor_tensor(out=v3(tbo), in0=v3(tvb), in1=b16(s16[1]), op=ALU.mult), r=[tvb, s16[1]], w=[tbo])
        for (nm, tt) in (("r", tr), ("kk", tkk), ("bonus", tbo), ("v", tvb)):
            dma(fw, "sp", A[nm][rows, :], tt[:], [tt], [A[nm]])
        fw.act(lambda e: e.activation(out=tg[:], in_=pp[2][:], func=AF.Copy), r=[pp[2]], w=[tg])
        dma(fw, "sp", A["g"][rows, :], tg[:], [tg], [A["g"]])
    fw.flush()
def phase_rw_scan(fw, k, b):
    A = k.rw
    tri = fw.sbuf("tri", [128, 4, 128], F32)
    dma(fw, "sp", tri[:], k.c_tri[:, :, :], [], [tri])
    mexp = [fw.sbuf("mexp%d" % m, [128, 4, 128], F32) for m in range(4)]
    for m in range(4):
        for hh in range(4):
            fw.pool(lambda e, m=m, hh=hh: e.tensor_copy(mexp[m][:, hh, :], tri[:, m, :]), r=[tri], ww=[mexp[m]])
    onesc = fw.sbuf("onesc", [128, 1], F32)
    fw.pool(lambda e: e.memset(onesc[:], 1.0), w=[onesc])
    Lr = fw.sbuf("Lr", [128, D], F32)
    Lkk = fw.sbuf("Lkk", [128, D], F32)
    Lk = fw.sbuf("Lk", [128, D], F32)
    Lb = fw.sbuf("Lb", [128, D], F32)
    Ls = fw.sbuf("Ls", [128, D], F32)
    Lv = fw.sbuf("Lv", [128, D], BF16)
    gam = fw.sbuf("gam", [128, D], F32)
    gin = fw.sbuf("gin", [128, D], F32)
    gex = fw.sbuf("gex", [128, D], F32)
    At = fw.sbuf("At", [128, D], BF16)
    Rt = fw.sbuf("Rt", [128, D], BF16)
    Bt = fw.sbuf("Bt", [128, D], BF16)
    Kt = fw.sbuf("Kt", [128, D], BF16)
    ART = fw.sbuf("ART", [128, 8, 256], BF16)
    BT = fw.sbuf("BT", [128, 8, 128], BF16)
    KTt = fw.sbuf("KTt", [128, 8, 128], BF16)
    gLT = fw.sbuf("gLT", [128, 8], F32)
    ST = fw.sbuf("ST", [128, 8, 64], F32)
    STb = fw.sbuf("STb", [128, 8, 64], BF16)
    Sn = fw.sbuf("Sn", [128, 8, 64], F32)
    PDT = F32 if getattr(k, "scan_fp32", True) else BF16
    P = [[fw.sbuf("P%d_%d" % (g, i), [128, 4, 128], PDT) for i in range(7)] for g in range(4)]
    Q = [[fw.sbuf("Q%d_%d" % (g, i), [128, 4, 128], PDT) for i in range(2)] for g in range(4)]
    Br = [fw.sbuf("Br%d" % g, [128, 4, 128], BF16) for g in range(4)]
    Aak = [fw.sbuf("Aak%d" % g, [128, 4, 128], BF16) for g in range(4)]
    Kr = [fw.sbuf("Kr%d" % g, [128, 4, 128], BF16) for g in range(4)]
    Xb = [[fw.sbuf("Xb%d_%d" % (g, i), [128, 4, 64], BF16) for i in range(2)] for g in range(4)]
    Ub = [fw.sbuf("Ub%d" % g, [128, 4, 64], BF16) for g in range(4)]
    Xf = [fw.sbuf("Xf%d" % g, [128, 4, 64], F32) for g in range(4)]
    Yt = fw.sbuf("Yt", [128, D], F32)
    Yp_ = fw.sbuf("Yprev", [128, D], F32)
    pr = [fw.psum("sc_pr%d" % i, [128, 1024], F32) for i in range(4)]
    rb = [Res("bank%d" % i) for i in range(8)]
    lo = lambda i: pr[i][:, 0:512]
    hi = lambda i: pr[i][:, 512:1024]
    v4 = lambda ap, n: ap.rearrange("p (a t) -> p a t", a=n)
    bcm = lambda m: tri[:, m, :].unsqueeze(1).to_broadcast([128, 4, 128])
    MASK = {0: dict(cum=0, strict=1, incl=0, strictT=2), 1: dict(cum=3, strict=2, incl=3, strictT=1)}

    def chunk(d, ti, want_y, second):
        mk = MASK[d]
        rows = slice(ti * 128, (ti + 1) * 128)
        for (dst, nm) in ((Lr, "r"), (Lkk, "kk"), (Lk, "k%d" % d), (Lb, "b%d" % d), (Ls, "s%d" % d), (Lv, "v")):
            dma(fw, "sp", dst[:], A[nm][rows, :], [A[nm]], [dst])
        cum = pr[3]
        for half in range(2):
            fw.pe(lambda e, half=half: e.matmul(cum[:, half * 512:(half + 1) * 512], tri[:, mk["cum"], :],
                                               Ls[:, half * 512:(half + 1) * 512], start=True, stop=True),
                  r=[tri, Ls], w=[rb[6], rb[7]])
        fw.act(lambda e: e.activation(out=gex[:], in_=cum[:], func=AF.Copy), r=[rb[6], rb[7]], w=[gex])
        fw.act(lambda e: e.activation(out=gam[:], in_=gex[:], func=AF.Exp, scale=-C0), r=[gex], w=[gam])
        fw.act(lambda e: e.activation(out=gin[:], in_=gex[:], func=AF.Exp, scale=C0), r=[gex], w=[gin])
        fw.dve(lambda e: e.tensor_tensor(out=gex[:], in0=gex[:], in1=Ls[:], op=ALU.subtract), r=[gex, Ls], w=[gex])
        fw.act(lambda e: e.activation(out=gex[:], in_=gex[:], func=AF.Exp, scale=-C0), r=[gex], w=[gex])
        glp = pr[2][:, 512:520]
        for c in range(8):
            fw.pe(lambda e, c=c: e.matmul(pr[2][:, 512 + c:513 + c], Ls[:, c * 128:(c + 1) * 128], onesc[:, 0:1],
                                         start=True, stop=True), r=[Ls, onesc], w=[rb[5]])
        fw.act(lambda e: e.activation(out=gLT[:], in_=glp, func=AF.Exp, scale=-C0), r=[rb[5]], w=[gLT])
        fw.dve(lambda e: e.scalar_tensor_tensor(out=At[:], in0=Lkk[:], scalar=-1.0, in1=gex[:], op0=ALU.mult, op1=ALU.mult),
               r=[Lkk, gex], w=[At])
        fw.pool(lambda e: e.tensor_tensor(out=Rt[:], in0=Lr[:], in1=gam[:], op=ALU.mult), r=[Lr, gam], w=[Rt])
        fw.dve(lambda e: e.tensor_tensor(out=Bt[:], in0=Lb[:], in1=gin[:], op=ALU.mult), r=[Lb, gin], w=[Bt])
        fw.pool(lambda e: e.tensor_tensor(out=Kt[:], in0=Lk[:], in1=gin[:], op=ALU.mult), r=[Lk, gin], w=[Kt])
        if getattr(k, 'scan_stage', 99) <= 1:
            return
        tps = [(At, lo(0), rb[0], lambda: ART[:, :, 0:128], ART), (Rt, hi(0), rb[1], lambda: ART[:, :, 128:256], ART),
               (Bt, lo(1), rb[2], lambda: BT[:, :, :], BT), (Kt, hi(1), rb[3], lambda: KTt[:, :, :], KTt)]
        for n_, (src_, bank, res_, dstf, dstT) in enumerate(tps):
            tpv = v4(bank.bitcast(BF16), 8)
            for c in range(8):
                fw.pe(lambda e, c=c, src_=src_, tpv=tpv: e.transpose(tpv[:, c, :], src_[:, c * 128:(c + 1) * 128], k.ident_bf[:]),
                      r=[src_, k.ident_bf], w=[res_])
            if n_ % 2 == 0:
                fw.act(lambda e, tpv=tpv, dstf=dstf: e.activation(out=dstf(), in_=tpv, func=AF.Copy), r=[res_], ww=[dstT])
            else:
                fw.dve(lambda e, tpv=tpv, dstf=dstf: e.tensor_copy(dstf(), tpv), r=[res_], w=[dstT])
        if getattr(k, 'scan_stage', 99) <= 2:
            return
        for g in range(4):
            outs = [(v4(lo(0), 4), rb[0]), (v4(hi(0), 4), rb[1]), (v4(lo(1), 4), rb[2]), (v4(hi(1), 4), rb[3]), (v4(lo(2), 4), rb[4])]
            for hh in range(4):
                h = 4 * g + hh
                c, bp = h // 2, 64 * (h % 2)
                ops_ = [(BT, ART, 0, False), (BT, ART, 128, False), (KTt, ART, 0, False), (KTt, ART, 128, False), (ART, BT, 0, True)]
                fw.pe(lambda e: e.matmul(pr[2][:, 1023:1024], BT[:, 0, :], ART[:, 0, 0:1], start=True, stop=True),
                      r=[BT, ART], w=[rb[5]])
                for oi, (lt, rt_, off, swap) in enumerate(ops_):
                    ps_, rs_ = outs[oi]
                    if not swap:
                        fw.pe(lambda e, hh=hh, c=c, bp=bp, ps_=ps_, lt=lt, off=off: e.matmul(
                            ps_[:, hh, :], lt[bp:bp + 64, c, :], ART[bp:bp + 64, c, off:off + 128], start=True, stop=True),
                            r=[lt, ART], w=[rs_])
                    else:
                        fw.pe(lambda e, hh=hh, c=c, bp=bp, ps_=ps_: e.matmul(
                            ps_[:, hh, :], ART[bp:bp + 64, c, 0:128], BT[bp:bp + 64, c, :], start=True, stop=True),
                            r=[BT, ART], w=[rs_])
            dsts = [(P[g][0], "strict"), (Br[g], "incl"), (Aak[g], "strict"), (Kr[g], "incl"), (Q[g][0], "strictT")]
            svar = getattr(k, "scan_var", "")
            if svar == "mm":
                dsts = []
            for oi, (dt_, mname) in enumerate(dsts):
                ps_, rs_ = outs[oi]
                if oi % 2 == 0:
                    fw.act(lambda e, ps_=ps_, dt_=dt_: e.activation(out=dt_[:], in_=ps_, func=AF.Copy), r=[rs_], w=[dt_])
                else:
                    fw.dve(lambda e, ps_=ps_, dt_=dt_: e.tensor_copy(dt_[:], ps_), r=[rs_], w=[dt_])
                mx = mexp[mk[mname]]
                if svar == "cp":
                    continue
                if oi % 2 == 0:
                    fw.pool(lambda e, dt_=dt_, mx=mx: e.tensor_tensor(out=dt_[:], in0=dt_[:], in1=mx[:], op=ALU.mult), r=[dt_, mx], w=[dt_])
                else:
                    fw.dve(lambda e, dt_=dt_, mx=mx: e.tensor_tensor(out=dt_[:], in0=dt_[:], in1=mx[:], op=ALU.mult), r=[dt_, mx], w=[dt_])
        if getattr(k, 'scan_stage', 99) <= 3:
            return
        for kk_ in range(6):
            for g in range(4):
                pi = 2 + (g % 2)
                Pn, Qn = v4(lo(pi), 4), v4(hi(pi), 4)
                rP, rQ = rb[2 * pi], rb[2 * pi + 1]
                Pk, Qk, Qn_sb = P[g][kk_], Q[g][kk_ % 2], Q[g][(kk_ + 1) % 2]
                for hh in range(4):
                    fw.pe(lambda e, hh=hh, Pn=Pn, Pk=Pk, Qk=Qk: e.matmul(Pn[:, hh, :], Qk[:, hh, :], Pk[:, hh, :], start=True, stop=True),
                          r=[Pk, Qk], w=[rP])
                if kk_ < 5:
                    for hh in range(4):
                        fw.pe(lambda e, hh=hh, Qn=Qn, Pk=Pk, Qk=Qk: e.matmul(Qn[:, hh, :], Pk[:, hh, :], Qk[:, hh, :], start=True, stop=True),
                              r=[Pk, Qk], w=[rQ])
                fw.act(lambda e, g=g, kk_=kk_, Pn=Pn: e.activation(out=P[g][kk_ + 1][:], in_=Pn, func=AF.Copy), r=[rP], w=[P[g][kk_ + 1]])
                if kk_ < 5:
                    fw.dve(lambda e, Qn=Qn, Qn_sb=Qn_sb: e.tensor_copy(Qn_sb[:], Qn), r=[rQ], w=[Qn_sb])
        if getattr(k, 'scan_stage', 99) <= 4:
            return
        Xp = [v4(pr[0][:, g * 256:(g + 1) * 256], 4) for g in range(4)]
        rX = [rb[0], rb[0], rb[1], rb[1]]
        for g in range(4):
            for hh in range(4):
                h = 4 * g + hh
                c, bp = h // 2, 64 * (h % 2)
                fw.pe(lambda e, g=g, hh=hh, c=c, bp=bp: e.matmul(Xp[g][:, hh, :], ART[bp:bp + 64, c, 0:128], STb[bp:bp + 64, c, :],
                                                                start=True, stop=False), r=[ART, STb], w=[rX[g]])
                fw.pe(lambda e, g=g, hh=hh, h=h: e.matmul(Xp[g][:, hh, :], Aak[g][:, hh, :], Lv[:, h * 64:(h + 1) * 64],
                                                         start=False, stop=True), r=[Aak[g], Lv], w=[rX[g]])
        for g in range(4):
            fw.dve(lambda e, g=g: e.tensor_copy(Xf[g][:], Xp[g]), r=[rX[g]], w=[Xf[g]])
            if PDT != F32:
                fw.dve(lambda e, g=g: e.tensor_copy(Xb[g][0][:], Xf[g][:]), r=[Xf[g]], w=[Xb[g][0]])
        for kk_ in range(7):
            for g in range(4):
                xb = Xf[g] if PDT == F32 else Xb[g][kk_ % 2]
                xn = Ub[g] if kk_ == 6 else Xb[g][(kk_ + 1) % 2]
                for hh in range(4):
                    fw.pe(lambda e, g=g, hh=hh, kk_=kk_, xb=xb: e.matmul(Xp[g][:, hh, :], P[g][kk_][:, hh, :], xb[:, hh, :],
                                                                        start=True, stop=True), r=[P[g][kk_], xb], w=[rX[g]])
                fw.dve(lambda e, g=g: e.tensor_tensor(out=Xf[g][:], in0=Xp[g], in1=Xf[g][:], op=ALU.add), r=[rX[g], Xf[g]], w=[Xf[g]])
                fw.act(lambda e, g=g, xn=xn: e.activation(out=xn[:], in_=Xf[g][:], func=AF.Copy), r=[Xf[g]], w=[xn])
        if getattr(k, 'scan_stage', 99) <= 5:
            return
        if want_y:
            Yp = pr[1]
            if second:
                dma(fw, "sp", Yp_[:], A["y"][rows, :], [A["y"]], [Yp_])
            for g in range(4):
                for hh in range(4):
                    h = 4 * g + hh
                    c, bp = h // 2, 64 * (h % 2)
                    rY = rb[2] if h < 8 else rb[3]
                    fw.pe(lambda e, h=h, c=c, bp=bp: e.matmul(Yp[:, h * 64:(h + 1) * 64], ART[bp:bp + 64, c, 128:256], STb[bp:bp + 64, c, :],
                                                             start=True, stop=False), r=[ART, STb], w=[rY])
                    fw.pe(lambda e, h=h, g=g, hh=hh: e.matmul(Yp[:, h * 64:(h + 1) * 64], Br[g][:, hh, :], Ub[g][:, hh, :],
                                                             start=False, stop=False), r=[Br[g], Ub[g]], w=[rY])
                    fw.pe(lambda e, h=h, g=g, hh=hh: e.matmul(Yp[:, h * 64:(h + 1) * 64], Kr[g][:, hh, :], Lv[:, h * 64:(h + 1) * 64],
                                                             start=False, stop=True), r=[Kr[g], Lv], w=[rY])
            fw.act(lambda e: e.activation(out=Yt[:], in_=Yp[:], func=AF.Copy), r=[rb[2], rb[3]], w=[Yt])
            if second:
                fw.pool(lambda e: e.tensor_tensor(out=Yt[:], in0=Yt[:], in1=Yp_[:], op=ALU.add), r=[Yt, Yp_], w=[Yt])
            dma(fw, "sp", A["y"][rows, :], Yt[:], [Yt], [A["y"]])
        if getattr(k, 'scan_stage', 99) <= 6:
            return
        Sp = pr[2][:, :].rearrange("p (c w i) -> p c w i", c=8, w=2)
        for c in range(8):
            for wch in range(2):
                h = 2 * c + wch
                g, hh = h // 4, h % 4
                fw.pe(lambda e, c=c, wch=wch, g=g, hh=hh: e.matmul(Sp[:, c, wch, :], Bt[:, c * 128:(c + 1) * 128], Ub[g][:, hh, :],
                                                                  start=True, stop=False), r=[Bt, Ub[g]], w=[rb[4], rb[5]])
                fw.pe(lambda e, c=c, wch=wch, h=h: e.matmul(Sp[:, c, wch, :], Kt[:, c * 128:(c + 1) * 128], Lv[:, h * 64:(h + 1) * 64],
                                                           start=False, stop=True), r=[Kt, Lv], w=[rb[4], rb[5]])
        for cb in range(2):
            fw.dve(lambda e, cb=cb: e.tensor_copy(Sn[0:64, 4 * cb:4 * cb + 4, :], Sp[0:64, 4 * cb:4 * cb + 4, 0, :]), r=[rb[4 + cb]], ww=[Sn])
            fw.dve(lambda e, cb=cb: e.tensor_copy(Sn[64:128, 4 * cb:4 * cb + 4, :], Sp[64:128, 4 * cb:4 * cb + 4, 1, :]), r=[rb[4 + cb]], ww=[Sn])
        fw.dve(lambda e: e.tensor_tensor(out=ST[:], in0=ST[:], in1=Sn[:], op=ALU.add), r=[ST, Sn], w=[ST])
        fw.dve(lambda e: e.tensor_tensor(out=ST[:], in0=ST[:], in1=gLT[:].unsqueeze(2).to_broadcast([128, 8, 64]), op=ALU.mult),
               r=[ST, gLT], w=[ST])
        fw.act(lambda e: e.activation(out=STb[:], in_=ST[:], func=AF.Copy), r=[ST], w=[STb])

    nt_dbg = getattr(k, "scan_tiles", 18)
    for d in range(2):
        fw.pool(lambda e: e.memset(ST[:], 0.0), w=[ST])
        fw.pool(lambda e: e.memset(STb[:], 0.0), w=[STb])
        order = list(range(18)) if d == 0 else [1, 0] + list(range(17, 1, -1))
        if nt_dbg < 18:
            order = [t_ for t_ in order if t_ < nt_dbg]
        for ti in order:
            chunk(d, ti, ti >= 2, d == 1)
    fw.flush()


def phase_rw_out(fw, k, b, src, dst):
    l = 1
    t0 = 18 * b
    A = k.rw
    wo = fw.sbuf("rw_wo", [128, 8, D], BF16)
    for c in range(8):
        dma(fw, "pool", wo[:, c, :], k.rw_w_o[0, c * 128:(c + 1) * 128, :], [], [wo])
    lnw = bc_load(fw, "lnw", k.rw_ln_w[0:1, :])
    lnb = bc_load(fw, "lnb", k.rw_ln_b[0:1, :])
    ty = fw.sbuf("o_y", [128, D], F32)
    tg = fw.sbuf("o_g", [128, D], F32)
    tb = fw.sbuf("o_b", [128, D], F32)
    tq = fw.sbuf("o_q", [128, D], F32)
    zb = fw.sbuf("o_zb", [128, D], BF16)
    zT = fw.sbuf("o_zT", [128, 8, 128], BF16)
    resT = fw.sbuf("o_resT", [128, 8, 128], F32)
    s1 = fw.sbuf("o_s1", [128, 16], F32)
    s2 = fw.sbuf("o_s2", [128, 16], F32)
    ht = [fw.sbuf("o_ht%d" % i, [128, D], F32) for i in range(2)]
    big = fw.psum("o_big", [128, 1024], F32)
    pz = fw.psum("o_pz", [128, 8, 128], BF16)
    pq = fw.psum("o_pq", [128, 1024], F32)
    be = Back(fw, k, ht, big)
    v3 = lambda t_: t_[:].rearrange("p (h d) -> p h d", d=64)
    b16 = lambda s_: s_[:].unsqueeze(2).to_broadcast([128, 16, 64])
    pOP = pq[:, :].rearrange("p (d t) -> p d t", d=8)
    for i in range(2, getattr(k, "scan_tiles", 18)):
        t = t0 + i
        rows = slice(i * 128, (i + 1) * 128)
        dma(fw, "sp", ty[:], A["y"][rows, :], [A["y"]], [ty])
        dma(fw, "sp", tg[:], A["g"][rows, :], [A["g"]], [tg])
        dma(fw, "sp", tb[:], A["bonus"][rows, :], [A["bonus"]], [tb])
        fw.dve(lambda e: e.reduce_sum(out=s1[:], in_=v3(ty), axis=AX.X), r=[ty], w=[s1])
        fw.dve(lambda e: e.tensor_scalar(out=s1[:], in0=s1[:], scalar1=1.0 / 64, scalar2=None, op0=ALU.mult), r=[s1], w=[s1])
        fw.dve(lambda e: e.tensor_tensor(out=v3(ty), in0=v3(ty), in1=b16(s1), op=ALU.subtract), r=[ty, s1], w=[ty])
        fw.pool(lambda e: e.tensor_tensor(out=tq[:], in0=ty[:], in1=ty[:], op=ALU.mult), r=[ty], w=[tq])
        fw.dve(lambda e: e.reduce_sum(out=s2[:], in_=v3(tq), axis=AX.X), r=[tq], w=[s2])
        fw.dve(lambda e: e.tensor_scalar(out=s2[:], in0=s2[:], scalar1=1.0 / 64, scalar2=64e-5, op0=ALU.mult, op1=ALU.add), r=[s2], w=[s2])
        fw.act(lambda e: e.activation(out=s2[:], in_=s2[:], func=AF.Sqrt), r=[s2], w=[s2])
        fw.dve(lambda e: e.reciprocal(out=s2[:], in_=s2[:]), r=[s2], w=[s2])
        fw.dve(lambda e: e.tensor_tensor(out=v3(ty), in0=v3(ty), in1=b16(s2), op=ALU.mult), r=[ty, s2], w=[ty])
        fw.pool(lambda e: e.tensor_tensor(out=ty[:], in0=ty[:], in1=lnw[:], op=ALU.mult), r=[ty, lnw], w=[ty])
        fw.pool(lambda e: e.tensor_tensor(out=tb[:], in0=tb[:], in1=lnb[:], op=ALU.add), r=[tb, lnb], w=[tb])
        fw.dve(lambda e: e.tensor_tensor(out=ty[:], in0=ty[:], in1=tb[:], op=ALU.add), r=[ty, tb], w=[ty])
        fw.dve(lambda e: e.tensor_tensor(out=zb[:], in0=ty[:], in1=tg[:], op=ALU.mult), r=[ty, tg], w=[zb])
        for c in range(8):
            fw.pe(lambda e, c=c: e.transpose(pz[:, c, :], zb[:, c * 128:(c + 1) * 128], k.ident_bf[:]), r=[zb, k.ident_bf], w=[pz])
        fw.act(lambda e: e.activation(out=zT[:], in_=pz[:], func=AF.Copy), r=[pz], w=[zT])
        for d in range(8):
            for c in range(8):
                fw.pe(lambda e, c=c, d=d: e.matmul(pOP[:, d, :], wo[:, c, d * 128:(d + 1) * 128], zT[:, c, :],
                                                  start=(c == 0), stop=(c == 7)), r=[wo, zT], w=[pq])
        fw.act(lambda e: e.activation(out=resT[:].rearrange("p d t -> p (d t)"), in_=pq[:, :], func=AF.Copy), r=[pq], w=[resT])
        be.run(lambda c: resT[:, c, :], resT, l, 16, rowtype(t), src[t * 128:(t + 1) * 128, :], src,
               dst[t * 128:(t + 1) * 128, :], dst)
    fw.flush()
WSPEC = [
    ("ada_w", [2, D, 6 * D]), ("ada_b", [2, 6 * D]), ("norm_mix_g", [2, D]), ("norm_ffn_g", [2, D]),
    ("router_w", [2, D, NE]), ("router_b", [2, NE]), ("exp_w_in", [2, NE, D, 2 * D]), ("exp_b_in", [2, NE, 2 * D]),
    ("exp_w_out", [2, NE, D, D]), ("exp_b_out", [2, NE, D]),
    ("ab_w_in", [1, D, 2048]), ("na_q_g", [1, 64]), ("na_k_g", [1, 64]),
    ("pool_w", [1, 4, 128, 128]), ("pool_scale", [1, 512]), ("ab_w_out", [1, D, D]),
    ("rw_mu", [1, 6, D]), ("rw_w_r", [1, D, D]), ("rw_w_k", [1, D, D]), ("rw_w_v", [1, D, D]), ("rw_w_o", [1, D, D]),
    ("rw_w0", [1, 2, D]), ("rw_w1", [1, 2, D, 64]), ("rw_w2", [1, 2, 64, D]), ("rw_a0", [1, 2, D]),
    ("rw_a1", [1, 2, D, 64]), ("rw_a2", [1, 2, 64, D]), ("rw_g1", [1, D, 128]), ("rw_g2", [1, 128, D]),
    ("rw_k_k", [1, D]), ("rw_k_a", [1, D]), ("rw_r_k", [1, 16, 64]), ("rw_ln_w", [1, D]), ("rw_ln_b", [1, D]),
]


def declare(fw, k):
    ne_decl = 1 if getattr(k, "small", False) else NE
    k.xin = fw.dram("xin", [NT * 128, D], F32, kind="ExternalInput")
    k.cc = fw.dram("cc", [3, D], F32, kind="ExternalInput")
    for name, shp in WSPEC:
        if name.startswith("exp_w"):
            shp = [shp[0], ne_decl] + shp[2:]
        setattr(k, name, fw.dram(name, shp, F32, kind="ExternalInput"))
    k.rpb_g = fw.dram("rpb_g", [8, 8, 512, 64], F32, kind="ExternalInput")
    k.c_ident = fw.dram("c_ident", [128, 128], F32, kind="ExternalInput")
    k.c_namask = fw.dram("c_namask", [512, 64], F32, kind="ExternalInput")
    k.c_poolrc = fw.dram("c_poolrc", [4, 2, 2048 + 32], F32, kind="ExternalInput")
    k.c_tri = fw.dram("c_tri", [128, 4, 128], F32, kind="ExternalInput")
    k.out = fw.dram("out", [32 * 128, D], F32, kind="ExternalOutput")
    k.H = [fw.dram("H%d" % i, [NT * 128, D], F32) for i in range(2)]
    k.combT_d = fw.dram("combT_d", [NE, 1152], F32)
    fw.persist = True
    k.modT = [fw.sbuf("modT%d" % l, [128, 48, 3], F32) for l in range(2)]
    k.ident_f = fw.sbuf("ident_f", [128, 128], F32)
    k.ident_bf = fw.sbuf("ident_bf", [128, 128], BF16)
    fw.persist = False
    dma(fw, "sp", k.ident_f[:], k.c_ident[:, :], [], [k.ident_f])
    dma(fw, "pool", k.ident_bf[:], k.c_ident[:, :], [], [k.ident_bf])


def host_consts():
    c = {}
    c["c_ident"] = np.eye(128, dtype=np.float32)
    qc = np.arange(64)
    cs = np.clip(qc - 8, 0, 48)
    kc = np.arange(64)
    valid = (kc[:, None] >= cs[None, :]) & (kc[:, None] < cs[None, :] + 16)
    m = np.where(valid, 0.0, -30000.0).astype(np.float32)
    c["c_namask"] = np.tile(m, (8, 1)).astype(np.float32)
    rc = np.zeros((4, 2, 2048 + 32), np.float32)
    for g, w in enumerate((2, 4, 8, 16)):
        for s, L in enumerate((2048, 256)):
            t = np.arange(L)
            lo = np.clip(t - w // 2, 0, L)
            hi = np.clip(t + w - w // 2, 0, L)
            rc[g, s, 16:16 + L] = 1.0 / (hi - lo)
    c["c_poolrc"] = rc
    tri = np.zeros((128, 4, 128), np.float32)
    s_ = np.arange(128)[:, None]
    t_ = np.arange(128)[None, :]
    tri[:, 0, :] = (s_ <= t_)
    tri[:, 1, :] = (s_ < t_)
    tri[:, 2, :] = (s_ > t_)
    tri[:, 3, :] = (s_ >= t_)
    c["c_tri"] = tri
    return c


def gather_rpb(rpb):
    j = np.arange(8)
    o = np.arange(8)
    kc = np.arange(64)
    qc = np.arange(64)
    ri = j[None, :] - o[:, None] + 7
    ci = np.clip(kc[:, None] - qc[None, :] + 15, 0, 30)
    g = rpb[:, ri[:, :, None, None], ci[None, None, :, :]]
    return np.ascontiguousarray(g.reshape(8, 8, 512, 64)).astype(np.float32)


def shard_inputs(inp, small=False):
    consts = host_consts()
    maps = []
    wts = {name: np.ascontiguousarray(inp[name], dtype=np.float32) for name, _ in WSPEC}
    if small:
        for nm in ("exp_w_in", "exp_w_out"):
            wts[nm] = np.ascontiguousarray(wts[nm][:, 0:1])
    rpbg = gather_rpb(np.asarray(inp["na_rpb"])[0])
    for core in range(8):
        rows = []
        for b in (2 * core, 2 * core + 1):
            rows.append(inp["ctx"][b])
            rows.append(inp["x"][b])
        m = {"xin": np.ascontiguousarray(np.concatenate(rows, axis=0), dtype=np.float32),
             "cc": np.ascontiguousarray(np.stack([inp["c"][2 * core], inp["c"][2 * core + 1], inp["c_ctx"]]), dtype=np.float32),
             "rpb_g": rpbg}
        m.update(wts)
        m.update(consts)
        maps.append(m)
    return maps
def build_program(k=None):
    nc = bass.Bass("TRN2", target_bir_lowering=False)
    fw = FW(nc)
    if k is None:
        k = K()
    declare(fw, k)
    rw_declare(fw, k)
    phase_ada(fw, k)
    for b in range(2):
        phase_ab(fw, k, b)
    for blk in range(4):
        tiles = list(range(9 * blk, 9 * blk + 9))
        phase_moe(fw, k, 0, tiles, k.H[0], lambda t: (k.H[1][t * 128:(t + 1) * 128, :], k.H[1]), blk == 0)
    for b in range(2):
        phase_rw_feat(fw, k, b, k.H[1])
        phase_rw_scan(fw, k, b)
        phase_rw_out(fw, k, b, k.H[1], k.H[0])

    def dst1(t):
        b, i = t // 18, t % 18
        o = b * 16 + (i - 2)
        return (k.out[o * 128:(o + 1) * 128, :], k.out)
    for b in range(2):
        for hb in range(2):
            tiles = [18 * b + 2 + 8 * hb + j for j in range(8)]
            phase_moe(fw, k, 1, tiles, k.H[0], dst1, False)
    fw.flush(final=True)
    return nc, fw


_CACHE = {}


def kernel(**inputs):
    from concourse.bass_utils import run_bass_kernel_spmd
    inp = {n: np.asarray(v) for n, v in inputs.items()}
    if "nc" not in _CACHE:
        _CACHE["nc"] = build_program()[0]
    nc = _CACHE["nc"]
    maps = shard_inputs(inp)
    res = run_bass_kernel_spmd(nc, maps, core_ids=list(range(8)))
    outs = [np.asarray(r["out"]).reshape(2, 2048, D) for r in res.results]
    return np.concatenate(outs, axis=0).astype(np.float32)
```

```python
import numpy as np
import concourse.bass as bass
import concourse.mybir as mybir

F32 = mybir.dt.float32
BF16 = mybir.dt.bfloat16
ALU = mybir.AluOpType
AF = mybir.ActivationFunctionType
AX = mybir.AxisListType

KDMA = 8
ENGS = ("pe", "dve", "act", "pool", "sp")


class Res:
    __slots__ = ("name", "w", "r")

    def __init__(self, name=""):
        self.name = name
        self.w = None
        self.r = {}


class T:
    def __init__(self, t, name):
        self.t = t
        self.res = Res(name)

    def __getitem__(self, k):
        return self.t[k]

    def parts(self, n):
        if not hasattr(self, "_parts"):
            self._parts = [Res("%s.%d" % (self.res.name, i)) for i in range(n)]
        return self._parts


class V(T):
    def __init__(self, ap, res):
        self.t = ap
        self.res = res


def _res(x):
    return x.res if isinstance(x, T) else x


class Op:
    __slots__ = ("waits", "fn", "marked", "kind", "dma_m")

    def __init__(self, fn, kind):
        self.waits = []
        self.fn = fn
        self.marked = False
        self.kind = kind
        self.dma_m = -1


class FW:
    def __init__(self, nc):
        self.nc = nc
        self.ops = {e: [] for e in ENGS}
        self.seen = {e: {} for e in ENGS}
        self.ndma = {e: 0 for e in ENGS}
        self.ctx = []
        self.pctx = []
        self.emitted = {e: 0 for e in ENGS}
        self.phase_end = {e: [] for e in ENGS}
        self.cnt = {e: [] for e in ENGS}
        self.sems = None
        self.persist = False

    def sbuf(self, name, shape, dt):
        self.uid = getattr(self, "uid", 0) + 1
        name = "s%d_%s" % (self.uid, name)
        g = self.nc.sbuf_tensor(name, list(shape), dt)
        t = g.__enter__()
        (self.ctx if self.persist else self.pctx).append(g)
        return T(t, name)

    def psum(self, name, shape, dt=F32):
        self.uid = getattr(self, "uid", 0) + 1
        name = "p%d_%s" % (self.uid, name)
        g = self.nc.psum_tensor(name, list(shape), dt)
        t = g.__enter__()
        (self.ctx if self.persist else self.pctx).append(g)
        return T(t, name)

    def dram(self, name, shape, dt, kind="Internal"):
        t = self.nc.dram_tensor(name, list(shape), dt, kind=kind)
        return T(t.ap(), name)

    def _need(self, eng, tok, op):
        if tok is None:
            return
        if tok[0] == "c":
            _, e, idx = tok
            if e == "pe" and eng == "pe":
                return
            key = ("c", e)
            if idx < self.emitted[e] and not self.ops[e][idx].marked:
                idx = min(i for i in self.phase_end[e] if i >= idx)
                tok = ("c", e, idx)
            if self.seen[eng].get(key, -1) >= idx:
                return
            self.seen[eng][key] = idx
            self.ops[e][idx].marked = True
            op.waits.append(tok)
        else:
            _, q, m = tok
            key = ("d", q, m % KDMA)
            if self.seen[eng].get(key, -1) >= m:
                return
            self.seen[eng][key] = m
            op.waits.append(tok)

    def _deps(self, eng, op, r, w, ww=()):
        for x in r:
            x = _res(x)
            self._need(eng, x.w, op)
        for x in w:
            x = _res(x)
            self._need(eng, x.w, op)
            for tok in x.r.values():
                self._need(eng, tok, op)
        for x in ww:
            x = _res(x)
            if x.w is not None and not (x.w[0] == "c" and x.w[1] == eng):
                self._need(eng, x.w, op)
            for tok in x.r.values():
                self._need(eng, tok, op)

    def _commit(self, tok, r, w):
        for x in r:
            x = _res(x)
            if tok[0] == "c":
                x.r[("c", tok[1])] = tok
            else:
                x.r[("d", tok[1], tok[2] % KDMA)] = tok
        for x in w:
            x = _res(x)
            x.w = tok
            x.r = {}

    def op(self, eng, fn, r=(), w=(), ww=()):
        o = Op(fn, "c")
        self._deps(eng, o, r, w, ww)
        idx = len(self.ops[eng])
        self.ops[eng].append(o)
        self._commit(("c", eng, idx), r, list(w) + list(ww))
        return o

    def dma(self, eng, fn, r=(), w=()):
        o = Op(fn, "d")
        m = self.ndma[eng]
        self.ndma[eng] += 1
        o.dma_m = m
        if m >= KDMA:
            self._need(eng, ("d", eng, m - KDMA), o)
        self._deps(eng, o, r, w)
        self.ops[eng].append(o)
        self._commit(("d", eng, m), r, w)
        return o

    def pe(self, fn, r=(), w=(), ww=()):
        return self.op("pe", fn, r, w, ww)

    def dve(self, fn, r=(), w=(), ww=()):
        return self.op("dve", fn, r, w, ww)

    def act(self, fn, r=(), w=(), ww=()):
        return self.op("act", fn, r, w, ww)

    def pool(self, fn, r=(), w=(), ww=()):
        return self.op("pool", fn, r, w, ww)

    def barrier(self):
        for eng in ENGS:
            o = Op(None, "n")
            for e in ENGS:
                for i in range(len(self.ops[e]) - 1, -1, -1):
                    if self.ops[e][i].kind == "c":
                        self._need(eng, ("c", e, i), o)
                        break
                n = self.ndma[e]
                for m in range(max(0, n - KDMA), n):
                    self._need(eng, ("d", e, m), o)
            self.ops[eng].append(o)

    def finish_waits(self):
        o = Op(None, "n")
        for q in ENGS:
            n = self.ndma[q]
            for m in range(max(0, n - KDMA), n):
                self._need("sp", ("d", q, m), o)
        self.ops["sp"].append(o)

    def _mksems(self):
        nc = self.nc
        self.sems = {}
        for e in ENGS:
            g = nc.semaphore("c_" + e)
            self.sems[("c", e)] = g.__enter__()
            self.ctx.append(g)
            for k in range(KDMA):
                g = nc.semaphore("d_%s_%d" % (e, k))
                self.sems[("d", e, k)] = g.__enter__()
                self.ctx.append(g)

    def flush(self, final=False):
        nc = self.nc
        if self.sems is None:
            self._mksems()
        if final:
            self.finish_waits()
        sems = self.sems
        start = dict(self.emitted)
        for e in ENGS:
            ops = self.ops[e]
            for i in range(len(ops) - 1, start[e] - 1, -1):
                if ops[i].kind == "c":
                    ops[i].marked = True
                    self.phase_end[e].append(i)
                    break
            c = self.cnt[e][-1] if self.cnt[e] else 0
            for o in ops[start[e]:]:
                if o.kind == "c" and o.marked:
                    c += 1
                self.cnt[e].append(c)
        cnt = self.cnt

        def run(ename, eng):
            for o in self.ops[ename][start[ename]:]:
                for tok in o.waits:
                    if tok[0] == "c":
                        eng.wait_ge(sems[("c", tok[1])], cnt[tok[1]][tok[2]])
                    else:
                        m = tok[2]
                        eng.wait_ge(sems[("d", tok[1], m % KDMA)], 16 * (m // KDMA + 1))
                if o.kind == "n":
                    continue
                ins = o.fn(eng)
                if o.kind == "d":
                    ins.then_inc(sems[("d", ename, o.dma_m % KDMA)], 16)
                elif o.marked:
                    ins.then_inc(sems[("c", ename)], 1)

        blk = nc.Block()
        block = blk.__enter__()

        @block.tensor
        def _(e):
            run("pe", e)

        @block.vector
        def _(e):
            run("dve", e)

        @block.scalar
        def _(e):
            run("act", e)

        @block.gpsimd
        def _(e):
            run("pool", e)

        @block.sync
        def _(e):
            run("sp", e)

        blk.__exit__(None, None, None)
        for e in ENGS:
            self.emitted[e] = len(self.ops[e])
        if not final:
            self.barrier()
        for g in reversed(self.pctx):
            g.__exit__(None, None, None)
        self.pctx = []
        if final:
            for g in reversed(self.ctx):
                g.__exit__(None, None, None)
            self.ctx = []

    def emit(self):
        self.flush(final=True)

    def stats(self):
        return {e: (len(self.ops[e]), sum(1 for o in self.ops[e] if o.marked),
                    sum(len(o.waits) for o in self.ops[e])) for e in ENGS}
D = 1024
NT = 36
NE = 32
EPS = 1e-6


def rowtype(t):
    return 2 if (t % 18) < 2 else t // 18


class K:
    pass


def dma(fw, q, out, in_, r, w, **kw):
    fw.dma(q, lambda e: e.dma_start(out=out, in_=in_, **kw), r=r, w=w)


def phase_ada(fw, k):
    ccT = fw.sbuf("ccT", [128, 8, 3], F32)
    scT = fw.sbuf("scT", [128, 8, 3], F32)
    for r_ in range(3):
        dma(fw, "sp", ccT[:, :, r_], k.cc[r_, :].rearrange("(c p) -> p c", p=128), [k.cc], [ccT],
            allow_slow_non_contiguous=True)
    fw.act(lambda e: e.activation(out=scT[:], in_=ccT[:], func=AF.Silu), r=[ccT], w=[scT])
    aw = [fw.sbuf("aw%d" % i, [128, 8, 768], F32) for i in range(2)]
    abT = fw.sbuf("abT", [128, 48], F32)
    ps = fw.psum("adaps", [128, 48, 3], F32)
    n = 0
    for l in range(2):
        dma(fw, "sp", abT[:], k.ada_b[l, :].rearrange("(j p) -> p j", p=128), [k.ada_b], [abT],
            allow_slow_non_contiguous=True)
        for blk in range(8):
            a = aw[n % 2]
            n += 1
            dma(fw, "sp", a[:], k.ada_w[l, :, blk * 768:(blk + 1) * 768].rearrange("(c p) f -> p c f", p=128),
                [k.ada_w], [a])
            for j in range(6):
                jj = blk * 6 + j
                for c in range(8):
                    fw.pe(lambda e, a=a, j=j, c=c, jj=jj: e.matmul(
                        ps[:, jj, :], a[:, c, j * 128:(j + 1) * 128], scT[:, c, :],
                        start=(c == 0), stop=(c == 7)), r=[a, scT], w=[ps])
        fw.dve(lambda e, l=l: e.tensor_tensor(
            out=k.modT[l][:], in0=ps[:], in1=abT[:].unsqueeze(2).to_broadcast([128, 48, 3]), op=ALU.add),
            r=[ps, abT], w=[k.modT[l]])
    fw.flush()


def load_gain_T(fw, k, name, src_row):
    t = fw.sbuf(name, [128, 8], F32)
    dma(fw, "sp", t[:], src_row.rearrange("(c p) -> p c", p=128), [], [t], allow_slow_non_contiguous=True)
    return t


def make_GS(fw, k, l, which, gain_T):
    G = fw.sbuf("G%d" % which, [128, 8, 3], F32)
    base = 0 if which == 0 else 24
    m = k.modT[l]
    fw.dve(lambda e: e.scalar_tensor_tensor(
        out=G[:], in0=m[:, base + 8:base + 16, :], scalar=1.0,
        in1=gain_T[:].unsqueeze(2).to_broadcast([128, 8, 3]), op0=ALU.add, op1=ALU.mult),
        r=[m, gain_T], w=[G])
    return G, base


class Front:
    def __init__(self, fw, k, nbuf=2):
        self.fw = fw
        self.k = k
        self.xt = [fw.sbuf("fe_xt%d" % i, [128, D], F32) for i in range(nbuf)]
        self.xh = [fw.sbuf("fe_xh%d" % i, [128, D], BF16) for i in range(2)]
        self.ss = [fw.sbuf("fe_ss%d" % i, [128, 1], F32) for i in range(2)]
        self.rstd = [fw.sbuf("fe_rs%d" % i, [128, 1], F32) for i in range(2)]
        self.tp = [fw.psum("fe_tp%d" % i, [128, 8, 128], BF16) for i in range(1)]
        self.n = 0

    def run(self, src_rows, src_res, G, l, base, row, dst_fn, dst_res):
        fw, k = self.fw, self.k
        i = self.n
        self.n += 1
        xt = self.xt[i % len(self.xt)]
        xh = self.xh[i % 2]
        ss = self.ss[i % 2]
        rstd = self.rstd[i % 2]
        tp = self.tp[0]
        junk = xh
        m = k.modT[l]
        dma(fw, "sp", xt[:], src_rows, [src_res], [xt])
        fw.pool(lambda e: e.memset(ss[:], 0.0), w=[ss])
        fw.act(lambda e: e.activation(out=junk[:], in_=xt[:], func=AF.Square, accum_out=ss[:]),
               r=[xt], w=[xh, ss])
        fw.dve(lambda e: e.tensor_scalar(out=rstd[:], in0=ss[:], scalar1=1.0 / D, scalar2=EPS,
                                         op0=ALU.mult, op1=ALU.add), r=[ss], w=[rstd])
        fw.act(lambda e: e.activation(out=rstd[:], in_=rstd[:], func=AF.Sqrt), r=[rstd], w=[rstd])
        fw.dve(lambda e: e.reciprocal(out=rstd[:], in_=rstd[:]), r=[rstd], w=[rstd])
        fw.dve(lambda e: e.tensor_scalar(out=xh[:], in0=xt[:], scalar1=rstd[:, 0:1], scalar2=None,
                                         op0=ALU.mult), r=[xt, rstd], w=[xh])
        for c in range(8):
            fw.pe(lambda e, c=c: e.transpose(tp[:, c, :], xh[:, c * 128:(c + 1) * 128], k.ident_bf[:]),
                  r=[xh, k.ident_bf], w=[tp])
        for c in range(8):
            fw.act(lambda e, c=c: e.activation(out=dst_fn(c), in_=tp[:, c, :], func=AF.Identity,
                                               scale=G[:, c, row:row + 1], bias=m[:, base + c, row:row + 1]),
                   r=[tp, G, m], ww=[dst_res])
        return xt


class Back:
    def __init__(self, fw, k, ht_bufs, ps_big):
        self.fw = fw
        self.k = k
        self.fT = [fw.sbuf("be_fT%d" % i, [128, 8, 128], F32) for i in range(1)]
        self.ht = ht_bufs
        self.ps = [ps_big]
        self.n = 0

    def run(self, srcT_fn, src_res, l, gbase, row, h_rows, h_res, dst_rows, dst_res):
        fw, k = self.fw, self.k
        i = self.n
        self.n += 1
        fT = self.fT[0]
        ht = self.ht[i % len(self.ht)]
        ps = self.ps[0]
        m = k.modT[l]
        dma(fw, "sp", ht[:], h_rows, [h_res], [ht])
        fp = fT.parts(8)
        for c in range(8):
            if c % 2 == 0:
                fw.dve(lambda e, c=c: e.tensor_scalar(out=fT[:, c, :], in0=srcT_fn(c),
                                                      scalar1=m[:, gbase + c, row:row + 1], scalar2=None,
                                                      op0=ALU.mult), r=[src_res, m], w=[fp[c]])
            else:
                fw.act(lambda e, c=c: e.activation(out=fT[:, c, :], in_=srcT_fn(c), func=AF.Copy,
                                                   scale=m[:, gbase + c, row:row + 1]), r=[src_res, m], w=[fp[c]])
        for c in range(8):
            fw.pe(lambda e, c=c: e.transpose(ps[:, c * 128:(c + 1) * 128], fT[:, c, :], k.ident_f[:]),
                  r=[fp[c], k.ident_f], w=[ps])
        fw.dve(lambda e: e.tensor_tensor(out=ht[:], in0=ps[:], in1=ht[:], op=ALU.add), r=[ps, ht], w=[ht])
        dma(fw, "sp", dst_rows, ht[:], [ht], [dst_res])
def subs(T):
    out = []
    o = 0
    while o < T:
        n = min(512, T - o)
        out.append((o, n))
        o += n
    return out


def phase_moe(fw, k, l, tiles, src, dst_fn, first_block):
    nt = len(tiles)
    TB = nt * 128
    SB = subs(TB)
    ynT = fw.sbuf("ynT", [128, 8, TB], BF16)
    acc = fw.sbuf("acc", [128, 8, TB], F32)
    actT = [fw.sbuf("actT%d" % i, [128, 8, TB], BF16) for i in range(2)]
    gbc = [fw.sbuf("gbc%d" % i, [128, TB], F32) for i in range(1)]
    st1 = [fw.sbuf("st1_%d" % i, [128, 8, 256], F32) for i in range(2)]
    w1b = [fw.sbuf("w1b%d" % i, [128, 8, 2, 128], BF16) for i in range(3)]
    st2 = [fw.sbuf("st2_%d" % i, [128, 1024], F32) for i in range(2)]
    w2b = fw.sbuf("w2b", [128, 8, 1024], BF16)
    w2p = w2b.parts(8)
    ub = [[fw.sbuf("ub%d_%d" % (i, j), [128, 512], F32) for j in range(3)] for i in range(2)]
    wr = fw.sbuf("wr", [128, 8, NE], BF16)
    rb = fw.sbuf("rb", [128, NE], F32)
    bout = V(st2[0][0:NE, :], st2[0].res)
    bin_sb = V(st1[0][0:NE, :, :].rearrange("p c f -> p (c f)"), st1[0].res)
    binT = fw.sbuf("binT", [128, 16, NE], F32)
    combT = fw.sbuf("combT", [NE, TB], F32)
    lg = fw.sbuf("lg", [128, NE], F32)
    m8 = fw.sbuf("m8", [128, 8], F32)
    nmx = fw.sbuf("nmx", [128, 1], F32)
    msk = fw.sbuf("msk", [128, NE], F32)
    ex = fw.sbuf("ex", [128, NE], F32)
    sm = fw.sbuf("sm", [128, 1], F32)
    comb = fw.sbuf("comb", [128, NE], F32)
    gn = load_gain_T(fw, k, "gnf", k.norm_ffn_g[l, :])
    G, base = make_GS(fw, k, l, 1, gn)
    fe = Front(fw, k)
    ps_big = fw.psum("ps_big", [128, 1024], F32)
    be = Back(fw, k, fe.xt, ps_big)
    ps_misc = fw.psum("ps_misc", [128, 512], F32)
    ps_g = [V(ps_big[:, 0:512], ps_big.res), fw.psum("ps_g1", [128, 512], F32)]
    ps_l = [V(ps_big[:, 512:1024], Res("ps_l0")), fw.psum("ps_l1", [128, 512], F32)]
    ps_y = [fw.psum("ps_y%d" % i, [128, 512], F32) for i in range(1)]
    combT_d = k.combT_d

    dma(fw, "pool", wr[:], k.router_w[l].rearrange("(c p) e -> p c e", p=128), [], [wr])
    dma(fw, "sp", rb[:], k.router_b[l:l + 1, :].partition_broadcast(128), [], [rb])
    dma(fw, "sp", bout[:], k.exp_b_out[l], [], [bout])
    dma(fw, "sp", bin_sb[:], k.exp_b_in[l], [], [bin_sb])
    for mt in range(16):
        m_, two = mt // 2, mt % 2
        fw.pe(lambda e, mt=mt, m_=m_, two=two: e.transpose(
            ps_misc[:, mt * NE:(mt + 1) * NE],
            bin_sb[:, two + 256 * m_: 256 * m_ + 256: 2], k.ident_f[0:NE, 0:NE]),
            r=[bin_sb, k.ident_f], w=[ps_misc])
    fw.dve(lambda e: e.tensor_copy(binT[:].rearrange("p a b -> p (a b)"), ps_misc[:, 0:16 * NE]),
           r=[ps_misc], w=[binT])
    fw.dve(lambda e: e.tensor_scalar(out=binT[:, 1::2, :], in0=binT[:, 1::2, :], scalar1=1.0, scalar2=None, op0=ALU.add),
           r=[binT], w=[binT])

    for i, t in enumerate(tiles):
        row = rowtype(t)
        fe.run(src[t * 128:(t + 1) * 128, :], src, G, l, base, row,
               lambda c, i=i: ynT[:, c, i * 128:(i + 1) * 128], ynT)
        for c in range(8):
            fw.pe(lambda e, c=c, i=i: e.matmul(ps_misc[:, 0:NE], ynT[:, c, i * 128:(i + 1) * 128], wr[:, c, :],
                                               start=(c == 0), stop=(c == 7)), r=[ynT, wr], w=[ps_misc])
        fw.dve(lambda e: e.tensor_tensor(out=lg[:], in0=ps_misc[:, 0:NE], in1=rb[:], op=ALU.add),
               r=[ps_misc, rb], w=[lg])
        fw.dve(lambda e: e.max(out=m8[:], in_=lg[:]), r=[lg], w=[m8])
        fw.dve(lambda e: e.tensor_scalar(out=msk[:], in0=lg[:], scalar1=m8[:, 3:4], scalar2=None, op0=ALU.is_ge),
               r=[lg, m8], w=[msk])
        fw.dve(lambda e: e.tensor_scalar(out=nmx[:], in0=m8[:, 0:1], scalar1=-1.0, scalar2=None, op0=ALU.mult),
               r=[m8], w=[nmx])
        fw.act(lambda e: e.activation(out=ex[:], in_=lg[:], func=AF.Exp, bias=nmx[:, 0:1]), r=[lg, nmx], w=[ex])
        fw.dve(lambda e: e.tensor_tensor(out=ex[:], in0=ex[:], in1=msk[:], op=ALU.mult), r=[ex, msk], w=[ex])
        fw.dve(lambda e: e.reduce_sum(out=sm[:], in_=ex[:], axis=AX.X), r=[ex], w=[sm])
        fw.dve(lambda e: e.reciprocal(out=sm[:], in_=sm[:]), r=[sm], w=[sm])
        fw.dve(lambda e: e.tensor_scalar(out=comb[:], in0=ex[:], scalar1=sm[:, 0:1], scalar2=None, op0=ALU.mult),
               r=[ex, sm], w=[comb])
        fw.pe(lambda e: e.transpose(ps_misc[0:NE, 128:256], comb[:], k.ident_f[:]), r=[comb, k.ident_f], w=[ps_misc])
        fw.act(lambda e, i=i: e.activation(out=combT[:, i * 128:(i + 1) * 128], in_=ps_misc[0:NE, 128:256], func=AF.Copy),
               r=[ps_misc], ww=[combT])
    dma(fw, "sp", combT_d[:, 0:TB], combT[:], [combT], [combT_d])

    for d in range(8):
        for (o, n) in SB:
            fw.pe(lambda e, d=d, o=o, n=n: e.matmul(ps_misc[:, 0:n], bout[:, d * 128:(d + 1) * 128], combT[:, o:o + n],
                                                    start=True, stop=True), r=[bout, combT], w=[ps_misc])
            fw.act(lambda e, d=d, o=o, n=n: e.activation(out=acc[:, d, o:o + n], in_=ps_misc[:, 0:n], func=AF.Copy),
                   r=[ps_misc], ww=[acc])

    cnt = {"w1": 0, "u": 0, "cast": 0}

    def load_w1(e_, m_):
        i = cnt["w1"]
        cnt["w1"] += 1
        s_ = st1[i % 2]
        wb = w1b[i % 3]
        dma(fw, "sp", s_[:], k.exp_w_in[l, e_].rearrange("(c p) f -> p c f", p=128)[:, :, m_ * 256:(m_ + 1) * 256],
            [], [s_])
        if True:
            fw.act(lambda e: e.activation(out=wb[:], in_=s_[:].rearrange("p c (j two) -> p c two j", two=2), func=AF.Copy),
                   r=[s_], w=[wb])
        else:
            fw.pool(lambda e: e.tensor_copy(wb[:], s_[:].rearrange("p c (j two) -> p c two j", two=2)),
                    r=[s_], w=[wb])
        return wb

    def load_w2_piece(e_, m_):
        i = cnt["cast"]
        cnt["cast"] += 1
        s_ = st2[i % 2]
        dma(fw, "sp", s_[:], k.exp_w_out[l, e_, m_ * 128:(m_ + 1) * 128, :], [], [s_])
        if i % 2 == 0:
            fw.pool(lambda e, m_=m_, s_=s_: e.tensor_copy(w2b[:, m_, :], s_[:]), r=[s_], w=[w2p[m_]])
        else:
            fw.act(lambda e, m_=m_, s_=s_: e.activation(out=w2b[:, m_, :], in_=s_[:], func=AF.Copy), r=[s_], w=[w2p[m_]])

    def mm1(e_, w2_of=None):
        g = gbc[0]
        dma(fw, "sp", g[:], combT_d[e_:e_ + 1, 0:TB].partition_broadcast(128), [combT_d], [g])
        aT = actT[e_ % 2]
        for m_ in range(8):
            wb = load_w1(e_, m_)
            if w2_of is not None:
                load_w2_piece(w2_of, m_)
            for (o, n) in SB:
                u = cnt["u"]
                cnt["u"] += 1
                pg, pl = ps_g[u % 2], ps_l[u % 2]
                A, B, C = ub[u % 2]
                for c in range(8):
                    fw.pe(lambda e, c=c, o=o, n=n, wb=wb, pg=pg: e.matmul(pg[:, 0:n], wb[:, c, 0, :], ynT[:, c, o:o + n],
                                                                          start=(c == 0), stop=(c == 7)), r=[wb, ynT], w=[pg])
                for c in range(8):
                    fw.pe(lambda e, c=c, o=o, n=n, wb=wb, pl=pl: e.matmul(pl[:, 0:n], wb[:, c, 1, :], ynT[:, c, o:o + n],
                                                                          start=(c == 0), stop=(c == 7)), r=[wb, ynT], w=[pl])
                bg = binT[:, 2 * m_, e_:e_ + 1]
                bl = binT[:, 2 * m_ + 1, e_:e_ + 1]
                fw.dve(lambda e, n=n, pg=pg, A=A, bg=bg: e.tensor_scalar(out=A[:, 0:n], in0=pg[:, 0:n], scalar1=bg, scalar2=7.0,
                                                                        op0=ALU.add, op1=ALU.min), r=[pg, binT], w=[A])
                fw.act(lambda e, n=n, A=A, B=B: e.activation(out=B[:, 0:n], in_=A[:, 0:n], func=AF.Sigmoid, scale=1.702),
                       r=[A], w=[B])
                fw.act(lambda e, n=n, pl=pl, C=C, bl=bl: e.activation(out=C[:, 0:n], in_=pl[:, 0:n], func=AF.Identity, bias=bl),
                       r=[pl, binT], w=[C])
                fw.pool(lambda e, n=n, C=C: e.tensor_scalar(out=C[:, 0:n], in0=C[:, 0:n], scalar1=8.0, scalar2=-6.0,
                                                           op0=ALU.min, op1=ALU.max), r=[C], w=[C])
                fw.dve(lambda e, n=n, A=A, B=B: e.tensor_tensor(out=A[:, 0:n], in0=A[:, 0:n], in1=B[:, 0:n], op=ALU.mult),
                       r=[A, B], w=[A])
                fw.pool(lambda e, n=n, A=A, C=C: e.tensor_tensor(out=C[:, 0:n], in0=C[:, 0:n], in1=A[:, 0:n], op=ALU.mult),
                        r=[A, C], w=[C])
                fw.dve(lambda e, n=n, o=o, C=C, g=g, aT=aT, m_=m_: e.tensor_tensor(out=aT[:, m_, o:o + n], in0=C[:, 0:n],
                                                                                  in1=g[:, o:o + n], op=ALU.mult),
                       r=[C, g], ww=[aT])

    def mm2(e_):
        aT = actT[e_ % 2]
        for d in range(8):
            for (o, n) in SB:
                py = ps_y[0]
                for m_ in range(8):
                    fw.pe(lambda e, m_=m_, d=d, o=o, n=n, py=py: e.matmul(py[:, 0:n], w2b[:, m_, d * 128:(d + 1) * 128],
                                                                          aT[:, m_, o:o + n], start=(m_ == 0), stop=(m_ == 7)),
                          r=[w2p[m_], aT], w=[py])
                fw.dve(lambda e, d=d, o=o, n=n, py=py: e.tensor_tensor(out=acc[:, d, o:o + n], in0=py[:, 0:n],
                                                                      in1=acc[:, d, o:o + n], op=ALU.add),
                       r=[py], ww=[acc])

    ne = k.n_experts_dbg if hasattr(k, "n_experts_dbg") else NE
    mm1(0)
    for e_ in range(ne):
        if e_ + 1 < ne:
            mm1(e_ + 1, w2_of=e_)
        else:
            for m_ in range(8):
                load_w2_piece(e_, m_)
        mm2(e_)

    for i, t in enumerate(tiles):
        row = rowtype(t)
        drows, dres = dst_fn(t)
        if drows is None:
            continue
        be.run(lambda c, i=i: acc[:, c, i * 128:(i + 1) * 128], acc, l, 40, row,
               src[t * 128:(t + 1) * 128, :], src, drows, dres)
    fw.flush()
def ucol(s):
    return 16 + s if s < 256 else s + 48


def phase_ab(fw, k, b):
    l = 0
    t0 = 18 * b
    TS = 2304
    xnT = fw.sbuf("xnT", [128, 8, TS], BF16)
    OT = xnT
    QT = fw.sbuf("QT", [128, 4, TS], BF16)
    KT = fw.sbuf("KT", [128, 4, TS], BF16)
    Vt = fw.sbuf("Vt", [128, 18, 512], BF16)
    Vo = fw.sbuf("Vo", [128, 17, 512], BF16)
    UT = fw.sbuf("UT", [128, 4, 2368], BF16)
    w_in = fw.sbuf("w_in", [128, 8, 2048], BF16)
    EB = V(w_in[:].rearrange("p h (o jp q) -> p h o jp q", o=8, jp=4), w_in.res)
    w_out = V(UT[:].rearrange("p g x -> p (g x)")[:, 0:8 * D].rearrange("p (c f) -> p c f", c=8), UT.res)
    pw = fw.sbuf("pw", [128, 4, 128], BF16)
    pscale = fw.sbuf("pscale", [128, 4], F32)
    gq = fw.sbuf("gq", [128, 1], F32)
    gk = fw.sbuf("gk", [128, 1], F32)
    bd = fw.sbuf("bd", [128, 128], BF16)
    ones = fw.sbuf("ones", [128, 128], BF16)
    sqb = [fw.sbuf("sqb%d" % i, [128, 512], BF16) for i in range(2)]
    rstd = [fw.sbuf("qrstd%d" % i, [128, 512], F32) for i in range(2)]
    tA = fw.sbuf("ptA", [128, 544], F32)
    tB = fw.sbuf("ptB", [128, 544], F32)
    rcb = fw.sbuf("rcb", [128, 512], F32)
    pooled = fw.sbuf("pooled", [128, 512], BF16)
    mk = fw.sbuf("mk", [128, 4, 64], F32)
    rbt = [fw.sbuf("rbt%d" % i, [128, 4, 64], F32) for i in range(2)]
    Pb = [fw.sbuf("Pb%d" % i, [128, 6, 64], BF16) for i in range(3)]
    rd = [fw.sbuf("rd%d" % i, [128, 64], F32) for i in range(2)]
    resT = fw.sbuf("resT", [128, 8, 128], F32)
    gn = load_gain_T(fw, k, "gnm", k.norm_mix_g[l, :])
    G, base = make_GS(fw, k, l, 0, gn)
    fe = Front(fw, k)
    big = fw.psum("ab_big", [128, 1024], F32)
    pq = fw.psum("ab_pq", [128, 2048], F32)
    be = Back(fw, k, fe.xt, big)
    pAB = [V(big[:, 0:512], Res("pA")), V(big[:, 512:1024], Res("pB"))]
    pS = V(pq[:, 0:512], Res("pS"))
    pO = [V(pq[:, 0:64], pS.res), V(pq[:, 512:576], Res("pO1"))]
    pD = [V(pq[:, 1024:1088], Res("pD0")), V(pq[:, 1536:1600], Res("pD1"))]

    for c in range(8):
        dma(fw, "pool", w_in[:, c, :], k.ab_w_in[0, c * 128:(c + 1) * 128, :], [], [w_in])
    fw.pool(lambda e: e.memset(UT[:], 0.0), w=[UT])
    for (g_, src) in ((gq, k.na_q_g), (gk, k.na_k_g)):
        for hl in range(2):
            dma(fw, "sp", g_[hl * 64:(hl + 1) * 64, :], src[0, :].rearrange("(p o) -> p o", o=1), [], [g_])
    fw.dve(lambda e: e.tensor_scalar(out=gq[:], in0=gq[:], scalar1=0.125, scalar2=None, op0=ALU.mult), r=[gq], w=[gq])
    fw.pool(lambda e: e.memset(bd[:], 0.0), w=[bd])
    fw.pool(lambda e: e.memset(bd[0:64, 0:64], 1.0), w=[bd])
    fw.pool(lambda e: e.memset(bd[64:128, 64:128], 1.0), w=[bd])
    fw.pool(lambda e: e.memset(ones[:], 1.0), w=[ones])
    dma(fw, "pool", pw[:], k.pool_w[0].rearrange("g c d -> c g d"), [], [pw])
    dma(fw, "sp", pscale[:], k.pool_scale[0, :].rearrange("(g p) -> p g", p=128), [], [pscale],
        allow_slow_non_contiguous=True)
    dma(fw, "sp", mk[:], k.c_namask[:, :].rearrange("(jp p) q -> p jp q", p=128), [], [mk])

    for i in range(18):
        t = t0 + i
        fe.run(k.xin[t * 128:(t + 1) * 128, :], k.xin, G, l, base, rowtype(t),
               lambda c, i=i: xnT[:, c, i * 128:(i + 1) * 128], xnT)

    n_ = 0
    for m in range(8):
        dstT, gg = (QT, gq) if m < 4 else (KT, gk)
        mm = m % 4
        for (o, n) in subs(TS):
            ps = pAB[n_ % 2]
            sq = sqb[n_ % 2]
            rs = rstd[n_ % 2]
            n_ += 1
            for c in range(8):
                fw.pe(lambda e, c=c, m=m, o=o, n=n, ps=ps: e.matmul(ps[:, 0:n], w_in[:, c, m * 128:(m + 1) * 128],
                                                                   xnT[:, c, o:o + n], start=(c == 0), stop=(c == 7)),
                      r=[w_in, xnT], w=[ps])
            fw.act(lambda e, n=n, ps=ps, sq=sq: e.activation(out=sq[:, 0:n], in_=ps[:, 0:n], func=AF.Square), r=[ps], w=[sq])
            fw.pe(lambda e, n=n, sq=sq: e.matmul(pS[:, 0:n], bd[:], sq[:, 0:n], start=True, stop=True), r=[bd, sq], w=[pS])
            fw.dve(lambda e, n=n, rs=rs: e.tensor_scalar(out=rs[:, 0:n], in0=pS[:, 0:n], scalar1=1.0 / 64, scalar2=EPS,
                                                        op0=ALU.mult, op1=ALU.add), r=[pS], w=[rs])
            fw.act(lambda e, n=n, rs=rs: e.activation(out=rs[:, 0:n], in_=rs[:, 0:n], func=AF.Sqrt), r=[rs], w=[rs])
            fw.dve(lambda e, n=n, rs=rs: e.reciprocal(out=rs[:, 0:n], in_=rs[:, 0:n]), r=[rs], w=[rs])
            fw.dve(lambda e, n=n, o=o, rs=rs, ps=ps, dstT=dstT, gg=gg, mm=mm: e.scalar_tensor_tensor(
                out=dstT[:, mm, o:o + n], in0=ps[:, 0:n], scalar=gg[:, 0:1], in1=rs[:, 0:n], op0=ALU.mult, op1=ALU.mult),
                r=[ps, rs, gg], ww=[dstT])
    for (dst, ntile, off) in ((Vt, 18, 0), (Vo, 17, 64)):
        for i in range(ntile):
            ps = pAB[n_ % 2]
            n_ += 1
            for c in range(8):
                fw.pe(lambda e, c=c, i=i, off=off, ps=ps: e.matmul(ps[:, :], xnT[:, c, off + i * 128: off + (i + 1) * 128],
                                                                  w_in[:, c, 1024:1536], start=(c == 0), stop=(c == 7)),
                      r=[w_in, xnT], w=[ps])
            fw.act(lambda e, i=i, ps=ps, dst=dst: e.activation(out=dst[:, i, :], in_=ps[:, :], func=AF.Copy), r=[ps], ww=[dst])
    for g in range(4):
        for (o, n) in subs(TS):
            ps = pAB[n_ % 2]
            n_ += 1
            for c in range(8):
                fw.pe(lambda e, c=c, g=g, o=o, n=n, ps=ps: e.matmul(ps[:, 0:n], w_in[:, c, 1536 + g * 128:1536 + (g + 1) * 128],
                                                                   xnT[:, c, o:o + n], start=(c == 0), stop=(c == 7)),
                      r=[w_in, xnT], w=[ps])
            pieces = [(o, n)] if o >= 256 else [(0, 256), (256, n - 256)]
            for (po_, pn) in pieces:
                fw.dve(lambda e, g=g, po_=po_, pn=pn, o=o, ps=ps: e.tensor_copy(UT[:, g, ucol(po_):ucol(po_) + pn],
                                                                              ps[:, po_ - o:po_ - o + pn]), r=[ps], ww=[UT])

    for g, w in enumerate((2, 4, 8, 16)):
        nlev = (2, 4, 8, 16).index(w) + 1
        plist = [(16, 256, 0, 1, 0)] + [(304 + 512 * j, 512, 256 + 512 * j, 0, 512 * j) for j in range(4)]
        for (lo, n, s0, seg, p0) in plist:
            W = n + 32
            xb = lo - 16
            fw.dve(lambda e, g=g, W=W, xb=xb: e.tensor_tensor(out=tA[:, 1:W], in0=UT[:, g, xb + 1:xb + W],
                                                             in1=UT[:, g, xb:xb + W - 1], op=ALU.add), r=[UT], w=[tA])
            cur, oth = tA, tB
            sh = 2
            valid = 1
            for lev in range(1, nlev):
                lo_x = valid + sh
                fw.dve(lambda e, W=W, cur=cur, oth=oth, lo_x=lo_x, sh=sh: e.tensor_tensor(
                    out=oth[:, lo_x:W], in0=cur[:, lo_x:W], in1=cur[:, lo_x - sh:W - sh], op=ALU.add), r=[cur], w=[oth])
                cur, oth = oth, cur
                valid = lo_x
                sh *= 2
            X0 = 16 + w // 2 - 1
            dma(fw, "sp", rcb[:, 0:n], k.c_poolrc[g, seg:seg + 1, 16 + p0:16 + p0 + n].partition_broadcast(128), [], [rcb])
            fw.dve(lambda e, n=n, cur=cur, oth=oth, X0=X0: e.tensor_tensor(out=oth[:, 0:n], in0=cur[:, X0:X0 + n],
                                                                         in1=rcb[:, 0:n], op=ALU.mult), r=[cur, rcb], w=[oth])
            fw.pool(lambda e, n=n, oth=oth, g=g, lo=lo: e.tensor_tensor(out=pooled[:, 0:n], in0=oth[:, 0:n],
                                                                       in1=UT[:, g, lo:lo + n], op=ALU.subtract),
                    r=[oth, UT], w=[pooled])
            ps = pAB[n_ % 2]
            n_ += 1
            fw.pe(lambda e, n=n, g=g, ps=ps: e.matmul(ps[:, 0:n], pw[:, g, :], pooled[:, 0:n], start=True, stop=True),
                  r=[pw, pooled], w=[ps])
            fw.act(lambda e, n=n, g=g, s0=s0, ps=ps: e.activation(out=OT[:, 4 + g, s0:s0 + n], in_=ps[:, 0:n], func=AF.Copy,
                                                                 scale=pscale[:, g:g + 1]), r=[ps, pscale], ww=[OT])

    n2 = 0
    for h in range(8):
        for o in range(8):
            rb_ = rbt[n2 % 2]
            n2 += 1
            dma(fw, "sp", rb_[:], k.rpb_g[h, o].rearrange("(jp p) q -> p jp q", p=128), [], [rb_])
            fw.pool(lambda e, rb_=rb_: e.tensor_tensor(out=rb_[:], in0=rb_[:], in1=mk[:], op=ALU.add), r=[rb_, mk], w=[rb_])
            fw.act(lambda e, rb_=rb_, h=h, o=o: e.activation(out=EB[:, h, o, :, :], in_=rb_[:], func=AF.Exp), r=[rb_], ww=[EB])

    for c in range(8):
        dma(fw, "pool", w_out[:, c, :], k.ab_w_out[0, c * 128:(c + 1) * 128, :], [], [w_out])

    cnt = {"u": 0}
    pst = pAB

    def unit(qs, m, hl, ktiles, eb):
        u = cnt["u"]
        cnt["u"] += 1
        bp = 64 * hl
        ps = pst[u % 2]
        P = Pb[u % 3]
        rdd = rd[u % 2]
        nk = len(ktiles)
        for kt, (ks, vt) in enumerate(ktiles):
            fw.pe(lambda e, kt=kt, ks=ks, ps=ps: e.matmul(ps[:, kt * 64:(kt + 1) * 64], KT[bp:bp + 64, m, ks:ks + 128],
                                                         QT[bp:bp + 64, m, qs:qs + 64], start=True, stop=True),
                  r=[KT, QT], w=[ps])
        fw.act(lambda e, nk=nk, ps=ps, P=P: e.activation(out=P[:, 0:nk, :].rearrange("p a b -> p (a b)"), in_=ps[:, 0:nk * 64],
                                                        func=AF.Exp), r=[ps], w=[P])
        if eb is not None:
            fw.dve(lambda e, P=P, eb=eb: e.tensor_tensor(out=P[:, 0:4, :], in0=P[:, 0:4, :], in1=eb, op=ALU.mult),
                   r=[P, EB], w=[P])
        for kt, (ks, vt) in enumerate(ktiles):
            fw.pe(lambda e, kt=kt, vt=vt, P=P: e.matmul(pO[hl][:, :], vt[:, m * 128:(m + 1) * 128], P[:, kt, :],
                                                       start=(kt == 0), stop=(kt == nk - 1)), r=[Vt, Vo, P], w=[pO[hl]])
        for kt, (ks, vt) in enumerate(ktiles):
            fw.pe(lambda e, kt=kt, P=P: e.matmul(pD[hl][:, :], ones[:], P[:, kt, :],
                                                start=(kt == 0), stop=(kt == nk - 1)), r=[ones, P], w=[pD[hl]])
        fw.dve(lambda e, rdd=rdd: e.reciprocal(out=rdd[bp:bp + 64, :], in_=pD[hl][bp:bp + 64, :]), r=[pD[hl]], w=[rdd])
        fw.dve(lambda e, rdd=rdd: e.tensor_tensor(out=OT[bp:bp + 64, m, qs:qs + 64], in0=pO[hl][bp:bp + 64, :],
                                                 in1=rdd[bp:bp + 64, :], op=ALU.mult), r=[pO[hl], rdd], ww=[OT])

    ctx_tiles = [(0, Vt[:, 0, :]), (128, Vt[:, 1, :])]
    for m in range(4):
        for qb in range(4):
            for hl in range(2):
                unit(qb * 64, m, hl, ctx_tiles, None)
        for r in range(32):
            rs_ = min(max(r - 4, 0), 24)
            o = r - rs_
            kts = []
            for kt in range(4):
                rho = rs_ + 2 * kt
                ks = 256 + rho * 64
                vt = Vt[:, 2 + rho // 2, :] if rho % 2 == 0 else Vo[:, (3 + rho) // 2, :]
                kts.append((ks, vt))
            kts += ctx_tiles
            for hl in range(2):
                unit(256 + r * 64, m, hl, kts, EB[:, 2 * m + hl, o, :, :])

    pOP = V(pq[:, 0:1024].rearrange("p (d t) -> p d t", d=8), pS.res)
    for i in range(18):
        t = t0 + i
        for d in range(8):
            for c in range(8):
                fw.pe(lambda e, c=c, d=d, i=i: e.matmul(pOP[:, d, :], w_out[:, c, d * 128:(d + 1) * 128],
                                                       OT[:, c, i * 128:(i + 1) * 128], start=(c == 0), stop=(c == 7)),
                      r=[w_out, OT], w=[pOP])
        fw.act(lambda e: e.activation(out=resT[:].rearrange("p d t -> p (d t)"), in_=pq[:, 0:1024], func=AF.Copy),
               r=[pOP], w=[resT])
        be.run(lambda c: resT[:, c, :], resT, l, 16, rowtype(t),
               k.xin[t * 128:(t + 1) * 128, :], k.xin, k.H[0][t * 128:(t + 1) * 128, :], k.H[0])
    fw.flush()
C0 = 0.6065306597126334
TSQ = 2304


def rw_declare(fw, k):
    k.rw = {}
    for nm in ("r", "kk", "k0", "k1", "b0", "b1", "s0", "s1", "g", "bonus", "y"):
        k.rw[nm] = fw.dram("rw_" + nm, [TSQ, D], F32)
    k.rw["v"] = fw.dram("rw_v", [TSQ, D], BF16)


def bc_load(fw, name, row_ap):
    t = fw.sbuf(name, [128, D], F32)
    dma(fw, "sp", t[:], row_ap.partition_broadcast(128), [], [t])
    return t


def phase_rw_feat(fw, k, b, src):
    l = 1
    t0 = 18 * b
    A = k.rw
    xp = fw.sbuf("xp", [128, 8, 2310], BF16)
    col = lambda s: 2 + s if s < 256 else 4 + s
    wr = fw.sbuf("rw_wr", [128, 8, D], BF16)
    wk = fw.sbuf("rw_wk", [128, 8, D], BF16)
    wv = fw.sbuf("rw_wv", [128, 8, D], BF16)
    w1 = fw.sbuf("rw_w1", [128, 2, 8, 64], BF16)
    a1 = fw.sbuf("rw_a1", [128, 2, 8, 64], BF16)
    g1 = fw.sbuf("rw_g1", [128, 8, 128], BF16)
    w2 = fw.sbuf("rw_w2", [128, 2, D], BF16)
    a2 = fw.sbuf("rw_a2", [128, 2, D], BF16)
    g2 = fw.sbuf("rw_g2", [128, D], BF16)
    mu = fw.sbuf("rw_mu", [128, 6, 8], F32)
    w0b = [bc_load(fw, "w0b%d" % d, k.rw_w0[0, d:d + 1, :]) for d in range(2)]
    a0b = [bc_load(fw, "a0b%d" % d, k.rw_a0[0, d:d + 1, :]) for d in range(2)]
    kkb = bc_load(fw, "kkb", k.rw_k_k[0:1, :])
    kab = bc_load(fw, "kab", k.rw_k_a[0:1, :])
    rkb = bc_load(fw, "rkb", k.rw_r_k[0:1, :, :].rearrange("o h d -> o (h d)"))
    for (dst, srcw) in ((wr, k.rw_w_r), (wk, k.rw_w_k), (wv, k.rw_w_v)):
        for c in range(8):
            dma(fw, "pool", dst[:, c, :], srcw[0, c * 128:(c + 1) * 128, :], [], [dst])
    for d in range(2):
        dma(fw, "pool", w1[:, d, :, :], k.rw_w1[0, d].rearrange("(c p) n -> p c n", p=128), [], [w1])
        dma(fw, "pool", a1[:, d, :, :], k.rw_a1[0, d].rearrange("(c p) n -> p c n", p=128), [], [a1])
        dma(fw, "pool", w2[0:64, d, :], k.rw_w2[0, d], [], [w2])
        dma(fw, "pool", a2[0:64, d, :], k.rw_a2[0, d], [], [a2])
    dma(fw, "pool", g1[:], k.rw_g1[0].rearrange("(c p) n -> p c n", p=128), [], [g1])
    dma(fw, "pool", g2[:], k.rw_g2[0], [], [g2])
    for j in range(6):
        dma(fw, "sp", mu[:, j, :], k.rw_mu[0, j, :].rearrange("(c p) -> p c", p=128), [], [mu], allow_slow_non_contiguous=True)
    fw.pool(lambda e: e.memset(xp[:], 0.0), w=[xp])
    gn = load_gain_T(fw, k, "gnm1", k.norm_mix_g[l, :])
    G, base = make_GS(fw, k, l, 0, gn)
    fe = Front(fw, k)
    for i in range(18):
        t = t0 + i
        fe.run(src[t * 128:(t + 1) * 128, :], src, G, l, base, rowtype(t),
               lambda c, i=i: xp[:, c, col(i * 128):col(i * 128) + 128], xp)

    xx = fw.sbuf("rw_xx", [128, 8, 128], F32)
    tmpx = fw.sbuf("rw_tmpx", [128, 8, 128], F32)
    xm = [fw.sbuf("rw_xm%d" % j, [128, 8, 128], BF16) for j in range(6)]
    hT = [fw.sbuf("rw_hT%d" % i, [128, 128], BF16) for i in range(5)]
    pp = [fw.psum("rwf_pp%d" % i, [128, 1024], F32) for i in range(3)]
    ph = fw.psum("rwf_ph", [128, 4, 128], F32)
    tr = fw.sbuf("t_r", [128, D], F32)
    tk = fw.sbuf("t_k", [128, D], F32)
    tvb = fe.xh[0]
    tkk = fw.sbuf("t_kk", [128, D], F32)
    tsq = fe.xt[0]
    ta = [fw.sbuf("t_a", [128, D], F32)] * 2
    tsg = [fw.sbuf("t_sg", [128, D], F32)] * 2
    tkd = [fw.sbuf("t_kd", [128, D], F32)] * 2
    tbt = [fw.sbuf("t_bt", [128, D], F32)] * 2
    tg = tsq
    tbo = fe.xt[1]
    s16 = [fw.sbuf("t_s16_%d" % i, [128, 16], F32) for i in range(3)]
    v3 = lambda t_: t_[:].rearrange("p (h d) -> p h d", d=64)
    b16 = lambda s_: s_[:].unsqueeze(2).to_broadcast([128, 16, 64])

    for i in range(getattr(k, "rwf_tiles", 18)):
        c0_ = col(i * 128)
        rows = slice(i * 128, (i + 1) * 128)
        fw.pool(lambda e, c0_=c0_: e.tensor_tensor(out=tmpx[:], in0=xp[:, :, c0_ - 1:c0_ + 127], in1=xp[:, :, c0_ + 1:c0_ + 129],
                                                  op=ALU.add), r=[xp], w=[tmpx])
        fw.dve(lambda e, c0_=c0_: e.scalar_tensor_tensor(out=xx[:], in0=tmpx[:], scalar=0.5, in1=xp[:, :, c0_:c0_ + 128],
                                                        op0=ALU.mult, op1=ALU.subtract), r=[tmpx, xp], w=[xx])
        for j in range(6):
            fw.pool(lambda e, j=j: e.tensor_tensor(out=tmpx[:], in0=xx[:], in1=mu[:, j, :].unsqueeze(2).to_broadcast([128, 8, 128]),
                                                  op=ALU.mult), r=[xx, mu], w=[tmpx])
            fw.pool(lambda e, j=j, c0_=c0_: e.tensor_tensor(out=xm[j][:], in0=tmpx[:], in1=xp[:, :, c0_:c0_ + 128], op=ALU.add),
                    r=[tmpx, xp], w=[xm[j]])
        if getattr(k, 'rwf_stage', 99) <= 1:
            continue
        for (pi, xj, wt) in ((0, 0, wr), (1, 2, wk), (2, 3, wv)):
            for half in range(2):
                for c in range(8):
                    fw.pe(lambda e, pi=pi, xj=xj, wt=wt, half=half, c=c: e.matmul(
                        pp[pi][:, half * 512:(half + 1) * 512], xm[xj][:, c, :], wt[:, c, half * 512:(half + 1) * 512],
                        start=(c == 0), stop=(c == 7)), r=[xm[xj], wt], w=[pp[pi]])
        var = getattr(k, "rwf_var", "")
        if var != "noevac":
            if var != "nor":
                fw.act(lambda e: e.activation(out=tr[:], in_=pp[0][:], func=AF.Copy), r=[pp[0]], w=[tr])
            if var != "nok":
                fw.act(lambda e: e.activation(out=tk[:], in_=pp[1][:], func=AF.Copy), r=[pp[1]], w=[tk])
            if var != "nokk":
                fw.pool(lambda e: e.tensor_tensor(out=tkk[:], in0=tk[:], in1=kkb[:], op=ALU.mult), r=[tk, kkb], w=[tkk])
            if var != "nov":
                fw.act(lambda e: e.activation(out=tvb[:], in_=pp[2][:], func=AF.Copy), r=[pp[2]], w=[tvb])
        if getattr(k, 'rwf_stage', 99) <= 2:
            continue
        for (hi, xj, wt, d, M) in ((0, 1, w1, 0, 64), (1, 1, w1, 1, 64), (2, 4, a1, 0, 64), (3, 4, a1, 1, 64), (4, 5, g1, None, 128)):
            for c in range(8):
                lhs = (lambda wt=wt, d=d, c=c: wt[:, c, :]) if d is None else (lambda wt=wt, d=d, c=c: wt[:, d, c, :])
                if hi < 4:
                    fw.pe(lambda e, hi=hi, xj=xj, c=c, M=M, lhs=lhs: e.matmul(ph[0:M, hi, :], lhs(), xm[xj][:, c, :],
                                                                            start=(c == 0), stop=(c == 7)), r=[xm[xj], wt], w=[ph])
                else:
                    fw.pe(lambda e, xj=xj, c=c, lhs=lhs: e.matmul(pp[2][:, 0:128], lhs(), xm[xj][:, c, :],
                                                                 start=(c == 0), stop=(c == 7)), r=[xm[xj], wt], w=[pp[2]])
        for hi, (fn, M) in enumerate(((AF.Tanh, 64), (AF.Tanh, 64), (AF.Copy, 64), (AF.Copy, 64), (AF.Sigmoid, 128))):
            if hi < 4:
                fw.act(lambda e, hi=hi, fn=fn, M=M: e.activation(out=hT[hi][0:M, :], in_=ph[0:M, hi, :], func=fn), r=[ph], w=[hT[hi]])
            else:
                fw.act(lambda e, hi=hi, fn=fn: e.activation(out=hT[hi][:, :], in_=pp[2][:, 0:128], func=fn), r=[pp[2]], w=[hT[hi]])
        if getattr(k, 'rwf_stage', 99) <= 3:
            continue
        for half in range(2):
            fw.pe(lambda e, half=half: e.matmul(pp[2][:, half * 512:(half + 1) * 512], hT[4][:, :], g2[:, half * 512:(half + 1) * 512],
                                               start=True, stop=True), r=[hT[4], g2], w=[pp[2]])
        fw.pool(lambda e: e.tensor_tensor(out=tsq[:], in0=tkk[:], in1=tkk[:], op=ALU.mult), r=[tkk], w=[tsq])
        fw.dve(lambda e: e.reduce_sum(out=s16[0][:], in_=v3(tsq), axis=AX.X), r=[tsq], w=[s16[0]])
        fw.dve(lambda e: e.tensor_scalar(out=s16[0][:], in0=s16[0][:], scalar1=1e-12, scalar2=None, op0=ALU.add), r=[s16[0]], w=[s16[0]])
        fw.act(lambda e: e.activation(out=s16[0][:], in_=s16[0][:], func=AF.Sqrt), r=[s16[0]], w=[s16[0]])
        fw.dve(lambda e: e.reciprocal(out=s16[0][:], in_=s16[0][:]), r=[s16[0]], w=[s16[0]])
        fw.dve(lambda e: e.tensor_tensor(out=v3(tkk), in0=v3(tkk), in1=b16(s16[0]), op=ALU.mult), r=[tkk, s16[0]], w=[tkk])
        fw.pool(lambda e: e.tensor_tensor(out=tsq[:], in0=tr[:], in1=rkb[:], op=ALU.mult), r=[tr, rkb], w=[tsq])
        for d in range(2):
            for half in range(2):
                fw.pe(lambda e, d=d, half=half: e.matmul(pp[0][:, half * 512:(half + 1) * 512], hT[d][0:64, :],
                                                        w2[0:64, d, half * 512:(half + 1) * 512], start=True, stop=True),
                      r=[hT[d], w2], w=[pp[0]])
                fw.pe(lambda e, d=d, half=half: e.matmul(pp[1][:, half * 512:(half + 1) * 512], hT[2 + d][0:64, :],
                                                        a2[0:64, d, half * 512:(half + 1) * 512], start=True, stop=True),
                      r=[hT[2 + d], a2], w=[pp[1]])
            fw.dve(lambda e, d=d: e.tensor_tensor(out=tsg[d][:], in0=pp[0][:], in1=w0b[d][:], op=ALU.add), r=[pp[0], w0b[d]], w=[tsg[d]])
            fw.act(lambda e, d=d: e.activation(out=tsg[d][:], in_=tsg[d][:], func=AF.Sigmoid), r=[tsg[d]], w=[tsg[d]])
            fw.dve(lambda e, d=d: e.tensor_tensor(out=ta[d][:], in0=pp[1][:], in1=a0b[d][:], op=ALU.add), r=[pp[1], a0b[d]], w=[ta[d]])
            fw.act(lambda e, d=d: e.activation(out=ta[d][:], in_=ta[d][:], func=AF.Sigmoid), r=[ta[d]], w=[ta[d]])
            fw.dve(lambda e, d=d: e.scalar_tensor_tensor(out=tkd[d][:], in0=ta[d][:], scalar=-1.0, in1=kab[:], op0=ALU.add, op1=ALU.mult),
                   r=[ta[d], kab], w=[tkd[d]])
            fw.dve(lambda e, d=d: e.scalar_tensor_tensor(out=tkd[d][:], in0=tkd[d][:], scalar=1.0, in1=tk[:], op0=ALU.add, op1=ALU.mult),
                   r=[tkd[d], tk], w=[tkd[d]])
            fw.pool(lambda e, d=d: e.tensor_tensor(out=tbt[d][:], in0=tkk[:], in1=ta[d][:], op=ALU.mult), r=[tkk, ta[d]], w=[tbt[d]])
            fw.pool(lambda e, d=d: e.tensor_tensor(out=tbo[:], in0=tsq[:], in1=tkd[d][:], op=ALU.mult), r=[tsq, tkd[d]], w=[tbo])
            fw.dve(lambda e, d=d: e.reduce_sum(out=s16[1 + d][:], in_=v3(tbo), axis=AX.X), r=[tbo], w=[s16[1 + d]])
            for (nm, tt) in (("k%d" % d, tkd[d]), ("b%d" % d, tbt[d]), ("s%d" % d, tsg[d])):
                dma(fw, "sp", A[nm][rows, :], tt[:], [tt], [A[nm]])
        fw.dve(lambda e: e.tensor_tensor(out=s16[1][:], in0=s16[1][:], in1=s16[2][:], op=ALU.add), r=[s16[1], s16[2]], w=[s16[1]])
        fw.dve(lambda e: e.tensor_tensor(out=v3(tbo), in0=v3(tvb), in1=b16(s16[1]), op=ALU.mult), r=[tvb, s16[1]], w=[tbo])
        for (nm, tt) in (("r", tr), ("kk", tkk), ("bonus", tbo), ("v", tvb)):
            dma(fw, "sp", A[nm][rows, :], tt[:], [tt], [A[nm]])
        fw.act(lambda e: e.activation(out=tg[:], in_=pp[2][:], func=AF.Copy), r=[pp[2]], w=[tg])
        dma(fw, "sp", A["g"][rows, :], tg[:], [tg], [A["g"]])
    fw.flush()
def phase_rw_scan(fw, k, b):
    A = k.rw
    tri = fw.sbuf("tri", [128, 4, 128], F32)
    dma(fw, "sp", tri[:], k.c_tri[:, :, :], [], [tri])
    mexp = [fw.sbuf("mexp%d" % m, [128, 4, 128], F32) for m in range(4)]
    for m in range(4):
        for hh in range(4):
            fw.pool(lambda e, m=m, hh=hh: e.tensor_copy(mexp[m][:, hh, :], tri[:, m, :]), r=[tri], ww=[mexp[m]])
    onesc = fw.sbuf("onesc", [128, 1], F32)
    fw.pool(lambda e: e.memset(onesc[:], 1.0), w=[onesc])
    Lr = fw.sbuf("Lr", [128, D], F32)
    Lkk = fw.sbuf("Lkk", [128, D], F32)
    Lk = fw.sbuf("Lk", [128, D], F32)
    Lb = fw.sbuf("Lb", [128, D], F32)
    Ls = fw.sbuf("Ls", [128, D], F32)
    Lv = fw.sbuf("Lv", [128, D], BF16)
    gam = fw.sbuf("gam", [128, D], F32)
    gin = fw.sbuf("gin", [128, D], F32)
    gex = fw.sbuf("gex", [128, D], F32)
    At = fw.sbuf("At", [128, D], BF16)
    Rt = fw.sbuf("Rt", [128, D], BF16)
    Bt = fw.sbuf("Bt", [128, D], BF16)
    Kt = fw.sbuf("Kt", [128, D], BF16)
    ART = fw.sbuf("ART", [128, 8, 256], BF16)
    BT = fw.sbuf("BT", [128, 8, 128], BF16)
    KTt = fw.sbuf("KTt", [128, 8, 128], BF16)
    gLT = fw.sbuf("gLT", [128, 8], F32)
    ST = fw.sbuf("ST", [128, 8, 64], F32)
    STb = fw.sbuf("STb", [128, 8, 64], BF16)
    Sn = fw.sbuf("Sn", [128, 8, 64], F32)
    PDT = F32 if getattr(k, "scan_fp32", True) else BF16
    P = [[fw.sbuf("P%d_%d" % (g, i), [128, 4, 128], PDT) for i in range(7)] for g in range(4)]
    Q = [[fw.sbuf("Q%d_%d" % (g, i), [128, 4, 128], PDT) for i in range(2)] for g in range(4)]
    Br = [fw.sbuf("Br%d" % g, [128, 4, 128], BF16) for g in range(4)]
    Aak = [fw.sbuf("Aak%d" % g, [128, 4, 128], BF16) for g in range(4)]
    Kr = [fw.sbuf("Kr%d" % g, [128, 4, 128], BF16) for g in range(4)]
    Xb = [[fw.sbuf("Xb%d_%d" % (g, i), [128, 4, 64], BF16) for i in range(2)] for g in range(4)]
    Ub = [fw.sbuf("Ub%d" % g, [128, 4, 64], BF16) for g in range(4)]
    Xf = [fw.sbuf("Xf%d" % g, [128, 4, 64], F32) for g in range(4)]
    Yt = fw.sbuf("Yt", [128, D], F32)
    Yp_ = fw.sbuf("Yprev", [128, D], F32)
    pr = [fw.psum("sc_pr%d" % i, [128, 1024], F32) for i in range(4)]
    rb = [Res("bank%d" % i) for i in range(8)]
    lo = lambda i: pr[i][:, 0:512]
    hi = lambda i: pr[i][:, 512:1024]
    v4 = lambda ap, n: ap.rearrange("p (a t) -> p a t", a=n)
    bcm = lambda m: tri[:, m, :].unsqueeze(1).to_broadcast([128, 4, 128])
    MASK = {0: dict(cum=0, strict=1, incl=0, strictT=2), 1: dict(cum=3, strict=2, incl=3, strictT=1)}

    def chunk(d, ti, want_y, second):
        mk = MASK[d]
        rows = slice(ti * 128, (ti + 1) * 128)
        for (dst, nm) in ((Lr, "r"), (Lkk, "kk"), (Lk, "k%d" % d), (Lb, "b%d" % d), (Ls, "s%d" % d), (Lv, "v")):
            dma(fw, "sp", dst[:], A[nm][rows, :], [A[nm]], [dst])
        cum = pr[3]
        for half in range(2):
            fw.pe(lambda e, half=half: e.matmul(cum[:, half * 512:(half + 1) * 512], tri[:, mk["cum"], :],
                                               Ls[:, half * 512:(half + 1) * 512], start=True, stop=True),
                  r=[tri, Ls], w=[rb[6], rb[7]])
        fw.act(lambda e: e.activation(out=gex[:], in_=cum[:], func=AF.Copy), r=[rb[6], rb[7]], w=[gex])
        fw.act(lambda e: e.activation(out=gam[:], in_=gex[:], func=AF.Exp, scale=-C0), r=[gex], w=[gam])
        fw.act(lambda e: e.activation(out=gin[:], in_=gex[:], func=AF.Exp, scale=C0), r=[gex], w=[gin])
        fw.dve(lambda e: e.tensor_tensor(out=gex[:], in0=gex[:], in1=Ls[:], op=ALU.subtract), r=[gex, Ls], w=[gex])
        fw.act(lambda e: e.activation(out=gex[:], in_=gex[:], func=AF.Exp, scale=-C0), r=[gex], w=[gex])
        glp = pr[2][:, 512:520]
        for c in range(8):
            fw.pe(lambda e, c=c: e.matmul(pr[2][:, 512 + c:513 + c], Ls[:, c * 128:(c + 1) * 128], onesc[:, 0:1],
                                         start=True, stop=True), r=[Ls, onesc], w=[rb[5]])
        fw.act(lambda e: e.activation(out=gLT[:], in_=glp, func=AF.Exp, scale=-C0), r=[rb[5]], w=[gLT])
        fw.dve(lambda e: e.scalar_tensor_tensor(out=At[:], in0=Lkk[:], scalar=-1.0, in1=gex[:], op0=ALU.mult, op1=ALU.mult),
               r=[Lkk, gex], w=[At])
        fw.pool(lambda e: e.tensor_tensor(out=Rt[:], in0=Lr[:], in1=gam[:], op=ALU.mult), r=[Lr, gam], w=[Rt])
        fw.dve(lambda e: e.tensor_tensor(out=Bt[:], in0=Lb[:], in1=gin[:], op=ALU.mult), r=[Lb, gin], w=[Bt])
        fw.pool(lambda e: e.tensor_tensor(out=Kt[:], in0=Lk[:], in1=gin[:], op=ALU.mult), r=[Lk, gin], w=[Kt])
        if getattr(k, 'scan_stage', 99) <= 1:
            return
        tps = [(At, lo(0), rb[0], lambda: ART[:, :, 0:128], ART), (Rt, hi(0), rb[1], lambda: ART[:, :, 128:256], ART),
               (Bt, lo(1), rb[2], lambda: BT[:, :, :], BT), (Kt, hi(1), rb[3], lambda: KTt[:, :, :], KTt)]
        for n_, (src_, bank, res_, dstf, dstT) in enumerate(tps):
            tpv = v4(bank.bitcast(BF16), 8)
            for c in range(8):
                fw.pe(lambda e, c=c, src_=src_, tpv=tpv: e.transpose(tpv[:, c, :], src_[:, c * 128:(c + 1) * 128], k.ident_bf[:]),
                      r=[src_, k.ident_bf], w=[res_])
            if n_ % 2 == 0:
                fw.act(lambda e, tpv=tpv, dstf=dstf: e.activation(out=dstf(), in_=tpv, func=AF.Copy), r=[res_], ww=[dstT])
            else:
                fw.dve(lambda e, tpv=tpv, dstf=dstf: e.tensor_copy(dstf(), tpv), r=[res_], w=[dstT])
        if getattr(k, 'scan_stage', 99) <= 2:
            return
        for g in range(4):
            outs = [(v4(lo(0), 4), rb[0]), (v4(hi(0), 4), rb[1]), (v4(lo(1), 4), rb[2]), (v4(hi(1), 4), rb[3]), (v4(lo(2), 4), rb[4])]
            for hh in range(4):
                h = 4 * g + hh
                c, bp = h // 2, 64 * (h % 2)
                ops_ = [(BT, ART, 0, False), (BT, ART, 128, False), (KTt, ART, 0, False), (KTt, ART, 128, False), (ART, BT, 0, True)]
                fw.pe(lambda e: e.matmul(pr[2][:, 1023:1024], BT[:, 0, :], ART[:, 0, 0:1], start=True, stop=True),
                      r=[BT, ART], w=[rb[5]])
                for oi, (lt, rt_, off, swap) in enumerate(ops_):
                    ps_, rs_ = outs[oi]
                    if not swap:
                        fw.pe(lambda e, hh=hh, c=c, bp=bp, ps_=ps_, lt=lt, off=off: e.matmul(
                            ps_[:, hh, :], lt[bp:bp + 64, c, :], ART[bp:bp + 64, c, off:off + 128], start=True, stop=True),
                            r=[lt, ART], w=[rs_])
                    else:
                        fw.pe(lambda e, hh=hh, c=c, bp=bp, ps_=ps_: e.matmul(
                            ps_[:, hh, :], ART[bp:bp + 64, c, 0:128], BT[bp:bp + 64, c, :], start=True, stop=True),
                            r=[BT, ART], w=[rs_])
            dsts = [(P[g][0], "strict"), (Br[g], "incl"), (Aak[g], "strict"), (Kr[g], "incl"), (Q[g][0], "strictT")]
            svar = getattr(k, "scan_var", "")
            if svar == "mm":
                dsts = []
            for oi, (dt_, mname) in enumerate(dsts):
                ps_, rs_ = outs[oi]
                if oi % 2 == 0:
                    fw.act(lambda e, ps_=ps_, dt_=dt_: e.activation(out=dt_[:], in_=ps_, func=AF.Copy), r=[rs_], w=[dt_])
                else:
                    fw.dve(lambda e, ps_=ps_, dt_=dt_: e.tensor_copy(dt_[:], ps_), r=[rs_], w=[dt_])
                mx = mexp[mk[mname]]
                if svar == "cp":
                    continue
                if oi % 2 == 0:
                    fw.pool(lambda e, dt_=dt_, mx=mx: e.tensor_tensor(out=dt_[:], in0=dt_[:], in1=mx[:], op=ALU.mult), r=[dt_, mx], w=[dt_])
                else:
                    fw.dve(lambda e, dt_=dt_, mx=mx: e.tensor_tensor(out=dt_[:], in0=dt_[:], in1=mx[:], op=ALU.mult), r=[dt_, mx], w=[dt_])
        if getattr(k, 'scan_stage', 99) <= 3:
            return
        for kk_ in range(6):
            for g in range(4):
                pi = 2 + (g % 2)
                Pn, Qn = v4(lo(pi), 4), v4(hi(pi), 4)
                rP, rQ = rb[2 * pi], rb[2 * pi + 1]
                Pk, Qk, Qn_sb = P[g][kk_], Q[g][kk_ % 2], Q[g][(kk_ + 1) % 2]
                for hh in range(4):
                    fw.pe(lambda e, hh=hh, Pn=Pn, Pk=Pk, Qk=Qk: e.matmul(Pn[:, hh, :], Qk[:, hh, :], Pk[:, hh, :], start=True, stop=True),
                          r=[Pk, Qk], w=[rP])
                if kk_ < 5:
                    for hh in range(4):
                        fw.pe(lambda e, hh=hh, Qn=Qn, Pk=Pk, Qk=Qk: e.matmul(Qn[:, hh, :], Pk[:, hh, :], Qk[:, hh, :], start=True, stop=True),
                              r=[Pk, Qk], w=[rQ])
                fw.act(lambda e, g=g, kk_=kk_, Pn=Pn: e.activation(out=P[g][kk_ + 1][:], in_=Pn, func=AF.Copy), r=[rP], w=[P[g][kk_ + 1]])
                if kk_ < 5:
                    fw.dve(lambda e, Qn=Qn, Qn_sb=Qn_sb: e.tensor_copy(Qn_sb[:], Qn), r=[rQ], w=[Qn_sb])
        if getattr(k, 'scan_stage', 99) <= 4:
            return
        Xp = [v4(pr[0][:, g * 256:(g + 1) * 256], 4) for g in range(4)]
        rX = [rb[0], rb[0], rb[1], rb[1]]
        for g in range(4):
            for hh in range(4):
                h = 4 * g + hh
                c, bp = h // 2, 64 * (h % 2)
                fw.pe(lambda e, g=g, hh=hh, c=c, bp=bp: e.matmul(Xp[g][:, hh, :], ART[bp:bp + 64, c, 0:128], STb[bp:bp + 64, c, :],
                                                                start=True, stop=False), r=[ART, STb], w=[rX[g]])
                fw.pe(lambda e, g=g, hh=hh, h=h: e.matmul(Xp[g][:, hh, :], Aak[g][:, hh, :], Lv[:, h * 64:(h + 1) * 64],
                                                         start=False, stop=True), r=[Aak[g], Lv], w=[rX[g]])
        for g in range(4):
            fw.dve(lambda e, g=g: e.tensor_copy(Xf[g][:], Xp[g]), r=[rX[g]], w=[Xf[g]])
            if PDT != F32:
                fw.dve(lambda e, g=g: e.tensor_copy(Xb[g][0][:], Xf[g][:]), r=[Xf[g]], w=[Xb[g][0]])
        for kk_ in range(7):
            for g in range(4):
                xb = Xf[g] if PDT == F32 else Xb[g][kk_ % 2]
                xn = Ub[g] if kk_ == 6 else Xb[g][(kk_ + 1) % 2]
                for hh in range(4):
                    fw.pe(lambda e, g=g, hh=hh, kk_=kk_, xb=xb: e.matmul(Xp[g][:, hh, :], P[g][kk_][:, hh, :], xb[:, hh, :],
                                                                        start=True, stop=True), r=[P[g][kk_], xb], w=[rX[g]])
                fw.dve(lambda e, g=g: e.tensor_tensor(out=Xf[g][:], in0=Xp[g], in1=Xf[g][:], op=ALU.add), r=[rX[g], Xf[g]], w=[Xf[g]])
                fw.act(lambda e, g=g, xn=xn: e.activation(out=xn[:], in_=Xf[g][:], func=AF.Copy), r=[Xf[g]], w=[xn])
        if getattr(k, 'scan_stage', 99) <= 5:
            return
        if want_y:
            Yp = pr[1]
            if second:
                dma(fw, "sp", Yp_[:], A["y"][rows, :], [A["y"]], [Yp_])
            for g in range(4):
                for hh in range(4):
                    h = 4 * g + hh
                    c, bp = h // 2, 64 * (h % 2)
                    rY = rb[2] if h < 8 else rb[3]
                    fw.pe(lambda e, h=h, c=c, bp=bp: e.matmul(Yp[:, h * 64:(h + 1) * 64], ART[bp:bp + 64, c, 128:256], STb[bp:bp + 64, c, :],
                                                             start=True, stop=False), r=[ART, STb], w=[rY])
                    fw.pe(lambda e, h=h, g=g, hh=hh: e.matmul(Yp[:, h * 64:(h + 1) * 64], Br[g][:, hh, :], Ub[g][:, hh, :],
                                                             start=False, stop=False), r=[Br[g], Ub[g]], w=[rY])
                    fw.pe(lambda e, h=h, g=g, hh=hh: e.matmul(Yp[:, h * 64:(h + 1) * 64], Kr[g][:, hh, :], Lv[:, h * 64:(h + 1) * 64],
                                                             start=False, stop=True), r=[Kr[g], Lv], w=[rY])
            fw.act(lambda e: e.activation(out=Yt[:], in_=Yp[:], func=AF.Copy), r=[rb[2], rb[3]], w=[Yt])
            if second:
                fw.pool(lambda e: e.tensor_tensor(out=Yt[:], in0=Yt[:], in1=Yp_[:], op=ALU.add), r=[Yt, Yp_], w=[Yt])
            dma(fw, "sp", A["y"][rows, :], Yt[:], [Yt], [A["y"]])
        if getattr(k, 'scan_stage', 99) <= 6:
            return
        Sp = pr[2][:, :].rearrange("p (c w i) -> p c w i", c=8, w=2)
        for c in range(8):
            for wch in range(2):
                h = 2 * c + wch
                g, hh = h // 4, h % 4
                fw.pe(lambda e, c=c, wch=wch, g=g, hh=hh: e.matmul(Sp[:, c, wch, :], Bt[:, c * 128:(c + 1) * 128], Ub[g][:, hh, :],
                                                                  start=True, stop=False), r=[Bt, Ub[g]], w=[rb[4], rb[5]])
                fw.pe(lambda e, c=c, wch=wch, h=h: e.matmul(Sp[:, c, wch, :], Kt[:, c * 128:(c + 1) * 128], Lv[:, h * 64:(h + 1) * 64],
                                                           start=False, stop=True), r=[Kt, Lv], w=[rb[4], rb[5]])
        for cb in range(2):
            fw.dve(lambda e, cb=cb: e.tensor_copy(Sn[0:64, 4 * cb:4 * cb + 4, :], Sp[0:64, 4 * cb:4 * cb + 4, 0, :]), r=[rb[4 + cb]], ww=[Sn])
            fw.dve(lambda e, cb=cb: e.tensor_copy(Sn[64:128, 4 * cb:4 * cb + 4, :], Sp[64:128, 4 * cb:4 * cb + 4, 1, :]), r=[rb[4 + cb]], ww=[Sn])
        fw.dve(lambda e: e.tensor_tensor(out=ST[:], in0=ST[:], in1=Sn[:], op=ALU.add), r=[ST, Sn], w=[ST])
        fw.dve(lambda e: e.tensor_tensor(out=ST[:], in0=ST[:], in1=gLT[:].unsqueeze(2).to_broadcast([128, 8, 64]), op=ALU.mult),
               r=[ST, gLT], w=[ST])
        fw.act(lambda e: e.activation(out=STb[:], in_=ST[:], func=AF.Copy), r=[ST], w=[STb])

    nt_dbg = getattr(k, "scan_tiles", 18)
    for d in range(2):
        fw.pool(lambda e: e.memset(ST[:], 0.0), w=[ST])
        fw.pool(lambda e: e.memset(STb[:], 0.0), w=[STb])
        order = list(range(18)) if d == 0 else [1, 0] + list(range(17, 1, -1))
        if nt_dbg < 18:
            order = [t_ for t_ in order if t_ < nt_dbg]
        for ti in order:
            chunk(d, ti, ti >= 2, d == 1)
    fw.flush()


def phase_rw_out(fw, k, b, src, dst):
    l = 1
    t0 = 18 * b
    A = k.rw
    wo = fw.sbuf("rw_wo", [128, 8, D], BF16)
    for c in range(8):
        dma(fw, "pool", wo[:, c, :], k.rw_w_o[0, c * 128:(c + 1) * 128, :], [], [wo])
    lnw = bc_load(fw, "lnw", k.rw_ln_w[0:1, :])
    lnb = bc_load(fw, "lnb", k.rw_ln_b[0:1, :])
    ty = fw.sbuf("o_y", [128, D], F32)
    tg = fw.sbuf("o_g", [128, D], F32)
    tb = fw.sbuf("o_b", [128, D], F32)
    tq = fw.sbuf("o_q", [128, D], F32)
    zb = fw.sbuf("o_zb", [128, D], BF16)
    zT = fw.sbuf("o_zT", [128, 8, 128], BF16)
    resT = fw.sbuf("o_resT", [128, 8, 128], F32)
    s1 = fw.sbuf("o_s1", [128, 16], F32)
    s2 = fw.sbuf("o_s2", [128, 16], F32)
    ht = [fw.sbuf("o_ht%d" % i, [128, D], F32) for i in range(2)]
    big = fw.psum("o_big", [128, 1024], F32)
    pz = fw.psum("o_pz", [128, 8, 128], BF16)
    pq = fw.psum("o_pq", [128, 1024], F32)
    be = Back(fw, k, ht, big)
    v3 = lambda t_: t_[:].rearrange("p (h d) -> p h d", d=64)
    b16 = lambda s_: s_[:].unsqueeze(2).to_broadcast([128, 16, 64])
    pOP = pq[:, :].rearrange("p (d t) -> p d t", d=8)
    for i in range(2, getattr(k, "scan_tiles", 18)):
        t = t0 + i
        rows = slice(i * 128, (i + 1) * 128)
        dma(fw, "sp", ty[:], A["y"][rows, :], [A["y"]], [ty])
        dma(fw, "sp", tg[:], A["g"][rows, :], [A["g"]], [tg])
        dma(fw, "sp", tb[:], A["bonus"][rows, :], [A["bonus"]], [tb])
        fw.dve(lambda e: e.reduce_sum(out=s1[:], in_=v3(ty), axis=AX.X), r=[ty], w=[s1])
        fw.dve(lambda e: e.tensor_scalar(out=s1[:], in0=s1[:], scalar1=1.0 / 64, scalar2=None, op0=ALU.mult), r=[s1], w=[s1])
        fw.dve(lambda e: e.tensor_tensor(out=v3(ty), in0=v3(ty), in1=b16(s1), op=ALU.subtract), r=[ty, s1], w=[ty])
        fw.pool(lambda e: e.tensor_tensor(out=tq[:], in0=ty[:], in1=ty[:], op=ALU.mult), r=[ty], w=[tq])
        fw.dve(lambda e: e.reduce_sum(out=s2[:], in_=v3(tq), axis=AX.X), r=[tq], w=[s2])
        fw.dve(lambda e: e.tensor_scalar(out=s2[:], in0=s2[:], scalar1=1.0 / 64, scalar2=64e-5, op0=ALU.mult, op1=ALU.add), r=[s2], w=[s2])
        fw.act(lambda e: e.activation(out=s2[:], in_=s2[:], func=AF.Sqrt), r=[s2], w=[s2])
        fw.dve(lambda e: e.reciprocal(out=s2[:], in_=s2[:]), r=[s2], w=[s2])
        fw.dve(lambda e: e.tensor_tensor(out=v3(ty), in0=v3(ty), in1=b16(s2), op=ALU.mult), r=[ty, s2], w=[ty])
        fw.pool(lambda e: e.tensor_tensor(out=ty[:], in0=ty[:], in1=lnw[:], op=ALU.mult), r=[ty, lnw], w=[ty])
        fw.pool(lambda e: e.tensor_tensor(out=tb[:], in0=tb[:], in1=lnb[:], op=ALU.add), r=[tb, lnb], w=[tb])
        fw.dve(lambda e: e.tensor_tensor(out=ty[:], in0=ty[:], in1=tb[:], op=ALU.add), r=[ty, tb], w=[ty])
        fw.dve(lambda e: e.tensor_tensor(out=zb[:], in0=ty[:], in1=tg[:], op=ALU.mult), r=[ty, tg], w=[zb])
        for c in range(8):
            fw.pe(lambda e, c=c: e.transpose(pz[:, c, :], zb[:, c * 128:(c + 1) * 128], k.ident_bf[:]), r=[zb, k.ident_bf], w=[pz])
        fw.act(lambda e: e.activation(out=zT[:], in_=pz[:], func=AF.Copy), r=[pz], w=[zT])
        for d in range(8):
            for c in range(8):
                fw.pe(lambda e, c=c, d=d: e.matmul(pOP[:, d, :], wo[:, c, d * 128:(d + 1) * 128], zT[:, c, :],
                                                  start=(c == 0), stop=(c == 7)), r=[wo, zT], w=[pq])
        fw.act(lambda e: e.activation(out=resT[:].rearrange("p d t -> p (d t)"), in_=pq[:, :], func=AF.Copy), r=[pq], w=[resT])
        be.run(lambda c: resT[:, c, :], resT, l, 16, rowtype(t), src[t * 128:(t + 1) * 128, :], src,
               dst[t * 128:(t + 1) * 128, :], dst)
    fw.flush()
WSPEC = [
    ("ada_w", [2, D, 6 * D]), ("ada_b", [2, 6 * D]), ("norm_mix_g", [2, D]), ("norm_ffn_g", [2, D]),
    ("router_w", [2, D, NE]), ("router_b", [2, NE]), ("exp_w_in", [2, NE, D, 2 * D]), ("exp_b_in", [2, NE, 2 * D]),
    ("exp_w_out", [2, NE, D, D]), ("exp_b_out", [2, NE, D]),
    ("ab_w_in", [1, D, 2048]), ("na_q_g", [1, 64]), ("na_k_g", [1, 64]),
    ("pool_w", [1, 4, 128, 128]), ("pool_scale", [1, 512]), ("ab_w_out", [1, D, D]),
    ("rw_mu", [1, 6, D]), ("rw_w_r", [1, D, D]), ("rw_w_k", [1, D, D]), ("rw_w_v", [1, D, D]), ("rw_w_o", [1, D, D]),
    ("rw_w0", [1, 2, D]), ("rw_w1", [1, 2, D, 64]), ("rw_w2", [1, 2, 64, D]), ("rw_a0", [1, 2, D]),
    ("rw_a1", [1, 2, D, 64]), ("rw_a2", [1, 2, 64, D]), ("rw_g1", [1, D, 128]), ("rw_g2", [1, 128, D]),
    ("rw_k_k", [1, D]), ("rw_k_a", [1, D]), ("rw_r_k", [1, 16, 64]), ("rw_ln_w", [1, D]), ("rw_ln_b", [1, D]),
]


def declare(fw, k):
    ne_decl = 1 if getattr(k, "small", False) else NE
    k.xin = fw.dram("xin", [NT * 128, D], F32, kind="ExternalInput")
    k.cc = fw.dram("cc", [3, D], F32, kind="ExternalInput")
    for name, shp in WSPEC:
        if name.startswith("exp_w"):
            shp = [shp[0], ne_decl] + shp[2:]
        setattr(k, name, fw.dram(name, shp, F32, kind="ExternalInput"))
    k.rpb_g = fw.dram("rpb_g", [8, 8, 512, 64], F32, kind="ExternalInput")
    k.c_ident = fw.dram("c_ident", [128, 128], F32, kind="ExternalInput")
    k.c_namask = fw.dram("c_namask", [512, 64], F32, kind="ExternalInput")
    k.c_poolrc = fw.dram("c_poolrc", [4, 2, 2048 + 32], F32, kind="ExternalInput")
    k.c_tri = fw.dram("c_tri", [128, 4, 128], F32, kind="ExternalInput")
    k.out = fw.dram("out", [32 * 128, D], F32, kind="ExternalOutput")
    k.H = [fw.dram("H%d" % i, [NT * 128, D], F32) for i in range(2)]
    k.combT_d = fw.dram("combT_d", [NE, 1152], F32)
    fw.persist = True
    k.modT = [fw.sbuf("modT%d" % l, [128, 48, 3], F32) for l in range(2)]
    k.ident_f = fw.sbuf("ident_f", [128, 128], F32)
    k.ident_bf = fw.sbuf("ident_bf", [128, 128], BF16)
    fw.persist = False
    dma(fw, "sp", k.ident_f[:], k.c_ident[:, :], [], [k.ident_f])
    dma(fw, "pool", k.ident_bf[:], k.c_ident[:, :], [], [k.ident_bf])


def host_consts():
    c = {}
    c["c_ident"] = np.eye(128, dtype=np.float32)
    qc = np.arange(64)
    cs = np.clip(qc - 8, 0, 48)
    kc = np.arange(64)
    valid = (kc[:, None] >= cs[None, :]) & (kc[:, None] < cs[None, :] + 16)
    m = np.where(valid, 0.0, -30000.0).astype(np.float32)
    c["c_namask"] = np.tile(m, (8, 1)).astype(np.float32)
    rc = np.zeros((4, 2, 2048 + 32), np.float32)
    for g, w in enumerate((2, 4, 8, 16)):
        for s, L in enumerate((2048, 256)):
            t = np.arange(L)
            lo = np.clip(t - w // 2, 0, L)
            hi = np.clip(t + w - w // 2, 0, L)
            rc[g, s, 16:16 + L] = 1.0 / (hi - lo)
    c["c_poolrc"] = rc
    tri = np.zeros((128, 4, 128), np.float32)
    s_ = np.arange(128)[:, None]
    t_ = np.arange(128)[None, :]
    tri[:, 0, :] = (s_ <= t_)
    tri[:, 1, :] = (s_ < t_)
    tri[:, 2, :] = (s_ > t_)
    tri[:, 3, :] = (s_ >= t_)
    c["c_tri"] = tri
    return c


def gather_rpb(rpb):
    j = np.arange(8)
    o = np.arange(8)
    kc = np.arange(64)
    qc = np.arange(64)
    ri = j[None, :] - o[:, None] + 7
    ci = np.clip(kc[:, None] - qc[None, :] + 15, 0, 30)
    g = rpb[:, ri[:, :, None, None], ci[None, None, :, :]]
    return np.ascontiguousarray(g.reshape(8, 8, 512, 64)).astype(np.float32)


def shard_inputs(inp, small=False):
    consts = host_consts()
    maps = []
    wts = {name: np.ascontiguousarray(inp[name], dtype=np.float32) for name, _ in WSPEC}
    if small:
        for nm in ("exp_w_in", "exp_w_out"):
            wts[nm] = np.ascontiguousarray(wts[nm][:, 0:1])
    rpbg = gather_rpb(np.asarray(inp["na_rpb"])[0])
    for core in range(8):
        rows = []
        for b in (2 * core, 2 * core + 1):
            rows.append(inp["ctx"][b])
            rows.append(inp["x"][b])
        m = {"xin": np.ascontiguousarray(np.concatenate(rows, axis=0), dtype=np.float32),
             "cc": np.ascontiguousarray(np.stack([inp["c"][2 * core], inp["c"][2 * core + 1], inp["c_ctx"]]), dtype=np.float32),
             "rpb_g": rpbg}
        m.update(wts)
        m.update(consts)
        maps.append(m)
    return maps
def build_program(k=None):
    nc = bass.Bass("TRN2", target_bir_lowering=False)
    fw = FW(nc)
    if k is None:
        k = K()
    declare(fw, k)
    rw_declare(fw, k)
    phase_ada(fw, k)
    for b in range(2):
        phase_ab(fw, k, b)
    for blk in range(4):
        tiles = list(range(9 * blk, 9 * blk + 9))
        phase_moe(fw, k, 0, tiles, k.H[0], lambda t: (k.H[1][t * 128:(t + 1) * 128, :], k.H[1]), blk == 0)
    for b in range(2):
        phase_rw_feat(fw, k, b, k.H[1])
        phase_rw_scan(fw, k, b)
        phase_rw_out(fw, k, b, k.H[1], k.H[0])

    def dst1(t):
        b, i = t // 18, t % 18
        o = b * 16 + (i - 2)
        return (k.out[o * 128:(o + 1) * 128, :], k.out)
    for b in range(2):
        for hb in range(2):
            tiles = [18 * b + 2 + 8 * hb + j for j in range(8)]
            phase_moe(fw, k, 1, tiles, k.H[0], dst1, False)
    fw.flush(final=True)
    return nc, fw


_CACHE = {}


def kernel(**inputs):
    from concourse.bass_utils import run_bass_kernel_spmd
    inp = {n: np.asarray(v) for n, v in inputs.items()}
    if "nc" not in _CACHE:
        _CACHE["nc"] = build_program()[0]
    nc = _CACHE["nc"]
    maps = shard_inputs(inp)
    res = run_bass_kernel_spmd(nc, maps, core_ids=list(range(8)))
    outs = [np.asarray(r["out"]).reshape(2, 2048, D) for r in res.results]
    return np.concatenate(outs, axis=0).astype(np.float32)
```

```python
import numpy as np
import concourse.bass as bass
import concourse.mybir as mybir

F32 = mybir.dt.float32
BF16 = mybir.dt.bfloat16
ALU = mybir.AluOpType
AF = mybir.ActivationFunctionType
AX = mybir.AxisListType

KDMA = 8
ENGS = ("pe", "dve", "act", "pool", "sp")


class Res:
    __slots__ = ("name", "w", "r")

    def __init__(self, name=""):
        self.name = name
        self.w = None
        self.r = {}


class T:
    def __init__(self, t, name):
        self.t = t
        self.res = Res(name)

    def __getitem__(self, k):
        return self.t[k]

    def parts(self, n):
        if not hasattr(self, "_parts"):
            self._parts = [Res("%s.%d" % (self.res.name, i)) for i in range(n)]
        return self._parts


class V(T):
    def __init__(self, ap, res):
        self.t = ap
        self.res = res


def _res(x):
    return x.res if isinstance(x, T) else x


class Op:
    __slots__ = ("waits", "fn", "marked", "kind", "dma_m")

    def __init__(self, fn, kind):
        self.waits = []
        self.fn = fn
        self.marked = False
        self.kind = kind
        self.dma_m = -1


class FW:
    def __init__(self, nc):
        self.nc = nc
        self.ops = {e: [] for e in ENGS}
        self.seen = {e: {} for e in ENGS}
        self.ndma = {e: 0 for e in ENGS}
        self.ctx = []
        self.pctx = []
        self.emitted = {e: 0 for e in ENGS}
        self.phase_end = {e: [] for e in ENGS}
        self.cnt = {e: [] for e in ENGS}
        self.sems = None
        self.persist = False

    def sbuf(self, name, shape, dt):
        self.uid = getattr(self, "uid", 0) + 1
        name = "s%d_%s" % (self.uid, name)
        g = self.nc.sbuf_tensor(name, list(shape), dt)
        t = g.__enter__()
        (self.ctx if self.persist else self.pctx).append(g)
        return T(t, name)

    def psum(self, name, shape, dt=F32):
        self.uid = getattr(self, "uid", 0) + 1
        name = "p%d_%s" % (self.uid, name)
        g = self.nc.psum_tensor(name, list(shape), dt)
        t = g.__enter__()
        (self.ctx if self.persist else self.pctx).append(g)
        return T(t, name)

    def dram(self, name, shape, dt, kind="Internal"):
        t = self.nc.dram_tensor(name, list(shape), dt, kind=kind)
        return T(t.ap(), name)

    def _need(self, eng, tok, op):
        if tok is None:
            return
        if tok[0] == "c":
            _, e, idx = tok
            if e == "pe" and eng == "pe":
                return
            key = ("c", e)
            if idx < self.emitted[e] and not self.ops[e][idx].marked:
                idx = min(i for i in self.phase_end[e] if i >= idx)
                tok = ("c", e, idx)
            if self.seen[eng].get(key, -1) >= idx:
                return
            self.seen[eng][key] = idx
            self.ops[e][idx].marked = True
            op.waits.append(tok)
        else:
            _, q, m = tok
            key = ("d", q, m % KDMA)
            if self.seen[eng].get(key, -1) >= m:
                return
            self.seen[eng][key] = m
            op.waits.append(tok)

    def _deps(self, eng, op, r, w, ww=()):
        for x in r:
            x = _res(x)
            self._need(eng, x.w, op)
        for x in w:
            x = _res(x)
            self._need(eng, x.w, op)
            for tok in x.r.values():
                self._need(eng, tok, op)
        for x in ww:
            x = _res(x)
            if x.w is not None and not (x.w[0] == "c" and x.w[1] == eng):
                self._need(eng, x.w, op)
            for tok in x.r.values():
                self._need(eng, tok, op)

    def _commit(self, tok, r, w):
        for x in r:
            x = _res(x)
            if tok[0] == "c":
                x.r[("c", tok[1])] = tok
            else:
                x.r[("d", tok[1], tok[2] % KDMA)] = tok
        for x in w:
            x = _res(x)
            x.w = tok
            x.r = {}

    def op(self, eng, fn, r=(), w=(), ww=()):
        o = Op(fn, "c")
        self._deps(eng, o, r, w, ww)
        idx = len(self.ops[eng])
        self.ops[eng].append(o)
        self._commit(("c", eng, idx), r, list(w) + list(ww))
        return o

    def dma(self, eng, fn, r=(), w=()):
        o = Op(fn, "d")
        m = self.ndma[eng]
        self.ndma[eng] += 1
        o.dma_m = m
        if m >= KDMA:
            self._need(eng, ("d", eng, m - KDMA), o)
        self._deps(eng, o, r, w)
        self.ops[eng].append(o)
        self._commit(("d", eng, m), r, w)
        return o

    def pe(self, fn, r=(), w=(), ww=()):
        return self.op("pe", fn, r, w, ww)

    def dve(self, fn, r=(), w=(), ww=()):
        return self.op("dve", fn, r, w, ww)

    def act(self, fn, r=(), w=(), ww=()):
        return self.op("act", fn, r, w, ww)

    def pool(self, fn, r=(), w=(), ww=()):
        return self.op("pool", fn, r, w, ww)

    def barrier(self):
        for eng in ENGS:
            o = Op(None, "n")
            for e in ENGS:
                for i in range(len(self.ops[e]) - 1, -1, -1):
                    if self.ops[e][i].kind == "c":
                        self._need(eng, ("c", e, i), o)
                        break
                n = self.ndma[e]
                for m in range(max(0, n - KDMA), n):
                    self._need(eng, ("d", e, m), o)
            self.ops[eng].append(o)

    def finish_waits(self):
        o = Op(None, "n")
        for q in ENGS:
            n = self.ndma[q]
            for m in range(max(0, n - KDMA), n):
                self._need("sp", ("d", q, m), o)
        self.ops["sp"].append(o)

    def _mksems(self):
        nc = self.nc
        self.sems = {}
        for e in ENGS:
            g = nc.semaphore("c_" + e)
            self.sems[("c", e)] = g.__enter__()
            self.ctx.append(g)
            for k in range(KDMA):
                g = nc.semaphore("d_%s_%d" % (e, k))
                self.sems[("d", e, k)] = g.__enter__()
                self.ctx.append(g)

    def flush(self, final=False):
        nc = self.nc
        if self.sems is None:
            self._mksems()
        if final:
            self.finish_waits()
        sems = self.sems
        start = dict(self.emitted)
        for e in ENGS:
            ops = self.ops[e]
            for i in range(len(ops) - 1, start[e] - 1, -1):
                if ops[i].kind == "c":
                    ops[i].marked = True
                    self.phase_end[e].append(i)
                    break
            c = self.cnt[e][-1] if self.cnt[e] else 0
            for o in ops[start[e]:]:
                if o.kind == "c" and o.marked:
                    c += 1
                self.cnt[e].append(c)
        cnt = self.cnt

        def run(ename, eng):
            for o in self.ops[ename][start[ename]:]:
                for tok in o.waits:
                    if tok[0] == "c":
                        eng.wait_ge(sems[("c", tok[1])], cnt[tok[1]][tok[2]])
                    else:
                        m = tok[2]
                        eng.wait_ge(sems[("d", tok[1], m % KDMA)], 16 * (m // KDMA + 1))
                if o.kind == "n":
                    continue
                ins = o.fn(eng)
                if o.kind == "d":
                    ins.then_inc(sems[("d", ename, o.dma_m % KDMA)], 16)
                elif o.marked:
                    ins.then_inc(sems[("c", ename)], 1)

        blk = nc.Block()
        block = blk.__enter__()

        @block.tensor
        def _(e):
            run("pe", e)

        @block.vector
        def _(e):
            run("dve", e)

        @block.scalar
        def _(e):
            run("act", e)

        @block.gpsimd
        def _(e):
            run("pool", e)

        @block.sync
        def _(e):
            run("sp", e)

        blk.__exit__(None, None, None)
        for e in ENGS:
            self.emitted[e] = len(self.ops[e])
        if not final:
            self.barrier()
        for g in reversed(self.pctx):
            g.__exit__(None, None, None)
        self.pctx = []
        if final:
            for g in reversed(self.ctx):
                g.__exit__(None, None, None)
            self.ctx = []

    def emit(self):
        self.flush(final=True)

    def stats(self):
        return {e: (len(self.ops[e]), sum(1 for o in self.ops[e] if o.marked),
                    sum(len(o.waits) for o in self.ops[e])) for e in ENGS}
D = 1024
NT = 36
NE = 32
EPS = 1e-6


def rowtype(t):
    return 2 if (t % 18) < 2 else t // 18


class K:
    pass


def dma(fw, q, out, in_, r, w, **kw):
    fw.dma(q, lambda e: e.dma_start(out=out, in_=in_, **kw), r=r, w=w)


def phase_ada(fw, k):
    ccT = fw.sbuf("ccT", [128, 8, 3], F32)
    scT = fw.sbuf("scT", [128, 8, 3], F32)
    for r_ in range(3):
        dma(fw, "sp", ccT[:, :, r_], k.cc[r_, :].rearrange("(c p) -> p c", p=128), [k.cc], [ccT],
            allow_slow_non_contiguous=True)
    fw.act(lambda e: e.activation(out=scT[:], in_=ccT[:], func=AF.Silu), r=[ccT], w=[scT])
    aw = [fw.sbuf("aw%d" % i, [128, 8, 768], F32) for i in range(2)]
    abT = fw.sbuf("abT", [128, 48], F32)
    ps = fw.psum("adaps", [128, 48, 3], F32)
    n = 0
    for l in range(2):
        dma(fw, "sp", abT[:], k.ada_b[l, :].rearrange("(j p) -> p j", p=128), [k.ada_b], [abT],
            allow_slow_non_contiguous=True)
        for blk in range(8):
            a = aw[n % 2]
            n += 1
            dma(fw, "sp", a[:], k.ada_w[l, :, blk * 768:(blk + 1) * 768].rearrange("(c p) f -> p c f", p=128),
                [k.ada_w], [a])
            for j in range(6):
                jj = blk * 6 + j
                for c in range(8):
                    fw.pe(lambda e, a=a, j=j, c=c, jj=jj: e.matmul(
                        ps[:, jj, :], a[:, c, j * 128:(j + 1) * 128], scT[:, c, :],
                        start=(c == 0), stop=(c == 7)), r=[a, scT], w=[ps])
        fw.dve(lambda e, l=l: e.tensor_tensor(
            out=k.modT[l][:], in0=ps[:], in1=abT[:].unsqueeze(2).to_broadcast([128, 48, 3]), op=ALU.add),
            r=[ps, abT], w=[k.modT[l]])
    fw.flush()


def load_gain_T(fw, k, name, src_row):
    t = fw.sbuf(name, [128, 8], F32)
    dma(fw, "sp", t[:], src_row.rearrange("(c p) -> p c", p=128), [], [t], allow_slow_non_contiguous=True)
    return t


def make_GS(fw, k, l, which, gain_T):
    G = fw.sbuf("G%d" % which, [128, 8, 3], F32)
    base = 0 if which == 0 else 24
    m = k.modT[l]
    fw.dve(lambda e: e.scalar_tensor_tensor(
        out=G[:], in0=m[:, base + 8:base + 16, :], scalar=1.0,
        in1=gain_T[:].unsqueeze(2).to_broadcast([128, 8, 3]), op0=ALU.add, op1=ALU.mult),
        r=[m, gain_T], w=[G])
    return G, base


class Front:
    def __init__(self, fw, k, nbuf=2):
        self.fw = fw
        self.k = k
        self.xt = [fw.sbuf("fe_xt%d" % i, [128, D], F32) for i in range(nbuf)]
        self.xh = [fw.sbuf("fe_xh%d" % i, [128, D], BF16) for i in range(2)]
        self.ss = [fw.sbuf("fe_ss%d" % i, [128, 1], F32) for i in range(2)]
        self.rstd = [fw.sbuf("fe_rs%d" % i, [128, 1], F32) for i in range(2)]
        self.tp = [fw.psum("fe_tp%d" % i, [128, 8, 128], BF16) for i in range(1)]
        self.n = 0

    def run(self, src_rows, src_res, G, l, base, row, dst_fn, dst_res):
        fw, k = self.fw, self.k
        i = self.n
        self.n += 1
        xt = self.xt[i % len(self.xt)]
        xh = self.xh[i % 2]
        ss = self.ss[i % 2]
        rstd = self.rstd[i % 2]
        tp = self.tp[0]
        junk = xh
        m = k.modT[l]
        dma(fw, "sp", xt[:], src_rows, [src_res], [xt])
        fw.pool(lambda e: e.memset(ss[:], 0.0), w=[ss])
        fw.act(lambda e: e.activation(out=junk[:], in_=xt[:], func=AF.Square, accum_out=ss[:]),
               r=[xt], w=[xh, ss])
        fw.dve(lambda e: e.tensor_scalar(out=rstd[:], in0=ss[:], scalar1=1.0 / D, scalar2=EPS,
                                         op0=ALU.mult, op1=ALU.add), r=[ss], w=[rstd])
        fw.act(lambda e: e.activation(out=rstd[:], in_=rstd[:], func=AF.Sqrt), r=[rstd], w=[rstd])
        fw.dve(lambda e: e.reciprocal(out=rstd[:], in_=rstd[:]), r=[rstd], w=[rstd])
        fw.dve(lambda e: e.tensor_scalar(out=xh[:], in0=xt[:], scalar1=rstd[:, 0:1], scalar2=None,
                                         op0=ALU.mult), r=[xt, rstd], w=[xh])
        for c in range(8):
            fw.pe(lambda e, c=c: e.transpose(tp[:, c, :], xh[:, c * 128:(c + 1) * 128], k.ident_bf[:]),
                  r=[xh, k.ident_bf], w=[tp])
        for c in range(8):
            fw.act(lambda e, c=c: e.activation(out=dst_fn(c), in_=tp[:, c, :], func=AF.Identity,
                                               scale=G[:, c, row:row + 1], bias=m[:, base + c, row:row + 1]),
                   r=[tp, G, m], ww=[dst_res])
        return xt


class Back:
    def __init__(self, fw, k, ht_bufs, ps_big):
        self.fw = fw
        self.k = k
        self.fT = [fw.sbuf("be_fT%d" % i, [128, 8, 128], F32) for i in range(1)]
        self.ht = ht_bufs
        self.ps = [ps_big]
        self.n = 0

    def run(self, srcT_fn, src_res, l, gbase, row, h_rows, h_res, dst_rows, dst_res):
        fw, k = self.fw, self.k
        i = self.n
        self.n += 1
        fT = self.fT[0]
        ht = self.ht[i % len(self.ht)]
        ps = self.ps[0]
        m = k.modT[l]
        dma(fw, "sp", ht[:], h_rows, [h_res], [ht])
        fp = fT.parts(8)
        for c in range(8):
            if c % 2 == 0:
                fw.dve(lambda e, c=c: e.tensor_scalar(out=fT[:, c, :], in0=srcT_fn(c),
                                                      scalar1=m[:, gbase + c, row:row + 1], scalar2=None,
                                                      op0=ALU.mult), r=[src_res, m], w=[fp[c]])
            else:
                fw.act(lambda e, c=c: e.activation(out=fT[:, c, :], in_=srcT_fn(c), func=AF.Copy,
                                                   scale=m[:, gbase + c, row:row + 1]), r=[src_res, m], w=[fp[c]])
        for c in range(8):
            fw.pe(lambda e, c=c: e.transpose(ps[:, c * 128:(c + 1) * 128], fT[:, c, :], k.ident_f[:]),
                  r=[fp[c], k.ident_f], w=[ps])
        fw.dve(lambda e: e.tensor_tensor(out=ht[:], in0=ps[:], in1=ht[:], op=ALU.add), r=[ps, ht], w=[ht])
        dma(fw, "sp", dst_rows, ht[:], [ht], [dst_res])
def subs(T):
    out = []
    o = 0
    while o < T:
        n = min(512, T - o)
        out.append((o, n))
        o += n
    return out


def phase_moe(fw, k, l, tiles, src, dst_fn, first_block):
    nt = len(tiles)
    TB = nt * 128
    SB = subs(TB)
    ynT = fw.sbuf("ynT", [128, 8, TB], BF16)
    acc = fw.sbuf("acc", [128, 8, TB], F32)
    actT = [fw.sbuf("actT%d" % i, [128, 8, TB], BF16) for i in range(2)]
    gbc = [fw.sbuf("gbc%d" % i, [128, TB], F32) for i in range(1)]
    st1 = [fw.sbuf("st1_%d" % i, [128, 8, 256], F32) for i in range(2)]
    w1b = [fw.sbuf("w1b%d" % i, [128, 8, 2, 128], BF16) for i in range(4)]
    st2 = [fw.sbuf("st2_%d" % i, [128, 1024], F32) for i in range(2)]
    w2bs = [fw.sbuf("w2b%d" % i, [128, 8, 1024], BF16) for i in range(2)]
    w2ps = [w.parts(8) for w in w2bs]
    ub = [[fw.sbuf("ub%d_%d" % (i, j), [128, 512], F32) for j in range(3)] for i in range(2)]
    wr = fw.sbuf("wr", [128, 8, NE], BF16)
    rb = fw.sbuf("rb", [128, NE], F32)
    bout = V(st2[0][0:NE, :], st2[0].res)
    bin_sb = V(st1[0][0:NE, :, :].rearrange("p c f -> p (c f)"), st1[0].res)
    binT = fw.sbuf("binT", [128, 16, NE], F32)
    combT = fw.sbuf("combT", [NE, TB], F32)
    lg = fw.sbuf("lg", [128, NE], F32)
    m8 = fw.sbuf("m8", [128, 8], F32)
    nmx = fw.sbuf("nmx", [128, 1], F32)
    msk = fw.sbuf("msk", [128, NE], F32)
    ex = fw.sbuf("ex", [128, NE], F32)
    sm = fw.sbuf("sm", [128, 1], F32)
    comb = fw.sbuf("comb", [128, NE], F32)
    gn = load_gain_T(fw, k, "gnf", k.norm_ffn_g[l, :])
    G, base = make_GS(fw, k, l, 1, gn)
    fe = Front(fw, k)
    ps_big = fw.psum("ps_big", [128, 1024], F32)
    be = Back(fw, k, fe.xt, ps_big)
    ps_misc = fw.psum("ps_misc", [128, 512], F32)
    ps_g = [V(ps_big[:, 0:512], ps_big.res), fw.psum("ps_g1", [128, 512], F32)]
    ps_l = [V(ps_big[:, 512:1024], Res("ps_l0")), fw.psum("ps_l1", [128, 512], F32)]
    ps_y = [fw.psum("ps_y%d" % i, [128, 512], F32) for i in range(2)]
    combT_d = k.combT_d

    dma(fw, "pool", wr[:], k.router_w[l].rearrange("(c p) e -> p c e", p=128), [], [wr])
    dma(fw, "sp", rb[:], k.router_b[l:l + 1, :].partition_broadcast(128), [], [rb])
    dma(fw, "sp", bout[:], k.exp_b_out[l], [], [bout])
    dma(fw, "sp", bin_sb[:], k.exp_b_in[l], [], [bin_sb])
    for mt in range(16):
        m_, two = mt // 2, mt % 2
        fw.pe(lambda e, mt=mt, m_=m_, two=two: e.transpose(
            ps_misc[:, mt * NE:(mt + 1) * NE],
            bin_sb[:, two + 256 * m_: 256 * m_ + 256: 2], k.ident_f[0:NE, 0:NE]),
            r=[bin_sb, k.ident_f], w=[ps_misc])
    fw.dve(lambda e: e.tensor_copy(binT[:].rearrange("p a b -> p (a b)"), ps_misc[:, 0:16 * NE]),
           r=[ps_misc], w=[binT])
    fw.dve(lambda e: e.tensor_scalar(out=binT[:, 1::2, :], in0=binT[:, 1::2, :], scalar1=1.0, scalar2=None, op0=ALU.add),
           r=[binT], w=[binT])

    for i, t in enumerate(tiles):
        row = rowtype(t)
        fe.run(src[t * 128:(t + 1) * 128, :], src, G, l, base, row,
               lambda c, i=i: ynT[:, c, i * 128:(i + 1) * 128], ynT)
        for c in range(8):
            fw.pe(lambda e, c=c, i=i: e.matmul(ps_misc[:, 0:NE], ynT[:, c, i * 128:(i + 1) * 128], wr[:, c, :],
                                               start=(c == 0), stop=(c == 7)), r=[ynT, wr], w=[ps_misc])
        fw.dve(lambda e: e.tensor_tensor(out=lg[:], in0=ps_misc[:, 0:NE], in1=rb[:], op=ALU.add),
               r=[ps_misc, rb], w=[lg])
        fw.dve(lambda e: e.max(out=m8[:], in_=lg[:]), r=[lg], w=[m8])
        fw.dve(lambda e: e.tensor_scalar(out=msk[:], in0=lg[:], scalar1=m8[:, 3:4], scalar2=None, op0=ALU.is_ge),
               r=[lg, m8], w=[msk])
        fw.dve(lambda e: e.tensor_scalar(out=nmx[:], in0=m8[:, 0:1], scalar1=-1.0, scalar2=None, op0=ALU.mult),
               r=[m8], w=[nmx])
        fw.act(lambda e: e.activation(out=ex[:], in_=lg[:], func=AF.Exp, bias=nmx[:, 0:1]), r=[lg, nmx], w=[ex])
        fw.dve(lambda e: e.tensor_tensor(out=ex[:], in0=ex[:], in1=msk[:], op=ALU.mult), r=[ex, msk], w=[ex])
        fw.dve(lambda e: e.reduce_sum(out=sm[:], in_=ex[:], axis=AX.X), r=[ex], w=[sm])
        fw.dve(lambda e: e.reciprocal(out=sm[:], in_=sm[:]), r=[sm], w=[sm])
        fw.dve(lambda e: e.tensor_scalar(out=comb[:], in0=ex[:], scalar1=sm[:, 0:1], scalar2=None, op0=ALU.mult),
               r=[ex, sm], w=[comb])
        fw.pe(lambda e: e.transpose(ps_misc[0:NE, 128:256], comb[:], k.ident_f[:]), r=[comb, k.ident_f], w=[ps_misc])
        fw.act(lambda e, i=i: e.activation(out=combT[:, i * 128:(i + 1) * 128], in_=ps_misc[0:NE, 128:256], func=AF.Copy),
               r=[ps_misc], ww=[combT])
    dma(fw, "sp", combT_d[:, 0:TB], combT[:], [combT], [combT_d])

    for d in range(8):
        for (o, n) in SB:
            fw.pe(lambda e, d=d, o=o, n=n: e.matmul(ps_misc[:, 0:n], bout[:, d * 128:(d + 1) * 128], combT[:, o:o + n],
                                                    start=True, stop=True), r=[bout, combT], w=[ps_misc])
            fw.act(lambda e, d=d, o=o, n=n: e.activation(out=acc[:, d, o:o + n], in_=ps_misc[:, 0:n], func=AF.Copy),
                   r=[ps_misc], ww=[acc])

    cnt = {"w1": 0, "u": 0, "cast": 0, "y": 0}

    def load_w1(e_, m_):
        i = cnt["w1"]
        cnt["w1"] += 1
        s_ = st1[i % 2]
        wb = w1b[i % 4]
        dma(fw, "sp", s_[:], k.exp_w_in[l, e_].rearrange("(c p) f -> p c f", p=128)[:, :, m_ * 256:(m_ + 1) * 256],
            [], [s_])
        if True:
            fw.act(lambda e: e.activation(out=wb[:], in_=s_[:].rearrange("p c (j two) -> p c two j", two=2), func=AF.Copy),
                   r=[s_], w=[wb])
        else:
            fw.pool(lambda e: e.tensor_copy(wb[:], s_[:].rearrange("p c (j two) -> p c two j", two=2)),
                    r=[s_], w=[wb])
        return wb

    def load_w2_piece(e_, m_):
        i = cnt["cast"]
        cnt["cast"] += 1
        s_ = st2[i % 2]
        w2b, w2p = w2bs[e_ % 2], w2ps[e_ % 2]
        dma(fw, "sp", s_[:], k.exp_w_out[l, e_, m_ * 128:(m_ + 1) * 128, :], [], [s_])
        if i % 2 == 0:
            fw.pool(lambda e, m_=m_, s_=s_, w2b=w2b: e.tensor_copy(w2b[:, m_, :], s_[:]), r=[s_], w=[w2p[m_]])
        else:
            fw.act(lambda e, m_=m_, s_=s_, w2b=w2b: e.activation(out=w2b[:, m_, :], in_=s_[:], func=AF.Copy), r=[s_], w=[w2p[m_]])

    def mm1(e_, w2_of=None):
        g = gbc[0]
        dma(fw, "sp", g[:], combT_d[e_:e_ + 1, 0:TB].partition_broadcast(128), [combT_d], [g])
        aT = actT[e_ % 2]
        for m_ in range(8):
            wb = load_w1(e_, m_)
            if w2_of is not None:
                load_w2_piece(w2_of, m_)
            for (o, n) in SB:
                u = cnt["u"]
                cnt["u"] += 1
                pg, pl = ps_g[u % 2], ps_l[u % 2]
                A, B, C = ub[u % 2]
                for c in range(8):
                    fw.pe(lambda e, c=c, o=o, n=n, wb=wb, pg=pg: e.matmul(pg[:, 0:n], wb[:, c, 0, :], ynT[:, c, o:o + n],
                                                                          start=(c == 0), stop=(c == 7)), r=[wb, ynT], w=[pg])
                for c in range(8):
                    fw.pe(lambda e, c=c, o=o, n=n, wb=wb, pl=pl: e.matmul(pl[:, 0:n], wb[:, c, 1, :], ynT[:, c, o:o + n],
                                                                          start=(c == 0), stop=(c == 7)), r=[wb, ynT], w=[pl])
                bg = binT[:, 2 * m_, e_:e_ + 1]
                bl = binT[:, 2 * m_ + 1, e_:e_ + 1]
                fw.dve(lambda e, n=n, pg=pg, A=A, bg=bg: e.tensor_scalar(out=A[:, 0:n], in0=pg[:, 0:n], scalar1=bg, scalar2=7.0,
                                                                        op0=ALU.add, op1=ALU.min), r=[pg, binT], w=[A])
                fw.act(lambda e, n=n, A=A, B=B: e.activation(out=B[:, 0:n], in_=A[:, 0:n], func=AF.Sigmoid, scale=1.702),
                       r=[A], w=[B])
                fw.act(lambda e, n=n, pl=pl, C=C, bl=bl: e.activation(out=C[:, 0:n], in_=pl[:, 0:n], func=AF.Identity, bias=bl),
                       r=[pl, binT], w=[C])
                fw.pool(lambda e, n=n, C=C: e.tensor_scalar(out=C[:, 0:n], in0=C[:, 0:n], scalar1=8.0, scalar2=-6.0,
                                                           op0=ALU.min, op1=ALU.max), r=[C], w=[C])
                fw.dve(lambda e, n=n, A=A, B=B: e.tensor_tensor(out=A[:, 0:n], in0=A[:, 0:n], in1=B[:, 0:n], op=ALU.mult),
                       r=[A, B], w=[A])
                fw.pool(lambda e, n=n, A=A, C=C: e.tensor_tensor(out=C[:, 0:n], in0=C[:, 0:n], in1=A[:, 0:n], op=ALU.mult),
                        r=[A, C], w=[C])
                fw.dve(lambda e, n=n, o=o, C=C, g=g, aT=aT, m_=m_: e.tensor_tensor(out=aT[:, m_, o:o + n], in0=C[:, 0:n],
                                                                                  in1=g[:, o:o + n], op=ALU.mult),
                       r=[C, g], ww=[aT])

    def mm2(e_):
        aT = actT[e_ % 2]
        w2b, w2p = w2bs[e_ % 2], w2ps[e_ % 2]
        for d in range(8):
            for (o, n) in SB:
                py = ps_y[cnt["y"] % 2]
                cnt["y"] += 1
                for m_ in range(8):
                    fw.pe(lambda e, m_=m_, d=d, o=o, n=n, py=py, w2b=w2b: e.matmul(py[:, 0:n], w2b[:, m_, d * 128:(d + 1) * 128],
                                                                          aT[:, m_, o:o + n], start=(m_ == 0), stop=(m_ == 7)),
                          r=[w2p[m_], aT], w=[py])
                fw.dve(lambda e, d=d, o=o, n=n, py=py: e.tensor_tensor(out=acc[:, d, o:o + n], in0=py[:, 0:n],
                                                                      in1=acc[:, d, o:o + n], op=ALU.add),
                       r=[py], ww=[acc])

    ne = k.n_experts_dbg if hasattr(k, "n_experts_dbg") else NE
    mm1(0)
    for e_ in range(ne):
        if e_ + 1 < ne:
            mm1(e_ + 1, w2_of=e_)
        else:
            for m_ in range(8):
                load_w2_piece(e_, m_)
        mm2(e_)

    for i, t in enumerate(tiles):
        row = rowtype(t)
        drows, dres = dst_fn(t)
        if drows is None:
            continue
        be.run(lambda c, i=i: acc[:, c, i * 128:(i + 1) * 128], acc, l, 40, row,
               src[t * 128:(t + 1) * 128, :], src, drows, dres)
    fw.flush()
def ucol(s):
    return 16 + s if s < 256 else s + 48


def phase_ab(fw, k, b):
    l = 0
    t0 = 18 * b
    TS = 2304
    xnT = fw.sbuf("xnT", [128, 8, TS], BF16)
    OT = xnT
    QT = fw.sbuf("QT", [128, 4, TS], BF16)
    KT = fw.sbuf("KT", [128, 4, TS], BF16)
    Vt = fw.sbuf("Vt", [128, 18, 512], BF16)
    Vo = fw.sbuf("Vo", [128, 17, 512], BF16)
    UT = fw.sbuf("UT", [128, 4, 2368], BF16)
    w_in = fw.sbuf("w_in", [128, 8, 2048], BF16)
    EB = V(w_in[:].rearrange("p h (o jp q) -> p h o jp q", o=8, jp=4), w_in.res)
    w_out = V(UT[:].rearrange("p g x -> p (g x)")[:, 0:8 * D].rearrange("p (c f) -> p c f", c=8), UT.res)
    pw = fw.sbuf("pw", [128, 4, 128], BF16)
    pscale = fw.sbuf("pscale", [128, 4], F32)
    gq = fw.sbuf("gq", [128, 1], F32)
    gk = fw.sbuf("gk", [128, 1], F32)
    bd = fw.sbuf("bd", [128, 128], BF16)
    ones = fw.sbuf("ones", [128, 128], BF16)
    sqb = [fw.sbuf("sqb%d" % i, [128, 512], BF16) for i in range(2)]
    rstd = [fw.sbuf("qrstd%d" % i, [128, 512], F32) for i in range(2)]
    tA = fw.sbuf("ptA", [128, 544], F32)
    tB = fw.sbuf("ptB", [128, 544], F32)
    rcb = fw.sbuf("rcb", [128, 512], F32)
    pooled = fw.sbuf("pooled", [128, 512], BF16)
    mk = fw.sbuf("mk", [128, 4, 64], F32)
    rbt = [fw.sbuf("rbt%d" % i, [128, 4, 64], F32) for i in range(2)]
    Pb = [fw.sbuf("Pb%d" % i, [128, 6, 64], BF16) for i in range(3)]
    rd = [fw.sbuf("rd%d" % i, [128, 64], F32) for i in range(2)]
    resT = fw.sbuf("resT", [128, 8, 128], F32)
    gn = load_gain_T(fw, k, "gnm", k.norm_mix_g[l, :])
    G, base = make_GS(fw, k, l, 0, gn)
    fe = Front(fw, k)
    big = fw.psum("ab_big", [128, 1024], F32)
    pq = fw.psum("ab_pq", [128, 2048], F32)
    be = Back(fw, k, fe.xt, big)
    pAB = [V(big[:, 0:512], Res("pA")), V(big[:, 512:1024], Res("pB"))]
    pS = V(pq[:, 0:512], Res("pS"))
    pO = [V(pq[:, 0:64], pS.res), V(pq[:, 512:576], Res("pO1"))]
    pD = [V(pq[:, 1024:1088], Res("pD0")), V(pq[:, 1536:1600], Res("pD1"))]

    for c in range(8):
        dma(fw, "pool", w_in[:, c, :], k.ab_w_in[0, c * 128:(c + 1) * 128, :], [], [w_in])
    fw.pool(lambda e: e.memset(UT[:], 0.0), w=[UT])
    for (g_, src) in ((gq, k.na_q_g), (gk, k.na_k_g)):
        for hl in range(2):
            dma(fw, "sp", g_[hl * 64:(hl + 1) * 64, :], src[0, :].rearrange("(p o) -> p o", o=1), [], [g_])
    fw.dve(lambda e: e.tensor_scalar(out=gq[:], in0=gq[:], scalar1=0.125, scalar2=None, op0=ALU.mult), r=[gq], w=[gq])
    fw.pool(lambda e: e.memset(bd[:], 0.0), w=[bd])
    fw.pool(lambda e: e.memset(bd[0:64, 0:64], 1.0), w=[bd])
    fw.pool(lambda e: e.memset(bd[64:128, 64:128], 1.0), w=[bd])
    fw.pool(lambda e: e.memset(ones[:], 1.0), w=[ones])
    dma(fw, "pool", pw[:], k.pool_w[0].rearrange("g c d -> c g d"), [], [pw])
    dma(fw, "sp", pscale[:], k.pool_scale[0, :].rearrange("(g p) -> p g", p=128), [], [pscale],
        allow_slow_non_contiguous=True)
    dma(fw, "sp", mk[:], k.c_namask[:, :].rearrange("(jp p) q -> p jp q", p=128), [], [mk])

    for i in range(18):
        t = t0 + i
        fe.run(k.xin[t * 128:(t + 1) * 128, :], k.xin, G, l, base, rowtype(t),
               lambda c, i=i: xnT[:, c, i * 128:(i + 1) * 128], xnT)

    n_ = 0
    for m in range(8):
        dstT, gg = (QT, gq) if m < 4 else (KT, gk)
        mm = m % 4
        for (o, n) in subs(TS):
            ps = pAB[n_ % 2]
            sq = sqb[n_ % 2]
            rs = rstd[n_ % 2]
            n_ += 1
            for c in range(8):
                fw.pe(lambda e, c=c, m=m, o=o, n=n, ps=ps: e.matmul(ps[:, 0:n], w_in[:, c, m * 128:(m + 1) * 128],
                                                                   xnT[:, c, o:o + n], start=(c == 0), stop=(c == 7)),
                      r=[w_in, xnT], w=[ps])
            fw.act(lambda e, n=n, ps=ps, sq=sq: e.activation(out=sq[:, 0:n], in_=ps[:, 0:n], func=AF.Square), r=[ps], w=[sq])
            fw.pe(lambda e, n=n, sq=sq: e.matmul(pS[:, 0:n], bd[:], sq[:, 0:n], start=True, stop=True), r=[bd, sq], w=[pS])
            fw.dve(lambda e, n=n, rs=rs: e.tensor_scalar(out=rs[:, 0:n], in0=pS[:, 0:n], scalar1=1.0 / 64, scalar2=EPS,
                                                        op0=ALU.mult, op1=ALU.add), r=[pS], w=[rs])
            fw.act(lambda e, n=n, rs=rs: e.activation(out=rs[:, 0:n], in_=rs[:, 0:n], func=AF.Sqrt), r=[rs], w=[rs])
            fw.dve(lambda e, n=n, rs=rs: e.reciprocal(out=rs[:, 0:n], in_=rs[:, 0:n]), r=[rs], w=[rs])
            fw.dve(lambda e, n=n, o=o, rs=rs, ps=ps, dstT=dstT, gg=gg, mm=mm: e.scalar_tensor_tensor(
                out=dstT[:, mm, o:o + n], in0=ps[:, 0:n], scalar=gg[:, 0:1], in1=rs[:, 0:n], op0=ALU.mult, op1=ALU.mult),
                r=[ps, rs, gg], ww=[dstT])
    for (dst, ntile, off) in ((Vt, 18, 0), (Vo, 17, 64)):
        for i in range(ntile):
            ps = pAB[n_ % 2]
            n_ += 1
            for c in range(8):
                fw.pe(lambda e, c=c, i=i, off=off, ps=ps: e.matmul(ps[:, :], xnT[:, c, off + i * 128: off + (i + 1) * 128],
                                                                  w_in[:, c, 1024:1536], start=(c == 0), stop=(c == 7)),
                      r=[w_in, xnT], w=[ps])
            fw.act(lambda e, i=i, ps=ps, dst=dst: e.activation(out=dst[:, i, :], in_=ps[:, :], func=AF.Copy), r=[ps], ww=[dst])
    for g in range(4):
        for (o, n) in subs(TS):
            ps = pAB[n_ % 2]
            n_ += 1
            for c in range(8):
                fw.pe(lambda e, c=c, g=g, o=o, n=n, ps=ps: e.matmul(ps[:, 0:n], w_in[:, c, 1536 + g * 128:1536 + (g + 1) * 128],
                                                                   xnT[:, c, o:o + n], start=(c == 0), stop=(c == 7)),
                      r=[w_in, xnT], w=[ps])
            pieces = [(o, n)] if o >= 256 else [(0, 256), (256, n - 256)]
            for (po_, pn) in pieces:
                fw.dve(lambda e, g=g, po_=po_, pn=pn, o=o, ps=ps: e.tensor_copy(UT[:, g, ucol(po_):ucol(po_) + pn],
                                                                              ps[:, po_ - o:po_ - o + pn]), r=[ps], ww=[UT])

    for g, w in enumerate((2, 4, 8, 16)):
        nlev = (2, 4, 8, 16).index(w) + 1
        plist = [(16, 256, 0, 1, 0)] + [(304 + 512 * j, 512, 256 + 512 * j, 0, 512 * j) for j in range(4)]
        for (lo, n, s0, seg, p0) in plist:
            W = n + 32
            xb = lo - 16
            fw.dve(lambda e, g=g, W=W, xb=xb: e.tensor_tensor(out=tA[:, 1:W], in0=UT[:, g, xb + 1:xb + W],
                                                             in1=UT[:, g, xb:xb + W - 1], op=ALU.add), r=[UT], w=[tA])
            cur, oth = tA, tB
            sh = 2
            valid = 1
            for lev in range(1, nlev):
                lo_x = valid + sh
                fw.dve(lambda e, W=W, cur=cur, oth=oth, lo_x=lo_x, sh=sh: e.tensor_tensor(
                    out=oth[:, lo_x:W], in0=cur[:, lo_x:W], in1=cur[:, lo_x - sh:W - sh], op=ALU.add), r=[cur], w=[oth])
                cur, oth = oth, cur
                valid = lo_x
                sh *= 2
            X0 = 16 + w // 2 - 1
            dma(fw, "sp", rcb[:, 0:n], k.c_poolrc[g, seg:seg + 1, 16 + p0:16 + p0 + n].partition_broadcast(128), [], [rcb])
            fw.dve(lambda e, n=n, cur=cur, oth=oth, X0=X0: e.tensor_tensor(out=oth[:, 0:n], in0=cur[:, X0:X0 + n],
                                                                         in1=rcb[:, 0:n], op=ALU.mult), r=[cur, rcb], w=[oth])
            fw.pool(lambda e, n=n, oth=oth, g=g, lo=lo: e.tensor_tensor(out=pooled[:, 0:n], in0=oth[:, 0:n],
                                                                       in1=UT[:, g, lo:lo + n], op=ALU.subtract),
                    r=[oth, UT], w=[pooled])
            ps = pAB[n_ % 2]
            n_ += 1
            fw.pe(lambda e, n=n, g=g, ps=ps: e.matmul(ps[:, 0:n], pw[:, g, :], pooled[:, 0:n], start=True, stop=True),
                  r=[pw, pooled], w=[ps])
            fw.act(lambda e, n=n, g=g, s0=s0, ps=ps: e.activation(out=OT[:, 4 + g, s0:s0 + n], in_=ps[:, 0:n], func=AF.Copy,
                                                                 scale=pscale[:, g:g + 1]), r=[ps, pscale], ww=[OT])

    n2 = 0
    for h in range(8):
        for o in range(8):
            rb_ = rbt[n2 % 2]
            n2 += 1
            dma(fw, "sp", rb_[:], k.rpb_g[h, o].rearrange("(jp p) q -> p jp q", p=128), [], [rb_])
            fw.pool(lambda e, rb_=rb_: e.tensor_tensor(out=rb_[:], in0=rb_[:], in1=mk[:], op=ALU.add), r=[rb_, mk], w=[rb_])
            fw.act(lambda e, rb_=rb_, h=h, o=o: e.activation(out=EB[:, h, o, :, :], in_=rb_[:], func=AF.Exp), r=[rb_], ww=[EB])

    for c in range(8):
        dma(fw, "pool", w_out[:, c, :], k.ab_w_out[0, c * 128:(c + 1) * 128, :], [], [w_out])

    cnt = {"u": 0}
    pst = pAB

    def unit(qs, m, hl, ktiles, eb):
        u = cnt["u"]
        cnt["u"] += 1
        bp = 64 * hl
        ps = pst[u % 2]
        P = Pb[u % 3]
        rdd = rd[u % 2]
        nk = len(ktiles)
        for kt, (ks, vt) in enumerate(ktiles):
            fw.pe(lambda e, kt=kt, ks=ks, ps=ps: e.matmul(ps[:, kt * 64:(kt + 1) * 64], KT[bp:bp + 64, m, ks:ks + 128],
                                                         QT[bp:bp + 64, m, qs:qs + 64], start=True, stop=True),
                  r=[KT, QT], w=[ps])
        fw.act(lambda e, nk=nk, ps=ps, P=P: e.activation(out=P[:, 0:nk, :].rearrange("p a b -> p (a b)"), in_=ps[:, 0:nk * 64],
                                                        func=AF.Exp), r=[ps], w=[P])
        if eb is not None:
            fw.dve(lambda e, P=P, eb=eb: e.tensor_tensor(out=P[:, 0:4, :], in0=P[:, 0:4, :], in1=eb, op=ALU.mult),
                   r=[P, EB], w=[P])
        for kt, (ks, vt) in enumerate(ktiles):
            fw.pe(lambda e, kt=kt, vt=vt, P=P: e.matmul(pO[hl][:, :], vt[:, m * 128:(m + 1) * 128], P[:, kt, :],
                                                       start=(kt == 0), stop=(kt == nk - 1)), r=[Vt, Vo, P], w=[pO[hl]])
        for kt, (ks, vt) in enumerate(ktiles):
            fw.pe(lambda e, kt=kt, P=P: e.matmul(pD[hl][:, :], ones[:], P[:, kt, :],
                                                start=(kt == 0), stop=(kt == nk - 1)), r=[ones, P], w=[pD[hl]])
        fw.dve(lambda e, rdd=rdd: e.reciprocal(out=rdd[bp:bp + 64, :], in_=pD[hl][bp:bp + 64, :]), r=[pD[hl]], w=[rdd])
        fw.dve(lambda e, rdd=rdd: e.tensor_tensor(out=OT[bp:bp + 64, m, qs:qs + 64], in0=pO[hl][bp:bp + 64, :],
                                                 in1=rdd[bp:bp + 64, :], op=ALU.mult), r=[pO[hl], rdd], ww=[OT])

    ctx_tiles = [(0, Vt[:, 0, :]), (128, Vt[:, 1, :])]
    for m in range(4):
        for qb in range(4):
            for hl in range(2):
                unit(qb * 64, m, hl, ctx_tiles, None)
        for r in range(32):
            rs_ = min(max(r - 4, 0), 24)
            o = r - rs_
            kts = []
            for kt in range(4):
                rho = rs_ + 2 * kt
                ks = 256 + rho * 64
                vt = Vt[:, 2 + rho // 2, :] if rho % 2 == 0 else Vo[:, (3 + rho) // 2, :]
                kts.append((ks, vt))
            kts += ctx_tiles
            for hl in range(2):
                unit(256 + r * 64, m, hl, kts, EB[:, 2 * m + hl, o, :, :])

    pOP = V(pq[:, 0:1024].rearrange("p (d t) -> p d t", d=8), pS.res)
    for i in range(18):
        t = t0 + i
        for d in range(8):
            for c in range(8):
                fw.pe(lambda e, c=c, d=d, i=i: e.matmul(pOP[:, d, :], w_out[:, c, d * 128:(d + 1) * 128],
                                                       OT[:, c, i * 128:(i + 1) * 128], start=(c == 0), stop=(c == 7)),
                      r=[w_out, OT], w=[pOP])
        fw.act(lambda e: e.activation(out=resT[:].rearrange("p d t -> p (d t)"), in_=pq[:, 0:1024], func=AF.Copy),
               r=[pOP], w=[resT])
        be.run(lambda c: resT[:, c, :], resT, l, 16, rowtype(t),
               k.xin[t * 128:(t + 1) * 128, :], k.xin, k.H[0][t * 128:(t + 1) * 128, :], k.H[0])
    fw.flush()
C0 = 0.6065306597126334
TSQ = 2304


def rw_declare(fw, k):
    k.rw = {}
    for nm in ("r", "kk", "k0", "k1", "b0", "b1", "s0", "s1", "g", "bonus", "y"):
        k.rw[nm] = fw.dram("rw_" + nm, [TSQ, D], F32)
    k.rw["v"] = fw.dram("rw_v", [TSQ, D], BF16)


def bc_load(fw, name, row_ap):
    t = fw.sbuf(name, [128, D], F32)
    dma(fw, "sp", t[:], row_ap.partition_broadcast(128), [], [t])
    return t


def phase_rw_feat(fw, k, b, src):
    l = 1
    t0 = 18 * b
    A = k.rw
    xp = fw.sbuf("xp", [128, 8, 2310], BF16)
    col = lambda s: 2 + s if s < 256 else 4 + s
    wr = fw.sbuf("rw_wr", [128, 8, D], BF16)
    wk = fw.sbuf("rw_wk", [128, 8, D], BF16)
    wv = fw.sbuf("rw_wv", [128, 8, D], BF16)
    w1 = fw.sbuf("rw_w1", [128, 2, 8, 64], BF16)
    a1 = fw.sbuf("rw_a1", [128, 2, 8, 64], BF16)
    g1 = fw.sbuf("rw_g1", [128, 8, 128], BF16)
    w2 = fw.sbuf("rw_w2", [128, 2, D], BF16)
    a2 = fw.sbuf("rw_a2", [128, 2, D], BF16)
    g2 = fw.sbuf("rw_g2", [128, D], BF16)
    mu = fw.sbuf("rw_mu", [128, 6, 8], F32)
    w0b = [bc_load(fw, "w0b%d" % d, k.rw_w0[0, d:d + 1, :]) for d in range(2)]
    a0b = [bc_load(fw, "a0b%d" % d, k.rw_a0[0, d:d + 1, :]) for d in range(2)]
    kkb = bc_load(fw, "kkb", k.rw_k_k[0:1, :])
    kab = bc_load(fw, "kab", k.rw_k_a[0:1, :])
    rkb = bc_load(fw, "rkb", k.rw_r_k[0:1, :, :].rearrange("o h d -> o (h d)"))
    for (dst, srcw) in ((wr, k.rw_w_r), (wk, k.rw_w_k), (wv, k.rw_w_v)):
        for c in range(8):
            dma(fw, "pool", dst[:, c, :], srcw[0, c * 128:(c + 1) * 128, :], [], [dst])
    for d in range(2):
        dma(fw, "pool", w1[:, d, :, :], k.rw_w1[0, d].rearrange("(c p) n -> p c n", p=128), [], [w1])
        dma(fw, "pool", a1[:, d, :, :], k.rw_a1[0, d].rearrange("(c p) n -> p c n", p=128), [], [a1])
        dma(fw, "pool", w2[0:64, d, :], k.rw_w2[0, d], [], [w2])
        dma(fw, "pool", a2[0:64, d, :], k.rw_a2[0, d], [], [a2])
    dma(fw, "pool", g1[:], k.rw_g1[0].rearrange("(c p) n -> p c n", p=128), [], [g1])
    dma(fw, "pool", g2[:], k.rw_g2[0], [], [g2])
    for j in range(6):
        dma(fw, "sp", mu[:, j, :], k.rw_mu[0, j, :].rearrange("(c p) -> p c", p=128), [], [mu], allow_slow_non_contiguous=True)
    fw.pool(lambda e: e.memset(xp[:], 0.0), w=[xp])
    gn = load_gain_T(fw, k, "gnm1", k.norm_mix_g[l, :])
    G, base = make_GS(fw, k, l, 0, gn)
    fe = Front(fw, k)
    for i in range(18):
        t = t0 + i
        fe.run(src[t * 128:(t + 1) * 128, :], src, G, l, base, rowtype(t),
               lambda c, i=i: xp[:, c, col(i * 128):col(i * 128) + 128], xp)

    xx = fw.sbuf("rw_xx", [128, 8, 128], F32)
    tmpx = fw.sbuf("rw_tmpx", [128, 8, 128], F32)
    xm = [fw.sbuf("rw_xm%d" % j, [128, 8, 128], BF16) for j in range(6)]
    hT = [fw.sbuf("rw_hT%d" % i, [128, 128], BF16) for i in range(5)]
    pp = [fw.psum("rwf_pp%d" % i, [128, 1024], F32) for i in range(3)]
    ph = fw.psum("rwf_ph", [128, 4, 128], F32)
    tr = fw.sbuf("t_r", [128, D], F32)
    tk = fw.sbuf("t_k", [128, D], F32)
    tvb = fe.xh[0]
    tkk = fw.sbuf("t_kk", [128, D], F32)
    tsq = fe.xt[0]
    ta = [fw.sbuf("t_a", [128, D], F32)] * 2
    tsg = [fw.sbuf("t_sg", [128, D], F32)] * 2
    tkd = [fw.sbuf("t_kd", [128, D], F32)] * 2
    tbt = [fw.sbuf("t_bt", [128, D], F32)] * 2
    tg = tsq
    tbo = fe.xt[1]
    s16 = [fw.sbuf("t_s16_%d" % i, [128, 16], F32) for i in range(3)]
    v3 = lambda t_: t_[:].rearrange("p (h d) -> p h d", d=64)
    b16 = lambda s_: s_[:].unsqueeze(2).to_broadcast([128, 16, 64])

    for i in range(getattr(k, "rwf_tiles", 18)):
        c0_ = col(i * 128)
        rows = slice(i * 128, (i + 1) * 128)
        fw.pool(lambda e, c0_=c0_: e.tensor_tensor(out=tmpx[:], in0=xp[:, :, c0_ - 1:c0_ + 127], in1=xp[:, :, c0_ + 1:c0_ + 129],
                                                  op=ALU.add), r=[xp], w=[tmpx])
        fw.dve(lambda e, c0_=c0_: e.scalar_tensor_tensor(out=xx[:], in0=tmpx[:], scalar=0.5, in1=xp[:, :, c0_:c0_ + 128],
                                                        op0=ALU.mult, op1=ALU.subtract), r=[tmpx, xp], w=[xx])
        for j in range(6):
            fw.pool(lambda e, j=j: e.tensor_tensor(out=tmpx[:], in0=xx[:], in1=mu[:, j, :].unsqueeze(2).to_broadcast([128, 8, 128]),
                                                  op=ALU.mult), r=[xx, mu], w=[tmpx])
            fw.pool(lambda e, j=j, c0_=c0_: e.tensor_tensor(out=xm[j][:], in0=tmpx[:], in1=xp[:, :, c0_:c0_ + 128], op=ALU.add),
                    r=[tmpx, xp], w=[xm[j]])
        if getattr(k, 'rwf_stage', 99) <= 1:
            continue
        for (pi, xj, wt) in ((0, 0, wr), (1, 2, wk), (2, 3, wv)):
            for half in range(2):
                for c in range(8):
                    fw.pe(lambda e, pi=pi, xj=xj, wt=wt, half=half, c=c: e.matmul(
                        pp[pi][:, half * 512:(half + 1) * 512], xm[xj][:, c, :], wt[:, c, half * 512:(half + 1) * 512],
                        start=(c == 0), stop=(c == 7)), r=[xm[xj], wt], w=[pp[pi]])
        var = getattr(k, "rwf_var", "")
        if var != "noevac":
            if var != "nor":
                fw.act(lambda e: e.activation(out=tr[:], in_=pp[0][:], func=AF.Copy), r=[pp[0]], w=[tr])
            if var != "nok":
                fw.act(lambda e: e.activation(out=tk[:], in_=pp[1][:], func=AF.Copy), r=[pp[1]], w=[tk])
            if var != "nokk":
                fw.pool(lambda e: e.tensor_tensor(out=tkk[:], in0=tk[:], in1=kkb[:], op=ALU.mult), r=[tk, kkb], w=[tkk])
            if var != "nov":
                fw.act(lambda e: e.activation(out=tvb[:], in_=pp[2][:], func=AF.Copy), r=[pp[2]], w=[tvb])
        if getattr(k, 'rwf_stage', 99) <= 2:
            continue
        for (hi, xj, wt, d, M) in ((0, 1, w1, 0, 64), (1, 1, w1, 1, 64), (2, 4, a1, 0, 64), (3, 4, a1, 1, 64), (4, 5, g1, None, 128)):
            for c in range(8):
                lhs = (lambda wt=wt, d=d, c=c: wt[:, c, :]) if d is None else (lambda wt=wt, d=d, c=c: wt[:, d, c, :])
                if hi < 4:
                    fw.pe(lambda e, hi=hi, xj=xj, c=c, M=M, lhs=lhs: e.matmul(ph[0:M, hi, :], lhs(), xm[xj][:, c, :],
                                                                            start=(c == 0), stop=(c == 7)), r=[xm[xj], wt], w=[ph])
                else:
                    fw.pe(lambda e, xj=xj, c=c, lhs=lhs: e.matmul(pp[2][:, 0:128], lhs(), xm[xj][:, c, :],
                                                                 start=(c == 0), stop=(c == 7)), r=[xm[xj], wt], w=[pp[2]])
        for hi, (fn, M) in enumerate(((AF.Tanh, 64), (AF.Tanh, 64), (AF.Copy, 64), (AF.Copy, 64), (AF.Sigmoid, 128))):
            if hi < 4:
                fw.act(lambda e, hi=hi, fn=fn, M=M: e.activation(out=hT[hi][0:M, :], in_=ph[0:M, hi, :], func=fn), r=[ph], w=[hT[hi]])
            else:
                fw.act(lambda e, hi=hi, fn=fn: e.activation(out=hT[hi][:, :], in_=pp[2][:, 0:128], func=fn), r=[pp[2]], w=[hT[hi]])
        if getattr(k, 'rwf_stage', 99) <= 3:
            continue
        for half in range(2):
            fw.pe(lambda e, half=half: e.matmul(pp[2][:, half * 512:(half + 1) * 512], hT[4][:, :], g2[:, half * 512:(half + 1) * 512],
                                               start=True, stop=True), r=[hT[4], g2], w=[pp[2]])
        fw.pool(lambda e: e.tensor_tensor(out=tsq[:], in0=tkk[:], in1=tkk[:], op=ALU.mult), r=[tkk], w=[tsq])
        fw.dve(lambda e: e.reduce_sum(out=s16[0][:], in_=v3(tsq), axis=AX.X), r=[tsq], w=[s16[0]])
        fw.dve(lambda e: e.tensor_scalar(out=s16[0][:], in0=s16[0][:], scalar1=1e-12, scalar2=None, op0=ALU.add), r=[s16[0]], w=[s16[0]])
        fw.act(lambda e: e.activation(out=s16[0][:], in_=s16[0][:], func=AF.Sqrt), r=[s16[0]], w=[s16[0]])
        fw.dve(lambda e: e.reciprocal(out=s16[0][:], in_=s16[0][:]), r=[s16[0]], w=[s16[0]])
        fw.dve(lambda e: e.tensor_tensor(out=v3(tkk), in0=v3(tkk), in1=b16(s16[0]), op=ALU.mult), r=[tkk, s16[0]], w=[tkk])
        fw.pool(lambda e: e.tensor_tensor(out=tsq[:], in0=tr[:], in1=rkb[:], op=ALU.mult), r=[tr, rkb], w=[tsq])
        for d in range(2):
            for half in range(2):
                fw.pe(lambda e, d=d, half=half: e.matmul(pp[0][:, half * 512:(half + 1) * 512], hT[d][0:64, :],
                                                        w2[0:64, d, half * 512:(half + 1) * 512], start=True, stop=True),
                      r=[hT[d], w2], w=[pp[0]])
                fw.pe(lambda e, d=d, half=half: e.matmul(pp[1][:, half * 512:(half + 1) * 512], hT[2 + d][0:64, :],
                                                        a2[0:64, d, half * 512:(half + 1) * 512], start=True, stop=True),
                      r=[hT[2 + d], a2], w=[pp[1]])
            fw.dve(lambda e, d=d: e.tensor_tensor(out=tsg[d][:], in0=pp[0][:], in1=w0b[d][:], op=ALU.add), r=[pp[0], w0b[d]], w=[tsg[d]])
            fw.act(lambda e, d=d: e.activation(out=tsg[d][:], in_=tsg[d][:], func=AF.Sigmoid), r=[tsg[d]], w=[tsg[d]])
            fw.dve(lambda e, d=d: e.tensor_tensor(out=ta[d][:], in0=pp[1][:], in1=a0b[d][:], op=ALU.add), r=[pp[1], a0b[d]], w=[ta[d]])
            fw.act(lambda e, d=d: e.activation(out=ta[d][:], in_=ta[d][:], func=AF.Sigmoid), r=[ta[d]], w=[ta[d]])
            fw.dve(lambda e, d=d: e.scalar_tensor_tensor(out=tkd[d][:], in0=ta[d][:], scalar=-1.0, in1=kab[:], op0=ALU.add, op1=ALU.mult),
                   r=[ta[d], kab], w=[tkd[d]])
            fw.dve(lambda e, d=d: e.scalar_tensor_tensor(out=tkd[d][:], in0=tkd[d][:], scalar=1.0, in1=tk[:], op0=ALU.add, op1=ALU.mult),
                   r=[tkd[d], tk], w=[tkd[d]])
            fw.pool(lambda e, d=d: e.tensor_tensor(out=tbt[d][:], in0=tkk[:], in1=ta[d][:], op=ALU.mult), r=[tkk, ta[d]], w=[tbt[d]])
            fw.pool(lambda e, d=d: e.tensor_tensor(out=tbo[:], in0=tsq[:], in1=tkd[d][:], op=ALU.mult), r=[tsq, tkd[d]], w=[tbo])
            fw.dve(lambda e, d=d: e.reduce_sum(out=s16[1 + d][:], in_=v3(tbo), axis=AX.X), r=[tbo], w=[s16[1 + d]])
            for (nm, tt) in (("k%d" % d, tkd[d]), ("b%d" % d, tbt[d]), ("s%d" % d, tsg[d])):
                dma(fw, "sp", A[nm][rows, :], tt[:], [tt], [A[nm]])
        fw.dve(lambda e: e.tensor_tensor(out=s16[1][:], in0=s16[1][:], in1=s16[2][:], op=ALU.add), r=[s16[1], s16[2]], w=[s16[1]])
        fw.dve(lambda e: e.tensor_tensor(out=v3(tbo), in0=v3(tvb), in1=b16(s16[1]), op=ALU.mult), r=[tvb, s16[1]], w=[tbo])
        for (nm, tt) in (("r", tr), ("kk", tkk), ("bonus", tbo), ("v", tvb)):
            dma(fw, "sp", A[nm][rows, :], tt[:], [tt], [A[nm]])
        fw.act(lambda e: e.activation(out=tg[:], in_=pp[2][:], func=AF.Copy), r=[pp[2]], w=[tg])
        dma(fw, "sp", A["g"][rows, :], tg[:], [tg], [A["g"]])
    fw.flush()
def phase_rw_scan(fw, k, b):
    A = k.rw
    tri = fw.sbuf("tri", [128, 4, 128], F32)
    dma(fw, "sp", tri[:], k.c_tri[:, :, :], [], [tri])
    mexp = [fw.sbuf("mexp%d" % m, [128, 4, 128], F32) for m in range(4)]
    for m in range(4):
        for hh in range(4):
            fw.pool(lambda e, m=m, hh=hh: e.tensor_copy(mexp[m][:, hh, :], tri[:, m, :]), r=[tri], ww=[mexp[m]])
    onesc = fw.sbuf("onesc", [128, 1], F32)
    fw.pool(lambda e: e.memset(onesc[:], 1.0), w=[onesc])
    Lr = fw.sbuf("Lr", [128, D], F32)
    Lkk = fw.sbuf("Lkk", [128, D], F32)
    Lk = fw.sbuf("Lk", [128, D], F32)
    Lb = fw.sbuf("Lb", [128, D], F32)
    Ls = fw.sbuf("Ls", [128, D], F32)
    Lv = fw.sbuf("Lv", [128, D], BF16)
    gam = fw.sbuf("gam", [128, D], F32)
    gin = fw.sbuf("gin", [128, D], F32)
    gex = fw.sbuf("gex", [128, D], F32)
    At = fw.sbuf("At", [128, D], BF16)
    Rt = fw.sbuf("Rt", [128, D], BF16)
    Bt = fw.sbuf("Bt", [128, D], BF16)
    Kt = fw.sbuf("Kt", [128, D], BF16)
    ART = fw.sbuf("ART", [128, 8, 256], BF16)
    BT = fw.sbuf("BT", [128, 8, 128], BF16)
    KTt = fw.sbuf("KTt", [128, 8, 128], BF16)
    gLT = fw.sbuf("gLT", [128, 8], F32)
    ST = fw.sbuf("ST", [128, 8, 64], F32)
    STb = fw.sbuf("STb", [128, 8, 64], BF16)
    Sn = fw.sbuf("Sn", [128, 8, 64], F32)
    PDT = F32 if getattr(k, "scan_fp32", True) else BF16
    P = [[fw.sbuf("P%d_%d" % (g, i), [128, 4, 128], PDT) for i in range(7)] for g in range(4)]
    Q = [[fw.sbuf("Q%d_%d" % (g, i), [128, 4, 128], PDT) for i in range(2)] for g in range(4)]
    Br = [fw.sbuf("Br%d" % g, [128, 4, 128], BF16) for g in range(4)]
    Aak = [fw.sbuf("Aak%d" % g, [128, 4, 128], BF16) for g in range(4)]
    Kr = [fw.sbuf("Kr%d" % g, [128, 4, 128], BF16) for g in range(4)]
    Xb = [[fw.sbuf("Xb%d_%d" % (g, i), [128, 4, 64], BF16) for i in range(2)] for g in range(4)]
    Ub = [fw.sbuf("Ub%d" % g, [128, 4, 64], BF16) for g in range(4)]
    Xf = [fw.sbuf("Xf%d" % g, [128, 4, 64], F32) for g in range(4)]
    Yt = fw.sbuf("Yt", [128, D], F32)
    Yp_ = fw.sbuf("Yprev", [128, D], F32)
    pr = [fw.psum("sc_pr%d" % i, [128, 1024], F32) for i in range(4)]
    rb = [Res("bank%d" % i) for i in range(8)]
    lo = lambda i: pr[i][:, 0:512]
    hi = lambda i: pr[i][:, 512:1024]
    v4 = lambda ap, n: ap.rearrange("p (a t) -> p a t", a=n)
    bcm = lambda m: tri[:, m, :].unsqueeze(1).to_broadcast([128, 4, 128])
    MASK = {0: dict(cum=0, strict=1, incl=0, strictT=2), 1: dict(cum=3, strict=2, incl=3, strictT=1)}

    def chunk(d, ti, want_y, second):
        mk = MASK[d]
        rows = slice(ti * 128, (ti + 1) * 128)
        for (dst, nm) in ((Lr, "r"), (Lkk, "kk"), (Lk, "k%d" % d), (Lb, "b%d" % d), (Ls, "s%d" % d), (Lv, "v")):
            dma(fw, "sp", dst[:], A[nm][rows, :], [A[nm]], [dst])
        cum = pr[3]
        for half in range(2):
            fw.pe(lambda e, half=half: e.matmul(cum[:, half * 512:(half + 1) * 512], tri[:, mk["cum"], :],
                                               Ls[:, half * 512:(half + 1) * 512], start=True, stop=True),
                  r=[tri, Ls], w=[rb[6], rb[7]])
        fw.act(lambda e: e.activation(out=gex[:], in_=cum[:], func=AF.Copy), r=[rb[6], rb[7]], w=[gex])
        fw.act(lambda e: e.activation(out=gam[:], in_=gex[:], func=AF.Exp, scale=-C0), r=[gex], w=[gam])
        fw.act(lambda e: e.activation(out=gin[:], in_=gex[:], func=AF.Exp, scale=C0), r=[gex], w=[gin])
        fw.dve(lambda e: e.tensor_tensor(out=gex[:], in0=gex[:], in1=Ls[:], op=ALU.subtract), r=[gex, Ls], w=[gex])
        fw.act(lambda e: e.activation(out=gex[:], in_=gex[:], func=AF.Exp, scale=-C0), r=[gex], w=[gex])
        glp = pr[2][:, 512:520]
        for c in range(8):
            fw.pe(lambda e, c=c: e.matmul(pr[2][:, 512 + c:513 + c], Ls[:, c * 128:(c + 1) * 128], onesc[:, 0:1],
                                         start=True, stop=True), r=[Ls, onesc], w=[rb[5]])
        fw.act(lambda e: e.activation(out=gLT[:], in_=glp, func=AF.Exp, scale=-C0), r=[rb[5]], w=[gLT])
        fw.dve(lambda e: e.scalar_tensor_tensor(out=At[:], in0=Lkk[:], scalar=-1.0, in1=gex[:], op0=ALU.mult, op1=ALU.mult),
               r=[Lkk, gex], w=[At])
        fw.pool(lambda e: e.tensor_tensor(out=Rt[:], in0=Lr[:], in1=gam[:], op=ALU.mult), r=[Lr, gam], w=[Rt])
        fw.dve(lambda e: e.tensor_tensor(out=Bt[:], in0=Lb[:], in1=gin[:], op=ALU.mult), r=[Lb, gin], w=[Bt])
        fw.pool(lambda e: e.tensor_tensor(out=Kt[:], in0=Lk[:], in1=gin[:], op=ALU.mult), r=[Lk, gin], w=[Kt])
        if getattr(k, 'scan_stage', 99) <= 1:
            return
        tps = [(At, lo(0), rb[0], lambda: ART[:, :, 0:128], ART), (Rt, hi(0), rb[1], lambda: ART[:, :, 128:256], ART),
               (Bt, lo(1), rb[2], lambda: BT[:, :, :], BT), (Kt, hi(1), rb[3], lambda: KTt[:, :, :], KTt)]
        for n_, (src_, bank, res_, dstf, dstT) in enumerate(tps):
            tpv = v4(bank.bitcast(BF16), 8)
            for c in range(8):
                fw.pe(lambda e, c=c, src_=src_, tpv=tpv: e.transpose(tpv[:, c, :], src_[:, c * 128:(c + 1) * 128], k.ident_bf[:]),
                      r=[src_, k.ident_bf], w=[res_])
            if n_ % 2 == 0:
                fw.act(lambda e, tpv=tpv, dstf=dstf: e.activation(out=dstf(), in_=tpv, func=AF.Copy), r=[res_], ww=[dstT])
            else:
                fw.dve(lambda e, tpv=tpv, dstf=dstf: e.tensor_copy(dstf(), tpv), r=[res_], w=[dstT])
        if getattr(k, 'scan_stage', 99) <= 2:
            return
        for g in range(4):
            outs = [(v4(lo(0), 4), rb[0]), (v4(hi(0), 4), rb[1]), (v4(lo(1), 4), rb[2]), (v4(hi(1), 4), rb[3]), (v4(lo(2), 4), rb[4])]
            for hh in range(4):
                h = 4 * g + hh
                c, bp = h // 2, 64 * (h % 2)
                ops_ = [(BT, ART, 0, False), (BT, ART, 128, False), (KTt, ART, 0, False), (KTt, ART, 128, False), (ART, BT, 0, True)]
                fw.pe(lambda e: e.matmul(pr[2][:, 1023:1024], BT[:, 0, :], ART[:, 0, 0:1], start=True, stop=True),
                      r=[BT, ART], w=[rb[5]])
                for oi, (lt, rt_, off, swap) in enumerate(ops_):
                    ps_, rs_ = outs[oi]
                    if not swap:
                        fw.pe(lambda e, hh=hh, c=c, bp=bp, ps_=ps_, lt=lt, off=off: e.matmul(
                            ps_[:, hh, :], lt[bp:bp + 64, c, :], ART[bp:bp + 64, c, off:off + 128], start=True, stop=True),
                            r=[lt, ART], w=[rs_])
                    else:
                        fw.pe(lambda e, hh=hh, c=c, bp=bp, ps_=ps_: e.matmul(
                            ps_[:, hh, :], ART[bp:bp + 64, c, 0:128], BT[bp:bp + 64, c, :], start=True, stop=True),
                            r=[BT, ART], w=[rs_])
            dsts = [(P[g][0], "strict"), (Br[g], "incl"), (Aak[g], "strict"), (Kr[g], "incl"), (Q[g][0], "strictT")]
            svar = getattr(k, "scan_var", "")
            if svar == "mm":
                dsts = []
            for oi, (dt_, mname) in enumerate(dsts):
                ps_, rs_ = outs[oi]
                if oi % 2 == 0:
                    fw.act(lambda e, ps_=ps_, dt_=dt_: e.activation(out=dt_[:], in_=ps_, func=AF.Copy), r=[rs_], w=[dt_])
                else:
                    fw.dve(lambda e, ps_=ps_, dt_=dt_: e.tensor_copy(dt_[:], ps_), r=[rs_], w=[dt_])
                mx = mexp[mk[mname]]
                if svar == "cp":
                    continue
                if oi % 2 == 0:
                    fw.pool(lambda e, dt_=dt_, mx=mx: e.tensor_tensor(out=dt_[:], in0=dt_[:], in1=mx[:], op=ALU.mult), r=[dt_, mx], w=[dt_])
                else:
                    fw.dve(lambda e, dt_=dt_, mx=mx: e.tensor_tensor(out=dt_[:], in0=dt_[:], in1=mx[:], op=ALU.mult), r=[dt_, mx], w=[dt_])
        if getattr(k, 'scan_stage', 99) <= 3:
            return
        for kk_ in range(6):
            for g in range(4):
                pi = 2 + (g % 2)
                Pn, Qn = v4(lo(pi), 4), v4(hi(pi), 4)
                rP, rQ = rb[2 * pi], rb[2 * pi + 1]
                Pk, Qk, Qn_sb = P[g][kk_], Q[g][kk_ % 2], Q[g][(kk_ + 1) % 2]
                for hh in range(4):
                    fw.pe(lambda e, hh=hh, Pn=Pn, Pk=Pk, Qk=Qk: e.matmul(Pn[:, hh, :], Qk[:, hh, :], Pk[:, hh, :], start=True, stop=True),
                          r=[Pk, Qk], w=[rP])
                if kk_ < 5:
                    for hh in range(4):
                        fw.pe(lambda e, hh=hh, Qn=Qn, Pk=Pk, Qk=Qk: e.matmul(Qn[:, hh, :], Pk[:, hh, :], Qk[:, hh, :], start=True, stop=True),
                              r=[Pk, Qk], w=[rQ])
                fw.act(lambda e, g=g, kk_=kk_, Pn=Pn: e.activation(out=P[g][kk_ + 1][:], in_=Pn, func=AF.Copy), r=[rP], w=[P[g][kk_ + 1]])
                if kk_ < 5:
                    fw.dve(lambda e, Qn=Qn, Qn_sb=Qn_sb: e.tensor_copy(Qn_sb[:], Qn), r=[rQ], w=[Qn_sb])
        if getattr(k, 'scan_stage', 99) <= 4:
            return
        Xp = [v4(pr[0][:, g * 256:(g + 1) * 256], 4) for g in range(4)]
        rX = [rb[0], rb[0], rb[1], rb[1]]
        for g in range(4):
            for hh in range(4):
                h = 4 * g + hh
                c, bp = h // 2, 64 * (h % 2)
                fw.pe(lambda e, g=g, hh=hh, c=c, bp=bp: e.matmul(Xp[g][:, hh, :], ART[bp:bp + 64, c, 0:128], STb[bp:bp + 64, c, :],
                                                                start=True, stop=False), r=[ART, STb], w=[rX[g]])
                fw.pe(lambda e, g=g, hh=hh, h=h: e.matmul(Xp[g][:, hh, :], Aak[g][:, hh, :], Lv[:, h * 64:(h + 1) * 64],
                                                         start=False, stop=True), r=[Aak[g], Lv], w=[rX[g]])
        for g in range(4):
            fw.dve(lambda e, g=g: e.tensor_copy(Xf[g][:], Xp[g]), r=[rX[g]], w=[Xf[g]])
            if PDT != F32:
                fw.dve(lambda e, g=g: e.tensor_copy(Xb[g][0][:], Xf[g][:]), r=[Xf[g]], w=[Xb[g][0]])
        for kk_ in range(7):
            for g in range(4):
                xb = Xf[g] if PDT == F32 else Xb[g][kk_ % 2]
                xn = Ub[g] if kk_ == 6 else Xb[g][(kk_ + 1) % 2]
                for hh in range(4):
                    fw.pe(lambda e, g=g, hh=hh, kk_=kk_, xb=xb: e.matmul(Xp[g][:, hh, :], P[g][kk_][:, hh, :], xb[:, hh, :],
                                                                        start=True, stop=True), r=[P[g][kk_], xb], w=[rX[g]])
                fw.dve(lambda e, g=g: e.tensor_tensor(out=Xf[g][:], in0=Xp[g], in1=Xf[g][:], op=ALU.add), r=[rX[g], Xf[g]], w=[Xf[g]])
                fw.act(lambda e, g=g, xn=xn: e.activation(out=xn[:], in_=Xf[g][:], func=AF.Copy), r=[Xf[g]], w=[xn])
        if getattr(k, 'scan_stage', 99) <= 5:
            return
        if want_y:
            Yp = pr[1]
            if second:
                dma(fw, "sp", Yp_[:], A["y"][rows, :], [A["y"]], [Yp_])
            for g in range(4):
                for hh in range(4):
                    h = 4 * g + hh
                    c, bp = h // 2, 64 * (h % 2)
                    rY = rb[2] if h < 8 else rb[3]
                    fw.pe(lambda e, h=h, c=c, bp=bp: e.matmul(Yp[:, h * 64:(h + 1) * 64], ART[bp:bp + 64, c, 128:256], STb[bp:bp + 64, c, :],
                                                             start=True, stop=False), r=[ART, STb], w=[rY])
                    fw.pe(lambda e, h=h, g=g, hh=hh: e.matmul(Yp[:, h * 64:(h + 1) * 64], Br[g][:, hh, :], Ub[g][:, hh, :],
                                                             start=False, stop=False), r=[Br[g], Ub[g]], w=[rY])
                    fw.pe(lambda e, h=h, g=g, hh=hh: e.matmul(Yp[:, h * 64:(h + 1) * 64], Kr[g][:, hh, :], Lv[:, h * 64:(h + 1) * 64],
                                                             start=False, stop=True), r=[Kr[g], Lv], w=[rY])
            fw.act(lambda e: e.activation(out=Yt[:], in_=Yp[:], func=AF.Copy), r=[rb[2], rb[3]], w=[Yt])
            if second:
                fw.pool(lambda e: e.tensor_tensor(out=Yt[:], in0=Yt[:], in1=Yp_[:], op=ALU.add), r=[Yt, Yp_], w=[Yt])
            dma(fw, "sp", A["y"][rows, :], Yt[:], [Yt], [A["y"]])
        if getattr(k, 'scan_stage', 99) <= 6:
            return
        Sp = pr[2][:, :].rearrange("p (c w i) -> p c w i", c=8, w=2)
        for c in range(8):
            for wch in range(2):
                h = 2 * c + wch
                g, hh = h // 4, h % 4
                fw.pe(lambda e, c=c, wch=wch, g=g, hh=hh: e.matmul(Sp[:, c, wch, :], Bt[:, c * 128:(c + 1) * 128], Ub[g][:, hh, :],
                                                                  start=True, stop=False), r=[Bt, Ub[g]], w=[rb[4], rb[5]])
                fw.pe(lambda e, c=c, wch=wch, h=h: e.matmul(Sp[:, c, wch, :], Kt[:, c * 128:(c + 1) * 128], Lv[:, h * 64:(h + 1) * 64],
                                                           start=False, stop=True), r=[Kt, Lv], w=[rb[4], rb[5]])
        for cb in range(2):
            fw.dve(lambda e, cb=cb: e.tensor_copy(Sn[0:64, 4 * cb:4 * cb + 4, :], Sp[0:64, 4 * cb:4 * cb + 4, 0, :]), r=[rb[4 + cb]], ww=[Sn])
            fw.dve(lambda e, cb=cb: e.tensor_copy(Sn[64:128, 4 * cb:4 * cb + 4, :], Sp[64:128, 4 * cb:4 * cb + 4, 1, :]), r=[rb[4 + cb]], ww=[Sn])
        fw.dve(lambda e: e.tensor_tensor(out=ST[:], in0=ST[:], in1=Sn[:], op=ALU.add), r=[ST, Sn], w=[ST])
        fw.dve(lambda e: e.tensor_tensor(out=ST[:], in0=ST[:], in1=gLT[:].unsqueeze(2).to_broadcast([128, 8, 64]), op=ALU.mult),
               r=[ST, gLT], w=[ST])
        fw.act(lambda e: e.activation(out=STb[:], in_=ST[:], func=AF.Copy), r=[ST], w=[STb])

    nt_dbg = getattr(k, "scan_tiles", 18)
    for d in range(2):
        fw.pool(lambda e: e.memset(ST[:], 0.0), w=[ST])
        fw.pool(lambda e: e.memset(STb[:], 0.0), w=[STb])
        order = list(range(18)) if d == 0 else [1, 0] + list(range(17, 1, -1))
        if nt_dbg < 18:
            order = [t_ for t_ in order if t_ < nt_dbg]
        for ti in order:
            chunk(d, ti, ti >= 2, d == 1)
    fw.flush()


def phase_rw_out(fw, k, b, src, dst):
    l = 1
    t0 = 18 * b
    A = k.rw
    wo = fw.sbuf("rw_wo", [128, 8, D], BF16)
    for c in range(8):
        dma(fw, "pool", wo[:, c, :], k.rw_w_o[0, c * 128:(c + 1) * 128, :], [], [wo])
    lnw = bc_load(fw, "lnw", k.rw_ln_w[0:1, :])
    lnb = bc_load(fw, "lnb", k.rw_ln_b[0:1, :])
    ty = fw.sbuf("o_y", [128, D], F32)
    tg = fw.sbuf("o_g", [128, D], F32)
    tb = fw.sbuf("o_b", [128, D], F32)
    tq = fw.sbuf("o_q", [128, D], F32)
    zb = fw.sbuf("o_zb", [128, D], BF16)
    zT = fw.sbuf("o_zT", [128, 8, 128], BF16)
    resT = fw.sbuf("o_resT", [128, 8, 128], F32)
    s1 = fw.sbuf("o_s1", [128, 16], F32)
    s2 = fw.sbuf("o_s2", [128, 16], F32)
    ht = [fw.sbuf("o_ht%d" % i, [128, D], F32) for i in range(2)]
    big = fw.psum("o_big", [128, 1024], F32)
    pz = fw.psum("o_pz", [128, 8, 128], BF16)
    pq = fw.psum("o_pq", [128, 1024], F32)
    be = Back(fw, k, ht, big)
    v3 = lambda t_: t_[:].rearrange("p (h d) -> p h d", d=64)
    b16 = lambda s_: s_[:].unsqueeze(2).to_broadcast([128, 16, 64])
    pOP = pq[:, :].rearrange("p (d t) -> p d t", d=8)
    for i in range(2, getattr(k, "scan_tiles", 18)):
        t = t0 + i
        rows = slice(i * 128, (i + 1) * 128)
        dma(fw, "sp", ty[:], A["y"][rows, :], [A["y"]], [ty])
        dma(fw, "sp", tg[:], A["g"][rows, :], [A["g"]], [tg])
        dma(fw, "sp", tb[:], A["bonus"][rows, :], [A["bonus"]], [tb])
        fw.dve(lambda e: e.reduce_sum(out=s1[:], in_=v3(ty), axis=AX.X), r=[ty], w=[s1])
        fw.dve(lambda e: e.tensor_scalar(out=s1[:], in0=s1[:], scalar1=1.0 / 64, scalar2=None, op0=ALU.mult), r=[s1], w=[s1])
        fw.dve(lambda e: e.tensor_tensor(out=v3(ty), in0=v3(ty), in1=b16(s1), op=ALU.subtract), r=[ty, s1], w=[ty])
        fw.pool(lambda e: e.tensor_tensor(out=tq[:], in0=ty[:], in1=ty[:], op=ALU.mult), r=[ty], w=[tq])
        fw.dve(lambda e: e.reduce_sum(out=s2[:], in_=v3(tq), axis=AX.X), r=[tq], w=[s2])
        fw.dve(lambda e: e.tensor_scalar(out=s2[:], in0=s2[:], scalar1=1.0 / 64, scalar2=64e-5, op0=ALU.mult, op1=ALU.add), r=[s2], w=[s2])
        fw.act(lambda e: e.activation(out=s2[:], in_=s2[:], func=AF.Sqrt), r=[s2], w=[s2])
        fw.dve(lambda e: e.reciprocal(out=s2[:], in_=s2[:]), r=[s2], w=[s2])
        fw.dve(lambda e: e.tensor_tensor(out=v3(ty), in0=v3(ty), in1=b16(s2), op=ALU.mult), r=[ty, s2], w=[ty])
        fw.pool(lambda e: e.tensor_tensor(out=ty[:], in0=ty[:], in1=lnw[:], op=ALU.mult), r=[ty, lnw], w=[ty])
        fw.pool(lambda e: e.tensor_tensor(out=tb[:], in0=tb[:], in1=lnb[:], op=ALU.add), r=[tb, lnb], w=[tb])
        fw.dve(lambda e: e.tensor_tensor(out=ty[:], in0=ty[:], in1=tb[:], op=ALU.add), r=[ty, tb], w=[ty])
        fw.dve(lambda e: e.tensor_tensor(out=zb[:], in0=ty[:], in1=tg[:], op=ALU.mult), r=[ty, tg], w=[zb])
        for c in range(8):
            fw.pe(lambda e, c=c: e.transpose(pz[:, c, :], zb[:, c * 128:(c + 1) * 128], k.ident_bf[:]), r=[zb, k.ident_bf], w=[pz])
        fw.act(lambda e: e.activation(out=zT[:], in_=pz[:], func=AF.Copy), r=[pz], w=[zT])
        for d in range(8):
            for c in range(8):
                fw.pe(lambda e, c=c, d=d: e.matmul(pOP[:, d, :], wo[:, c, d * 128:(d + 1) * 128], zT[:, c, :],
                                                  start=(c == 0), stop=(c == 7)), r=[wo, zT], w=[pq])
        fw.act(lambda e: e.activation(out=resT[:].rearrange("p d t -> p (d t)"), in_=pq[:, :], func=AF.Copy), r=[pq], w=[resT])
        be.run(lambda c: resT[:, c, :], resT, l, 16, rowtype(t), src[t * 128:(t + 1) * 128, :], src,
               dst[t * 128:(t + 1) * 128, :], dst)
    fw.flush()
WSPEC = [
    ("ada_w", [2, D, 6 * D]), ("ada_b", [2, 6 * D]), ("norm_mix_g", [2, D]), ("norm_ffn_g", [2, D]),
    ("router_w", [2, D, NE]), ("router_b", [2, NE]), ("exp_w_in", [2, NE, D, 2 * D]), ("exp_b_in", [2, NE, 2 * D]),
    ("exp_w_out", [2, NE, D, D]), ("exp_b_out", [2, NE, D]),
    ("ab_w_in", [1, D, 2048]), ("na_q_g", [1, 64]), ("na_k_g", [1, 64]),
    ("pool_w", [1, 4, 128, 128]), ("pool_scale", [1, 512]), ("ab_w_out", [1, D, D]),
    ("rw_mu", [1, 6, D]), ("rw_w_r", [1, D, D]), ("rw_w_k", [1, D, D]), ("rw_w_v", [1, D, D]), ("rw_w_o", [1, D, D]),
    ("rw_w0", [1, 2, D]), ("rw_w1", [1, 2, D, 64]), ("rw_w2", [1, 2, 64, D]), ("rw_a0", [1, 2, D]),
    ("rw_a1", [1, 2, D, 64]), ("rw_a2", [1, 2, 64, D]), ("rw_g1", [1, D, 128]), ("rw_g2", [1, 128, D]),
    ("rw_k_k", [1, D]), ("rw_k_a", [1, D]), ("rw_r_k", [1, 16, 64]), ("rw_ln_w", [1, D]), ("rw_ln_b", [1, D]),
]


def declare(fw, k):
    ne_decl = 1 if getattr(k, "small", False) else NE
    k.xin = fw.dram("xin", [NT * 128, D], F32, kind="ExternalInput")
    k.cc = fw.dram("cc", [3, D], F32, kind="ExternalInput")
    for name, shp in WSPEC:
        if name.startswith("exp_w"):
            shp = [shp[0], ne_decl] + shp[2:]
        setattr(k, name, fw.dram(name, shp, F32, kind="ExternalInput"))
    k.rpb_g = fw.dram("rpb_g", [8, 8, 512, 64], F32, kind="ExternalInput")
    k.c_ident = fw.dram("c_ident", [128, 128], F32, kind="ExternalInput")
    k.c_namask = fw.dram("c_namask", [512, 64], F32, kind="ExternalInput")
    k.c_poolrc = fw.dram("c_poolrc", [4, 2, 2048 + 32], F32, kind="ExternalInput")
    k.c_tri = fw.dram("c_tri", [128, 4, 128], F32, kind="ExternalInput")
    k.out = fw.dram("out", [32 * 128, D], F32, kind="ExternalOutput")
    k.H = [fw.dram("H%d" % i, [NT * 128, D], F32) for i in range(2)]
    k.combT_d = fw.dram("combT_d", [NE, 1152], F32)
    fw.persist = True
    k.modT = [fw.sbuf("modT%d" % l, [128, 48, 3], F32) for l in range(2)]
    k.ident_f = fw.sbuf("ident_f", [128, 128], F32)
    k.ident_bf = fw.sbuf("ident_bf", [128, 128], BF16)
    fw.persist = False
    dma(fw, "sp", k.ident_f[:], k.c_ident[:, :], [], [k.ident_f])
    dma(fw, "pool", k.ident_bf[:], k.c_ident[:, :], [], [k.ident_bf])


def host_consts():
    c = {}
    c["c_ident"] = np.eye(128, dtype=np.float32)
    qc = np.arange(64)
    cs = np.clip(qc - 8, 0, 48)
    kc = np.arange(64)
    valid = (kc[:, None] >= cs[None, :]) & (kc[:, None] < cs[None, :] + 16)
    m = np.where(valid, 0.0, -30000.0).astype(np.float32)
    c["c_namask"] = np.tile(m, (8, 1)).astype(np.float32)
    rc = np.zeros((4, 2, 2048 + 32), np.float32)
    for g, w in enumerate((2, 4, 8, 16)):
        for s, L in enumerate((2048, 256)):
            t = np.arange(L)
            lo = np.clip(t - w // 2, 0, L)
            hi = np.clip(t + w - w // 2, 0, L)
            rc[g, s, 16:16 + L] = 1.0 / (hi - lo)
    c["c_poolrc"] = rc
    tri = np.zeros((128, 4, 128), np.float32)
    s_ = np.arange(128)[:, None]
    t_ = np.arange(128)[None, :]
    tri[:, 0, :] = (s_ <= t_)
    tri[:, 1, :] = (s_ < t_)
    tri[:, 2, :] = (s_ > t_)
    tri[:, 3, :] = (s_ >= t_)
    c["c_tri"] = tri
    return c


def gather_rpb(rpb):
    j = np.arange(8)
    o = np.arange(8)
    kc = np.arange(64)
    qc = np.arange(64)
    ri = j[None, :] - o[:, None] + 7
    ci = np.clip(kc[:, None] - qc[None, :] + 15, 0, 30)
    g = rpb[:, ri[:, :, None, None], ci[None, None, :, :]]
    return np.ascontiguousarray(g.reshape(8, 8, 512, 64)).astype(np.float32)


def shard_inputs(inp, small=False):
    consts = host_consts()
    maps = []
    wts = {name: np.ascontiguousarray(inp[name], dtype=np.float32) for name, _ in WSPEC}
    if small:
        for nm in ("exp_w_in", "exp_w_out"):
            wts[nm] = np.ascontiguousarray(wts[nm][:, 0:1])
    rpbg = gather_rpb(np.asarray(inp["na_rpb"])[0])
    for core in range(8):
        rows = []
        for b in (2 * core, 2 * core + 1):
            rows.append(inp["ctx"][b])
            rows.append(inp["x"][b])
        m = {"xin": np.ascontiguousarray(np.concatenate(rows, axis=0), dtype=np.float32),
             "cc": np.ascontiguousarray(np.stack([inp["c"][2 * core], inp["c"][2 * core + 1], inp["c_ctx"]]), dtype=np.float32),
             "rpb_g": rpbg}
        m.update(wts)
        m.update(consts)
        maps.append(m)
    return maps
def build_program(k=None):
    nc = bass.Bass("TRN2", target_bir_lowering=False)
    fw = FW(nc)
    if k is None:
        k = K()
    declare(fw, k)
    rw_declare(fw, k)
    phase_ada(fw, k)
    for b in range(2):
        phase_ab(fw, k, b)
    for blk in range(4):
        tiles = list(range(9 * blk, 9 * blk + 9))
        phase_moe(fw, k, 0, tiles, k.H[0], lambda t: (k.H[1][t * 128:(t + 1) * 128, :], k.H[1]), blk == 0)
    for b in range(2):
        phase_rw_feat(fw, k, b, k.H[1])
        phase_rw_scan(fw, k, b)
        phase_rw_out(fw, k, b, k.H[1], k.H[0])

    def dst1(t):
        b, i = t // 18, t % 18
        o = b * 16 + (i - 2)
        return (k.out[o * 128:(o + 1) * 128, :], k.out)
    for b in range(2):
        for hb in range(2):
            tiles = [18 * b + 2 + 8 * hb + j for j in range(8)]
            phase_moe(fw, k, 1, tiles, k.H[0], dst1, False)
    fw.flush(final=True)
    return nc, fw


_CACHE = {}


def kernel(**inputs):
    from concourse.bass_utils import run_bass_kernel_spmd
    inp = {n: np.asarray(v) for n, v in inputs.items()}
    if "nc" not in _CACHE:
        _CACHE["nc"] = build_program()[0]
    nc = _CACHE["nc"]
    maps = shard_inputs(inp)
    res = run_bass_kernel_spmd(nc, maps, core_ids=list(range(8)))
    outs = [np.asarray(r["out"]).reshape(2, 2048, D) for r in res.results]
    return np.concatenate(outs, axis=0).astype(np.float32)
```

```python
import numpy as np
import concourse.bass as bass
import concourse.mybir as mybir

F32 = mybir.dt.float32
BF16 = mybir.dt.bfloat16
ALU = mybir.AluOpType
AF = mybir.ActivationFunctionType
AX = mybir.AxisListType

KDMA = 8
ENGS = ("pe", "dve", "act", "pool", "sp")


class Res:
    __slots__ = ("name", "w", "r")

    def __init__(self, name=""):
        self.name = name
        self.w = None
        self.r = {}


class T:
    def __init__(self, t, name):
        self.t = t
        self.res = Res(name)

    def __getitem__(self, k):
        return self.t[k]

    def parts(self, n):
        if not hasattr(self, "_parts"):
            self._parts = [Res("%s.%d" % (self.res.name, i)) for i in range(n)]
        return self._parts


class V(T):
    def __init__(self, ap, res):
        self.t = ap
        self.res = res


def _res(x):
    return x.res if isinstance(x, T) else x


class Op:
    __slots__ = ("waits", "fn", "marked", "kind", "dma_m")

    def __init__(self, fn, kind):
        self.waits = []
        self.fn = fn
        self.marked = False
        self.kind = kind
        self.dma_m = -1


class FW:
    def __init__(self, nc):
        self.nc = nc
        self.ops = {e: [] for e in ENGS}
        self.seen = {e: {} for e in ENGS}
        self.ndma = {e: 0 for e in ENGS}
        self.ctx = []
        self.pctx = []
        self.emitted = {e: 0 for e in ENGS}
        self.phase_end = {e: [] for e in ENGS}
        self.cnt = {e: [] for e in ENGS}
        self.sems = None
        self.persist = False

    def sbuf(self, name, shape, dt):
        self.uid = getattr(self, "uid", 0) + 1
        name = "s%d_%s" % (self.uid, name)
        g = self.nc.sbuf_tensor(name, list(shape), dt)
        t = g.__enter__()
        (self.ctx if self.persist else self.pctx).append(g)
        return T(t, name)

    def psum(self, name, shape, dt=F32):
        self.uid = getattr(self, "uid", 0) + 1
        name = "p%d_%s" % (self.uid, name)
        g = self.nc.psum_tensor(name, list(shape), dt)
        t = g.__enter__()
        (self.ctx if self.persist else self.pctx).append(g)
        return T(t, name)

    def dram(self, name, shape, dt, kind="Internal"):
        t = self.nc.dram_tensor(name, list(shape), dt, kind=kind)
        return T(t.ap(), name)

    def _need(self, eng, tok, op):
        if tok is None:
            return
        if tok[0] == "c":
            _, e, idx = tok
            if e == "pe" and eng == "pe":
                return
            key = ("c", e)
            if idx < self.emitted[e] and not self.ops[e][idx].marked:
                idx = min(i for i in self.phase_end[e] if i >= idx)
                tok = ("c", e, idx)
            if self.seen[eng].get(key, -1) >= idx:
                return
            self.seen[eng][key] = idx
            self.ops[e][idx].marked = True
            op.waits.append(tok)
        else:
            _, q, m = tok
            key = ("d", q, m % KDMA)
            if self.seen[eng].get(key, -1) >= m:
                return
            self.seen[eng][key] = m
            op.waits.append(tok)

    def _deps(self, eng, op, r, w, ww=()):
        for x in r:
            x = _res(x)
            self._need(eng, x.w, op)
        for x in w:
            x = _res(x)
            self._need(eng, x.w, op)
            for tok in x.r.values():
                self._need(eng, tok, op)
        for x in ww:
            x = _res(x)
            if x.w is not None and not (x.w[0] == "c" and x.w[1] == eng):
                self._need(eng, x.w, op)
            for tok in x.r.values():
                self._need(eng, tok, op)

    def _commit(self, tok, r, w):
        for x in r:
            x = _res(x)
            if tok[0] == "c":
                x.r[("c", tok[1])] = tok
            else:
                x.r[("d", tok[1], tok[2] % KDMA)] = tok
        for x in w:
            x = _res(x)
            x.w = tok
            x.r = {}

    def op(self, eng, fn, r=(), w=(), ww=()):
        o = Op(fn, "c")
        self._deps(eng, o, r, w, ww)
        idx = len(self.ops[eng])
        self.ops[eng].append(o)
        self._commit(("c", eng, idx), r, list(w) + list(ww))
        return o

    def dma(self, eng, fn, r=(), w=()):
        o = Op(fn, "d")
        m = self.ndma[eng]
        self.ndma[eng] += 1
        o.dma_m = m
        if m >= KDMA:
            self._need(eng, ("d", eng, m - KDMA), o)
        self._deps(eng, o, r, w)
        self.ops[eng].append(o)
        self._commit(("d", eng, m), r, w)
        return o

    def pe(self, fn, r=(), w=(), ww=()):
        return self.op("pe", fn, r, w, ww)

    def dve(self, fn, r=(), w=(), ww=()):
        return self.op("dve", fn, r, w, ww)

    def act(self, fn, r=(), w=(), ww=()):
        return self.op("act", fn, r, w, ww)

    def pool(self, fn, r=(), w=(), ww=()):
        return self.op("pool", fn, r, w, ww)

    def barrier(self):
        for eng in ENGS:
            o = Op(None, "n")
            for e in ENGS:
                for i in range(len(self.ops[e]) - 1, -1, -1):
                    if self.ops[e][i].kind == "c":
                        self._need(eng, ("c", e, i), o)
                        break
                n = self.ndma[e]
                for m in range(max(0, n - KDMA), n):
                    self._need(eng, ("d", e, m), o)
            self.ops[eng].append(o)

    def finish_waits(self):
        o = Op(None, "n")
        for q in ENGS:
            n = self.ndma[q]
            for m in range(max(0, n - KDMA), n):
                self._need("sp", ("d", q, m), o)
        self.ops["sp"].append(o)

    def _mksems(self):
        nc = self.nc
        self.sems = {}
        for e in ENGS:
            g = nc.semaphore("c_" + e)
            self.sems[("c", e)] = g.__enter__()
            self.ctx.append(g)
            for k in range(KDMA):
                g = nc.semaphore("d_%s_%d" % (e, k))
                self.sems[("d", e, k)] = g.__enter__()
                self.ctx.append(g)

    def flush(self, final=False):
        nc = self.nc
        if self.sems is None:
            self._mksems()
        if final:
            self.finish_waits()
        sems = self.sems
        start = dict(self.emitted)
        for e in ENGS:
            ops = self.ops[e]
            for i in range(len(ops) - 1, start[e] - 1, -1):
                if ops[i].kind == "c":
                    ops[i].marked = True
                    self.phase_end[e].append(i)
                    break
            c = self.cnt[e][-1] if self.cnt[e] else 0
            for o in ops[start[e]:]:
                if o.kind == "c" and o.marked:
                    c += 1
                self.cnt[e].append(c)
        cnt = self.cnt

        def run(ename, eng):
            for o in self.ops[ename][start[ename]:]:
                for tok in o.waits:
                    if tok[0] == "c":
                        eng.wait_ge(sems[("c", tok[1])], cnt[tok[1]][tok[2]])
                    else:
                        m = tok[2]
                        eng.wait_ge(sems[("d", tok[1], m % KDMA)], 16 * (m // KDMA + 1))
                if o.kind == "n":
                    continue
                ins = o.fn(eng)
                if o.kind == "d":
                    ins.then_inc(sems[("d", ename, o.dma_m % KDMA)], 16)
                elif o.marked:
                    ins.then_inc(sems[("c", ename)], 1)

        blk = nc.Block()
        block = blk.__enter__()

        @block.tensor
        def _(e):
            run("pe", e)

        @block.vector
        def _(e):
            run("dve", e)

        @block.scalar
        def _(e):
            run("act", e)

        @block.gpsimd
        def _(e):
            run("pool", e)

        @block.sync
        def _(e):
            run("sp", e)

        blk.__exit__(None, None, None)
        for e in ENGS:
            self.emitted[e] = len(self.ops[e])
        if not final:
            self.barrier()
        for g in reversed(self.pctx):
            g.__exit__(None, None, None)
        self.pctx = []
        if final:
            for g in reversed(self.ctx):
                g.__exit__(None, None, None)
            self.ctx = []

    def emit(self):
        self.flush(final=True)

    def stats(self):
        return {e: (len(self.ops[e]), sum(1 for o in self.ops[e] if o.marked),
                    sum(len(o.waits) for o in self.ops[e])) for e in ENGS}
D = 1024
NT = 36
NE = 32
EPS = 1e-6


def rowtype(t):
    return 2 if (t % 18) < 2 else t // 18


class K:
    pass


def dma(fw, q, out, in_, r, w, **kw):
    fw.dma(q, lambda e: e.dma_start(out=out, in_=in_, **kw), r=r, w=w)


def phase_ada(fw, k):
    ccT = fw.sbuf("ccT", [128, 8, 3], F32)
    scT = fw.sbuf("scT", [128, 8, 3], F32)
    for r_ in range(3):
        dma(fw, "sp", ccT[:, :, r_], k.cc[r_, :].rearrange("(c p) -> p c", p=128), [k.cc], [ccT],
            allow_slow_non_contiguous=True)
    fw.act(lambda e: e.activation(out=scT[:], in_=ccT[:], func=AF.Silu), r=[ccT], w=[scT])
    aw = [fw.sbuf("aw%d" % i, [128, 8, 768], F32) for i in range(2)]
    abT = fw.sbuf("abT", [128, 48], F32)
    ps = fw.psum("adaps", [128, 48, 3], F32)
    n = 0
    for l in range(2):
        dma(fw, "sp", abT[:], k.ada_b[l, :].rearrange("(j p) -> p j", p=128), [k.ada_b], [abT],
            allow_slow_non_contiguous=True)
        for blk in range(8):
            a = aw[n % 2]
            n += 1
            dma(fw, "sp", a[:], k.ada_w[l, :, blk * 768:(blk + 1) * 768].rearrange("(c p) f -> p c f", p=128),
                [k.ada_w], [a])
            for j in range(6):
                jj = blk * 6 + j
                for c in range(8):
                    fw.pe(lambda e, a=a, j=j, c=c, jj=jj: e.matmul(
                        ps[:, jj, :], a[:, c, j * 128:(j + 1) * 128], scT[:, c, :],
                        start=(c == 0), stop=(c == 7)), r=[a, scT], w=[ps])
        fw.dve(lambda e, l=l: e.tensor_tensor(
            out=k.modT[l][:], in0=ps[:], in1=abT[:].unsqueeze(2).to_broadcast([128, 48, 3]), op=ALU.add),
            r=[ps, abT], w=[k.modT[l]])
    fw.flush()


def load_gain_T(fw, k, name, src_row):
    t = fw.sbuf(name, [128, 8], F32)
    dma(fw, "sp", t[:], src_row.rearrange("(c p) -> p c", p=128), [], [t], allow_slow_non_contiguous=True)
    return t


def make_GS(fw, k, l, which, gain_T):
    G = fw.sbuf("G%d" % which, [128, 8, 3], F32)
    base = 0 if which == 0 else 24
    m = k.modT[l]
    fw.dve(lambda e: e.scalar_tensor_tensor(
        out=G[:], in0=m[:, base + 8:base + 16, :], scalar=1.0,
        in1=gain_T[:].unsqueeze(2).to_broadcast([128, 8, 3]), op0=ALU.add, op1=ALU.mult),
        r=[m, gain_T], w=[G])
    return G, base


class Front:
    def __init__(self, fw, k, nbuf=2):
        self.fw = fw
        self.k = k
        self.xt = [fw.sbuf("fe_xt%d" % i, [128, D], F32) for i in range(nbuf)]
        self.xh = [fw.sbuf("fe_xh%d" % i, [128, D], BF16) for i in range(2)]
        self.ss = [fw.sbuf("fe_ss%d" % i, [128, 1], F32) for i in range(2)]
        self.rstd = [fw.sbuf("fe_rs%d" % i, [128, 1], F32) for i in range(2)]
        self.tp = [fw.psum("fe_tp%d" % i, [128, 8, 128], BF16) for i in range(1)]
        self.n = 0

    def run(self, src_rows, src_res, G, l, base, row, dst_fn, dst_res):
        fw, k = self.fw, self.k
        i = self.n
        self.n += 1
        xt = self.xt[i % len(self.xt)]
        xh = self.xh[i % 2]
        ss = self.ss[i % 2]
        rstd = self.rstd[i % 2]
        tp = self.tp[0]
        junk = xh
        m = k.modT[l]
        dma(fw, "sp", xt[:], src_rows, [src_res], [xt])
        fw.pool(lambda e: e.memset(ss[:], 0.0), w=[ss])
        fw.act(lambda e: e.activation(out=junk[:], in_=xt[:], func=AF.Square, accum_out=ss[:]),
               r=[xt], w=[xh, ss])
        fw.dve(lambda e: e.tensor_scalar(out=rstd[:], in0=ss[:], scalar1=1.0 / D, scalar2=EPS,
                                         op0=ALU.mult, op1=ALU.add), r=[ss], w=[rstd])
        fw.act(lambda e: e.activation(out=rstd[:], in_=rstd[:], func=AF.Sqrt), r=[rstd], w=[rstd])
        fw.dve(lambda e: e.reciprocal(out=rstd[:], in_=rstd[:]), r=[rstd], w=[rstd])
        fw.dve(lambda e: e.tensor_scalar(out=xh[:], in0=xt[:], scalar1=rstd[:, 0:1], scalar2=None,
                                         op0=ALU.mult), r=[xt, rstd], w=[xh])
        for c in range(8):
            fw.pe(lambda e, c=c: e.transpose(tp[:, c, :], xh[:, c * 128:(c + 1) * 128], k.ident_bf[:]),
                  r=[xh, k.ident_bf], w=[tp])
        for c in range(8):
            fw.act(lambda e, c=c: e.activation(out=dst_fn(c), in_=tp[:, c, :], func=AF.Identity,
                                               scale=G[:, c, row:row + 1], bias=m[:, base + c, row:row + 1]),
                   r=[tp, G, m], ww=[dst_res])
        return xt


class Back:
    def __init__(self, fw, k, ht_bufs, ps_big):
        self.fw = fw
        self.k = k
        self.fT = [fw.sbuf("be_fT%d" % i, [128, 8, 128], F32) for i in range(1)]
        self.ht = ht_bufs
        self.ps = [ps_big]
        self.n = 0

    def run(self, srcT_fn, src_res, l, gbase, row, h_rows, h_res, dst_rows, dst_res):
        fw, k = self.fw, self.k
        i = self.n
        self.n += 1
        fT = self.fT[0]
        ht = self.ht[i % len(self.ht)]
        ps = self.ps[0]
        m = k.modT[l]
        dma(fw, "sp", ht[:], h_rows, [h_res], [ht])
        fp = fT.parts(8)
        for c in range(8):
            if c % 2 == 0:
                fw.dve(lambda e, c=c: e.tensor_scalar(out=fT[:, c, :], in0=srcT_fn(c),
                                                      scalar1=m[:, gbase + c, row:row + 1], scalar2=None,
                                                      op0=ALU.mult), r=[src_res, m], w=[fp[c]])
            else:
                fw.act(lambda e, c=c: e.activation(out=fT[:, c, :], in_=srcT_fn(c), func=AF.Copy,
                                                   scale=m[:, gbase + c, row:row + 1]), r=[src_res, m], w=[fp[c]])
        for c in range(8):
            fw.pe(lambda e, c=c: e.transpose(ps[:, c * 128:(c + 1) * 128], fT[:, c, :], k.ident_f[:]),
                  r=[fp[c], k.ident_f], w=[ps])
        fw.dve(lambda e: e.tensor_tensor(out=ht[:], in0=ps[:], in1=ht[:], op=ALU.add), r=[ps, ht], w=[ht])
        dma(fw, "sp", dst_rows, ht[:], [ht], [dst_res])
def subs(T):
    out = []
    o = 0
    while o < T:
        n = min(512, T - o)
        out.append((o, n))
        o += n
    return out


def phase_moe(fw, k, l, tiles, src, dst_fn, first_block):
    nt = len(tiles)
    TB = nt * 128
    SB = subs(TB)
    ynT = fw.sbuf("ynT", [128, 8, TB], BF16)
    acc = fw.sbuf("acc", [128, 8, TB], F32)
    actT = [fw.sbuf("actT%d" % i, [128, 8, TB], BF16) for i in range(2)]
    gbc = [fw.sbuf("gbc%d" % i, [128, TB], F32) for i in range(1)]
    st1 = [fw.sbuf("st1_%d" % i, [128, 8, 256], F32) for i in range(2)]
    w1b = [fw.sbuf("w1b%d" % i, [128, 8, 2, 128], BF16) for i in range(4)]
    st2 = [fw.sbuf("st2_%d" % i, [128, 1024], F32) for i in range(2)]
    w2bs = [fw.sbuf("w2b%d" % i, [128, 8, 1024], BF16) for i in range(2)]
    w2ps = [w.parts(8) for w in w2bs]
    ub = [[fw.sbuf("ub%d_%d" % (i, j), [128, 512], F32) for j in range(3)] for i in range(2)]
    wr = fw.sbuf("wr", [128, 8, NE], BF16)
    rb = fw.sbuf("rb", [128, NE], F32)
    bout = V(st2[0][0:NE, :], st2[0].res)
    bin_sb = V(st1[0][0:NE, :, :].rearrange("p c f -> p (c f)"), st1[0].res)
    binT = fw.sbuf("binT", [128, 16, NE], F32)
    combT = fw.sbuf("combT", [NE, TB], F32)
    lg = fw.sbuf("lg", [128, NE], F32)
    m8 = fw.sbuf("m8", [128, 8], F32)
    nmx = fw.sbuf("nmx", [128, 1], F32)
    msk = fw.sbuf("msk", [128, NE], F32)
    ex = fw.sbuf("ex", [128, NE], F32)
    sm = fw.sbuf("sm", [128, 1], F32)
    comb = fw.sbuf("comb", [128, NE], F32)
    gn = load_gain_T(fw, k, "gnf", k.norm_ffn_g[l, :])
    G, base = make_GS(fw, k, l, 1, gn)
    fe = Front(fw, k)
    ps_big = fw.psum("ps_big", [128, 1024], F32)
    be = Back(fw, k, fe.xt, ps_big)
    ps_misc = fw.psum("ps_misc", [128, 512], F32)
    ps_g = [V(ps_big[:, 0:512], ps_big.res), fw.psum("ps_g1", [128, 512], F32)]
    ps_l = [V(ps_big[:, 512:1024], Res("ps_l0")), fw.psum("ps_l1", [128, 512], F32)]
    ps_y = [fw.psum("ps_y%d" % i, [128, 512], F32) for i in range(2)]
    combT_d = k.combT_d

    dma(fw, "pool", wr[:], k.router_w[l].rearrange("(c p) e -> p c e", p=128), [], [wr])
    dma(fw, "sp", rb[:], k.router_b[l:l + 1, :].partition_broadcast(128), [], [rb])
    dma(fw, "sp", bout[:], k.exp_b_out[l], [], [bout])
    dma(fw, "sp", bin_sb[:], k.exp_b_in[l], [], [bin_sb])
    for mt in range(16):
        m_, two = mt // 2, mt % 2
        fw.pe(lambda e, mt=mt, m_=m_, two=two: e.transpose(
            ps_misc[:, mt * NE:(mt + 1) * NE],
            bin_sb[:, two + 256 * m_: 256 * m_ + 256: 2], k.ident_f[0:NE, 0:NE]),
            r=[bin_sb, k.ident_f], w=[ps_misc])
    fw.dve(lambda e: e.tensor_copy(binT[:].rearrange("p a b -> p (a b)"), ps_misc[:, 0:16 * NE]),
           r=[ps_misc], w=[binT])
    fw.dve(lambda e: e.tensor_scalar(out=binT[:, 1::2, :], in0=binT[:, 1::2, :], scalar1=1.0, scalar2=None, op0=ALU.add),
           r=[binT], w=[binT])

    for i, t in enumerate(tiles):
        row = rowtype(t)
        fe.run(src[t * 128:(t + 1) * 128, :], src, G, l, base, row,
               lambda c, i=i: ynT[:, c, i * 128:(i + 1) * 128], ynT)
        for c in range(8):
            fw.pe(lambda e, c=c, i=i: e.matmul(ps_misc[:, 0:NE], ynT[:, c, i * 128:(i + 1) * 128], wr[:, c, :],
                                               start=(c == 0), stop=(c == 7)), r=[ynT, wr], w=[ps_misc])
        fw.dve(lambda e: e.tensor_tensor(out=lg[:], in0=ps_misc[:, 0:NE], in1=rb[:], op=ALU.add),
               r=[ps_misc, rb], w=[lg])
        fw.dve(lambda e: e.max(out=m8[:], in_=lg[:]), r=[lg], w=[m8])
        fw.dve(lambda e: e.tensor_scalar(out=msk[:], in0=lg[:], scalar1=m8[:, 3:4], scalar2=None, op0=ALU.is_ge),
               r=[lg, m8], w=[msk])
        fw.dve(lambda e: e.tensor_scalar(out=nmx[:], in0=m8[:, 0:1], scalar1=-1.0, scalar2=None, op0=ALU.mult),
               r=[m8], w=[nmx])
        fw.act(lambda e: e.activation(out=ex[:], in_=lg[:], func=AF.Exp, bias=nmx[:, 0:1]), r=[lg, nmx], w=[ex])
        fw.dve(lambda e: e.tensor_tensor(out=ex[:], in0=ex[:], in1=msk[:], op=ALU.mult), r=[ex, msk], w=[ex])
        fw.dve(lambda e: e.reduce_sum(out=sm[:], in_=ex[:], axis=AX.X), r=[ex], w=[sm])
        fw.dve(lambda e: e.reciprocal(out=sm[:], in_=sm[:]), r=[sm], w=[sm])
        fw.dve(lambda e: e.tensor_scalar(out=comb[:], in0=ex[:], scalar1=sm[:, 0:1], scalar2=None, op0=ALU.mult),
               r=[ex, sm], w=[comb])
        fw.pe(lambda e: e.transpose(ps_misc[0:NE, 128:256], comb[:], k.ident_f[:]), r=[comb, k.ident_f], w=[ps_misc])
        fw.act(lambda e, i=i: e.activation(out=combT[:, i * 128:(i + 1) * 128], in_=ps_misc[0:NE, 128:256], func=AF.Copy),
               r=[ps_misc], ww=[combT])
    dma(fw, "sp", combT_d[:, 0:TB], combT[:], [combT], [combT_d])

    for d in range(8):
        for (o, n) in SB:
            fw.pe(lambda e, d=d, o=o, n=n: e.matmul(ps_misc[:, 0:n], bout[:, d * 128:(d + 1) * 128], combT[:, o:o + n],
                                                    start=True, stop=True), r=[bout, combT], w=[ps_misc])
            fw.act(lambda e, d=d, o=o, n=n: e.activation(out=acc[:, d, o:o + n], in_=ps_misc[:, 0:n], func=AF.Copy),
                   r=[ps_misc], ww=[acc])

    cnt = {"w1": 0, "u": 0, "cast": 0, "y": 0}
    ne = k.n_experts_dbg if hasattr(k, "n_experts_dbg") else NE

    def load_w1(e_, m_):
        i = cnt["w1"]
        cnt["w1"] += 1
        s_ = st1[i % 2]
        wb = w1b[i % 4]
        dma(fw, "sp", s_[:], k.exp_w_in[l, e_].rearrange("(c p) f -> p c f", p=128)[:, :, m_ * 256:(m_ + 1) * 256],
            [], [s_])
        if True:
            fw.act(lambda e: e.activation(out=wb[:], in_=s_[:].rearrange("p c (j two) -> p c two j", two=2), func=AF.Copy),
                   r=[s_], w=[wb])
        else:
            fw.pool(lambda e: e.tensor_copy(wb[:], s_[:].rearrange("p c (j two) -> p c two j", two=2)),
                    r=[s_], w=[wb])
        return wb

    def load_w2_piece(e_, m_):
        i = cnt["cast"]
        cnt["cast"] += 1
        s_ = st2[i % 2]
        w2b, w2p = w2bs[e_ % 2], w2ps[e_ % 2]
        dma(fw, "sp", s_[:], k.exp_w_out[l, e_, m_ * 128:(m_ + 1) * 128, :], [], [s_])
        if i % 2 == 0:
            fw.pool(lambda e, m_=m_, s_=s_, w2b=w2b: e.tensor_copy(w2b[:, m_, :], s_[:]), r=[s_], w=[w2p[m_]])
        else:
            fw.act(lambda e, m_=m_, s_=s_, w2b=w2b: e.activation(out=w2b[:, m_, :], in_=s_[:], func=AF.Copy), r=[s_], w=[w2p[m_]])

    PF = 3
    npieces = [0]
    ready = {}

    def ensure(q_target):
        while npieces[0] <= min(q_target, 8 * ne - 1):
            q = npieces[0]
            e2, m2 = q // 8, q % 8
            ready[q] = load_w1(e2, m2)
            if e2 >= 1:
                load_w2_piece(e2 - 1, m2)
            npieces[0] += 1

    def mm1(e_, w2_of=None):
        g = gbc[0]
        dma(fw, "sp", g[:], combT_d[e_:e_ + 1, 0:TB].partition_broadcast(128), [combT_d], [g])
        aT = actT[e_ % 2]
        for m_ in range(8):
            ensure(e_ * 8 + m_ + PF)
            wb = ready.pop(e_ * 8 + m_)
            for (o, n) in SB:
                u = cnt["u"]
                cnt["u"] += 1
                pg, pl = ps_g[u % 2], ps_l[u % 2]
                A, B, C = ub[u % 2]
                for c in range(8):
                    fw.pe(lambda e, c=c, o=o, n=n, wb=wb, pg=pg: e.matmul(pg[:, 0:n], wb[:, c, 0, :], ynT[:, c, o:o + n],
                                                                          start=(c == 0), stop=(c == 7)), r=[wb, ynT], w=[pg])
                for c in range(8):
                    fw.pe(lambda e, c=c, o=o, n=n, wb=wb, pl=pl: e.matmul(pl[:, 0:n], wb[:, c, 1, :], ynT[:, c, o:o + n],
                                                                          start=(c == 0), stop=(c == 7)), r=[wb, ynT], w=[pl])
                bg = binT[:, 2 * m_, e_:e_ + 1]
                bl = binT[:, 2 * m_ + 1, e_:e_ + 1]
                fw.dve(lambda e, n=n, pg=pg, A=A, bg=bg: e.tensor_scalar(out=A[:, 0:n], in0=pg[:, 0:n], scalar1=bg, scalar2=7.0,
                                                                        op0=ALU.add, op1=ALU.min), r=[pg, binT], w=[A])
                fw.act(lambda e, n=n, A=A, B=B: e.activation(out=B[:, 0:n], in_=A[:, 0:n], func=AF.Sigmoid, scale=1.702),
                       r=[A], w=[B])
                fw.act(lambda e, n=n, pl=pl, C=C, bl=bl: e.activation(out=C[:, 0:n], in_=pl[:, 0:n], func=AF.Identity, bias=bl),
                       r=[pl, binT], w=[C])
                fw.pool(lambda e, n=n, C=C: e.tensor_scalar(out=C[:, 0:n], in0=C[:, 0:n], scalar1=8.0, scalar2=-6.0,
                                                           op0=ALU.min, op1=ALU.max), r=[C], w=[C])
                fw.dve(lambda e, n=n, A=A, B=B: e.tensor_tensor(out=A[:, 0:n], in0=A[:, 0:n], in1=B[:, 0:n], op=ALU.mult),
                       r=[A, B], w=[A])
                fw.pool(lambda e, n=n, A=A, C=C: e.tensor_tensor(out=C[:, 0:n], in0=C[:, 0:n], in1=A[:, 0:n], op=ALU.mult),
                        r=[A, C], w=[C])
                fw.dve(lambda e, n=n, o=o, C=C, g=g, aT=aT, m_=m_: e.tensor_tensor(out=aT[:, m_, o:o + n], in0=C[:, 0:n],
                                                                                  in1=g[:, o:o + n], op=ALU.mult),
                       r=[C, g], ww=[aT])

    def mm2(e_):
        aT = actT[e_ % 2]
        w2b, w2p = w2bs[e_ % 2], w2ps[e_ % 2]
        for d in range(8):
            for (o, n) in SB:
                py = ps_y[cnt["y"] % 2]
                cnt["y"] += 1
                for m_ in range(8):
                    fw.pe(lambda e, m_=m_, d=d, o=o, n=n, py=py, w2b=w2b: e.matmul(py[:, 0:n], w2b[:, m_, d * 128:(d + 1) * 128],
                                                                          aT[:, m_, o:o + n], start=(m_ == 0), stop=(m_ == 7)),
                          r=[w2p[m_], aT], w=[py])
                fw.dve(lambda e, d=d, o=o, n=n, py=py: e.tensor_tensor(out=acc[:, d, o:o + n], in0=py[:, 0:n],
                                                                      in1=acc[:, d, o:o + n], op=ALU.add),
                       r=[py], ww=[acc])

    mm1(0)
    for e_ in range(ne):
        if e_ + 1 < ne:
            mm1(e_ + 1, w2_of=e_)
        else:
            for m_ in range(8):
                load_w2_piece(e_, m_)
        mm2(e_)

    for i, t in enumerate(tiles):
        row = rowtype(t)
        drows, dres = dst_fn(t)
        if drows is None:
            continue
        be.run(lambda c, i=i: acc[:, c, i * 128:(i + 1) * 128], acc, l, 40, row,
               src[t * 128:(t + 1) * 128, :], src, drows, dres)
    fw.flush()
def ucol(s):
    return 16 + s if s < 256 else s + 48


def phase_ab(fw, k, b):
    l = 0
    t0 = 18 * b
    TS = 2304
    xnT = fw.sbuf("xnT", [128, 8, TS], BF16)
    OT = xnT
    QT = fw.sbuf("QT", [128, 4, TS], BF16)
    KT = fw.sbuf("KT", [128, 4, TS], BF16)
    Vt = fw.sbuf("Vt", [128, 18, 512], BF16)
    Vo = fw.sbuf("Vo", [128, 17, 512], BF16)
    UT = fw.sbuf("UT", [128, 4, 2368], BF16)
    w_in = fw.sbuf("w_in", [128, 8, 2048], BF16)
    EB = V(w_in[:].rearrange("p h (o jp q) -> p h o jp q", o=8, jp=4), w_in.res)
    w_out = V(UT[:].rearrange("p g x -> p (g x)")[:, 0:8 * D].rearrange("p (c f) -> p c f", c=8), UT.res)
    pw = fw.sbuf("pw", [128, 4, 128], BF16)
    pscale = fw.sbuf("pscale", [128, 4], F32)
    gq = fw.sbuf("gq", [128, 1], F32)
    gk = fw.sbuf("gk", [128, 1], F32)
    bd = fw.sbuf("bd", [128, 128], BF16)
    ones = fw.sbuf("ones", [128, 128], BF16)
    sqb = [fw.sbuf("sqb%d" % i, [128, 512], BF16) for i in range(2)]
    rstd = [fw.sbuf("qrstd%d" % i, [128, 512], F32) for i in range(2)]
    tA = fw.sbuf("ptA", [128, 544], F32)
    tB = fw.sbuf("ptB", [128, 544], F32)
    rcb = fw.sbuf("rcb", [128, 512], F32)
    pooled = fw.sbuf("pooled", [128, 512], BF16)
    mk = fw.sbuf("mk", [128, 4, 64], F32)
    rbt = [fw.sbuf("rbt%d" % i, [128, 4, 64], F32) for i in range(2)]
    Pb = [fw.sbuf("Pb%d" % i, [128, 6, 64], BF16) for i in range(3)]
    rd = [fw.sbuf("rd%d" % i, [128, 64], F32) for i in range(2)]
    resT = fw.sbuf("resT", [128, 8, 128], F32)
    gn = load_gain_T(fw, k, "gnm", k.norm_mix_g[l, :])
    G, base = make_GS(fw, k, l, 0, gn)
    fe = Front(fw, k)
    big = fw.psum("ab_big", [128, 1024], F32)
    pq = fw.psum("ab_pq", [128, 2048], F32)
    be = Back(fw, k, fe.xt, big)
    pAB = [V(big[:, 0:512], Res("pA")), V(big[:, 512:1024], Res("pB"))]
    pS = V(pq[:, 0:512], Res("pS"))
    pO = [V(pq[:, 0:64], pS.res), V(pq[:, 512:576], Res("pO1"))]
    pD = [V(pq[:, 1024:1088], Res("pD0")), V(pq[:, 1536:1600], Res("pD1"))]

    for c in range(8):
        dma(fw, "pool", w_in[:, c, :], k.ab_w_in[0, c * 128:(c + 1) * 128, :], [], [w_in])
    fw.pool(lambda e: e.memset(UT[:], 0.0), w=[UT])
    for (g_, src) in ((gq, k.na_q_g), (gk, k.na_k_g)):
        for hl in range(2):
            dma(fw, "sp", g_[hl * 64:(hl + 1) * 64, :], src[0, :].rearrange("(p o) -> p o", o=1), [], [g_])
    fw.dve(lambda e: e.tensor_scalar(out=gq[:], in0=gq[:], scalar1=0.125, scalar2=None, op0=ALU.mult), r=[gq], w=[gq])
    fw.pool(lambda e: e.memset(bd[:], 0.0), w=[bd])
    fw.pool(lambda e: e.memset(bd[0:64, 0:64], 1.0), w=[bd])
    fw.pool(lambda e: e.memset(bd[64:128, 64:128], 1.0), w=[bd])
    fw.pool(lambda e: e.memset(ones[:], 1.0), w=[ones])
    dma(fw, "pool", pw[:], k.pool_w[0].rearrange("g c d -> c g d"), [], [pw])
    dma(fw, "sp", pscale[:], k.pool_scale[0, :].rearrange("(g p) -> p g", p=128), [], [pscale],
        allow_slow_non_contiguous=True)
    dma(fw, "sp", mk[:], k.c_namask[:, :].rearrange("(jp p) q -> p jp q", p=128), [], [mk])

    for i in range(18):
        t = t0 + i
        fe.run(k.xin[t * 128:(t + 1) * 128, :], k.xin, G, l, base, rowtype(t),
               lambda c, i=i: xnT[:, c, i * 128:(i + 1) * 128], xnT)

    n_ = 0
    for m in range(8):
        dstT, gg = (QT, gq) if m < 4 else (KT, gk)
        mm = m % 4
        for (o, n) in subs(TS):
            ps = pAB[n_ % 2]
            sq = sqb[n_ % 2]
            rs = rstd[n_ % 2]
            n_ += 1
            for c in range(8):
                fw.pe(lambda e, c=c, m=m, o=o, n=n, ps=ps: e.matmul(ps[:, 0:n], w_in[:, c, m * 128:(m + 1) * 128],
                                                                   xnT[:, c, o:o + n], start=(c == 0), stop=(c == 7)),
                      r=[w_in, xnT], w=[ps])
            fw.act(lambda e, n=n, ps=ps, sq=sq: e.activation(out=sq[:, 0:n], in_=ps[:, 0:n], func=AF.Square), r=[ps], w=[sq])
            fw.pe(lambda e, n=n, sq=sq: e.matmul(pS[:, 0:n], bd[:], sq[:, 0:n], start=True, stop=True), r=[bd, sq], w=[pS])
            fw.dve(lambda e, n=n, rs=rs: e.tensor_scalar(out=rs[:, 0:n], in0=pS[:, 0:n], scalar1=1.0 / 64, scalar2=EPS,
                                                        op0=ALU.mult, op1=ALU.add), r=[pS], w=[rs])
            fw.act(lambda e, n=n, rs=rs: e.activation(out=rs[:, 0:n], in_=rs[:, 0:n], func=AF.Sqrt), r=[rs], w=[rs])
            fw.dve(lambda e, n=n, rs=rs: e.reciprocal(out=rs[:, 0:n], in_=rs[:, 0:n]), r=[rs], w=[rs])
            fw.dve(lambda e, n=n, o=o, rs=rs, ps=ps, dstT=dstT, gg=gg, mm=mm: e.scalar_tensor_tensor(
                out=dstT[:, mm, o:o + n], in0=ps[:, 0:n], scalar=gg[:, 0:1], in1=rs[:, 0:n], op0=ALU.mult, op1=ALU.mult),
                r=[ps, rs, gg], ww=[dstT])
    for (dst, ntile, off) in ((Vt, 18, 0), (Vo, 17, 64)):
        for i in range(ntile):
            ps = pAB[n_ % 2]
            n_ += 1
            for c in range(8):
                fw.pe(lambda e, c=c, i=i, off=off, ps=ps: e.matmul(ps[:, :], xnT[:, c, off + i * 128: off + (i + 1) * 128],
                                                                  w_in[:, c, 1024:1536], start=(c == 0), stop=(c == 7)),
                      r=[w_in, xnT], w=[ps])
            fw.act(lambda e, i=i, ps=ps, dst=dst: e.activation(out=dst[:, i, :], in_=ps[:, :], func=AF.Copy), r=[ps], ww=[dst])
    for g in range(4):
        for (o, n) in subs(TS):
            ps = pAB[n_ % 2]
            n_ += 1
            for c in range(8):
                fw.pe(lambda e, c=c, g=g, o=o, n=n, ps=ps: e.matmul(ps[:, 0:n], w_in[:, c, 1536 + g * 128:1536 + (g + 1) * 128],
                                                                   xnT[:, c, o:o + n], start=(c == 0), stop=(c == 7)),
                      r=[w_in, xnT], w=[ps])
            pieces = [(o, n)] if o >= 256 else [(0, 256), (256, n - 256)]
            for (po_, pn) in pieces:
                fw.dve(lambda e, g=g, po_=po_, pn=pn, o=o, ps=ps: e.tensor_copy(UT[:, g, ucol(po_):ucol(po_) + pn],
                                                                              ps[:, po_ - o:po_ - o + pn]), r=[ps], ww=[UT])

    for g, w in enumerate((2, 4, 8, 16)):
        nlev = (2, 4, 8, 16).index(w) + 1
        plist = [(16, 256, 0, 1, 0)] + [(304 + 512 * j, 512, 256 + 512 * j, 0, 512 * j) for j in range(4)]
        for (lo, n, s0, seg, p0) in plist:
            W = n + 32
            xb = lo - 16
            fw.dve(lambda e, g=g, W=W, xb=xb: e.tensor_tensor(out=tA[:, 1:W], in0=UT[:, g, xb + 1:xb + W],
                                                             in1=UT[:, g, xb:xb + W - 1], op=ALU.add), r=[UT], w=[tA])
            cur, oth = tA, tB
            sh = 2
            valid = 1
            for lev in range(1, nlev):
                lo_x = valid + sh
                fw.dve(lambda e, W=W, cur=cur, oth=oth, lo_x=lo_x, sh=sh: e.tensor_tensor(
                    out=oth[:, lo_x:W], in0=cur[:, lo_x:W], in1=cur[:, lo_x - sh:W - sh], op=ALU.add), r=[cur], w=[oth])
                cur, oth = oth, cur
                valid = lo_x
                sh *= 2
            X0 = 16 + w // 2 - 1
            dma(fw, "sp", rcb[:, 0:n], k.c_poolrc[g, seg:seg + 1, 16 + p0:16 + p0 + n].partition_broadcast(128), [], [rcb])
            fw.dve(lambda e, n=n, cur=cur, oth=oth, X0=X0: e.tensor_tensor(out=oth[:, 0:n], in0=cur[:, X0:X0 + n],
                                                                         in1=rcb[:, 0:n], op=ALU.mult), r=[cur, rcb], w=[oth])
            fw.pool(lambda e, n=n, oth=oth, g=g, lo=lo: e.tensor_tensor(out=pooled[:, 0:n], in0=oth[:, 0:n],
                                                                       in1=UT[:, g, lo:lo + n], op=ALU.subtract),
                    r=[oth, UT], w=[pooled])
            ps = pAB[n_ % 2]
            n_ += 1
            fw.pe(lambda e, n=n, g=g, ps=ps: e.matmul(ps[:, 0:n], pw[:, g, :], pooled[:, 0:n], start=True, stop=True),
                  r=[pw, pooled], w=[ps])
            fw.act(lambda e, n=n, g=g, s0=s0, ps=ps: e.activation(out=OT[:, 4 + g, s0:s0 + n], in_=ps[:, 0:n], func=AF.Copy,
                                                                 scale=pscale[:, g:g + 1]), r=[ps, pscale], ww=[OT])

    n2 = 0
    for h in range(8):
        for o in range(8):
            rb_ = rbt[n2 % 2]
            n2 += 1
            dma(fw, "sp", rb_[:], k.rpb_g[h, o].rearrange("(jp p) q -> p jp q", p=128), [], [rb_])
            fw.pool(lambda e, rb_=rb_: e.tensor_tensor(out=rb_[:], in0=rb_[:], in1=mk[:], op=ALU.add), r=[rb_, mk], w=[rb_])
            fw.act(lambda e, rb_=rb_, h=h, o=o: e.activation(out=EB[:, h, o, :, :], in_=rb_[:], func=AF.Exp), r=[rb_], ww=[EB])

    for c in range(8):
        dma(fw, "pool", w_out[:, c, :], k.ab_w_out[0, c * 128:(c + 1) * 128, :], [], [w_out])

    cnt = {"u": 0}
    pst = pAB

    def unit(qs, m, hl, ktiles, eb):
        u = cnt["u"]
        cnt["u"] += 1
        bp = 64 * hl
        ps = pst[u % 2]
        P = Pb[u % 3]
        rdd = rd[u % 2]
        nk = len(ktiles)
        for kt, (ks, vt) in enumerate(ktiles):
            fw.pe(lambda e, kt=kt, ks=ks, ps=ps: e.matmul(ps[:, kt * 64:(kt + 1) * 64], KT[bp:bp + 64, m, ks:ks + 128],
                                                         QT[bp:bp + 64, m, qs:qs + 64], start=True, stop=True),
                  r=[KT, QT], w=[ps])
        fw.act(lambda e, nk=nk, ps=ps, P=P: e.activation(out=P[:, 0:nk, :].rearrange("p a b -> p (a b)"), in_=ps[:, 0:nk * 64],
                                                        func=AF.Exp), r=[ps], w=[P])
        if eb is not None:
            fw.dve(lambda e, P=P, eb=eb: e.tensor_tensor(out=P[:, 0:4, :], in0=P[:, 0:4, :], in1=eb, op=ALU.mult),
                   r=[P, EB], w=[P])
        for kt, (ks, vt) in enumerate(ktiles):
            fw.pe(lambda e, kt=kt, vt=vt, P=P: e.matmul(pO[hl][:, :], vt[:, m * 128:(m + 1) * 128], P[:, kt, :],
                                                       start=(kt == 0), stop=(kt == nk - 1)), r=[Vt, Vo, P], w=[pO[hl]])
        for kt, (ks, vt) in enumerate(ktiles):
            fw.pe(lambda e, kt=kt, P=P: e.matmul(pD[hl][:, :], ones[:], P[:, kt, :],
                                                start=(kt == 0), stop=(kt == nk - 1)), r=[ones, P], w=[pD[hl]])
        fw.dve(lambda e, rdd=rdd: e.reciprocal(out=rdd[bp:bp + 64, :], in_=pD[hl][bp:bp + 64, :]), r=[pD[hl]], w=[rdd])
        fw.dve(lambda e, rdd=rdd: e.tensor_tensor(out=OT[bp:bp + 64, m, qs:qs + 64], in0=pO[hl][bp:bp + 64, :],
                                                 in1=rdd[bp:bp + 64, :], op=ALU.mult), r=[pO[hl], rdd], ww=[OT])

    ctx_tiles = [(0, Vt[:, 0, :]), (128, Vt[:, 1, :])]
    for m in range(4):
        for qb in range(4):
            for hl in range(2):
                unit(qb * 64, m, hl, ctx_tiles, None)
        for r in range(32):
            rs_ = min(max(r - 4, 0), 24)
            o = r - rs_
            kts = []
            for kt in range(4):
                rho = rs_ + 2 * kt
                ks = 256 + rho * 64
                vt = Vt[:, 2 + rho // 2, :] if rho % 2 == 0 else Vo[:, (3 + rho) // 2, :]
                kts.append((ks, vt))
            kts += ctx_tiles
            for hl in range(2):
                unit(256 + r * 64, m, hl, kts, EB[:, 2 * m + hl, o, :, :])

    pOP = V(pq[:, 0:1024].rearrange("p (d t) -> p d t", d=8), pS.res)
    for i in range(18):
        t = t0 + i
        for d in range(8):
            for c in range(8):
                fw.pe(lambda e, c=c, d=d, i=i: e.matmul(pOP[:, d, :], w_out[:, c, d * 128:(d + 1) * 128],
                                                       OT[:, c, i * 128:(i + 1) * 128], start=(c == 0), stop=(c == 7)),
                      r=[w_out, OT], w=[pOP])
        fw.act(lambda e: e.activation(out=resT[:].rearrange("p d t -> p (d t)"), in_=pq[:, 0:1024], func=AF.Copy),
               r=[pOP], w=[resT])
        be.run(lambda c: resT[:, c, :], resT, l, 16, rowtype(t),
               k.xin[t * 128:(t + 1) * 128, :], k.xin, k.H[0][t * 128:(t + 1) * 128, :], k.H[0])
    fw.flush()
C0 = 0.6065306597126334
TSQ = 2304


def rw_declare(fw, k):
    k.rw = {}
    for nm in ("r", "kk", "k0", "k1", "b0", "b1", "s0", "s1", "g", "bonus", "y"):
        k.rw[nm] = fw.dram("rw_" + nm, [TSQ, D], F32)
    k.rw["v"] = fw.dram("rw_v", [TSQ, D], BF16)


def bc_load(fw, name, row_ap):
    t = fw.sbuf(name, [128, D], F32)
    dma(fw, "sp", t[:], row_ap.partition_broadcast(128), [], [t])
    return t


def phase_rw_feat(fw, k, b, src):
    l = 1
    t0 = 18 * b
    A = k.rw
    xp = fw.sbuf("xp", [128, 8, 2310], BF16)
    col = lambda s: 2 + s if s < 256 else 4 + s
    wr = fw.sbuf("rw_wr", [128, 8, D], BF16)
    wk = fw.sbuf("rw_wk", [128, 8, D], BF16)
    wv = fw.sbuf("rw_wv", [128, 8, D], BF16)
    w1 = fw.sbuf("rw_w1", [128, 2, 8, 64], BF16)
    a1 = fw.sbuf("rw_a1", [128, 2, 8, 64], BF16)
    g1 = fw.sbuf("rw_g1", [128, 8, 128], BF16)
    w2 = fw.sbuf("rw_w2", [128, 2, D], BF16)
    a2 = fw.sbuf("rw_a2", [128, 2, D], BF16)
    g2 = fw.sbuf("rw_g2", [128, D], BF16)
    mu = fw.sbuf("rw_mu", [128, 6, 8], F32)
    w0b = [bc_load(fw, "w0b%d" % d, k.rw_w0[0, d:d + 1, :]) for d in range(2)]
    a0b = [bc_load(fw, "a0b%d" % d, k.rw_a0[0, d:d + 1, :]) for d in range(2)]
    kkb = bc_load(fw, "kkb", k.rw_k_k[0:1, :])
    kab = bc_load(fw, "kab", k.rw_k_a[0:1, :])
    rkb = bc_load(fw, "rkb", k.rw_r_k[0:1, :, :].rearrange("o h d -> o (h d)"))
    for (dst, srcw) in ((wr, k.rw_w_r), (wk, k.rw_w_k), (wv, k.rw_w_v)):
        for c in range(8):
            dma(fw, "pool", dst[:, c, :], srcw[0, c * 128:(c + 1) * 128, :], [], [dst])
    for d in range(2):
        dma(fw, "pool", w1[:, d, :, :], k.rw_w1[0, d].rearrange("(c p) n -> p c n", p=128), [], [w1])
        dma(fw, "pool", a1[:, d, :, :], k.rw_a1[0, d].rearrange("(c p) n -> p c n", p=128), [], [a1])
        dma(fw, "pool", w2[0:64, d, :], k.rw_w2[0, d], [], [w2])
        dma(fw, "pool", a2[0:64, d, :], k.rw_a2[0, d], [], [a2])
    dma(fw, "pool", g1[:], k.rw_g1[0].rearrange("(c p) n -> p c n", p=128), [], [g1])
    dma(fw, "pool", g2[:], k.rw_g2[0], [], [g2])
    for j in range(6):
        dma(fw, "sp", mu[:, j, :], k.rw_mu[0, j, :].rearrange("(c p) -> p c", p=128), [], [mu], allow_slow_non_contiguous=True)
    fw.pool(lambda e: e.memset(xp[:], 0.0), w=[xp])
    gn = load_gain_T(fw, k, "gnm1", k.norm_mix_g[l, :])
    G, base = make_GS(fw, k, l, 0, gn)
    fe = Front(fw, k)
    for i in range(18):
        t = t0 + i
        fe.run(src[t * 128:(t + 1) * 128, :], src, G, l, base, rowtype(t),
               lambda c, i=i: xp[:, c, col(i * 128):col(i * 128) + 128], xp)

    xx = fw.sbuf("rw_xx", [128, 8, 128], F32)
    tmpx = fw.sbuf("rw_tmpx", [128, 8, 128], F32)
    xm = [fw.sbuf("rw_xm%d" % j, [128, 8, 128], BF16) for j in range(6)]
    hT = [fw.sbuf("rw_hT%d" % i, [128, 128], BF16) for i in range(5)]
    pp = [fw.psum("rwf_pp%d" % i, [128, 1024], F32) for i in range(3)]
    ph = fw.psum("rwf_ph", [128, 4, 128], F32)
    tr = fw.sbuf("t_r", [128, D], F32)
    tk = fw.sbuf("t_k", [128, D], F32)
    tvb = fe.xh[0]
    tkk = fw.sbuf("t_kk", [128, D], F32)
    tsq = fe.xt[0]
    ta = [fw.sbuf("t_a", [128, D], F32)] * 2
    tsg = [fw.sbuf("t_sg", [128, D], F32)] * 2
    tkd = [fw.sbuf("t_kd", [128, D], F32)] * 2
    tbt = [fw.sbuf("t_bt", [128, D], F32)] * 2
    tg = tsq
    tbo = fe.xt[1]
    s16 = [fw.sbuf("t_s16_%d" % i, [128, 16], F32) for i in range(3)]
    v3 = lambda t_: t_[:].rearrange("p (h d) -> p h d", d=64)
    b16 = lambda s_: s_[:].unsqueeze(2).to_broadcast([128, 16, 64])

    for i in range(getattr(k, "rwf_tiles", 18)):
        c0_ = col(i * 128)
        rows = slice(i * 128, (i + 1) * 128)
        fw.pool(lambda e, c0_=c0_: e.tensor_tensor(out=tmpx[:], in0=xp[:, :, c0_ - 1:c0_ + 127], in1=xp[:, :, c0_ + 1:c0_ + 129],
                                                  op=ALU.add), r=[xp], w=[tmpx])
        fw.dve(lambda e, c0_=c0_: e.scalar_tensor_tensor(out=xx[:], in0=tmpx[:], scalar=0.5, in1=xp[:, :, c0_:c0_ + 128],
                                                        op0=ALU.mult, op1=ALU.subtract), r=[tmpx, xp], w=[xx])
        for j in range(6):
            fw.pool(lambda e, j=j: e.tensor_tensor(out=tmpx[:], in0=xx[:], in1=mu[:, j, :].unsqueeze(2).to_broadcast([128, 8, 128]),
                                                  op=ALU.mult), r=[xx, mu], w=[tmpx])
            fw.pool(lambda e, j=j, c0_=c0_: e.tensor_tensor(out=xm[j][:], in0=tmpx[:], in1=xp[:, :, c0_:c0_ + 128], op=ALU.add),
                    r=[tmpx, xp], w=[xm[j]])
        if getattr(k, 'rwf_stage', 99) <= 1:
            continue
        for (pi, xj, wt) in ((0, 0, wr), (1, 2, wk), (2, 3, wv)):
            for half in range(2):
                for c in range(8):
                    fw.pe(lambda e, pi=pi, xj=xj, wt=wt, half=half, c=c: e.matmul(
                        pp[pi][:, half * 512:(half + 1) * 512], xm[xj][:, c, :], wt[:, c, half * 512:(half + 1) * 512],
                        start=(c == 0), stop=(c == 7)), r=[xm[xj], wt], w=[pp[pi]])
        var = getattr(k, "rwf_var", "")
        if var != "noevac":
            if var != "nor":
                fw.act(lambda e: e.activation(out=tr[:], in_=pp[0][:], func=AF.Copy), r=[pp[0]], w=[tr])
            if var != "nok":
                fw.act(lambda e: e.activation(out=tk[:], in_=pp[1][:], func=AF.Copy), r=[pp[1]], w=[tk])
            if var != "nokk":
                fw.pool(lambda e: e.tensor_tensor(out=tkk[:], in0=tk[:], in1=kkb[:], op=ALU.mult), r=[tk, kkb], w=[tkk])
            if var != "nov":
                fw.act(lambda e: e.activation(out=tvb[:], in_=pp[2][:], func=AF.Copy), r=[pp[2]], w=[tvb])
        if getattr(k, 'rwf_stage', 99) <= 2:
            continue
        for (hi, xj, wt, d, M) in ((0, 1, w1, 0, 64), (1, 1, w1, 1, 64), (2, 4, a1, 0, 64), (3, 4, a1, 1, 64), (4, 5, g1, None, 128)):
            for c in range(8):
                lhs = (lambda wt=wt, d=d, c=c: wt[:, c, :]) if d is None else (lambda wt=wt, d=d, c=c: wt[:, d, c, :])
                if hi < 4:
                    fw.pe(lambda e, hi=hi, xj=xj, c=c, M=M, lhs=lhs: e.matmul(ph[0:M, hi, :], lhs(), xm[xj][:, c, :],
                                                                            start=(c == 0), stop=(c == 7)), r=[xm[xj], wt], w=[ph])
                else:
                    fw.pe(lambda e, xj=xj, c=c, lhs=lhs: e.matmul(pp[2][:, 0:128], lhs(), xm[xj][:, c, :],
                                                                 start=(c == 0), stop=(c == 7)), r=[xm[xj], wt], w=[pp[2]])
        for hi, (fn, M) in enumerate(((AF.Tanh, 64), (AF.Tanh, 64), (AF.Copy, 64), (AF.Copy, 64), (AF.Sigmoid, 128))):
            if hi < 4:
                fw.act(lambda e, hi=hi, fn=fn, M=M: e.activation(out=hT[hi][0:M, :], in_=ph[0:M, hi, :], func=fn), r=[ph], w=[hT[hi]])
            else:
                fw.act(lambda e, hi=hi, fn=fn: e.activation(out=hT[hi][:, :], in_=pp[2][:, 0:128], func=fn), r=[pp[2]], w=[hT[hi]])
        if getattr(k, 'rwf_stage', 99) <= 3:
            continue
        for half in range(2):
            fw.pe(lambda e, half=half: e.matmul(pp[2][:, half * 512:(half + 1) * 512], hT[4][:, :], g2[:, half * 512:(half + 1) * 512],
                                               start=True, stop=True), r=[hT[4], g2], w=[pp[2]])
        fw.pool(lambda e: e.tensor_tensor(out=tsq[:], in0=tkk[:], in1=tkk[:], op=ALU.mult), r=[tkk], w=[tsq])
        fw.dve(lambda e: e.reduce_sum(out=s16[0][:], in_=v3(tsq), axis=AX.X), r=[tsq], w=[s16[0]])
        fw.dve(lambda e: e.tensor_scalar(out=s16[0][:], in0=s16[0][:], scalar1=1e-12, scalar2=None, op0=ALU.add), r=[s16[0]], w=[s16[0]])
        fw.act(lambda e: e.activation(out=s16[0][:], in_=s16[0][:], func=AF.Sqrt), r=[s16[0]], w=[s16[0]])
        fw.dve(lambda e: e.reciprocal(out=s16[0][:], in_=s16[0][:]), r=[s16[0]], w=[s16[0]])
        fw.dve(lambda e: e.tensor_tensor(out=v3(tkk), in0=v3(tkk), in1=b16(s16[0]), op=ALU.mult), r=[tkk, s16[0]], w=[tkk])
        fw.pool(lambda e: e.tensor_tensor(out=tsq[:], in0=tr[:], in1=rkb[:], op=ALU.mult), r=[tr, rkb], w=[tsq])
        for d in range(2):
            for half in range(2):
                fw.pe(lambda e, d=d, half=half: e.matmul(pp[0][:, half * 512:(half + 1) * 512], hT[d][0:64, :],
                                                        w2[0:64, d, half * 512:(half + 1) * 512], start=True, stop=True),
                      r=[hT[d], w2], w=[pp[0]])
                fw.pe(lambda e, d=d, half=half: e.matmul(pp[1][:, half * 512:(half + 1) * 512], hT[2 + d][0:64, :],
                                                        a2[0:64, d, half * 512:(half + 1) * 512], start=True, stop=True),
                      r=[hT[2 + d], a2], w=[pp[1]])
            fw.dve(lambda e, d=d: e.tensor_tensor(out=tsg[d][:], in0=pp[0][:], in1=w0b[d][:], op=ALU.add), r=[pp[0], w0b[d]], w=[tsg[d]])
            fw.act(lambda e, d=d: e.activation(out=tsg[d][:], in_=tsg[d][:], func=AF.Sigmoid), r=[tsg[d]], w=[tsg[d]])
            fw.dve(lambda e, d=d: e.tensor_tensor(out=ta[d][:], in0=pp[1][:], in1=a0b[d][:], op=ALU.add), r=[pp[1], a0b[d]], w=[ta[d]])
            fw.act(lambda e, d=d: e.activation(out=ta[d][:], in_=ta[d][:], func=AF.Sigmoid), r=[ta[d]], w=[ta[d]])
            fw.dve(lambda e, d=d: e.scalar_tensor_tensor(out=tkd[d][:], in0=ta[d][:], scalar=-1.0, in1=kab[:], op0=ALU.add, op1=ALU.mult),
                   r=[ta[d], kab], w=[tkd[d]])
            fw.dve(lambda e, d=d: e.scalar_tensor_tensor(out=tkd[d][:], in0=tkd[d][:], scalar=1.0, in1=tk[:], op0=ALU.add, op1=ALU.mult),
                   r=[tkd[d], tk], w=[tkd[d]])
            fw.pool(lambda e, d=d: e.tensor_tensor(out=tbt[d][:], in0=tkk[:], in1=ta[d][:], op=ALU.mult), r=[tkk, ta[d]], w=[tbt[d]])
            fw.pool(lambda e, d=d: e.tensor_tensor(out=tbo[:], in0=tsq[:], in1=tkd[d][:], op=ALU.mult), r=[tsq, tkd[d]], w=[tbo])
            fw.dve(lambda e, d=d: e.reduce_sum(out=s16[1 + d][:], in_=v3(tbo), axis=AX.X), r=[tbo], w=[s16[1 + d]])
            for (nm, tt) in (("k%d" % d, tkd[d]), ("b%d" % d, tbt[d]), ("s%d" % d, tsg[d])):
                dma(fw, "sp", A[nm][rows, :], tt[:], [tt], [A[nm]])
        fw.dve(lambda e: e.tensor_tensor(out=s16[1][:], in0=s16[1][:], in1=s16[2][:], op=ALU.add), r=[s16[1], s16[2]], w=[s16[1]])
        fw.dve(lambda e: e.tensor_tensor(out=v3(tbo), in0=v3(tvb), in1=b16(s16[1]), op=ALU.mult), r=[tvb, s16[1]], w=[tbo])
        for (nm, tt) in (("r", tr), ("kk", tkk), ("bonus", tbo), ("v", tvb)):
            dma(fw, "sp", A[nm][rows, :], tt[:], [tt], [A[nm]])
        fw.act(lambda e: e.activation(out=tg[:], in_=pp[2][:], func=AF.Copy), r=[pp[2]], w=[tg])
        dma(fw, "sp", A["g"][rows, :], tg[:], [tg], [A["g"]])
    fw.flush()
def phase_rw_scan(fw, k, b):
    A = k.rw
    tri = fw.sbuf("tri", [128, 4, 128], F32)
    dma(fw, "sp", tri[:], k.c_tri[:, :, :], [], [tri])
    mexp = [fw.sbuf("mexp%d" % m, [128, 4, 128], F32) for m in range(4)]
    for m in range(4):
        for hh in range(4):
            fw.pool(lambda e, m=m, hh=hh: e.tensor_copy(mexp[m][:, hh, :], tri[:, m, :]), r=[tri], ww=[mexp[m]])
    onesc = fw.sbuf("onesc", [128, 1], F32)
    fw.pool(lambda e: e.memset(onesc[:], 1.0), w=[onesc])
    Lr = fw.sbuf("Lr", [128, D], F32)
    Lkk = fw.sbuf("Lkk", [128, D], F32)
    Lk = fw.sbuf("Lk", [128, D], F32)
    Lb = fw.sbuf("Lb", [128, D], F32)
    Ls = fw.sbuf("Ls", [128, D], F32)
    Lv = fw.sbuf("Lv", [128, D], BF16)
    gam = fw.sbuf("gam", [128, D], F32)
    gin = fw.sbuf("gin", [128, D], F32)
    gex = fw.sbuf("gex", [128, D], F32)
    At = fw.sbuf("At", [128, D], BF16)
    Rt = fw.sbuf("Rt", [128, D], BF16)
    Bt = fw.sbuf("Bt", [128, D], BF16)
    Kt = fw.sbuf("Kt", [128, D], BF16)
    ART = fw.sbuf("ART", [128, 8, 256], BF16)
    BT = fw.sbuf("BT", [128, 8, 128], BF16)
    KTt = fw.sbuf("KTt", [128, 8, 128], BF16)
    gLT = fw.sbuf("gLT", [128, 8], F32)
    ST = fw.sbuf("ST", [128, 8, 64], F32)
    STb = fw.sbuf("STb", [128, 8, 64], BF16)
    Sn = fw.sbuf("Sn", [128, 8, 64], F32)
    PDT = F32 if getattr(k, "scan_fp32", True) else BF16
    P = [[fw.sbuf("P%d_%d" % (g, i), [128, 4, 128], PDT) for i in range(7)] for g in range(4)]
    Q = [[fw.sbuf("Q%d_%d" % (g, i), [128, 4, 128], PDT) for i in range(2)] for g in range(4)]
    Br = [fw.sbuf("Br%d" % g, [128, 4, 128], BF16) for g in range(4)]
    Aak = [fw.sbuf("Aak%d" % g, [128, 4, 128], BF16) for g in range(4)]
    Kr = [fw.sbuf("Kr%d" % g, [128, 4, 128], BF16) for g in range(4)]
    Xb = [[fw.sbuf("Xb%d_%d" % (g, i), [128, 4, 64], BF16) for i in range(2)] for g in range(4)]
    Ub = [fw.sbuf("Ub%d" % g, [128, 4, 64], BF16) for g in range(4)]
    Xf = [fw.sbuf("Xf%d" % g, [128, 4, 64], F32) for g in range(4)]
    Yt = fw.sbuf("Yt", [128, D], F32)
    Yp_ = fw.sbuf("Yprev", [128, D], F32)
    pr = [fw.psum("sc_pr%d" % i, [128, 1024], F32) for i in range(4)]
    rb = [Res("bank%d" % i) for i in range(8)]
    lo = lambda i: pr[i][:, 0:512]
    hi = lambda i: pr[i][:, 512:1024]
    v4 = lambda ap, n: ap.rearrange("p (a t) -> p a t", a=n)
    bcm = lambda m: tri[:, m, :].unsqueeze(1).to_broadcast([128, 4, 128])
    MASK = {0: dict(cum=0, strict=1, incl=0, strictT=2), 1: dict(cum=3, strict=2, incl=3, strictT=1)}

    def chunk(d, ti, want_y, second):
        mk = MASK[d]
        rows = slice(ti * 128, (ti + 1) * 128)
        for (dst, nm) in ((Lr, "r"), (Lkk, "kk"), (Lk, "k%d" % d), (Lb, "b%d" % d), (Ls, "s%d" % d), (Lv, "v")):
            dma(fw, "sp", dst[:], A[nm][rows, :], [A[nm]], [dst])
        cum = pr[3]
        for half in range(2):
            fw.pe(lambda e, half=half: e.matmul(cum[:, half * 512:(half + 1) * 512], tri[:, mk["cum"], :],
                                               Ls[:, half * 512:(half + 1) * 512], start=True, stop=True),
                  r=[tri, Ls], w=[rb[6], rb[7]])
        fw.act(lambda e: e.activation(out=gex[:], in_=cum[:], func=AF.Copy), r=[rb[6], rb[7]], w=[gex])
        fw.act(lambda e: e.activation(out=gam[:], in_=gex[:], func=AF.Exp, scale=-C0), r=[gex], w=[gam])
        fw.act(lambda e: e.activation(out=gin[:], in_=gex[:], func=AF.Exp, scale=C0), r=[gex], w=[gin])
        fw.dve(lambda e: e.tensor_tensor(out=gex[:], in0=gex[:], in1=Ls[:], op=ALU.subtract), r=[gex, Ls], w=[gex])
        fw.act(lambda e: e.activation(out=gex[:], in_=gex[:], func=AF.Exp, scale=-C0), r=[gex], w=[gex])
        glp = pr[2][:, 512:520]
        for c in range(8):
            fw.pe(lambda e, c=c: e.matmul(pr[2][:, 512 + c:513 + c], Ls[:, c * 128:(c + 1) * 128], onesc[:, 0:1],
                                         start=True, stop=True), r=[Ls, onesc], w=[rb[5]])
        fw.act(lambda e: e.activation(out=gLT[:], in_=glp, func=AF.Exp, scale=-C0), r=[rb[5]], w=[gLT])
        fw.dve(lambda e: e.scalar_tensor_tensor(out=At[:], in0=Lkk[:], scalar=-1.0, in1=gex[:], op0=ALU.mult, op1=ALU.mult),
               r=[Lkk, gex], w=[At])
        fw.pool(lambda e: e.tensor_tensor(out=Rt[:], in0=Lr[:], in1=gam[:], op=ALU.mult), r=[Lr, gam], w=[Rt])
        fw.dve(lambda e: e.tensor_tensor(out=Bt[:], in0=Lb[:], in1=gin[:], op=ALU.mult), r=[Lb, gin], w=[Bt])
        fw.pool(lambda e: e.tensor_tensor(out=Kt[:], in0=Lk[:], in1=gin[:], op=ALU.mult), r=[Lk, gin], w=[Kt])
        if getattr(k, 'scan_stage', 99) <= 1:
            return
        tps = [(At, lo(0), rb[0], lambda: ART[:, :, 0:128], ART), (Rt, hi(0), rb[1], lambda: ART[:, :, 128:256], ART),
               (Bt, lo(1), rb[2], lambda: BT[:, :, :], BT), (Kt, hi(1), rb[3], lambda: KTt[:, :, :], KTt)]
        for n_, (src_, bank, res_, dstf, dstT) in enumerate(tps):
            tpv = v4(bank.bitcast(BF16), 8)
            for c in range(8):
                fw.pe(lambda e, c=c, src_=src_, tpv=tpv: e.transpose(tpv[:, c, :], src_[:, c * 128:(c + 1) * 128], k.ident_bf[:]),
                      r=[src_, k.ident_bf], w=[res_])
            if n_ % 2 == 0:
                fw.act(lambda e, tpv=tpv, dstf=dstf: e.activation(out=dstf(), in_=tpv, func=AF.Copy), r=[res_], ww=[dstT])
            else:
                fw.dve(lambda e, tpv=tpv, dstf=dstf: e.tensor_copy(dstf(), tpv), r=[res_], w=[dstT])
        if getattr(k, 'scan_stage', 99) <= 2:
            return
        for g in range(4):
            outs = [(v4(lo(0), 4), rb[0]), (v4(hi(0), 4), rb[1]), (v4(lo(1), 4), rb[2]), (v4(hi(1), 4), rb[3]), (v4(lo(2), 4), rb[4])]
            for hh in range(4):
                h = 4 * g + hh
                c, bp = h // 2, 64 * (h % 2)
                ops_ = [(BT, ART, 0, False), (BT, ART, 128, False), (KTt, ART, 0, False), (KTt, ART, 128, False), (ART, BT, 0, True)]
                fw.pe(lambda e: e.matmul(pr[2][:, 1023:1024], BT[:, 0, :], ART[:, 0, 0:1], start=True, stop=True),
                      r=[BT, ART], w=[rb[5]])
                for oi, (lt, rt_, off, swap) in enumerate(ops_):
                    ps_, rs_ = outs[oi]
                    if not swap:
                        fw.pe(lambda e, hh=hh, c=c, bp=bp, ps_=ps_, lt=lt, off=off: e.matmul(
                            ps_[:, hh, :], lt[bp:bp + 64, c, :], ART[bp:bp + 64, c, off:off + 128], start=True, stop=True),
                            r=[lt, ART], w=[rs_])
                    else:
                        fw.pe(lambda e, hh=hh, c=c, bp=bp, ps_=ps_: e.matmul(
                            ps_[:, hh, :], ART[bp:bp + 64, c, 0:128], BT[bp:bp + 64, c, :], start=True, stop=True),
                            r=[BT, ART], w=[rs_])
            dsts = [(P[g][0], "strict"), (Br[g], "incl"), (Aak[g], "strict"), (Kr[g], "incl"), (Q[g][0], "strictT")]
            svar = getattr(k, "scan_var", "")
            if svar == "mm":
                dsts = []
            for oi, (dt_, mname) in enumerate(dsts):
                ps_, rs_ = outs[oi]
                if oi % 2 == 0:
                    fw.act(lambda e, ps_=ps_, dt_=dt_: e.activation(out=dt_[:], in_=ps_, func=AF.Copy), r=[rs_], w=[dt_])
                else:
                    fw.dve(lambda e, ps_=ps_, dt_=dt_: e.tensor_copy(dt_[:], ps_), r=[rs_], w=[dt_])
                mx = mexp[mk[mname]]
                if svar == "cp":
                    continue
                if oi % 2 == 0:
                    fw.pool(lambda e, dt_=dt_, mx=mx: e.tensor_tensor(out=dt_[:], in0=dt_[:], in1=mx[:], op=ALU.mult), r=[dt_, mx], w=[dt_])
                else:
                    fw.dve(lambda e, dt_=dt_, mx=mx: e.tensor_tensor(out=dt_[:], in0=dt_[:], in1=mx[:], op=ALU.mult), r=[dt_, mx], w=[dt_])
        if getattr(k, 'scan_stage', 99) <= 3:
            return
        for kk_ in range(6):
            for g in range(4):
                pi = 2 + (g % 2)
                Pn, Qn = v4(lo(pi), 4), v4(hi(pi), 4)
                rP, rQ = rb[2 * pi], rb[2 * pi + 1]
                Pk, Qk, Qn_sb = P[g][kk_], Q[g][kk_ % 2], Q[g][(kk_ + 1) % 2]
                for hh in range(4):
                    fw.pe(lambda e, hh=hh, Pn=Pn, Pk=Pk, Qk=Qk: e.matmul(Pn[:, hh, :], Qk[:, hh, :], Pk[:, hh, :], start=True, stop=True),
                          r=[Pk, Qk], w=[rP])
                if kk_ < 5:
                    for hh in range(4):
                        fw.pe(lambda e, hh=hh, Qn=Qn, Pk=Pk, Qk=Qk: e.matmul(Qn[:, hh, :], Pk[:, hh, :], Qk[:, hh, :], start=True, stop=True),
                              r=[Pk, Qk], w=[rQ])
                fw.act(lambda e, g=g, kk_=kk_, Pn=Pn: e.activation(out=P[g][kk_ + 1][:], in_=Pn, func=AF.Copy), r=[rP], w=[P[g][kk_ + 1]])
                if kk_ < 5:
                    fw.dve(lambda e, Qn=Qn, Qn_sb=Qn_sb: e.tensor_copy(Qn_sb[:], Qn), r=[rQ], w=[Qn_sb])
        if getattr(k, 'scan_stage', 99) <= 4:
            return
        Xp = [v4(pr[0][:, g * 256:(g + 1) * 256], 4) for g in range(4)]
        rX = [rb[0], rb[0], rb[1], rb[1]]
        for g in range(4):
            for hh in range(4):
                h = 4 * g + hh
                c, bp = h // 2, 64 * (h % 2)
                fw.pe(lambda e, g=g, hh=hh, c=c, bp=bp: e.matmul(Xp[g][:, hh, :], ART[bp:bp + 64, c, 0:128], STb[bp:bp + 64, c, :],
                                                                start=True, stop=False), r=[ART, STb], w=[rX[g]])
                fw.pe(lambda e, g=g, hh=hh, h=h: e.matmul(Xp[g][:, hh, :], Aak[g][:, hh, :], Lv[:, h * 64:(h + 1) * 64],
                                                         start=False, stop=True), r=[Aak[g], Lv], w=[rX[g]])
        for g in range(4):
            fw.dve(lambda e, g=g: e.tensor_copy(Xf[g][:], Xp[g]), r=[rX[g]], w=[Xf[g]])
            if PDT != F32:
                fw.dve(lambda e, g=g: e.tensor_copy(Xb[g][0][:], Xf[g][:]), r=[Xf[g]], w=[Xb[g][0]])
        for kk_ in range(7):
            for g in range(4):
                xb = Xf[g] if PDT == F32 else Xb[g][kk_ % 2]
                xn = Ub[g] if kk_ == 6 else Xb[g][(kk_ + 1) % 2]
                for hh in range(4):
                    fw.pe(lambda e, g=g, hh=hh, kk_=kk_, xb=xb: e.matmul(Xp[g][:, hh, :], P[g][kk_][:, hh, :], xb[:, hh, :],
                                                                        start=True, stop=True), r=[P[g][kk_], xb], w=[rX[g]])
                fw.dve(lambda e, g=g: e.tensor_tensor(out=Xf[g][:], in0=Xp[g], in1=Xf[g][:], op=ALU.add), r=[rX[g], Xf[g]], w=[Xf[g]])
                fw.act(lambda e, g=g, xn=xn: e.activation(out=xn[:], in_=Xf[g][:], func=AF.Copy), r=[Xf[g]], w=[xn])
        if getattr(k, 'scan_stage', 99) <= 5:
            return
        if want_y:
            Yp = pr[1]
            if second:
                dma(fw, "sp", Yp_[:], A["y"][rows, :], [A["y"]], [Yp_])
            for g in range(4):
                for hh in range(4):
                    h = 4 * g + hh
                    c, bp = h // 2, 64 * (h % 2)
                    rY = rb[2] if h < 8 else rb[3]
                    fw.pe(lambda e, h=h, c=c, bp=bp: e.matmul(Yp[:, h * 64:(h + 1) * 64], ART[bp:bp + 64, c, 128:256], STb[bp:bp + 64, c, :],
                                                             start=True, stop=False), r=[ART, STb], w=[rY])
                    fw.pe(lambda e, h=h, g=g, hh=hh: e.matmul(Yp[:, h * 64:(h + 1) * 64], Br[g][:, hh, :], Ub[g][:, hh, :],
                                                             start=False, stop=False), r=[Br[g], Ub[g]], w=[rY])
                    fw.pe(lambda e, h=h, g=g, hh=hh: e.matmul(Yp[:, h * 64:(h + 1) * 64], Kr[g][:, hh, :], Lv[:, h * 64:(h + 1) * 64],
                                                             start=False, stop=True), r=[Kr[g], Lv], w=[rY])
            fw.act(lambda e: e.activation(out=Yt[:], in_=Yp[:], func=AF.Copy), r=[rb[2], rb[3]], w=[Yt])
            if second:
                fw.pool(lambda e: e.tensor_tensor(out=Yt[:], in0=Yt[:], in1=Yp_[:], op=ALU.add), r=[Yt, Yp_], w=[Yt])
            dma(fw, "sp", A["y"][rows, :], Yt[:], [Yt], [A["y"]])
        if getattr(k, 'scan_stage', 99) <= 6:
            return
        Sp = pr[2][:, :].rearrange("p (c w i) -> p c w i", c=8, w=2)
        for c in range(8):
            for wch in range(2):
                h = 2 * c + wch
                g, hh = h // 4, h % 4
                fw.pe(lambda e, c=c, wch=wch, g=g, hh=hh: e.matmul(Sp[:, c, wch, :], Bt[:, c * 128:(c + 1) * 128], Ub[g][:, hh, :],
                                                                  start=True, stop=False), r=[Bt, Ub[g]], w=[rb[4], rb[5]])
                fw.pe(lambda e, c=c, wch=wch, h=h: e.matmul(Sp[:, c, wch, :], Kt[:, c * 128:(c + 1) * 128], Lv[:, h * 64:(h + 1) * 64],
                                                           start=False, stop=True), r=[Kt, Lv], w=[rb[4], rb[5]])
        for cb in range(2):
            fw.dve(lambda e, cb=cb: e.tensor_copy(Sn[0:64, 4 * cb:4 * cb + 4, :], Sp[0:64, 4 * cb:4 * cb + 4, 0, :]), r=[rb[4 + cb]], ww=[Sn])
            fw.dve(lambda e, cb=cb: e.tensor_copy(Sn[64:128, 4 * cb:4 * cb + 4, :], Sp[64:128, 4 * cb:4 * cb + 4, 1, :]), r=[rb[4 + cb]], ww=[Sn])
        fw.dve(lambda e: e.tensor_tensor(out=ST[:], in0=ST[:], in1=Sn[:], op=ALU.add), r=[ST, Sn], w=[ST])
        fw.dve(lambda e: e.tensor_tensor(out=ST[:], in0=ST[:], in1=gLT[:].unsqueeze(2).to_broadcast([128, 8, 64]), op=ALU.mult),
               r=[ST, gLT], w=[ST])
        fw.act(lambda e: e.activation(out=STb[:], in_=ST[:], func=AF.Copy), r=[ST], w=[STb])

    nt_dbg = getattr(k, "scan_tiles", 18)
    for d in range(2):
        fw.pool(lambda e: e.memset(ST[:], 0.0), w=[ST])
        fw.pool(lambda e: e.memset(STb[:], 0.0), w=[STb])
        order = list(range(18)) if d == 0 else [1, 0] + list(range(17, 1, -1))
        if nt_dbg < 18:
            order = [t_ for t_ in order if t_ < nt_dbg]
        for ti in order:
            chunk(d, ti, ti >= 2, d == 1)
    fw.flush()


def phase_rw_out(fw, k, b, src, dst):
    l = 1
    t0 = 18 * b
    A = k.rw
    wo = fw.sbuf("rw_wo", [128, 8, D], BF16)
    for c in range(8):
        dma(fw, "pool", wo[:, c, :], k.rw_w_o[0, c * 128:(c + 1) * 128, :], [], [wo])
    lnw = bc_load(fw, "lnw", k.rw_ln_w[0:1, :])
    lnb = bc_load(fw, "lnb", k.rw_ln_b[0:1, :])
    ty = fw.sbuf("o_y", [128, D], F32)
    tg = fw.sbuf("o_g", [128, D], F32)
    tb = fw.sbuf("o_b", [128, D], F32)
    tq = fw.sbuf("o_q", [128, D], F32)
    zb = fw.sbuf("o_zb", [128, D], BF16)
    zT = fw.sbuf("o_zT", [128, 8, 128], BF16)
    resT = fw.sbuf("o_resT", [128, 8, 128], F32)
    s1 = fw.sbuf("o_s1", [128, 16], F32)
    s2 = fw.sbuf("o_s2", [128, 16], F32)
    ht = [fw.sbuf("o_ht%d" % i, [128, D], F32) for i in range(2)]
    big = fw.psum("o_big", [128, 1024], F32)
    pz = fw.psum("o_pz", [128, 8, 128], BF16)
    pq = fw.psum("o_pq", [128, 1024], F32)
    be = Back(fw, k, ht, big)
    v3 = lambda t_: t_[:].rearrange("p (h d) -> p h d", d=64)
    b16 = lambda s_: s_[:].unsqueeze(2).to_broadcast([128, 16, 64])
    pOP = pq[:, :].rearrange("p (d t) -> p d t", d=8)
    for i in range(2, getattr(k, "scan_tiles", 18)):
        t = t0 + i
        rows = slice(i * 128, (i + 1) * 128)
        dma(fw, "sp", ty[:], A["y"][rows, :], [A["y"]], [ty])
        dma(fw, "sp", tg[:], A["g"][rows, :], [A["g"]], [tg])
        dma(fw, "sp", tb[:], A["bonus"][rows, :], [A["bonus"]], [tb])
        fw.dve(lambda e: e.reduce_sum(out=s1[:], in_=v3(ty), axis=AX.X), r=[ty], w=[s1])
        fw.dve(lambda e: e.tensor_scalar(out=s1[:], in0=s1[:], scalar1=1.0 / 64, scalar2=None, op0=ALU.mult), r=[s1], w=[s1])
        fw.dve(lambda e: e.tensor_tensor(out=v3(ty), in0=v3(ty), in1=b16(s1), op=ALU.subtract), r=[ty, s1], w=[ty])
        fw.pool(lambda e: e.tensor_tensor(out=tq[:], in0=ty[:], in1=ty[:], op=ALU.mult), r=[ty], w=[tq])
        fw.dve(lambda e: e.reduce_sum(out=s2[:], in_=v3(tq), axis=AX.X), r=[tq], w=[s2])
        fw.dve(lambda e: e.tensor_scalar(out=s2[:], in0=s2[:], scalar1=1.0 / 64, scalar2=64e-5, op0=ALU.mult, op1=ALU.add), r=[s2], w=[s2])
        fw.act(lambda e: e.activation(out=s2[:], in_=s2[:], func=AF.Sqrt), r=[s2], w=[s2])
        fw.dve(lambda e: e.reciprocal(out=s2[:], in_=s2[:]), r=[s2], w=[s2])
        fw.dve(lambda e: e.tensor_tensor(out=v3(ty), in0=v3(ty), in1=b16(s2), op=ALU.mult), r=[ty, s2], w=[ty])
        fw.pool(lambda e: e.tensor_tensor(out=ty[:], in0=ty[:], in1=lnw[:], op=ALU.mult), r=[ty, lnw], w=[ty])
        fw.pool(lambda e: e.tensor_tensor(out=tb[:], in0=tb[:], in1=lnb[:], op=ALU.add), r=[tb, lnb], w=[tb])
        fw.dve(lambda e: e.tensor_tensor(out=ty[:], in0=ty[:], in1=tb[:], op=ALU.add), r=[ty, tb], w=[ty])
        fw.dve(lambda e: e.tensor_tensor(out=zb[:], in0=ty[:], in1=tg[:], op=ALU.mult), r=[ty, tg], w=[zb])
        for c in range(8):
            fw.pe(lambda e, c=c: e.transpose(pz[:, c, :], zb[:, c * 128:(c + 1) * 128], k.ident_bf[:]), r=[zb, k.ident_bf], w=[pz])
        fw.act(lambda e: e.activation(out=zT[:], in_=pz[:], func=AF.Copy), r=[pz], w=[zT])
        for d in range(8):
            for c in range(8):
                fw.pe(lambda e, c=c, d=d: e.matmul(pOP[:, d, :], wo[:, c, d * 128:(d + 1) * 128], zT[:, c, :],
                                                  start=(c == 0), stop=(c == 7)), r=[wo, zT], w=[pq])
        fw.act(lambda e: e.activation(out=resT[:].rearrange("p d t -> p (d t)"), in_=pq[:, :], func=AF.Copy), r=[pq], w=[resT])
        be.run(lambda c: resT[:, c, :], resT, l, 16, rowtype(t), src[t * 128:(t + 1) * 128, :], src,
               dst[t * 128:(t + 1) * 128, :], dst)
    fw.flush()
WSPEC = [
    ("ada_w", [2, D, 6 * D]), ("ada_b", [2, 6 * D]), ("norm_mix_g", [2, D]), ("norm_ffn_g", [2, D]),
    ("router_w", [2, D, NE]), ("router_b", [2, NE]), ("exp_w_in", [2, NE, D, 2 * D]), ("exp_b_in", [2, NE, 2 * D]),
    ("exp_w_out", [2, NE, D, D]), ("exp_b_out", [2, NE, D]),
    ("ab_w_in", [1, D, 2048]), ("na_q_g", [1, 64]), ("na_k_g", [1, 64]),
    ("pool_w", [1, 4, 128, 128]), ("pool_scale", [1, 512]), ("ab_w_out", [1, D, D]),
    ("rw_mu", [1, 6, D]), ("rw_w_r", [1, D, D]), ("rw_w_k", [1, D, D]), ("rw_w_v", [1, D, D]), ("rw_w_o", [1, D, D]),
    ("rw_w0", [1, 2, D]), ("rw_w1", [1, 2, D, 64]), ("rw_w2", [1, 2, 64, D]), ("rw_a0", [1, 2, D]),
    ("rw_a1", [1, 2, D, 64]), ("rw_a2", [1, 2, 64, D]), ("rw_g1", [1, D, 128]), ("rw_g2", [1, 128, D]),
    ("rw_k_k", [1, D]), ("rw_k_a", [1, D]), ("rw_r_k", [1, 16, 64]), ("rw_ln_w", [1, D]), ("rw_ln_b", [1, D]),
]


def declare(fw, k):
    ne_decl = 1 if getattr(k, "small", False) else NE
    k.xin = fw.dram("xin", [NT * 128, D], F32, kind="ExternalInput")
    k.cc = fw.dram("cc", [3, D], F32, kind="ExternalInput")
    for name, shp in WSPEC:
        if name.startswith("exp_w"):
            shp = [shp[0], ne_decl] + shp[2:]
        setattr(k, name, fw.dram(name, shp, F32, kind="ExternalInput"))
    k.rpb_g = fw.dram("rpb_g", [8, 8, 512, 64], F32, kind="ExternalInput")
    k.c_ident = fw.dram("c_ident", [128, 128], F32, kind="ExternalInput")
    k.c_namask = fw.dram("c_namask", [512, 64], F32, kind="ExternalInput")
    k.c_poolrc = fw.dram("c_poolrc", [4, 2, 2048 + 32], F32, kind="ExternalInput")
    k.c_tri = fw.dram("c_tri", [128, 4, 128], F32, kind="ExternalInput")
    k.out = fw.dram("out", [32 * 128, D], F32, kind="ExternalOutput")
    k.H = [fw.dram("H%d" % i, [NT * 128, D], F32) for i in range(2)]
    k.combT_d = fw.dram("combT_d", [NE, 1152], F32)
    fw.persist = True
    k.modT = [fw.sbuf("modT%d" % l, [128, 48, 3], F32) for l in range(2)]
    k.ident_f = fw.sbuf("ident_f", [128, 128], F32)
    k.ident_bf = fw.sbuf("ident_bf", [128, 128], BF16)
    fw.persist = False
    dma(fw, "sp", k.ident_f[:], k.c_ident[:, :], [], [k.ident_f])
    dma(fw, "pool", k.ident_bf[:], k.c_ident[:, :], [], [k.ident_bf])


def host_consts():
    c = {}
    c["c_ident"] = np.eye(128, dtype=np.float32)
    qc = np.arange(64)
    cs = np.clip(qc - 8, 0, 48)
    kc = np.arange(64)
    valid = (kc[:, None] >= cs[None, :]) & (kc[:, None] < cs[None, :] + 16)
    m = np.where(valid, 0.0, -30000.0).astype(np.float32)
    c["c_namask"] = np.tile(m, (8, 1)).astype(np.float32)
    rc = np.zeros((4, 2, 2048 + 32), np.float32)
    for g, w in enumerate((2, 4, 8, 16)):
        for s, L in enumerate((2048, 256)):
            t = np.arange(L)
            lo = np.clip(t - w // 2, 0, L)
            hi = np.clip(t + w - w // 2, 0, L)
            rc[g, s, 16:16 + L] = 1.0 / (hi - lo)
    c["c_poolrc"] = rc
    tri = np.zeros((128, 4, 128), np.float32)
    s_ = np.arange(128)[:, None]
    t_ = np.arange(128)[None, :]
    tri[:, 0, :] = (s_ <= t_)
    tri[:, 1, :] = (s_ < t_)
    tri[:, 2, :] = (s_ > t_)
    tri[:, 3, :] = (s_ >= t_)
    c["c_tri"] = tri
    return c


def gather_rpb(rpb):
    j = np.arange(8)
    o = np.arange(8)
    kc = np.arange(64)
    qc = np.arange(64)
    ri = j[None, :] - o[:, None] + 7
    ci = np.clip(kc[:, None] - qc[None, :] + 15, 0, 30)
    g = rpb[:, ri[:, :, None, None], ci[None, None, :, :]]
    return np.ascontiguousarray(g.reshape(8, 8, 512, 64)).astype(np.float32)


def shard_inputs(inp, small=False):
    consts = host_consts()
    maps = []
    wts = {name: np.ascontiguousarray(inp[name], dtype=np.float32) for name, _ in WSPEC}
    if small:
        for nm in ("exp_w_in", "exp_w_out"):
            wts[nm] = np.ascontiguousarray(wts[nm][:, 0:1])
    rpbg = gather_rpb(np.asarray(inp["na_rpb"])[0])
    for core in range(8):
        rows = []
        for b in (2 * core, 2 * core + 1):
            rows.append(inp["ctx"][b])
            rows.append(inp["x"][b])
        m = {"xin": np.ascontiguousarray(np.concatenate(rows, axis=0), dtype=np.float32),
             "cc": np.ascontiguousarray(np.stack([inp["c"][2 * core], inp["c"][2 * core + 1], inp["c_ctx"]]), dtype=np.float32),
             "rpb_g": rpbg}
        m.update(wts)
        m.update(consts)
        maps.append(m)
    return maps
def build_program(k=None):
    nc = bass.Bass("TRN2", target_bir_lowering=False)
    fw = FW(nc)
    if k is None:
        k = K()
    declare(fw, k)
    rw_declare(fw, k)
    phase_ada(fw, k)
    for b in range(2):
        phase_ab(fw, k, b)
    for blk in range(4):
        tiles = list(range(9 * blk, 9 * blk + 9))
        phase_moe(fw, k, 0, tiles, k.H[0], lambda t: (k.H[1][t * 128:(t + 1) * 128, :], k.H[1]), blk == 0)
    for b in range(2):
        phase_rw_feat(fw, k, b, k.H[1])
        phase_rw_scan(fw, k, b)
        phase_rw_out(fw, k, b, k.H[1], k.H[0])

    def dst1(t):
        b, i = t // 18, t % 18
        o = b * 16 + (i - 2)
        return (k.out[o * 128:(o + 1) * 128, :], k.out)
    for b in range(2):
        for hb in range(2):
            tiles = [18 * b + 2 + 8 * hb + j for j in range(8)]
            phase_moe(fw, k, 1, tiles, k.H[0], dst1, False)
    fw.flush(final=True)
    return nc, fw


_CACHE = {}


def kernel(**inputs):
    from concourse.bass_utils import run_bass_kernel_spmd
    inp = {n: np.asarray(v) for n, v in inputs.items()}
    if "nc" not in _CACHE:
        _CACHE["nc"] = build_program()[0]
    nc = _CACHE["nc"]
    maps = shard_inputs(inp)
    res = run_bass_kernel_spmd(nc, maps, core_ids=list(range(8)))
    outs = [np.asarray(r["out"]).reshape(2, 2048, D) for r in res.results]
    return np.concatenate(outs, axis=0).astype(np.float32)
```

```python
import numpy as np
import concourse.bass as bass
import concourse.mybir as mybir

F32 = mybir.dt.float32
BF16 = mybir.dt.bfloat16
ALU = mybir.AluOpType
AF = mybir.ActivationFunctionType
AX = mybir.AxisListType

KDMA = 8
ENGS = ("pe", "dve", "act", "pool", "sp")


class Res:
    __slots__ = ("name", "w", "r")

    def __init__(self, name=""):
        self.name = name
        self.w = None
        self.r = {}


class T:
    def __init__(self, t, name):
        self.t = t
        self.res = Res(name)

    def __getitem__(self, k):
        return self.t[k]

    def parts(self, n):
        if not hasattr(self, "_parts"):
            self._parts = [Res("%s.%d" % (self.res.name, i)) for i in range(n)]
        return self._parts


class V(T):
    def __init__(self, ap, res):
        self.t = ap
        self.res = res


def _res(x):
    return x.res if isinstance(x, T) else x


class Op:
    __slots__ = ("waits", "fn", "marked", "kind", "dma_m")

    def __init__(self, fn, kind):
        self.waits = []
        self.fn = fn
        self.marked = False
        self.kind = kind
        self.dma_m = -1


class FW:
    def __init__(self, nc):
        self.nc = nc
        self.ops = {e: [] for e in ENGS}
        self.seen = {e: {} for e in ENGS}
        self.ndma = {e: 0 for e in ENGS}
        self.ctx = []
        self.pctx = []
        self.emitted = {e: 0 for e in ENGS}
        self.phase_end = {e: [] for e in ENGS}
        self.cnt = {e: [] for e in ENGS}
        self.sems = None
        self.persist = False

    def sbuf(self, name, shape, dt):
        self.uid = getattr(self, "uid", 0) + 1
        name = "s%d_%s" % (self.uid, name)
        g = self.nc.sbuf_tensor(name, list(shape), dt)
        t = g.__enter__()
        (self.ctx if self.persist else self.pctx).append(g)
        return T(t, name)

    def psum(self, name, shape, dt=F32):
        self.uid = getattr(self, "uid", 0) + 1
        name = "p%d_%s" % (self.uid, name)
        g = self.nc.psum_tensor(name, list(shape), dt)
        t = g.__enter__()
        (self.ctx if self.persist else self.pctx).append(g)
        return T(t, name)

    def dram(self, name, shape, dt, kind="Internal"):
        t = self.nc.dram_tensor(name, list(shape), dt, kind=kind)
        return T(t.ap(), name)

    def _need(self, eng, tok, op):
        if tok is None:
            return
        if tok[0] == "c":
            _, e, idx = tok
            if e == "pe" and eng == "pe":
                return
            key = ("c", e)
            if idx < self.emitted[e] and not self.ops[e][idx].marked:
                idx = min(i for i in self.phase_end[e] if i >= idx)
                tok = ("c", e, idx)
            if self.seen[eng].get(key, -1) >= idx:
                return
            self.seen[eng][key] = idx
            self.ops[e][idx].marked = True
            op.waits.append(tok)
        else:
            _, q, m = tok
            key = ("d", q, m % KDMA)
            if self.seen[eng].get(key, -1) >= m:
                return
            self.seen[eng][key] = m
            op.waits.append(tok)

    def _deps(self, eng, op, r, w, ww=()):
        for x in r:
            x = _res(x)
            self._need(eng, x.w, op)
        for x in w:
            x = _res(x)
            self._need(eng, x.w, op)
            for tok in x.r.values():
                self._need(eng, tok, op)
        for x in ww:
            x = _res(x)
            if x.w is not None and not (x.w[0] == "c" and x.w[1] == eng):
                self._need(eng, x.w, op)
            for tok in x.r.values():
                self._need(eng, tok, op)

    def _commit(self, tok, r, w):
        for x in r:
            x = _res(x)
            if tok[0] == "c":
                x.r[("c", tok[1])] = tok
            else:
                x.r[("d", tok[1], tok[2] % KDMA)] = tok
        for x in w:
            x = _res(x)
            x.w = tok
            x.r = {}

    def op(self, eng, fn, r=(), w=(), ww=()):
        o = Op(fn, "c")
        self._deps(eng, o, r, w, ww)
        idx = len(self.ops[eng])
        self.ops[eng].append(o)
        self._commit(("c", eng, idx), r, list(w) + list(ww))
        return o

    def dma(self, eng, fn, r=(), w=()):
        o = Op(fn, "d")
        m = self.ndma[eng]
        self.ndma[eng] += 1
        o.dma_m = m
        if m >= KDMA:
            self._need(eng, ("d", eng, m - KDMA), o)
        self._deps(eng, o, r, w)
        self.ops[eng].append(o)
        self._commit(("d", eng, m), r, w)
        return o

    def pe(self, fn, r=(), w=(), ww=()):
        return self.op("pe", fn, r, w, ww)

    def dve(self, fn, r=(), w=(), ww=()):
        return self.op("dve", fn, r, w, ww)

    def act(self, fn, r=(), w=(), ww=()):
        return self.op("act", fn, r, w, ww)

    def pool(self, fn, r=(), w=(), ww=()):
        return self.op("pool", fn, r, w, ww)

    def barrier(self):
        for eng in ENGS:
            o = Op(None, "n")
            for e in ENGS:
                for i in range(len(self.ops[e]) - 1, -1, -1):
                    if self.ops[e][i].kind == "c":
                        self._need(eng, ("c", e, i), o)
                        break
                n = self.ndma[e]
                for m in range(max(0, n - KDMA), n):
                    self._need(eng, ("d", e, m), o)
            self.ops[eng].append(o)

    def finish_waits(self):
        o = Op(None, "n")
        for q in ENGS:
            n = self.ndma[q]
            for m in range(max(0, n - KDMA), n):
                self._need("sp", ("d", q, m), o)
        self.ops["sp"].append(o)

    def _mksems(self):
        nc = self.nc
        self.sems = {}
        for e in ENGS:
            g = nc.semaphore("c_" + e)
            self.sems[("c", e)] = g.__enter__()
            self.ctx.append(g)
            for k in range(KDMA):
                g = nc.semaphore("d_%s_%d" % (e, k))
                self.sems[("d", e, k)] = g.__enter__()
                self.ctx.append(g)

    def flush(self, final=False):
        nc = self.nc
        if self.sems is None:
            self._mksems()
        if final:
            self.finish_waits()
        sems = self.sems
        start = dict(self.emitted)
        for e in ENGS:
            ops = self.ops[e]
            for i in range(len(ops) - 1, start[e] - 1, -1):
                if ops[i].kind == "c":
                    ops[i].marked = True
                    self.phase_end[e].append(i)
                    break
            c = self.cnt[e][-1] if self.cnt[e] else 0
            for o in ops[start[e]:]:
                if o.kind == "c" and o.marked:
                    c += 1
                self.cnt[e].append(c)
        cnt = self.cnt

        def run(ename, eng):
            for o in self.ops[ename][start[ename]:]:
                for tok in o.waits:
                    if tok[0] == "c":
                        eng.wait_ge(sems[("c", tok[1])], cnt[tok[1]][tok[2]])
                    else:
                        m = tok[2]
                        eng.wait_ge(sems[("d", tok[1], m % KDMA)], 16 * (m // KDMA + 1))
                if o.kind == "n":
                    continue
                ins = o.fn(eng)
                if o.kind == "d":
                    ins.then_inc(sems[("d", ename, o.dma_m % KDMA)], 16)
                elif o.marked:
                    ins.then_inc(sems[("c", ename)], 1)

        blk = nc.Block()
        block = blk.__enter__()

        @block.tensor
        def _(e):
            run("pe", e)

        @block.vector
        def _(e):
            run("dve", e)

        @block.scalar
        def _(e):
            run("act", e)

        @block.gpsimd
        def _(e):
            run("pool", e)

        @block.sync
        def _(e):
            run("sp", e)

        blk.__exit__(None, None, None)
        for e in ENGS:
            self.emitted[e] = len(self.ops[e])
        if not final:
            self.barrier()
        for g in reversed(self.pctx):
            g.__exit__(None, None, None)
        self.pctx = []
        if final:
            for g in reversed(self.ctx):
                g.__exit__(None, None, None)
            self.ctx = []

    def emit(self):
        self.flush(final=True)

    def stats(self):
        return {e: (len(self.ops[e]), sum(1 for o in self.ops[e] if o.marked),
                    sum(len(o.waits) for o in self.ops[e])) for e in ENGS}
D = 1024
NT = 36
NE = 32
EPS = 1e-6


def rowtype(t):
    return 2 if (t % 18) < 2 else t // 18


class K:
    pass


def dma(fw, q, out, in_, r, w, **kw):
    fw.dma(q, lambda e: e.dma_start(out=out, in_=in_, **kw), r=r, w=w)


def phase_ada(fw, k):
    ccT = fw.sbuf("ccT", [128, 8, 3], F32)
    scT = fw.sbuf("scT", [128, 8, 3], F32)
    for r_ in range(3):
        dma(fw, "sp", ccT[:, :, r_], k.cc[r_, :].rearrange("(c p) -> p c", p=128), [k.cc], [ccT],
            allow_slow_non_contiguous=True)
    fw.act(lambda e: e.activation(out=scT[:], in_=ccT[:], func=AF.Silu), r=[ccT], w=[scT])
    aw = [fw.sbuf("aw%d" % i, [128, 8, 768], F32) for i in range(2)]
    abT = fw.sbuf("abT", [128, 48], F32)
    ps = fw.psum("adaps", [128, 48, 3], F32)
    n = 0
    for l in range(2):
        dma(fw, "sp", abT[:], k.ada_b[l, :].rearrange("(j p) -> p j", p=128), [k.ada_b], [abT],
            allow_slow_non_contiguous=True)
        for blk in range(8):
            a = aw[n % 2]
            n += 1
            dma(fw, "sp", a[:], k.ada_w[l, :, blk * 768:(blk + 1) * 768].rearrange("(c p) f -> p c f", p=128),
                [k.ada_w], [a])
            for j in range(6):
                jj = blk * 6 + j
                for c in range(8):
                    fw.pe(lambda e, a=a, j=j, c=c, jj=jj: e.matmul(
                        ps[:, jj, :], a[:, c, j * 128:(j + 1) * 128], scT[:, c, :],
                        start=(c == 0), stop=(c == 7)), r=[a, scT], w=[ps])
        fw.dve(lambda e, l=l: e.tensor_tensor(
            out=k.modT[l][:], in0=ps[:], in1=abT[:].unsqueeze(2).to_broadcast([128, 48, 3]), op=ALU.add),
            r=[ps, abT], w=[k.modT[l]])
    fw.flush()


def load_gain_T(fw, k, name, src_row):
    t = fw.sbuf(name, [128, 8], F32)
    dma(fw, "sp", t[:], src_row.rearrange("(c p) -> p c", p=128), [], [t], allow_slow_non_contiguous=True)
    return t


def make_GS(fw, k, l, which, gain_T):
    G = fw.sbuf("G%d" % which, [128, 8, 3], F32)
    base = 0 if which == 0 else 24
    m = k.modT[l]
    fw.dve(lambda e: e.scalar_tensor_tensor(
        out=G[:], in0=m[:, base + 8:base + 16, :], scalar=1.0,
        in1=gain_T[:].unsqueeze(2).to_broadcast([128, 8, 3]), op0=ALU.add, op1=ALU.mult),
        r=[m, gain_T], w=[G])
    return G, base


class Front:
    def __init__(self, fw, k, nbuf=2):
        self.fw = fw
        self.k = k
        self.xt = [fw.sbuf("fe_xt%d" % i, [128, D], F32) for i in range(nbuf)]
        self.xh = [fw.sbuf("fe_xh%d" % i, [128, D], BF16) for i in range(2)]
        self.ss = [fw.sbuf("fe_ss%d" % i, [128, 1], F32) for i in range(2)]
        self.rstd = [fw.sbuf("fe_rs%d" % i, [128, 1], F32) for i in range(2)]
        self.tp = [fw.psum("fe_tp%d" % i, [128, 8, 128], BF16) for i in range(1)]
        self.n = 0

    def run(self, src_rows, src_res, G, l, base, row, dst_fn, dst_res):
        fw, k = self.fw, self.k
        i = self.n
        self.n += 1
        xt = self.xt[i % len(self.xt)]
        xh = self.xh[i % 2]
        ss = self.ss[i % 2]
        rstd = self.rstd[i % 2]
        tp = self.tp[0]
        junk = xh
        m = k.modT[l]
        dma(fw, "sp", xt[:], src_rows, [src_res], [xt])
        fw.pool(lambda e: e.memset(ss[:], 0.0), w=[ss])
        fw.act(lambda e: e.activation(out=junk[:], in_=xt[:], func=AF.Square, accum_out=ss[:]),
               r=[xt], w=[xh, ss])
        fw.dve(lambda e: e.tensor_scalar(out=rstd[:], in0=ss[:], scalar1=1.0 / D, scalar2=EPS,
                                         op0=ALU.mult, op1=ALU.add), r=[ss], w=[rstd])
        fw.act(lambda e: e.activation(out=rstd[:], in_=rstd[:], func=AF.Sqrt), r=[rstd], w=[rstd])
        fw.dve(lambda e: e.reciprocal(out=rstd[:], in_=rstd[:]), r=[rstd], w=[rstd])
        fw.dve(lambda e: e.tensor_scalar(out=xh[:], in0=xt[:], scalar1=rstd[:, 0:1], scalar2=None,
                                         op0=ALU.mult), r=[xt, rstd], w=[xh])
        for c in range(8):
            fw.pe(lambda e, c=c: e.transpose(tp[:, c, :], xh[:, c * 128:(c + 1) * 128], k.ident_bf[:]),
                  r=[xh, k.ident_bf], w=[tp])
        for c in range(8):
            fw.act(lambda e, c=c: e.activation(out=dst_fn(c), in_=tp[:, c, :], func=AF.Identity,
                                               scale=G[:, c, row:row + 1], bias=m[:, base + c, row:row + 1]),
                   r=[tp, G, m], ww=[dst_res])
        return xt


class Back:
    def __init__(self, fw, k, ht_bufs, ps_big):
        self.fw = fw
        self.k = k
        self.fT = [fw.sbuf("be_fT%d" % i, [128, 8, 128], F32) for i in range(1)]
        self.ht = ht_bufs
        self.ps = [ps_big]
        self.n = 0

    def run(self, srcT_fn, src_res, l, gbase, row, h_rows, h_res, dst_rows, dst_res):
        fw, k = self.fw, self.k
        i = self.n
        self.n += 1
        fT = self.fT[0]
        ht = self.ht[i % len(self.ht)]
        ps = self.ps[0]
        m = k.modT[l]
        dma(fw, "sp", ht[:], h_rows, [h_res], [ht])
        fp = fT.parts(8)
        for c in range(8):
            if c % 2 == 0:
                fw.dve(lambda e, c=c: e.tensor_scalar(out=fT[:, c, :], in0=srcT_fn(c),
                                                      scalar1=m[:, gbase + c, row:row + 1], scalar2=None,
                                                      op0=ALU.mult), r=[src_res, m], w=[fp[c]])
            else:
                fw.act(lambda e, c=c: e.activation(out=fT[:, c, :], in_=srcT_fn(c), func=AF.Copy,
                                                   scale=m[:, gbase + c, row:row + 1]), r=[src_res, m], w=[fp[c]])
        for c in range(8):
            fw.pe(lambda e, c=c: e.transpose(ps[:, c * 128:(c + 1) * 128], fT[:, c, :], k.ident_f[:]),
                  r=[fp[c], k.ident_f], w=[ps])
        fw.dve(lambda e: e.tensor_tensor(out=ht[:], in0=ps[:], in1=ht[:], op=ALU.add), r=[ps, ht], w=[ht])
        dma(fw, "sp", dst_rows, ht[:], [ht], [dst_res])
def subs(T):
    out = []
    o = 0
    while o < T:
        n = min(512, T - o)
        out.append((o, n))
        o += n
    return out


def phase_moe(fw, k, l, tiles, src, dst_fn, first_block):
    nt = len(tiles)
    TB = nt * 128
    SB = subs(TB)
    ynT = fw.sbuf("ynT", [128, 8, TB], BF16)
    acc = fw.sbuf("acc", [128, 8, TB], F32)
    actT = [fw.sbuf("actT%d" % i, [128, 8, TB], BF16) for i in range(2)]
    gbc = [fw.sbuf("gbc%d" % i, [128, TB], F32) for i in range(1)]
    st1 = [fw.sbuf("st1_%d" % i, [128, 8, 256], F32) for i in range(2)]
    w1b = [fw.sbuf("w1b%d" % i, [128, 8, 2, 128], BF16) for i in range(4)]
    st2 = [fw.sbuf("st2_%d" % i, [128, 1024], F32) for i in range(2)]
    w2bs = [fw.sbuf("w2b%d" % i, [128, 8, 1024], BF16) for i in range(2)]
    w2ps = [w.parts(8) for w in w2bs]
    ub = [[fw.sbuf("ub%d_%d" % (i, j), [128, 512], F32) for j in range(3)] for i in range(2)]
    wr = fw.sbuf("wr", [128, 8, NE], BF16)
    rb = fw.sbuf("rb", [128, NE], F32)
    bout = V(st2[0][0:NE, :], st2[0].res)
    bin_sb = V(st1[0][0:NE, :, :].rearrange("p c f -> p (c f)"), st1[0].res)
    binT = fw.sbuf("binT", [128, 16, NE], F32)
    combT = fw.sbuf("combT", [NE, TB], F32)
    lg = fw.sbuf("lg", [128, NE], F32)
    m8 = fw.sbuf("m8", [128, 8], F32)
    nmx = fw.sbuf("nmx", [128, 1], F32)
    msk = fw.sbuf("msk", [128, NE], F32)
    ex = fw.sbuf("ex", [128, NE], F32)
    sm = fw.sbuf("sm", [128, 1], F32)
    comb = fw.sbuf("comb", [128, NE], F32)
    gn = load_gain_T(fw, k, "gnf", k.norm_ffn_g[l, :])
    G, base = make_GS(fw, k, l, 1, gn)
    fe = Front(fw, k)
    ps_big = fw.psum("ps_big", [128, 1024], F32)
    be = Back(fw, k, fe.xt, ps_big)
    ps_misc = fw.psum("ps_misc", [128, 512], F32)
    ps_g = [V(ps_big[:, 0:512], ps_big.res), fw.psum("ps_g1", [128, 512], F32)]
    ps_l = [V(ps_big[:, 512:1024], Res("ps_l0")), fw.psum("ps_l1", [128, 512], F32)]
    ps_y = [fw.psum("ps_y%d" % i, [128, 512], F32) for i in range(2)]
    combT_d = k.combT_d

    dma(fw, "pool", wr[:], k.router_w[l].rearrange("(c p) e -> p c e", p=128), [], [wr])
    dma(fw, "sp", rb[:], k.router_b[l:l + 1, :].partition_broadcast(128), [], [rb])
    dma(fw, "sp", bout[:], k.exp_b_out[l], [], [bout])
    dma(fw, "sp", bin_sb[:], k.exp_b_in[l], [], [bin_sb])
    for mt in range(16):
        m_, two = mt // 2, mt % 2
        fw.pe(lambda e, mt=mt, m_=m_, two=two: e.transpose(
            ps_misc[:, mt * NE:(mt + 1) * NE],
            bin_sb[:, two + 256 * m_: 256 * m_ + 256: 2], k.ident_f[0:NE, 0:NE]),
            r=[bin_sb, k.ident_f], w=[ps_misc])
    fw.dve(lambda e: e.tensor_copy(binT[:].rearrange("p a b -> p (a b)"), ps_misc[:, 0:16 * NE]),
           r=[ps_misc], w=[binT])
    fw.dve(lambda e: e.tensor_scalar(out=binT[:, 1::2, :], in0=binT[:, 1::2, :], scalar1=1.0, scalar2=None, op0=ALU.add),
           r=[binT], w=[binT])

    for i, t in enumerate(tiles):
        row = rowtype(t)
        fe.run(src[t * 128:(t + 1) * 128, :], src, G, l, base, row,
               lambda c, i=i: ynT[:, c, i * 128:(i + 1) * 128], ynT)
        for c in range(8):
            fw.pe(lambda e, c=c, i=i: e.matmul(ps_misc[:, 0:NE], ynT[:, c, i * 128:(i + 1) * 128], wr[:, c, :],
                                               start=(c == 0), stop=(c == 7)), r=[ynT, wr], w=[ps_misc])
        fw.dve(lambda e: e.tensor_tensor(out=lg[:], in0=ps_misc[:, 0:NE], in1=rb[:], op=ALU.add),
               r=[ps_misc, rb], w=[lg])
        fw.dve(lambda e: e.max(out=m8[:], in_=lg[:]), r=[lg], w=[m8])
        fw.dve(lambda e: e.tensor_scalar(out=msk[:], in0=lg[:], scalar1=m8[:, 3:4], scalar2=None, op0=ALU.is_ge),
               r=[lg, m8], w=[msk])
        fw.dve(lambda e: e.tensor_scalar(out=nmx[:], in0=m8[:, 0:1], scalar1=-1.0, scalar2=None, op0=ALU.mult),
               r=[m8], w=[nmx])
        fw.act(lambda e: e.activation(out=ex[:], in_=lg[:], func=AF.Exp, bias=nmx[:, 0:1]), r=[lg, nmx], w=[ex])
        fw.dve(lambda e: e.tensor_tensor(out=ex[:], in0=ex[:], in1=msk[:], op=ALU.mult), r=[ex, msk], w=[ex])
        fw.dve(lambda e: e.reduce_sum(out=sm[:], in_=ex[:], axis=AX.X), r=[ex], w=[sm])
        fw.dve(lambda e: e.reciprocal(out=sm[:], in_=sm[:]), r=[sm], w=[sm])
        fw.dve(lambda e: e.tensor_scalar(out=comb[:], in0=ex[:], scalar1=sm[:, 0:1], scalar2=None, op0=ALU.mult),
               r=[ex, sm], w=[comb])
        fw.pe(lambda e: e.transpose(ps_misc[0:NE, 128:256], comb[:], k.ident_f[:]), r=[comb, k.ident_f], w=[ps_misc])
        fw.act(lambda e, i=i: e.activation(out=combT[:, i * 128:(i + 1) * 128], in_=ps_misc[0:NE, 128:256], func=AF.Copy),
               r=[ps_misc], ww=[combT])
    dma(fw, "sp", combT_d[:, 0:TB], combT[:], [combT], [combT_d])

    for d in range(8):
        for (o, n) in SB:
            fw.pe(lambda e, d=d, o=o, n=n: e.matmul(ps_misc[:, 0:n], bout[:, d * 128:(d + 1) * 128], combT[:, o:o + n],
                                                    start=True, stop=True), r=[bout, combT], w=[ps_misc])
            fw.act(lambda e, d=d, o=o, n=n: e.activation(out=acc[:, d, o:o + n], in_=ps_misc[:, 0:n], func=AF.Copy),
                   r=[ps_misc], ww=[acc])

    cnt = {"w1": 0, "u": 0, "cast": 0, "y": 0}
    ne = k.n_experts_dbg if hasattr(k, "n_experts_dbg") else NE

    def load_w1(e_, m_):
        i = cnt["w1"]
        cnt["w1"] += 1
        s_ = st1[i % 2]
        wb = w1b[i % 4]
        dma(fw, "sp", s_[:], k.exp_w_in[l, e_].rearrange("(c p) f -> p c f", p=128)[:, :, m_ * 256:(m_ + 1) * 256],
            [], [s_])
        if True:
            fw.act(lambda e: e.activation(out=wb[:], in_=s_[:].rearrange("p c (j two) -> p c two j", two=2), func=AF.Copy),
                   r=[s_], w=[wb])
        else:
            fw.pool(lambda e: e.tensor_copy(wb[:], s_[:].rearrange("p c (j two) -> p c two j", two=2)),
                    r=[s_], w=[wb])
        return wb

    def load_w2_piece(e_, m_):
        i = cnt["cast"]
        cnt["cast"] += 1
        s_ = st2[i % 2]
        w2b, w2p = w2bs[e_ % 2], w2ps[e_ % 2]
        dma(fw, "sp", s_[:], k.exp_w_out[l, e_, m_ * 128:(m_ + 1) * 128, :], [], [s_])
        if i % 2 == 0:
            fw.pool(lambda e, m_=m_, s_=s_, w2b=w2b: e.tensor_copy(w2b[:, m_, :], s_[:]), r=[s_], w=[w2p[m_]])
        else:
            fw.act(lambda e, m_=m_, s_=s_, w2b=w2b: e.activation(out=w2b[:, m_, :], in_=s_[:], func=AF.Copy), r=[s_], w=[w2p[m_]])

    PF = 3
    npieces = [0]
    ready = {}

    def ensure(q_target):
        while npieces[0] <= min(q_target, 8 * ne - 1):
            q = npieces[0]
            e2, m2 = q // 8, q % 8
            ready[q] = load_w1(e2, m2)
            if e2 >= 1:
                load_w2_piece(e2 - 1, m2)
            npieces[0] += 1

    def mm1(e_, w2_of=None):
        g = gbc[0]
        dma(fw, "sp", g[:], combT_d[e_:e_ + 1, 0:TB].partition_broadcast(128), [combT_d], [g])
        aT = actT[e_ % 2]
        pend = []
        for m_ in range(8):
            ensure(e_ * 8 + m_ + PF)
            wb = ready.pop(e_ * 8 + m_)
            for (o, n) in SB:
                u = cnt["u"]
                cnt["u"] += 1
                pg, pl = ps_g[u % 2], ps_l[u % 2]
                A, B, C = ub[u % 2]
                for c in range(8):
                    fw.pe(lambda e, c=c, o=o, n=n, wb=wb, pg=pg: e.matmul(pg[:, 0:n], wb[:, c, 0, :], ynT[:, c, o:o + n],
                                                                          start=(c == 0), stop=(c == 7)), r=[wb, ynT], w=[pg])
                for c in range(8):
                    fw.pe(lambda e, c=c, o=o, n=n, wb=wb, pl=pl: e.matmul(pl[:, 0:n], wb[:, c, 1, :], ynT[:, c, o:o + n],
                                                                          start=(c == 0), stop=(c == 7)), r=[wb, ynT], w=[pl])
                bg = binT[:, 2 * m_, e_:e_ + 1]
                bl = binT[:, 2 * m_ + 1, e_:e_ + 1]
                fw.dve(lambda e, n=n, pg=pg, A=A, bg=bg: e.tensor_scalar(out=A[:, 0:n], in0=pg[:, 0:n], scalar1=bg, scalar2=7.0,
                                                                        op0=ALU.add, op1=ALU.min), r=[pg, binT], w=[A])
                fw.act(lambda e, n=n, pl=pl, C=C, bl=bl: e.activation(out=C[:, 0:n], in_=pl[:, 0:n], func=AF.Identity, bias=bl),
                       r=[pl, binT], w=[C])
                if pend:
                    pend.pop()()

                def stage2(n=n, o=o, A=A, B=B, C=C, g=g, aT=aT, m_=m_):
                    fw.act(lambda e: e.activation(out=B[:, 0:n], in_=A[:, 0:n], func=AF.Sigmoid, scale=1.702), r=[A], w=[B])
                    fw.pool(lambda e: e.tensor_scalar(out=C[:, 0:n], in0=C[:, 0:n], scalar1=8.0, scalar2=-6.0,
                                                     op0=ALU.min, op1=ALU.max), r=[C], w=[C])
                    fw.dve(lambda e: e.tensor_tensor(out=A[:, 0:n], in0=A[:, 0:n], in1=B[:, 0:n], op=ALU.mult), r=[A, B], w=[A])
                    fw.pool(lambda e: e.tensor_tensor(out=C[:, 0:n], in0=C[:, 0:n], in1=A[:, 0:n], op=ALU.mult), r=[A, C], w=[C])
                    fw.dve(lambda e: e.tensor_tensor(out=aT[:, m_, o:o + n], in0=C[:, 0:n], in1=g[:, o:o + n], op=ALU.mult),
                           r=[C, g], ww=[aT])
                pend.append(stage2)
        if pend:
            pend.pop()()

    def mm2(e_):
        aT = actT[e_ % 2]
        w2b, w2p = w2bs[e_ % 2], w2ps[e_ % 2]
        for d in range(8):
            for (o, n) in SB:
                py = ps_y[cnt["y"] % 2]
                cnt["y"] += 1
                for m_ in range(8):
                    fw.pe(lambda e, m_=m_, d=d, o=o, n=n, py=py, w2b=w2b: e.matmul(py[:, 0:n], w2b[:, m_, d * 128:(d + 1) * 128],
                                                                          aT[:, m_, o:o + n], start=(m_ == 0), stop=(m_ == 7)),
                          r=[w2p[m_], aT], w=[py])
                fw.dve(lambda e, d=d, o=o, n=n, py=py: e.tensor_tensor(out=acc[:, d, o:o + n], in0=py[:, 0:n],
                                                                      in1=acc[:, d, o:o + n], op=ALU.add),
                       r=[py], ww=[acc])

    mm1(0)
    for e_ in range(ne):
        if e_ + 1 < ne:
            mm1(e_ + 1, w2_of=e_)
        else:
            for m_ in range(8):
                load_w2_piece(e_, m_)
        mm2(e_)

    for i, t in enumerate(tiles):
        row = rowtype(t)
        drows, dres = dst_fn(t)
        if drows is None:
            continue
        be.run(lambda c, i=i: acc[:, c, i * 128:(i + 1) * 128], acc, l, 40, row,
               src[t * 128:(t + 1) * 128, :], src, drows, dres)
    fw.flush()
def ucol(s):
    return 16 + s if s < 256 else s + 48


def phase_ab(fw, k, b):
    l = 0
    t0 = 18 * b
    TS = 2304
    xnT = fw.sbuf("xnT", [128, 8, TS], BF16)
    OT = xnT
    QT = fw.sbuf("QT", [128, 4, TS], BF16)
    KT = fw.sbuf("KT", [128, 4, TS], BF16)
    Vt = fw.sbuf("Vt", [128, 18, 512], BF16)
    Vo = fw.sbuf("Vo", [128, 17, 512], BF16)
    UT = fw.sbuf("UT", [128, 4, 2368], BF16)
    w_in = fw.sbuf("w_in", [128, 8, 2048], BF16)
    EB = V(w_in[:].rearrange("p h (o jp q) -> p h o jp q", o=8, jp=4), w_in.res)
    w_out = V(UT[:].rearrange("p g x -> p (g x)")[:, 0:8 * D].rearrange("p (c f) -> p c f", c=8), UT.res)
    pw = fw.sbuf("pw", [128, 4, 128], BF16)
    pscale = fw.sbuf("pscale", [128, 4], F32)
    gq = fw.sbuf("gq", [128, 1], F32)
    gk = fw.sbuf("gk", [128, 1], F32)
    bd = fw.sbuf("bd", [128, 128], BF16)
    ones = fw.sbuf("ones", [128, 128], BF16)
    sqb = [fw.sbuf("sqb%d" % i, [128, 512], BF16) for i in range(2)]
    rstd = [fw.sbuf("qrstd%d" % i, [128, 512], F32) for i in range(2)]
    tA = fw.sbuf("ptA", [128, 544], F32)
    tB = fw.sbuf("ptB", [128, 544], F32)
    rcb = fw.sbuf("rcb", [128, 512], F32)
    pooled = fw.sbuf("pooled", [128, 512], BF16)
    mk = fw.sbuf("mk", [128, 4, 64], F32)
    rbt = [fw.sbuf("rbt%d" % i, [128, 4, 64], F32) for i in range(2)]
    Pb = [fw.sbuf("Pb%d" % i, [128, 6, 64], BF16) for i in range(3)]
    rd = [fw.sbuf("rd%d" % i, [128, 64], F32) for i in range(2)]
    resT = fw.sbuf("resT", [128, 8, 128], F32)
    gn = load_gain_T(fw, k, "gnm", k.norm_mix_g[l, :])
    G, base = make_GS(fw, k, l, 0, gn)
    fe = Front(fw, k)
    big = fw.psum("ab_big", [128, 1024], F32)
    pq = fw.psum("ab_pq", [128, 2048], F32)
    be = Back(fw, k, fe.xt, big)
    pAB = [V(big[:, 0:512], Res("pA")), V(big[:, 512:1024], Res("pB"))]
    pS = V(pq[:, 0:512], Res("pS"))
    pO = [V(pq[:, 0:64], pS.res), V(pq[:, 512:576], Res("pO1"))]
    pD = [V(pq[:, 1024:1088], Res("pD0")), V(pq[:, 1536:1600], Res("pD1"))]

    for c in range(8):
        dma(fw, "pool", w_in[:, c, :], k.ab_w_in[0, c * 128:(c + 1) * 128, :], [], [w_in])
    fw.pool(lambda e: e.memset(UT[:], 0.0), w=[UT])
    for (g_, src) in ((gq, k.na_q_g), (gk, k.na_k_g)):
        for hl in range(2):
            dma(fw, "sp", g_[hl * 64:(hl + 1) * 64, :], src[0, :].rearrange("(p o) -> p o", o=1), [], [g_])
    fw.dve(lambda e: e.tensor_scalar(out=gq[:], in0=gq[:], scalar1=0.125, scalar2=None, op0=ALU.mult), r=[gq], w=[gq])
    fw.pool(lambda e: e.memset(bd[:], 0.0), w=[bd])
    fw.pool(lambda e: e.memset(bd[0:64, 0:64], 1.0), w=[bd])
    fw.pool(lambda e: e.memset(bd[64:128, 64:128], 1.0), w=[bd])
    fw.pool(lambda e: e.memset(ones[:], 1.0), w=[ones])
    dma(fw, "pool", pw[:], k.pool_w[0].rearrange("g c d -> c g d"), [], [pw])
    dma(fw, "sp", pscale[:], k.pool_scale[0, :].rearrange("(g p) -> p g", p=128), [], [pscale],
        allow_slow_non_contiguous=True)
    dma(fw, "sp", mk[:], k.c_namask[:, :].rearrange("(jp p) q -> p jp q", p=128), [], [mk])

    for i in range(18):
        t = t0 + i
        fe.run(k.xin[t * 128:(t + 1) * 128, :], k.xin, G, l, base, rowtype(t),
               lambda c, i=i: xnT[:, c, i * 128:(i + 1) * 128], xnT)

    n_ = 0
    for m in range(8):
        dstT, gg = (QT, gq) if m < 4 else (KT, gk)
        mm = m % 4
        for (o, n) in subs(TS):
            ps = pAB[n_ % 2]
            sq = sqb[n_ % 2]
            rs = rstd[n_ % 2]
            n_ += 1
            for c in range(8):
                fw.pe(lambda e, c=c, m=m, o=o, n=n, ps=ps: e.matmul(ps[:, 0:n], w_in[:, c, m * 128:(m + 1) * 128],
                                                                   xnT[:, c, o:o + n], start=(c == 0), stop=(c == 7)),
                      r=[w_in, xnT], w=[ps])
            fw.act(lambda e, n=n, ps=ps, sq=sq: e.activation(out=sq[:, 0:n], in_=ps[:, 0:n], func=AF.Square), r=[ps], w=[sq])
            fw.pe(lambda e, n=n, sq=sq: e.matmul(pS[:, 0:n], bd[:], sq[:, 0:n], start=True, stop=True), r=[bd, sq], w=[pS])
            fw.dve(lambda e, n=n, rs=rs: e.tensor_scalar(out=rs[:, 0:n], in0=pS[:, 0:n], scalar1=1.0 / 64, scalar2=EPS,
                                                        op0=ALU.mult, op1=ALU.add), r=[pS], w=[rs])
            fw.act(lambda e, n=n, rs=rs: e.activation(out=rs[:, 0:n], in_=rs[:, 0:n], func=AF.Sqrt), r=[rs], w=[rs])
            fw.dve(lambda e, n=n, rs=rs: e.reciprocal(out=rs[:, 0:n], in_=rs[:, 0:n]), r=[rs], w=[rs])
            fw.dve(lambda e, n=n, o=o, rs=rs, ps=ps, dstT=dstT, gg=gg, mm=mm: e.scalar_tensor_tensor(
                out=dstT[:, mm, o:o + n], in0=ps[:, 0:n], scalar=gg[:, 0:1], in1=rs[:, 0:n], op0=ALU.mult, op1=ALU.mult),
                r=[ps, rs, gg], ww=[dstT])
    for (dst, ntile, off) in ((Vt, 18, 0), (Vo, 17, 64)):
        for i in range(ntile):
            ps = pAB[n_ % 2]
            n_ += 1
            for c in range(8):
                fw.pe(lambda e, c=c, i=i, off=off, ps=ps: e.matmul(ps[:, :], xnT[:, c, off + i * 128: off + (i + 1) * 128],
                                                                  w_in[:, c, 1024:1536], start=(c == 0), stop=(c == 7)),
                      r=[w_in, xnT], w=[ps])
            fw.act(lambda e, i=i, ps=ps, dst=dst: e.activation(out=dst[:, i, :], in_=ps[:, :], func=AF.Copy), r=[ps], ww=[dst])
    for g in range(4):
        for (o, n) in subs(TS):
            ps = pAB[n_ % 2]
            n_ += 1
            for c in range(8):
                fw.pe(lambda e, c=c, g=g, o=o, n=n, ps=ps: e.matmul(ps[:, 0:n], w_in[:, c, 1536 + g * 128:1536 + (g + 1) * 128],
                                                                   xnT[:, c, o:o + n], start=(c == 0), stop=(c == 7)),
                      r=[w_in, xnT], w=[ps])
            pieces = [(o, n)] if o >= 256 else [(0, 256), (256, n - 256)]
            for (po_, pn) in pieces:
                fw.dve(lambda e, g=g, po_=po_, pn=pn, o=o, ps=ps: e.tensor_copy(UT[:, g, ucol(po_):ucol(po_) + pn],
                                                                              ps[:, po_ - o:po_ - o + pn]), r=[ps], ww=[UT])

    for g, w in enumerate((2, 4, 8, 16)):
        nlev = (2, 4, 8, 16).index(w) + 1
        plist = [(16, 256, 0, 1, 0)] + [(304 + 512 * j, 512, 256 + 512 * j, 0, 512 * j) for j in range(4)]
        for (lo, n, s0, seg, p0) in plist:
            W = n + 32
            xb = lo - 16
            fw.dve(lambda e, g=g, W=W, xb=xb: e.tensor_tensor(out=tA[:, 1:W], in0=UT[:, g, xb + 1:xb + W],
                                                             in1=UT[:, g, xb:xb + W - 1], op=ALU.add), r=[UT], w=[tA])
            cur, oth = tA, tB
            sh = 2
            valid = 1
            for lev in range(1, nlev):
                lo_x = valid + sh
                fw.dve(lambda e, W=W, cur=cur, oth=oth, lo_x=lo_x, sh=sh: e.tensor_tensor(
                    out=oth[:, lo_x:W], in0=cur[:, lo_x:W], in1=cur[:, lo_x - sh:W - sh], op=ALU.add), r=[cur], w=[oth])
                cur, oth = oth, cur
                valid = lo_x
                sh *= 2
            X0 = 16 + w // 2 - 1
            dma(fw, "sp", rcb[:, 0:n], k.c_poolrc[g, seg:seg + 1, 16 + p0:16 + p0 + n].partition_broadcast(128), [], [rcb])
            fw.dve(lambda e, n=n, cur=cur, oth=oth, X0=X0: e.tensor_tensor(out=oth[:, 0:n], in0=cur[:, X0:X0 + n],
                                                                         in1=rcb[:, 0:n], op=ALU.mult), r=[cur, rcb], w=[oth])
            fw.pool(lambda e, n=n, oth=oth, g=g, lo=lo: e.tensor_tensor(out=pooled[:, 0:n], in0=oth[:, 0:n],
                                                                       in1=UT[:, g, lo:lo + n], op=ALU.subtract),
                    r=[oth, UT], w=[pooled])
            ps = pAB[n_ % 2]
            n_ += 1
            fw.pe(lambda e, n=n, g=g, ps=ps: e.matmul(ps[:, 0:n], pw[:, g, :], pooled[:, 0:n], start=True, stop=True),
                  r=[pw, pooled], w=[ps])
            fw.act(lambda e, n=n, g=g, s0=s0, ps=ps: e.activation(out=OT[:, 4 + g, s0:s0 + n], in_=ps[:, 0:n], func=AF.Copy,
                                                                 scale=pscale[:, g:g + 1]), r=[ps, pscale], ww=[OT])

    n2 = 0
    for h in range(8):
        for o in range(8):
            rb_ = rbt[n2 % 2]
            n2 += 1
            dma(fw, "sp", rb_[:], k.rpb_g[h, o].rearrange("(jp p) q -> p jp q", p=128), [], [rb_])
            fw.pool(lambda e, rb_=rb_: e.tensor_tensor(out=rb_[:], in0=rb_[:], in1=mk[:], op=ALU.add), r=[rb_, mk], w=[rb_])
            fw.act(lambda e, rb_=rb_, h=h, o=o: e.activation(out=EB[:, h, o, :, :], in_=rb_[:], func=AF.Exp), r=[rb_], ww=[EB])

    for c in range(8):
        dma(fw, "pool", w_out[:, c, :], k.ab_w_out[0, c * 128:(c + 1) * 128, :], [], [w_out])

    cnt = {"u": 0}
    pst = pAB

    def unit(qs, m, hl, ktiles, eb):
        u = cnt["u"]
        cnt["u"] += 1
        bp = 64 * hl
        ps = pst[u % 2]
        P = Pb[u % 3]
        rdd = rd[u % 2]
        nk = len(ktiles)
        for kt, (ks, vt) in enumerate(ktiles):
            fw.pe(lambda e, kt=kt, ks=ks, ps=ps: e.matmul(ps[:, kt * 64:(kt + 1) * 64], KT[bp:bp + 64, m, ks:ks + 128],
                                                         QT[bp:bp + 64, m, qs:qs + 64], start=True, stop=True),
                  r=[KT, QT], w=[ps])
        fw.act(lambda e, nk=nk, ps=ps, P=P: e.activation(out=P[:, 0:nk, :].rearrange("p a b -> p (a b)"), in_=ps[:, 0:nk * 64],
                                                        func=AF.Exp), r=[ps], w=[P])
        if eb is not None:
            fw.dve(lambda e, P=P, eb=eb: e.tensor_tensor(out=P[:, 0:4, :], in0=P[:, 0:4, :], in1=eb, op=ALU.mult),
                   r=[P, EB], w=[P])
        for kt, (ks, vt) in enumerate(ktiles):
            fw.pe(lambda e, kt=kt, vt=vt, P=P: e.matmul(pO[hl][:, :], vt[:, m * 128:(m + 1) * 128], P[:, kt, :],
                                                       start=(kt == 0), stop=(kt == nk - 1)), r=[Vt, Vo, P], w=[pO[hl]])
        for kt, (ks, vt) in enumerate(ktiles):
            fw.pe(lambda e, kt=kt, P=P: e.matmul(pD[hl][:, :], ones[:], P[:, kt, :],
                                                start=(kt == 0), stop=(kt == nk - 1)), r=[ones, P], w=[pD[hl]])
        fw.dve(lambda e, rdd=rdd: e.reciprocal(out=rdd[bp:bp + 64, :], in_=pD[hl][bp:bp + 64, :]), r=[pD[hl]], w=[rdd])
        fw.dve(lambda e, rdd=rdd: e.tensor_tensor(out=OT[bp:bp + 64, m, qs:qs + 64], in0=pO[hl][bp:bp + 64, :],
                                                 in1=rdd[bp:bp + 64, :], op=ALU.mult), r=[pO[hl], rdd], ww=[OT])

    ctx_tiles = [(0, Vt[:, 0, :]), (128, Vt[:, 1, :])]
    for m in range(4):
        for qb in range(4):
            for hl in range(2):
                unit(qb * 64, m, hl, ctx_tiles, None)
        for r in range(32):
            rs_ = min(max(r - 4, 0), 24)
            o = r - rs_
            kts = []
            for kt in range(4):
                rho = rs_ + 2 * kt
                ks = 256 + rho * 64
                vt = Vt[:, 2 + rho // 2, :] if rho % 2 == 0 else Vo[:, (3 + rho) // 2, :]
                kts.append((ks, vt))
            kts += ctx_tiles
            for hl in range(2):
                unit(256 + r * 64, m, hl, kts, EB[:, 2 * m + hl, o, :, :])

    pOP = V(pq[:, 0:1024].rearrange("p (d t) -> p d t", d=8), pS.res)
    for i in range(18):
        t = t0 + i
        for d in range(8):
            for c in range(8):
                fw.pe(lambda e, c=c, d=d, i=i: e.matmul(pOP[:, d, :], w_out[:, c, d * 128:(d + 1) * 128],
                                                       OT[:, c, i * 128:(i + 1) * 128], start=(c == 0), stop=(c == 7)),
                      r=[w_out, OT], w=[pOP])
        fw.act(lambda e: e.activation(out=resT[:].rearrange("p d t -> p (d t)"), in_=pq[:, 0:1024], func=AF.Copy),
               r=[pOP], w=[resT])
        be.run(lambda c: resT[:, c, :], resT, l, 16, rowtype(t),
               k.xin[t * 128:(t + 1) * 128, :], k.xin, k.H[0][t * 128:(t + 1) * 128, :], k.H[0])
    fw.flush()
C0 = 0.6065306597126334
TSQ = 2304


def rw_declare(fw, k):
    k.rw = {}
    for nm in ("r", "kk", "k0", "k1", "b0", "b1", "s0", "s1", "g", "bonus", "y"):
        k.rw[nm] = fw.dram("rw_" + nm, [TSQ, D], F32)
    k.rw["v"] = fw.dram("rw_v", [TSQ, D], BF16)


def bc_load(fw, name, row_ap):
    t = fw.sbuf(name, [128, D], F32)
    dma(fw, "sp", t[:], row_ap.partition_broadcast(128), [], [t])
    return t


def phase_rw_feat(fw, k, b, src):
    l = 1
    t0 = 18 * b
    A = k.rw
    xp = fw.sbuf("xp", [128, 8, 2310], BF16)
    col = lambda s: 2 + s if s < 256 else 4 + s
    wr = fw.sbuf("rw_wr", [128, 8, D], BF16)
    wk = fw.sbuf("rw_wk", [128, 8, D], BF16)
    wv = fw.sbuf("rw_wv", [128, 8, D], BF16)
    w1 = fw.sbuf("rw_w1", [128, 2, 8, 64], BF16)
    a1 = fw.sbuf("rw_a1", [128, 2, 8, 64], BF16)
    g1 = fw.sbuf("rw_g1", [128, 8, 128], BF16)
    w2 = fw.sbuf("rw_w2", [128, 2, D], BF16)
    a2 = fw.sbuf("rw_a2", [128, 2, D], BF16)
    g2 = fw.sbuf("rw_g2", [128, D], BF16)
    mu = fw.sbuf("rw_mu", [128, 6, 8], F32)
    w0b = [bc_load(fw, "w0b%d" % d, k.rw_w0[0, d:d + 1, :]) for d in range(2)]
    a0b = [bc_load(fw, "a0b%d" % d, k.rw_a0[0, d:d + 1, :]) for d in range(2)]
    kkb = bc_load(fw, "kkb", k.rw_k_k[0:1, :])
    kab = bc_load(fw, "kab", k.rw_k_a[0:1, :])
    rkb = bc_load(fw, "rkb", k.rw_r_k[0:1, :, :].rearrange("o h d -> o (h d)"))
    for (dst, srcw) in ((wr, k.rw_w_r), (wk, k.rw_w_k), (wv, k.rw_w_v)):
        for c in range(8):
            dma(fw, "pool", dst[:, c, :], srcw[0, c * 128:(c + 1) * 128, :], [], [dst])
    for d in range(2):
        dma(fw, "pool", w1[:, d, :, :], k.rw_w1[0, d].rearrange("(c p) n -> p c n", p=128), [], [w1])
        dma(fw, "pool", a1[:, d, :, :], k.rw_a1[0, d].rearrange("(c p) n -> p c n", p=128), [], [a1])
        dma(fw, "pool", w2[0:64, d, :], k.rw_w2[0, d], [], [w2])
        dma(fw, "pool", a2[0:64, d, :], k.rw_a2[0, d], [], [a2])
    dma(fw, "pool", g1[:], k.rw_g1[0].rearrange("(c p) n -> p c n", p=128), [], [g1])
    dma(fw, "pool", g2[:], k.rw_g2[0], [], [g2])
    for j in range(6):
        dma(fw, "sp", mu[:, j, :], k.rw_mu[0, j, :].rearrange("(c p) -> p c", p=128), [], [mu], allow_slow_non_contiguous=True)
    fw.pool(lambda e: e.memset(xp[:], 0.0), w=[xp])
    gn = load_gain_T(fw, k, "gnm1", k.norm_mix_g[l, :])
    G, base = make_GS(fw, k, l, 0, gn)
    fe = Front(fw, k)
    for i in range(18):
        t = t0 + i
        fe.run(src[t * 128:(t + 1) * 128, :], src, G, l, base, rowtype(t),
               lambda c, i=i: xp[:, c, col(i * 128):col(i * 128) + 128], xp)

    xx = fw.sbuf("rw_xx", [128, 8, 128], F32)
    tmpx = fw.sbuf("rw_tmpx", [128, 8, 128], F32)
    xm = [fw.sbuf("rw_xm%d" % j, [128, 8, 128], BF16) for j in range(6)]
    hT = [fw.sbuf("rw_hT%d" % i, [128, 128], BF16) for i in range(5)]
    pp = [fw.psum("rwf_pp%d" % i, [128, 1024], F32) for i in range(3)]
    ph = fw.psum("rwf_ph", [128, 4, 128], F32)
    tr = fw.sbuf("t_r", [128, D], F32)
    tk = fw.sbuf("t_k", [128, D], F32)
    tvb = fe.xh[0]
    tkk = fw.sbuf("t_kk", [128, D], F32)
    tsq = fe.xt[0]
    ta = [fw.sbuf("t_a", [128, D], F32)] * 2
    tsg = [fw.sbuf("t_sg", [128, D], F32)] * 2
    tkd = [fw.sbuf("t_kd", [128, D], F32)] * 2
    tbt = [fw.sbuf("t_bt", [128, D], F32)] * 2
    tg = tsq
    tbo = fe.xt[1]
    s16 = [fw.sbuf("t_s16_%d" % i, [128, 16], F32) for i in range(3)]
    v3 = lambda t_: t_[:].rearrange("p (h d) -> p h d", d=64)
    b16 = lambda s_: s_[:].unsqueeze(2).to_broadcast([128, 16, 64])

    for i in range(getattr(k, "rwf_tiles", 18)):
        c0_ = col(i * 128)
        rows = slice(i * 128, (i + 1) * 128)
        fw.pool(lambda e, c0_=c0_: e.tensor_tensor(out=tmpx[:], in0=xp[:, :, c0_ - 1:c0_ + 127], in1=xp[:, :, c0_ + 1:c0_ + 129],
                                                  op=ALU.add), r=[xp], w=[tmpx])
        fw.dve(lambda e, c0_=c0_: e.scalar_tensor_tensor(out=xx[:], in0=tmpx[:], scalar=0.5, in1=xp[:, :, c0_:c0_ + 128],
                                                        op0=ALU.mult, op1=ALU.subtract), r=[tmpx, xp], w=[xx])
        for j in range(6):
            fw.pool(lambda e, j=j: e.tensor_tensor(out=tmpx[:], in0=xx[:], in1=mu[:, j, :].unsqueeze(2).to_broadcast([128, 8, 128]),
                                                  op=ALU.mult), r=[xx, mu], w=[tmpx])
            fw.pool(lambda e, j=j, c0_=c0_: e.tensor_tensor(out=xm[j][:], in0=tmpx[:], in1=xp[:, :, c0_:c0_ + 128], op=ALU.add),
                    r=[tmpx, xp], w=[xm[j]])
        if getattr(k, 'rwf_stage', 99) <= 1:
            continue
        for (pi, xj, wt) in ((0, 0, wr), (1, 2, wk), (2, 3, wv)):
            for half in range(2):
                for c in range(8):
                    fw.pe(lambda e, pi=pi, xj=xj, wt=wt, half=half, c=c: e.matmul(
                        pp[pi][:, half * 512:(half + 1) * 512], xm[xj][:, c, :], wt[:, c, half * 512:(half + 1) * 512],
                        start=(c == 0), stop=(c == 7)), r=[xm[xj], wt], w=[pp[pi]])
        var = getattr(k, "rwf_var", "")
        if var != "noevac":
            if var != "nor":
                fw.act(lambda e: e.activation(out=tr[:], in_=pp[0][:], func=AF.Copy), r=[pp[0]], w=[tr])
            if var != "nok":
                fw.act(lambda e: e.activation(out=tk[:], in_=pp[1][:], func=AF.Copy), r=[pp[1]], w=[tk])
            if var != "nokk":
                fw.pool(lambda e: e.tensor_tensor(out=tkk[:], in0=tk[:], in1=kkb[:], op=ALU.mult), r=[tk, kkb], w=[tkk])
            if var != "nov":
                fw.act(lambda e: e.activation(out=tvb[:], in_=pp[2][:], func=AF.Copy), r=[pp[2]], w=[tvb])
        if getattr(k, 'rwf_stage', 99) <= 2:
            continue
        for (hi, xj, wt, d, M) in ((0, 1, w1, 0, 64), (1, 1, w1, 1, 64), (2, 4, a1, 0, 64), (3, 4, a1, 1, 64), (4, 5, g1, None, 128)):
            for c in range(8):
                lhs = (lambda wt=wt, d=d, c=c: wt[:, c, :]) if d is None else (lambda wt=wt, d=d, c=c: wt[:, d, c, :])
                if hi < 4:
                    fw.pe(lambda e, hi=hi, xj=xj, c=c, M=M, lhs=lhs: e.matmul(ph[0:M, hi, :], lhs(), xm[xj][:, c, :],
                                                                            start=(c == 0), stop=(c == 7)), r=[xm[xj], wt], w=[ph])
                else:
                    fw.pe(lambda e, xj=xj, c=c, lhs=lhs: e.matmul(pp[2][:, 0:128], lhs(), xm[xj][:, c, :],
                                                                 start=(c == 0), stop=(c == 7)), r=[xm[xj], wt], w=[pp[2]])
        for hi, (fn, M) in enumerate(((AF.Tanh, 64), (AF.Tanh, 64), (AF.Copy, 64), (AF.Copy, 64), (AF.Sigmoid, 128))):
            if hi < 4:
                fw.act(lambda e, hi=hi, fn=fn, M=M: e.activation(out=hT[hi][0:M, :], in_=ph[0:M, hi, :], func=fn), r=[ph], w=[hT[hi]])
            else:
                fw.act(lambda e, hi=hi, fn=fn: e.activation(out=hT[hi][:, :], in_=pp[2][:, 0:128], func=fn), r=[pp[2]], w=[hT[hi]])
        if getattr(k, 'rwf_stage', 99) <= 3:
            continue
        for half in range(2):
            fw.pe(lambda e, half=half: e.matmul(pp[2][:, half * 512:(half + 1) * 512], hT[4][:, :], g2[:, half * 512:(half + 1) * 512],
                                               start=True, stop=True), r=[hT[4], g2], w=[pp[2]])
        fw.pool(lambda e: e.tensor_tensor(out=tsq[:], in0=tkk[:], in1=tkk[:], op=ALU.mult), r=[tkk], w=[tsq])
        fw.dve(lambda e: e.reduce_sum(out=s16[0][:], in_=v3(tsq), axis=AX.X), r=[tsq], w=[s16[0]])
        fw.dve(lambda e: e.tensor_scalar(out=s16[0][:], in0=s16[0][:], scalar1=1e-12, scalar2=None, op0=ALU.add), r=[s16[0]], w=[s16[0]])
        fw.act(lambda e: e.activation(out=s16[0][:], in_=s16[0][:], func=AF.Sqrt), r=[s16[0]], w=[s16[0]])
        fw.dve(lambda e: e.reciprocal(out=s16[0][:], in_=s16[0][:]), r=[s16[0]], w=[s16[0]])
        fw.dve(lambda e: e.tensor_tensor(out=v3(tkk), in0=v3(tkk), in1=b16(s16[0]), op=ALU.mult), r=[tkk, s16[0]], w=[tkk])
        fw.pool(lambda e: e.tensor_tensor(out=tsq[:], in0=tr[:], in1=rkb[:], op=ALU.mult), r=[tr, rkb], w=[tsq])
        for d in range(2):
            for half in range(2):
                fw.pe(lambda e, d=d, half=half: e.matmul(pp[0][:, half * 512:(half + 1) * 512], hT[d][0:64, :],
                                                        w2[0:64, d, half * 512:(half + 1) * 512], start=True, stop=True),
                      r=[hT[d], w2], w=[pp[0]])
                fw.pe(lambda e, d=d, half=half: e.matmul(pp[1][:, half * 512:(half + 1) * 512], hT[2 + d][0:64, :],
                                                        a2[0:64, d, half * 512:(half + 1) * 512], start=True, stop=True),
                      r=[hT[2 + d], a2], w=[pp[1]])
            fw.dve(lambda e, d=d: e.tensor_tensor(out=tsg[d][:], in0=pp[0][:], in1=w0b[d][:], op=ALU.add), r=[pp[0], w0b[d]], w=[tsg[d]])
            fw.act(lambda e, d=d: e.activation(out=tsg[d][:], in_=tsg[d][:], func=AF.Sigmoid), r=[tsg[d]], w=[tsg[d]])
            fw.dve(lambda e, d=d: e.tensor_tensor(out=ta[d][:], in0=pp[1][:], in1=a0b[d][:], op=ALU.add), r=[pp[1], a0b[d]], w=[ta[d]])
            fw.act(lambda e, d=d: e.activation(out=ta[d][:], in_=ta[d][:], func=AF.Sigmoid), r=[ta[d]], w=[ta[d]])
            fw.dve(lambda e, d=d: e.scalar_tensor_tensor(out=tkd[d][:], in0=ta[d][:], scalar=-1.0, in1=kab[:], op0=ALU.add, op1=ALU.mult),
                   r=[ta[d], kab], w=[tkd[d]])
            fw.dve(lambda e, d=d: e.scalar_tensor_tensor(out=tkd[d][:], in0=tkd[d][:], scalar=1.0, in1=tk[:], op0=ALU.add, op1=ALU.mult),
                   r=[tkd[d], tk], w=[tkd[d]])
            fw.pool(lambda e, d=d: e.tensor_tensor(out=tbt[d][:], in0=tkk[:], in1=ta[d][:], op=ALU.mult), r=[tkk, ta[d]], w=[tbt[d]])
            fw.pool(lambda e, d=d: e.tensor_tensor(out=tbo[:], in0=tsq[:], in1=tkd[d][:], op=ALU.mult), r=[tsq, tkd[d]], w=[tbo])
            fw.dve(lambda e, d=d: e.reduce_sum(out=s16[1 + d][:], in_=v3(tbo), axis=AX.X), r=[tbo], w=[s16[1 + d]])
            for (nm, tt) in (("k%d" % d, tkd[d]), ("b%d" % d, tbt[d]), ("s%d" % d, tsg[d])):
                dma(fw, "sp", A[nm][rows, :], tt[:], [tt], [A[nm]])
        fw.dve(lambda e: e.tensor_tensor(out=s16[1][:], in0=s16[1][:], in1=s16[2][:], op=ALU.add), r=[s16[1], s16[2]], w=[s16[1]])
        fw.dve(lambda e: e.tensor_tensor(out=v3(tbo), in0=v3(tvb), in1=b16(s16[1]), op=ALU.mult), r=[tvb, s16[1]], w=[tbo])
        for (nm, tt) in (("r", tr), ("kk", tkk), ("bonus", tbo), ("v", tvb)):
            dma(fw, "sp", A[nm][rows, :], tt[:], [tt], [A[nm]])
        fw.act(lambda e: e.activation(out=tg[:], in_=pp[2][:], func=AF.Copy), r=[pp[2]], w=[tg])
        dma(fw, "sp", A["g"][rows, :], tg[:], [tg], [A["g"]])
    fw.flush()
def phase_rw_scan(fw, k, b):
    A = k.rw
    tri = fw.sbuf("tri", [128, 4, 128], F32)
    dma(fw, "sp", tri[:], k.c_tri[:, :, :], [], [tri])
    mexp = [fw.sbuf("mexp%d" % m, [128, 4, 128], F32) for m in range(4)]
    for m in range(4):
        for hh in range(4):
            fw.pool(lambda e, m=m, hh=hh: e.tensor_copy(mexp[m][:, hh, :], tri[:, m, :]), r=[tri], ww=[mexp[m]])
    onesc = fw.sbuf("onesc", [128, 1], F32)
    fw.pool(lambda e: e.memset(onesc[:], 1.0), w=[onesc])
    Lr = fw.sbuf("Lr", [128, D], F32)
    Lkk = fw.sbuf("Lkk", [128, D], F32)
    Lk = fw.sbuf("Lk", [128, D], F32)
    Lb = fw.sbuf("Lb", [128, D], F32)
    Ls = fw.sbuf("Ls", [128, D], F32)
    Lv = fw.sbuf("Lv", [128, D], BF16)
    gam = fw.sbuf("gam", [128, D], F32)
    gin = fw.sbuf("gin", [128, D], F32)
    gex = fw.sbuf("gex", [128, D], F32)
    At = fw.sbuf("At", [128, D], BF16)
    Rt = fw.sbuf("Rt", [128, D], BF16)
    Bt = fw.sbuf("Bt", [128, D], BF16)
    Kt = fw.sbuf("Kt", [128, D], BF16)
    ART = fw.sbuf("ART", [128, 8, 256], BF16)
    BT = fw.sbuf("BT", [128, 8, 128], BF16)
    KTt = fw.sbuf("KTt", [128, 8, 128], BF16)
    gLT = fw.sbuf("gLT", [128, 8], F32)
    ST = fw.sbuf("ST", [128, 8, 64], F32)
    STb = fw.sbuf("STb", [128, 8, 64], BF16)
    Sn = fw.sbuf("Sn", [128, 8, 64], F32)
    PDT = F32 if getattr(k, "scan_fp32", True) else BF16
    P = [[fw.sbuf("P%d_%d" % (g, i), [128, 4, 128], PDT) for i in range(7)] for g in range(4)]
    Q = [[fw.sbuf("Q%d_%d" % (g, i), [128, 4, 128], PDT) for i in range(2)] for g in range(4)]
    Br = [fw.sbuf("Br%d" % g, [128, 4, 128], BF16) for g in range(4)]
    Aak = [fw.sbuf("Aak%d" % g, [128, 4, 128], BF16) for g in range(4)]
    Kr = [fw.sbuf("Kr%d" % g, [128, 4, 128], BF16) for g in range(4)]
    Xb = [[fw.sbuf("Xb%d_%d" % (g, i), [128, 4, 64], BF16) for i in range(2)] for g in range(4)]
    Ub = [fw.sbuf("Ub%d" % g, [128, 4, 64], BF16) for g in range(4)]
    Xf = [fw.sbuf("Xf%d" % g, [128, 4, 64], F32) for g in range(4)]
    Yt = fw.sbuf("Yt", [128, D], F32)
    Yp_ = fw.sbuf("Yprev", [128, D], F32)
    pr = [fw.psum("sc_pr%d" % i, [128, 1024], F32) for i in range(4)]
    rb = [Res("bank%d" % i) for i in range(8)]
    lo = lambda i: pr[i][:, 0:512]
    hi = lambda i: pr[i][:, 512:1024]
    v4 = lambda ap, n: ap.rearrange("p (a t) -> p a t", a=n)
    bcm = lambda m: tri[:, m, :].unsqueeze(1).to_broadcast([128, 4, 128])
    MASK = {0: dict(cum=0, strict=1, incl=0, strictT=2), 1: dict(cum=3, strict=2, incl=3, strictT=1)}

    def chunk(d, ti, want_y, second):
        mk = MASK[d]
        rows = slice(ti * 128, (ti + 1) * 128)
        for (dst, nm) in ((Lr, "r"), (Lkk, "kk"), (Lk, "k%d" % d), (Lb, "b%d" % d), (Ls, "s%d" % d), (Lv, "v")):
            dma(fw, "sp", dst[:], A[nm][rows, :], [A[nm]], [dst])
        cum = pr[3]
        for half in range(2):
            fw.pe(lambda e, half=half: e.matmul(cum[:, half * 512:(half + 1) * 512], tri[:, mk["cum"], :],
                                               Ls[:, half * 512:(half + 1) * 512], start=True, stop=True),
                  r=[tri, Ls], w=[rb[6], rb[7]])
        fw.act(lambda e: e.activation(out=gex[:], in_=cum[:], func=AF.Copy), r=[rb[6], rb[7]], w=[gex])
        fw.act(lambda e: e.activation(out=gam[:], in_=gex[:], func=AF.Exp, scale=-C0), r=[gex], w=[gam])
        fw.act(lambda e: e.activation(out=gin[:], in_=gex[:], func=AF.Exp, scale=C0), r=[gex], w=[gin])
        fw.dve(lambda e: e.tensor_tensor(out=gex[:], in0=gex[:], in1=Ls[:], op=ALU.subtract), r=[gex, Ls], w=[gex])
        fw.act(lambda e: e.activation(out=gex[:], in_=gex[:], func=AF.Exp, scale=-C0), r=[gex], w=[gex])
        glp = pr[2][:, 512:520]
        for c in range(8):
            fw.pe(lambda e, c=c: e.matmul(pr[2][:, 512 + c:513 + c], Ls[:, c * 128:(c + 1) * 128], onesc[:, 0:1],
                                         start=True, stop=True), r=[Ls, onesc], w=[rb[5]])
        fw.act(lambda e: e.activation(out=gLT[:], in_=glp, func=AF.Exp, scale=-C0), r=[rb[5]], w=[gLT])
        fw.dve(lambda e: e.scalar_tensor_tensor(out=At[:], in0=Lkk[:], scalar=-1.0, in1=gex[:], op0=ALU.mult, op1=ALU.mult),
               r=[Lkk, gex], w=[At])
        fw.pool(lambda e: e.tensor_tensor(out=Rt[:], in0=Lr[:], in1=gam[:], op=ALU.mult), r=[Lr, gam], w=[Rt])
        fw.dve(lambda e: e.tensor_tensor(out=Bt[:], in0=Lb[:], in1=gin[:], op=ALU.mult), r=[Lb, gin], w=[Bt])
        fw.pool(lambda e: e.tensor_tensor(out=Kt[:], in0=Lk[:], in1=gin[:], op=ALU.mult), r=[Lk, gin], w=[Kt])
        if getattr(k, 'scan_stage', 99) <= 1:
            return
        tps = [(At, lo(0), rb[0], lambda: ART[:, :, 0:128], ART), (Rt, hi(0), rb[1], lambda: ART[:, :, 128:256], ART),
               (Bt, lo(1), rb[2], lambda: BT[:, :, :], BT), (Kt, hi(1), rb[3], lambda: KTt[:, :, :], KTt)]
        for n_, (src_, bank, res_, dstf, dstT) in enumerate(tps):
            tpv = v4(bank.bitcast(BF16), 8)
            for c in range(8):
                fw.pe(lambda e, c=c, src_=src_, tpv=tpv: e.transpose(tpv[:, c, :], src_[:, c * 128:(c + 1) * 128], k.ident_bf[:]),
                      r=[src_, k.ident_bf], w=[res_])
            if n_ % 2 == 0:
                fw.act(lambda e, tpv=tpv, dstf=dstf: e.activation(out=dstf(), in_=tpv, func=AF.Copy), r=[res_], ww=[dstT])
            else:
                fw.dve(lambda e, tpv=tpv, dstf=dstf: e.tensor_copy(dstf(), tpv), r=[res_], w=[dstT])
        if getattr(k, 'scan_stage', 99) <= 2:
            return
        for g in range(4):
            outs = [(v4(lo(0), 4), rb[0]), (v4(hi(0), 4), rb[1]), (v4(lo(1), 4), rb[2]), (v4(hi(1), 4), rb[3]), (v4(lo(2), 4), rb[4])]
            for hh in range(4):
                h = 4 * g + hh
                c, bp = h // 2, 64 * (h % 2)
                ops_ = [(BT, ART, 0, False), (BT, ART, 128, False), (KTt, ART, 0, False), (KTt, ART, 128, False), (ART, BT, 0, True)]
                fw.pe(lambda e: e.matmul(pr[2][:, 1023:1024], BT[:, 0, :], ART[:, 0, 0:1], start=True, stop=True),
                      r=[BT, ART], w=[rb[5]])
                for oi, (lt, rt_, off, swap) in enumerate(ops_):
                    ps_, rs_ = outs[oi]
                    if not swap:
                        fw.pe(lambda e, hh=hh, c=c, bp=bp, ps_=ps_, lt=lt, off=off: e.matmul(
                            ps_[:, hh, :], lt[bp:bp + 64, c, :], ART[bp:bp + 64, c, off:off + 128], start=True, stop=True),
                            r=[lt, ART], w=[rs_])
                    else:
                        fw.pe(lambda e, hh=hh, c=c, bp=bp, ps_=ps_: e.matmul(
                            ps_[:, hh, :], ART[bp:bp + 64, c, 0:128], BT[bp:bp + 64, c, :], start=True, stop=True),
                            r=[BT, ART], w=[rs_])
            dsts = [(P[g][0], "strict"), (Br[g], "incl"), (Aak[g], "strict"), (Kr[g], "incl"), (Q[g][0], "strictT")]
            svar = getattr(k, "scan_var", "")
            if svar == "mm":
                dsts = []
            for oi, (dt_, mname) in enumerate(dsts):
                ps_, rs_ = outs[oi]
                if oi % 2 == 0:
                    fw.act(lambda e, ps_=ps_, dt_=dt_: e.activation(out=dt_[:], in_=ps_, func=AF.Copy), r=[rs_], w=[dt_])
                else:
                    fw.dve(lambda e, ps_=ps_, dt_=dt_: e.tensor_copy(dt_[:], ps_), r=[rs_], w=[dt_])
                mx = mexp[mk[mname]]
                if svar == "cp":
                    continue
                if oi % 2 == 0:
                    fw.pool(lambda e, dt_=dt_, mx=mx: e.tensor_tensor(out=dt_[:], in0=dt_[:], in1=mx[:], op=ALU.mult), r=[dt_, mx], w=[dt_])
                else:
                    fw.dve(lambda e, dt_=dt_, mx=mx: e.tensor_tensor(out=dt_[:], in0=dt_[:], in1=mx[:], op=ALU.mult), r=[dt_, mx], w=[dt_])
        if getattr(k, 'scan_stage', 99) <= 3:
            return
        for kk_ in range(6):
            for g in range(4):
                pi = 2 + (g % 2)
                Pn, Qn = v4(lo(pi), 4), v4(hi(pi), 4)
                rP, rQ = rb[2 * pi], rb[2 * pi + 1]
                Pk, Qk, Qn_sb = P[g][kk_], Q[g][kk_ % 2], Q[g][(kk_ + 1) % 2]
                for hh in range(4):
                    fw.pe(lambda e, hh=hh, Pn=Pn, Pk=Pk, Qk=Qk: e.matmul(Pn[:, hh, :], Qk[:, hh, :], Pk[:, hh, :], start=True, stop=True),
                          r=[Pk, Qk], w=[rP])
                if kk_ < 5:
                    for hh in range(4):
                        fw.pe(lambda e, hh=hh, Qn=Qn, Pk=Pk, Qk=Qk: e.matmul(Qn[:, hh, :], Pk[:, hh, :], Qk[:, hh, :], start=True, stop=True),
                              r=[Pk, Qk], w=[rQ])
                fw.act(lambda e, g=g, kk_=kk_, Pn=Pn: e.activation(out=P[g][kk_ + 1][:], in_=Pn, func=AF.Copy), r=[rP], w=[P[g][kk_ + 1]])
                if kk_ < 5:
                    fw.dve(lambda e, Qn=Qn, Qn_sb=Qn_sb: e.tensor_copy(Qn_sb[:], Qn), r=[rQ], w=[Qn_sb])
        if getattr(k, 'scan_stage', 99) <= 4:
            return
        Xp = [v4(pr[0][:, g * 256:(g + 1) * 256], 4) for g in range(4)]
        rX = [rb[0], rb[0], rb[1], rb[1]]
        for g in range(4):
            for hh in range(4):
                h = 4 * g + hh
                c, bp = h // 2, 64 * (h % 2)
                fw.pe(lambda e, g=g, hh=hh, c=c, bp=bp: e.matmul(Xp[g][:, hh, :], ART[bp:bp + 64, c, 0:128], STb[bp:bp + 64, c, :],
                                                                start=True, stop=False), r=[ART, STb], w=[rX[g]])
                fw.pe(lambda e, g=g, hh=hh, h=h: e.matmul(Xp[g][:, hh, :], Aak[g][:, hh, :], Lv[:, h * 64:(h + 1) * 64],
                                                         start=False, stop=True), r=[Aak[g], Lv], w=[rX[g]])
        for g in range(4):
            fw.dve(lambda e, g=g: e.tensor_copy(Xf[g][:], Xp[g]), r=[rX[g]], w=[Xf[g]])
            if PDT != F32:
                fw.dve(lambda e, g=g: e.tensor_copy(Xb[g][0][:], Xf[g][:]), r=[Xf[g]], w=[Xb[g][0]])
        for kk_ in range(7):
            for g in range(4):
                xb = Xf[g] if PDT == F32 else Xb[g][kk_ % 2]
                xn = Ub[g] if kk_ == 6 else Xb[g][(kk_ + 1) % 2]
                for hh in range(4):
                    fw.pe(lambda e, g=g, hh=hh, kk_=kk_, xb=xb: e.matmul(Xp[g][:, hh, :], P[g][kk_][:, hh, :], xb[:, hh, :],
                                                                        start=True, stop=True), r=[P[g][kk_], xb], w=[rX[g]])
                fw.dve(lambda e, g=g: e.tensor_tensor(out=Xf[g][:], in0=Xp[g], in1=Xf[g][:], op=ALU.add), r=[rX[g], Xf[g]], w=[Xf[g]])
                fw.act(lambda e, g=g, xn=xn: e.activation(out=xn[:], in_=Xf[g][:], func=AF.Copy), r=[Xf[g]], w=[xn])
        if getattr(k, 'scan_stage', 99) <= 5:
            return
        if want_y:
            Yp = pr[1]
            if second:
                dma(fw, "sp", Yp_[:], A["y"][rows, :], [A["y"]], [Yp_])
            for g in range(4):
                for hh in range(4):
                    h = 4 * g + hh
                    c, bp = h // 2, 64 * (h % 2)
                    rY = rb[2] if h < 8 else rb[3]
                    fw.pe(lambda e, h=h, c=c, bp=bp: e.matmul(Yp[:, h * 64:(h + 1) * 64], ART[bp:bp + 64, c, 128:256], STb[bp:bp + 64, c, :],
                                                             start=True, stop=False), r=[ART, STb], w=[rY])
                    fw.pe(lambda e, h=h, g=g, hh=hh: e.matmul(Yp[:, h * 64:(h + 1) * 64], Br[g][:, hh, :], Ub[g][:, hh, :],
                                                             start=False, stop=False), r=[Br[g], Ub[g]], w=[rY])
                    fw.pe(lambda e, h=h, g=g, hh=hh: e.matmul(Yp[:, h * 64:(h + 1) * 64], Kr[g][:, hh, :], Lv[:, h * 64:(h + 1) * 64],
                                                             start=False, stop=True), r=[Kr[g], Lv], w=[rY])
            fw.act(lambda e: e.activation(out=Yt[:], in_=Yp[:], func=AF.Copy), r=[rb[2], rb[3]], w=[Yt])
            if second:
                fw.pool(lambda e: e.tensor_tensor(out=Yt[:], in0=Yt[:], in1=Yp_[:], op=ALU.add), r=[Yt, Yp_], w=[Yt])
            dma(fw, "sp", A["y"][rows, :], Yt[:], [Yt], [A["y"]])
        if getattr(k, 'scan_stage', 99) <= 6:
            return
        Sp = pr[2][:, :].rearrange("p (c w i) -> p c w i", c=8, w=2)
        for c in range(8):
            for wch in range(2):
                h = 2 * c + wch
                g, hh = h // 4, h % 4
                fw.pe(lambda e, c=c, wch=wch, g=g, hh=hh: e.matmul(Sp[:, c, wch, :], Bt[:, c * 128:(c + 1) * 128], Ub[g][:, hh, :],
                                                                  start=True, stop=False), r=[Bt, Ub[g]], w=[rb[4], rb[5]])
                fw.pe(lambda e, c=c, wch=wch, h=h: e.matmul(Sp[:, c, wch, :], Kt[:, c * 128:(c + 1) * 128], Lv[:, h * 64:(h + 1) * 64],
                                                           start=False, stop=True), r=[Kt, Lv], w=[rb[4], rb[5]])
        for cb in range(2):
            fw.dve(lambda e, cb=cb: e.tensor_copy(Sn[0:64, 4 * cb:4 * cb + 4, :], Sp[0:64, 4 * cb:4 * cb + 4, 0, :]), r=[rb[4 + cb]], ww=[Sn])
            fw.dve(lambda e, cb=cb: e.tensor_copy(Sn[64:128, 4 * cb:4 * cb + 4, :], Sp[64:128, 4 * cb:4 * cb + 4, 1, :]), r=[rb[4 + cb]], ww=[Sn])
        fw.dve(lambda e: e.tensor_tensor(out=ST[:], in0=ST[:], in1=Sn[:], op=ALU.add), r=[ST, Sn], w=[ST])
        fw.dve(lambda e: e.tensor_tensor(out=ST[:], in0=ST[:], in1=gLT[:].unsqueeze(2).to_broadcast([128, 8, 64]), op=ALU.mult),
               r=[ST, gLT], w=[ST])
        fw.act(lambda e: e.activation(out=STb[:], in_=ST[:], func=AF.Copy), r=[ST], w=[STb])

    nt_dbg = getattr(k, "scan_tiles", 18)
    for d in range(2):
        fw.pool(lambda e: e.memset(ST[:], 0.0), w=[ST])
        fw.pool(lambda e: e.memset(STb[:], 0.0), w=[STb])
        order = list(range(18)) if d == 0 else [1, 0] + list(range(17, 1, -1))
        if nt_dbg < 18:
            order = [t_ for t_ in order if t_ < nt_dbg]
        for ti in order:
            chunk(d, ti, ti >= 2, d == 1)
    fw.flush()


def phase_rw_out(fw, k, b, src, dst):
    l = 1
    t0 = 18 * b
    A = k.rw
    wo = fw.sbuf("rw_wo", [128, 8, D], BF16)
    for c in range(8):
        dma(fw, "pool", wo[:, c, :], k.rw_w_o[0, c * 128:(c + 1) * 128, :], [], [wo])
    lnw = bc_load(fw, "lnw", k.rw_ln_w[0:1, :])
    lnb = bc_load(fw, "lnb", k.rw_ln_b[0:1, :])
    ty = fw.sbuf("o_y", [128, D], F32)
    tg = fw.sbuf("o_g", [128, D], F32)
    tb = fw.sbuf("o_b", [128, D], F32)
    tq = fw.sbuf("o_q", [128, D], F32)
    zb = fw.sbuf("o_zb", [128, D], BF16)
    zT = fw.sbuf("o_zT", [128, 8, 128], BF16)
    resT = fw.sbuf("o_resT", [128, 8, 128], F32)
    s1 = fw.sbuf("o_s1", [128, 16], F32)
    s2 = fw.sbuf("o_s2", [128, 16], F32)
    ht = [fw.sbuf("o_ht%d" % i, [128, D], F32) for i in range(2)]
    big = fw.psum("o_big", [128, 1024], F32)
    pz = fw.psum("o_pz", [128, 8, 128], BF16)
    pq = fw.psum("o_pq", [128, 1024], F32)
    be = Back(fw, k, ht, big)
    v3 = lambda t_: t_[:].rearrange("p (h d) -> p h d", d=64)
    b16 = lambda s_: s_[:].unsqueeze(2).to_broadcast([128, 16, 64])
    pOP = pq[:, :].rearrange("p (d t) -> p d t", d=8)
    for i in range(2, getattr(k, "scan_tiles", 18)):
        t = t0 + i
        rows = slice(i * 128, (i + 1) * 128)
        dma(fw, "sp", ty[:], A["y"][rows, :], [A["y"]], [ty])
        dma(fw, "sp", tg[:], A["g"][rows, :], [A["g"]], [tg])
        dma(fw, "sp", tb[:], A["bonus"][rows, :], [A["bonus"]], [tb])
        fw.dve(lambda e: e.reduce_sum(out=s1[:], in_=v3(ty), axis=AX.X), r=[ty], w=[s1])
        fw.dve(lambda e: e.tensor_scalar(out=s1[:], in0=s1[:], scalar1=1.0 / 64, scalar2=None, op0=ALU.mult), r=[s1], w=[s1])
        fw.dve(lambda e: e.tensor_tensor(out=v3(ty), in0=v3(ty), in1=b16(s1), op=ALU.subtract), r=[ty, s1], w=[ty])
        fw.pool(lambda e: e.tensor_tensor(out=tq[:], in0=ty[:], in1=ty[:], op=ALU.mult), r=[ty], w=[tq])
        fw.dve(lambda e: e.reduce_sum(out=s2[:], in_=v3(tq), axis=AX.X), r=[tq], w=[s2])
        fw.dve(lambda e: e.tensor_scalar(out=s2[:], in0=s2[:], scalar1=1.0 / 64, scalar2=64e-5, op0=ALU.mult, op1=ALU.add), r=[s2], w=[s2])
        fw.act(lambda e: e.activation(out=s2[:], in_=s2[:], func=AF.Sqrt), r=[s2], w=[s2])
        fw.dve(lambda e: e.reciprocal(out=s2[:], in_=s2[:]), r=[s2], w=[s2])
        fw.dve(lambda e: e.tensor_tensor(out=v3(ty), in0=v3(ty), in1=b16(s2), op=ALU.mult), r=[ty, s2], w=[ty])
        fw.pool(lambda e: e.tensor_tensor(out=ty[:], in0=ty[:], in1=lnw[:], op=ALU.mult), r=[ty, lnw], w=[ty])
        fw.pool(lambda e: e.tensor_tensor(out=tb[:], in0=tb[:], in1=lnb[:], op=ALU.add), r=[tb, lnb], w=[tb])
        fw.dve(lambda e: e.tensor_tensor(out=ty[:], in0=ty[:], in1=tb[:], op=ALU.add), r=[ty, tb], w=[ty])
        fw.dve(lambda e: e.tensor_tensor(out=zb[:], in0=ty[:], in1=tg[:], op=ALU.mult), r=[ty, tg], w=[zb])
        for c in range(8):
            fw.pe(lambda e, c=c: e.transpose(pz[:, c, :], zb[:, c * 128:(c + 1) * 128], k.ident_bf[:]), r=[zb, k.ident_bf], w=[pz])
        fw.act(lambda e: e.activation(out=zT[:], in_=pz[:], func=AF.Copy), r=[pz], w=[zT])
        for d in range(8):
            for c in range(8):
                fw.pe(lambda e, c=c, d=d: e.matmul(pOP[:, d, :], wo[:, c, d * 128:(d + 1) * 128], zT[:, c, :],
                                                  start=(c == 0), stop=(c == 7)), r=[wo, zT], w=[pq])
        fw.act(lambda e: e.activation(out=resT[:].rearrange("p d t -> p (d t)"), in_=pq[:, :], func=AF.Copy), r=[pq], w=[resT])
        be.run(lambda c: resT[:, c, :], resT, l, 16, rowtype(t), src[t * 128:(t + 1) * 128, :], src,
               dst[t * 128:(t + 1) * 128, :], dst)
    fw.flush()
WSPEC = [
    ("ada_w", [2, D, 6 * D]), ("ada_b", [2, 6 * D]), ("norm_mix_g", [2, D]), ("norm_ffn_g", [2, D]),
    ("router_w", [2, D, NE]), ("router_b", [2, NE]), ("exp_w_in", [2, NE, D, 2 * D]), ("exp_b_in", [2, NE, 2 * D]),
    ("exp_w_out", [2, NE, D, D]), ("exp_b_out", [2, NE, D]),
    ("ab_w_in", [1, D, 2048]), ("na_q_g", [1, 64]), ("na_k_g", [1, 64]),
    ("pool_w", [1, 4, 128, 128]), ("pool_scale", [1, 512]), ("ab_w_out", [1, D, D]),
    ("rw_mu", [1, 6, D]), ("rw_w_r", [1, D, D]), ("rw_w_k", [1, D, D]), ("rw_w_v", [1, D, D]), ("rw_w_o", [1, D, D]),
    ("rw_w0", [1, 2, D]), ("rw_w1", [1, 2, D, 64]), ("rw_w2", [1, 2, 64, D]), ("rw_a0", [1, 2, D]),
    ("rw_a1", [1, 2, D, 64]), ("rw_a2", [1, 2, 64, D]), ("rw_g1", [1, D, 128]), ("rw_g2", [1, 128, D]),
    ("rw_k_k", [1, D]), ("rw_k_a", [1, D]), ("rw_r_k", [1, 16, 64]), ("rw_ln_w", [1, D]), ("rw_ln_b", [1, D]),
]


def declare(fw, k):
    ne_decl = 1 if getattr(k, "small", False) else NE
    k.xin = fw.dram("xin", [NT * 128, D], F32, kind="ExternalInput")
    k.cc = fw.dram("cc", [3, D], F32, kind="ExternalInput")
    for name, shp in WSPEC:
        if name.startswith("exp_w"):
            shp = [shp[0], ne_decl] + shp[2:]
        setattr(k, name, fw.dram(name, shp, F32, kind="ExternalInput"))
    k.rpb_g = fw.dram("rpb_g", [8, 8, 512, 64], F32, kind="ExternalInput")
    k.c_ident = fw.dram("c_ident", [128, 128], F32, kind="ExternalInput")
    k.c_namask = fw.dram("c_namask", [512, 64], F32, kind="ExternalInput")
    k.c_poolrc = fw.dram("c_poolrc", [4, 2, 2048 + 32], F32, kind="ExternalInput")
    k.c_tri = fw.dram("c_tri", [128, 4, 128], F32, kind="ExternalInput")
    k.out = fw.dram("out", [32 * 128, D], F32, kind="ExternalOutput")
    k.H = [fw.dram("H%d" % i, [NT * 128, D], F32) for i in range(2)]
    k.combT_d = fw.dram("combT_d", [NE, 1152], F32)
    fw.persist = True
    k.modT = [fw.sbuf("modT%d" % l, [128, 48, 3], F32) for l in range(2)]
    k.ident_f = fw.sbuf("ident_f", [128, 128], F32)
    k.ident_bf = fw.sbuf("ident_bf", [128, 128], BF16)
    fw.persist = False
    dma(fw, "sp", k.ident_f[:], k.c_ident[:, :], [], [k.ident_f])
    dma(fw, "pool", k.ident_bf[:], k.c_ident[:, :], [], [k.ident_bf])


def host_consts():
    c = {}
    c["c_ident"] = np.eye(128, dtype=np.float32)
    qc = np.arange(64)
    cs = np.clip(qc - 8, 0, 48)
    kc = np.arange(64)
    valid = (kc[:, None] >= cs[None, :]) & (kc[:, None] < cs[None, :] + 16)
    m = np.where(valid, 0.0, -30000.0).astype(np.float32)
    c["c_namask"] = np.tile(m, (8, 1)).astype(np.float32)
    rc = np.zeros((4, 2, 2048 + 32), np.float32)
    for g, w in enumerate((2, 4, 8, 16)):
        for s, L in enumerate((2048, 256)):
            t = np.arange(L)
            lo = np.clip(t - w // 2, 0, L)
            hi = np.clip(t + w - w // 2, 0, L)
            rc[g, s, 16:16 + L] = 1.0 / (hi - lo)
    c["c_poolrc"] = rc
    tri = np.zeros((128, 4, 128), np.float32)
    s_ = np.arange(128)[:, None]
    t_ = np.arange(128)[None, :]
    tri[:, 0, :] = (s_ <= t_)
    tri[:, 1, :] = (s_ < t_)
    tri[:, 2, :] = (s_ > t_)
    tri[:, 3, :] = (s_ >= t_)
    c["c_tri"] = tri
    return c


def gather_rpb(rpb):
    j = np.arange(8)
    o = np.arange(8)
    kc = np.arange(64)
    qc = np.arange(64)
    ri = j[None, :] - o[:, None] + 7
    ci = np.clip(kc[:, None] - qc[None, :] + 15, 0, 30)
    g = rpb[:, ri[:, :, None, None], ci[None, None, :, :]]
    return np.ascontiguousarray(g.reshape(8, 8, 512, 64)).astype(np.float32)


def shard_inputs(inp, small=False):
    consts = host_consts()
    maps = []
    wts = {name: np.ascontiguousarray(inp[name], dtype=np.float32) for name, _ in WSPEC}
    if small:
        for nm in ("exp_w_in", "exp_w_out"):
            wts[nm] = np.ascontiguousarray(wts[nm][:, 0:1])
    rpbg = gather_rpb(np.asarray(inp["na_rpb"])[0])
    for core in range(8):
        rows = []
        for b in (2 * core, 2 * core + 1):
            rows.append(inp["ctx"][b])
            rows.append(inp["x"][b])
        m = {"xin": np.ascontiguousarray(np.concatenate(rows, axis=0), dtype=np.float32),
             "cc": np.ascontiguousarray(np.stack([inp["c"][2 * core], inp["c"][2 * core + 1], inp["c_ctx"]]), dtype=np.float32),
             "rpb_g": rpbg}
        m.update(wts)
        m.update(consts)
        maps.append(m)
    return maps
def build_program(k=None):
    nc = bass.Bass("TRN2", target_bir_lowering=False)
    fw = FW(nc)
    if k is None:
        k = K()
    declare(fw, k)
    rw_declare(fw, k)
    phase_ada(fw, k)
    for b in range(2):
        phase_ab(fw, k, b)
    for blk in range(4):
        tiles = list(range(9 * blk, 9 * blk + 9))
        phase_moe(fw, k, 0, tiles, k.H[0], lambda t: (k.H[1][t * 128:(t + 1) * 128, :], k.H[1]), blk == 0)
    for b in range(2):
        phase_rw_feat(fw, k, b, k.H[1])
        phase_rw_scan(fw, k, b)
        phase_rw_out(fw, k, b, k.H[1], k.H[0])

    def dst1(t):
        b, i = t // 18, t % 18
        o = b * 16 + (i - 2)
        return (k.out[o * 128:(o + 1) * 128, :], k.out)
    for b in range(2):
        for hb in range(2):
            tiles = [18 * b + 2 + 8 * hb + j for j in range(8)]
            phase_moe(fw, k, 1, tiles, k.H[0], dst1, False)
    fw.flush(final=True)
    return nc, fw


_CACHE = {}


def kernel(**inputs):
    from concourse.bass_utils import run_bass_kernel_spmd
    inp = {n: np.asarray(v) for n, v in inputs.items()}
    if "nc" not in _CACHE:
        _CACHE["nc"] = build_program()[0]
    nc = _CACHE["nc"]
    maps = shard_inputs(inp)
    res = run_bass_kernel_spmd(nc, maps, core_ids=list(range(8)))
    outs = [np.asarray(r["out"]).reshape(2, 2048, D) for r in res.results]
    return np.concatenate(outs, axis=0).astype(np.float32)
```
